# Optimizing a Trainium2 kernel written in Bass

```python
import math
import jax, jax.numpy as jnp
from jax import lax
import numpy as np

D_MODEL = 1024
BATCH = 8
SEQ = 4096
DEPTH = 1

DIFF_HEADS = 8
DIFF_HD = 64
NSA_HEADS = 16
NSA_KV = 4
NSA_HD = 64
CMP_LEN = 32
CMP_STRIDE = 16
SLC_LEN = 64
SLC_TOPN = 16
WIN = 512
PHI_HID = 256
Q_BLOCK = 128
NSA_Q_BLOCK = 64
N_EXPERTS = 32
TOP_K = 4
D_FF = 1024
SWIGLU_LIMIT = 7.0
SWIGLU_ALPHA = 1.702
MOE_BLOCK = 512
PLE_DIM = 256
LN_EPS = 1e-5
NEG = -1e30
BIG = 1e30
DEEPNORM_ALPHA = (2.0 * DEPTH) ** 0.25
DEEPNORM_BETA = (8.0 * DEPTH) ** -0.25
DIFF_QK = DIFF_HEADS * 2 * DIFF_HD
DIFF_V = DIFF_HEADS * 2 * DIFF_HD
NSA_Q = NSA_HEADS * NSA_HD
NSA_KVW = NSA_KV * NSA_HD
NSA_GATES = NSA_HEADS * 3
IN_SIZES = (DIFF_QK, DIFF_QK, DIFF_V, NSA_Q) + (NSA_KVW,) * 6 + (NSA_GATES,)
C_IN = sum(IN_SIZES)

kernel_name = 'hybrid_diffattn_nsa_moe_deepnorm'


def _alibi_slopes(n):
    return jnp.asarray(2.0 ** (-8.0 * np.arange(1, n + 1) / n), jnp.float32)


def _layer_norm(x, g, b):
    xf = x.astype(jnp.float32)
    mu = jnp.mean(xf, -1, keepdims=True)
    var = jnp.mean(jnp.square(xf - mu), -1, keepdims=True)
    return ((xf - mu) * lax.rsqrt(var + LN_EPS) * g + b).astype(x.dtype)


def _rms_norm(x, g):
    xf = x.astype(jnp.float32)
    return (xf * lax.rsqrt(jnp.mean(xf * xf, -1, keepdims=True) + LN_EPS) * g).astype(x.dtype)


def _masked_softmax(s, mask):
    pr = jax.nn.softmax(jnp.where(mask, s, NEG), axis=-1)
    return jnp.where(mask, pr, 0.0)


def _diff_attention(q, k, v, lq1, lk1, lq2, lk2, subln_g, lambda_init):
    B, S = q.shape[:2]
    lam = (jnp.exp(jnp.sum(lq1.astype(jnp.float32) * lk1))
           - jnp.exp(jnp.sum(lq2.astype(jnp.float32) * lk2)) + lambda_init)
    slopes = _alibi_slopes(DIFF_HEADS)[None, :, None, None, None]
    scale = DIFF_HD ** -0.5
    outs = []
    for qs in range(0, S, Q_BLOCK):
        qe = qs + Q_BLOCK
        s = jnp.einsum('bqhcd,bkhcd->bhcqk', q[:, qs:qe], k[:, :qe]).astype(jnp.float32) * scale
        dist = jnp.arange(qs, qe)[:, None] - jnp.arange(qe)[None, :]
        s = s - slopes * dist.astype(jnp.float32)
        pr = _masked_softmax(s, dist >= 0)
        a = pr[:, :, 0] - lam * pr[:, :, 1]
        outs.append(jnp.einsum('bhqk,bkhe->bqhe', a.astype(v.dtype), v[:, :qe]))
    o = _rms_norm(jnp.concatenate(outs, axis=1), subln_g) * (1.0 - lambda_init)
    return o.reshape(B, S, DIFF_V).astype(q.dtype)


def _nsa_attention(q, kc, vc, ks, vs, kw, vw, gates, pos_k, pos_v, phi_k1, phi_k2, phi_v1, phi_v2):
    B, S = q.shape[:2]
    G, HPG, QB = NSA_KV, NSA_HEADS // NSA_KV, NSA_Q_BLOCK
    scale = NSA_HD ** -0.5
    slopes = _alibi_slopes(NSA_HEADS).reshape(G, HPG)
    n_cmp = (S - CMP_LEN) // CMP_STRIDE + 1
    cmp_idx = np.arange(n_cmp)[:, None] * CMP_STRIDE + np.arange(CMP_LEN)[None, :]
    cmp_pos = jnp.asarray(cmp_idx[:, -1], jnp.int32)

    def compress(t, pos, w1, w2):
        blk = t[:, cmp_idx] + pos[:, None, :]
        blk = blk.transpose(0, 1, 3, 2, 4).reshape(B, n_cmp, G, CMP_LEN * NSA_HD)
        return jax.nn.silu(blk @ w1) @ w2

    k_cmp = compress(kc, pos_k, phi_k1, phi_k2)
    v_cmp = compress(vc, pos_v, phi_v1, phi_v2)
    n_slc = S // SLC_LEN
    n_top = min(SLC_TOPN, n_slc)
    c_lo = np.arange(n_cmp) * CMP_STRIDE
    s_lo = np.arange(n_slc) * SLC_LEN
    ov = (np.minimum(c_lo[:, None] + CMP_LEN, s_lo[None, :] + SLC_LEN)
          - np.maximum(c_lo[:, None], s_lo[None, :]))
    cmp_to_slc = jnp.asarray(np.clip(ov, 0, None) / CMP_LEN, jnp.float32)
    ks_blk = ks.reshape(B, n_slc, SLC_LEN, G, NSA_HD).transpose(0, 3, 1, 2, 4)
    vs_blk = vs.reshape(B, n_slc, SLC_LEN, G, NSA_HD).transpose(0, 3, 1, 2, 4)
    kw_pad = jnp.pad(kw, ((0, 0), (WIN, 0), (0, 0), (0, 0)))
    vw_pad = jnp.pad(vw, ((0, 0), (WIN, 0), (0, 0), (0, 0)))
    b_ix = jnp.arange(B)[:, None, None, None]
    g_ix = jnp.arange(G)[None, :, None, None]
    blk_ids = jnp.arange(n_slc, dtype=jnp.int32)
    nb = S // QB
    q_b = q.reshape(B, nb, QB, G, HPG, NSA_HD).transpose(1, 0, 2, 3, 4, 5)
    g_b = gates.reshape(B, nb, QB, G, HPG, 3).transpose(1, 0, 2, 3, 4, 5)
    starts = jnp.arange(nb, dtype=jnp.int32) * QB

    def block(args):
        qb, gb, qs = args
        t = qs + jnp.arange(QB, dtype=jnp.int32)
        d_c = t[:, None] - cmp_pos[None, :]
        sc = jnp.einsum('bqghd,bngd->bghqn', qb, k_cmp).astype(jnp.float32) * scale
        sc = sc - slopes[None, :, :, None, None] * d_c.astype(jnp.float32)
        pc = _masked_softmax(sc, d_c >= 0)
        o_cmp = jnp.einsum('bghqn,bngd->bqghd', pc.astype(v_cmp.dtype), v_cmp)
        imp = jnp.einsum('bghqn,nj->bgqj', pc, cmp_to_slc)
        cur = (t // SLC_LEN)[:, None]
        forced = (blk_ids[None] == 0) | (blk_ids[None] == cur) | (blk_ids[None] == cur - 1)
        imp = jnp.where(forced, BIG, jnp.where(blk_ids[None] <= cur, imp, NEG))
        _, sel = lax.top_k(imp, n_top)
        k_sel = ks_blk[b_ix, g_ix, sel]
        v_sel = vs_blk[b_ix, g_ix, sel]
        d_s = t[None, None, :, None, None] - (sel[..., None] * SLC_LEN
                                              + jnp.arange(SLC_LEN, dtype=jnp.int32))
        ss = jnp.einsum('bqghd,bgqnld->bghqnl', qb, k_sel).astype(jnp.float32) * scale
        ss = ss - slopes[None, :, :, None, None, None] * d_s[:, :, None].astype(jnp.float32)
        ps = _masked_softmax(ss.reshape(B, G, HPG, QB, n_top * SLC_LEN),
                             (d_s >= 0).reshape(B, G, 1, QB, n_top * SLC_LEN))
        o_slc = jnp.einsum('bghqnl,bgqnld->bqghd',
                           ps.reshape(B, G, HPG, QB, n_top, SLC_LEN).astype(v_sel.dtype), v_sel)
        k_win = lax.dynamic_slice_in_dim(kw_pad, qs, WIN + QB, axis=1)
        v_win = lax.dynamic_slice_in_dim(vw_pad, qs, WIN + QB, axis=1)
        d_w = t[:, None] - (qs - WIN + jnp.arange(WIN + QB, dtype=jnp.int32))[None, :]
        sw = jnp.einsum('bqghd,bkgd->bghqk', qb, k_win).astype(jnp.float32) * scale
        sw = sw - slopes[None, :, :, None, None] * d_w.astype(jnp.float32)
        pw = _masked_softmax(sw, (d_w >= 0) & (d_w < WIN) & (d_w <= t[:, None]))
        o_win = jnp.einsum('bghqk,bkgd->bqghd', pw.astype(v_win.dtype), v_win)
        out = gb[..., 0:1] * o_cmp + gb[..., 1:2] * o_slc + gb[..., 2:3] * o_win
        return out.astype(qb.dtype)

    o = lax.map(block, (q_b, g_b, starts))
    return o.transpose(1, 0, 2, 3, 4, 5).reshape(B, S, NSA_Q)


def _moe(h, w_router, b_router, w1, b1, w2, b2):
    B, S, D = h.shape
    xt = h.reshape(-1, D)
    T = xt.shape[0]
    logits = (xt @ w_router + b_router).astype(jnp.float32)
    top_v, top_e = lax.top_k(logits, TOP_K)
    gate = jax.nn.softmax(top_v, axis=-1)
    flat_e = top_e.reshape(-1).astype(jnp.int32)
    flat_g = gate.reshape(-1)
    flat_t = jnp.arange(T * TOP_K, dtype=jnp.int32) // TOP_K
    order = jnp.argsort(flat_e)
    se = flat_e[order]
    counts = jnp.zeros(N_EXPERTS, jnp.int32).at[flat_e].add(1)
    starts = jnp.cumsum(counts) - counts
    pcounts = (counts + MOE_BLOCK - 1) // MOE_BLOCK * MOE_BLOCK
    pend = jnp.cumsum(pcounts)
    pstart = pend - pcounts
    dest = pstart[se] + (jnp.arange(T * TOP_K, dtype=jnp.int32) - starts[se])
    n_blocks = (T * TOP_K + MOE_BLOCK - 1) // MOE_BLOCK + N_EXPERTS
    n_pad = n_blocks * MOE_BLOCK
    slot_tok = jnp.zeros(n_pad, jnp.int32).at[dest].set(flat_t[order])
    slot_gate = jnp.zeros(n_pad, jnp.float32).at[dest].set(flat_g[order])
    blk_expert = jnp.minimum(
        jnp.searchsorted(pend, jnp.arange(n_blocks, dtype=jnp.int32) * MOE_BLOCK, side='right'),
        N_EXPERTS - 1).astype(jnp.int32)

    def expert_block(args):
        e, tok = args
        hb = xt[tok] @ w1[e] + b1[e]
        x_glu, x_lin = jnp.split(hb, 2, axis=-1)
        x_glu = jnp.minimum(x_glu, SWIGLU_LIMIT)
        x_lin = jnp.clip(x_lin, -SWIGLU_LIMIT, SWIGLU_LIMIT)
        act = x_glu * jax.nn.sigmoid(SWIGLU_ALPHA * x_glu) * (x_lin + 1.0)
        return act @ w2[e] + b2[e]

    y = lax.map(expert_block, (blk_expert, slot_tok.reshape(n_blocks, MOE_BLOCK)))
    y = y.reshape(n_pad, D) * slot_gate[:, None].astype(y.dtype)
    out = jnp.zeros((T, D), y.dtype).at[slot_tok].add(y)
    return out.reshape(B, S, D).astype(h.dtype)


def setup_inputs(seed: int = 0) -> dict:
    key = jax.random.key(seed)
    keys = iter(jax.random.split(key, 40))

    def nrm(shape, scale):
        return jax.random.normal(next(keys), shape, jnp.float32) * scale

    L, D = DEPTH, D_MODEL
    return {
        'x': nrm((BATCH, SEQ, D), 1.0),
        'p': nrm((L, BATCH, SEQ, PLE_DIM), 1.0),
        'w_in': nrm((L, D, C_IN), D ** -0.5),
        'diff_lq1': nrm((L, DIFF_HD), 0.1),
        'diff_lk1': nrm((L, DIFF_HD), 0.1),
        'diff_lq2': nrm((L, DIFF_HD), 0.1),
        'diff_lk2': nrm((L, DIFF_HD), 0.1),
        'diff_subln_g': 1.0 + nrm((L, 2 * DIFF_HD), 0.02),
        'nsa_pos_k': nrm((L, CMP_LEN, NSA_HD), 0.1),
        'nsa_pos_v': nrm((L, CMP_LEN, NSA_HD), 0.1),
        'nsa_phi_k1': nrm((L, CMP_LEN * NSA_HD, PHI_HID), (CMP_LEN * NSA_HD) ** -0.5),
        'nsa_phi_k2': nrm((L, PHI_HID, NSA_HD), PHI_HID ** -0.5),
        'nsa_phi_v1': nrm((L, CMP_LEN * NSA_HD, PHI_HID), (CMP_LEN * NSA_HD) ** -0.5),
        'nsa_phi_v2': nrm((L, PHI_HID, NSA_HD), PHI_HID ** -0.5),
        'w_br_diff': nrm((L, DIFF_V, D), DIFF_V ** -0.5),
        'w_br_nsa': nrm((L, NSA_Q, D), NSA_Q ** -0.5),
        'w_mgate': nrm((L, D, 2 * D), D ** -0.5),
        'b_mgate': nrm((L, 2 * D), 0.01),
        'w_o': nrm((L, D, D), D ** -0.5 * DEEPNORM_BETA),
        'ln1_g': 1.0 + nrm((L, D), 0.02),
        'ln1_b': nrm((L, D), 0.01),
        'w_router': nrm((L, D, N_EXPERTS), D ** -0.5),
        'b_router': nrm((L, N_EXPERTS), 0.01),
        'w_e1': nrm((L, N_EXPERTS, D, 2 * D_FF), D ** -0.5),
        'b_e1': nrm((L, N_EXPERTS, 2 * D_FF), 0.01),
        'w_e2': nrm((L, N_EXPERTS, D_FF, D), D_FF ** -0.5 * DEEPNORM_BETA),
        'b_e2': nrm((L, N_EXPERTS, D), 0.01),
        'w_ple_gate': nrm((L, D, D), D ** -0.5),
        'b_ple_gate': nrm((L, D), 0.01),
        'w_ple_proj': nrm((L, PLE_DIM, D), PLE_DIM ** -0.5 * DEEPNORM_BETA),
        'ln2_g': 1.0 + nrm((L, D), 0.02),
        'ln2_b': nrm((L, D), 0.01),
    }


def reference(x, p, w_in, diff_lq1, diff_lk1, diff_lq2, diff_lk2, diff_subln_g,
              nsa_pos_k, nsa_pos_v, nsa_phi_k1, nsa_phi_k2, nsa_phi_v1, nsa_phi_v2,
              w_br_diff, w_br_nsa, w_mgate, b_mgate, w_o, ln1_g, ln1_b,
              w_router, b_router, w_e1, b_e1, w_e2, b_e2,
              w_ple_gate, b_ple_gate, w_ple_proj, ln2_g, ln2_b):
    B, S, D = x.shape
    splits = np.cumsum(IN_SIZES)[:-1].tolist()
    for i in range(DEPTH):
        lambda_init = 0.8 - 0.6 * math.exp(-0.3 * i)
        proj = x @ w_in[i]
        dq, dk, dv, nq, kc, vc, ks, vs, kw, vw, ng = jnp.split(proj, splits, axis=-1)
        o_diff = _diff_attention(
            dq.reshape(B, S, DIFF_HEADS, 2, DIFF_HD), dk.reshape(B, S, DIFF_HEADS, 2, DIFF_HD),
            dv.reshape(B, S, DIFF_HEADS, 2 * DIFF_HD),
            diff_lq1[i], diff_lk1[i], diff_lq2[i], diff_lk2[i], diff_subln_g[i], lambda_init)
        kvr = lambda t: t.reshape(B, S, NSA_KV, NSA_HD)
        o_nsa = _nsa_attention(
            nq.reshape(B, S, NSA_HEADS, NSA_HD), kvr(kc), kvr(vc), kvr(ks), kvr(vs), kvr(kw), kvr(vw),
            jax.nn.sigmoid(ng).reshape(B, S, NSA_HEADS, 3),
            nsa_pos_k[i], nsa_pos_v[i], nsa_phi_k1[i], nsa_phi_k2[i], nsa_phi_v1[i], nsa_phi_v2[i])
        mg = jax.nn.sigmoid(x @ w_mgate[i] + b_mgate[i])
        mixed = mg[..., :D] * (o_diff @ w_br_diff[i]) + mg[..., D:] * (o_nsa @ w_br_nsa[i])
        h = _layer_norm(DEEPNORM_ALPHA * x + mixed @ w_o[i], ln1_g[i], ln1_b[i])
        ple = jax.nn.sigmoid(h @ w_ple_gate[i] + b_ple_gate[i]) * (p[i] @ w_ple_proj[i])
        ffn = _moe(h, w_router[i], b_router[i], w_e1[i], b_e1[i], w_e2[i], b_e2[i])
        x = _layer_norm(DEEPNORM_ALPHA * h + ffn + ple, ln2_g[i], ln2_b[i])
    return x
```

```python
import contextlib
import os
import math
import numpy as np
import ml_dtypes
import concourse.bass as bass
import concourse.mybir as mybir
from concourse.bass_utils import run_bass_kernel_spmd

F32 = mybir.dt.float32
BF16 = mybir.dt.bfloat16
AF = mybir.ActivationFunctionType
ALU = mybir.AluOpType
AX = mybir.AxisListType

S = 4096
D = 1024
NQT = 32
NEGB = -131072.0
SCALE = 0.125
LN_EPS = 1e-5
ALPHA = 2.0 ** 0.25
LAMBDA_INIT = 0.2
SEM_LIMIT = 30000
SKIP_SAME = set(os.environ.get('KSKIP', '').split(',')) - {''}
CAP = 768
NSLOT = 32 * CAP + 128


class Res:
    __slots__ = ("w", "r")

    def __init__(self):
        self.w = None
        self.r = {}


class Eng:
    def __init__(self, sch, name, eng):
        self.sch = sch
        self.name = name
        self.eng = eng
        self.sem = sch.new_sem(name)
        self.cnt = 0
        self.known = {}
        self.slots = []
        self.slot_i = 0


class Sched:
    def __init__(self, nc, es):
        self.nc = nc
        self.es = es
        self.nsem = 0
        self.sems = {}
        self.pe = Eng(self, "pe", nc.tensor)
        self.act = Eng(self, "act", nc.scalar)
        self.dve = Eng(self, "dve", nc.vector)
        self.pool = Eng(self, "pool", nc.gpsimd)
        self.sp = Eng(self, "sp", nc.sync)
        self.engs = [self.pe, self.act, self.dve, self.pool, self.sp]
        for e, n in ((self.sp, 24), (self.pool, 16), (self.act, 4)):
            for i in range(n):
                e.slots.append([self.new_sem(f"{e.name}_d{i}"), 0])
        self.n_ins = 0

    def new_sem(self, name):
        self.nsem += 1
        s = self.es.enter_context(self.nc.semaphore(f"s{self.nsem}_{name}"))
        self.sems[id(s)] = s
        return s

    def _wait(self, E, toks):
        for sem, val in toks:
            k = id(sem)
            if E.known.get(k, 0) >= val:
                continue
            E.eng.wait_ge(sem, val)
            E.known[k] = val

    def _deps(self, E, reads, writes):
        deps = []
        for r in reads:
            if r.w is not None:
                deps.append(r.w)
        for w in writes:
            if w.w is not None:
                deps.append(w.w)
            deps.extend(w.r.values())
        if E is self.pe or E.name in SKIP_SAME:
            deps = [d for d in deps if d[0] is not E.sem]
        return deps

    def _commit(self, tok, reads, writes):
        for r in reads:
            k = id(tok[0])
            r.r[k] = tok
        for w in writes:
            w.w = tok
            w.r = {}

    def op(self, E, fn, reads=(), writes=()):
        self._wait(E, self._deps(E, reads, writes))
        ins = fn()
        E.cnt += 1
        ins.then_inc(E.sem, 1)
        tok = (E.sem, E.cnt)
        self._commit(tok, reads, writes)
        self.n_ins += 1
        if E.cnt >= SEM_LIMIT:
            E.sem = self.new_sem(E.name)
            E.cnt = 0
        return tok

    def dma(self, E, out, in_, reads=(), writes=(), **kw):
        slot = E.slots[E.slot_i]
        E.slot_i = (E.slot_i + 1) % len(E.slots)
        deps = self._deps(E, reads, writes)
        if slot[1] > 0:
            deps.append((slot[0], slot[1]))
        self._wait(E, deps)
        if slot[1] + 16 > SEM_LIMIT:
            slot[0] = self.new_sem(E.name + "_d")
            slot[1] = 0
        if callable(out):
            out().then_inc(slot[0], 16)
        else:
            E.eng.dma_start(out=out, in_=in_, **kw).then_inc(slot[0], 16)
        slot[1] += 16
        tok = (slot[0], slot[1])
        self._commit(tok, reads, writes)
        self.n_ins += 1
        return tok

    def barrier(self):
        toks = []
        for e in self.engs:
            if e.cnt > 0:
                toks.append((e.sem, e.cnt))
            for s in e.slots:
                if s[1] > 0:
                    toks.append((s[0], s[1]))
        for e in self.engs:
            self._wait(e, toks)


def _bf(x):
    return np.asarray(x, np.float32).astype(ml_dtypes.bfloat16)


def _hi_lo(v):
    v = np.asarray(v, np.float64)
    hi = v.astype(np.float32).astype(ml_dtypes.bfloat16).astype(np.float64)
    lo = (v - hi)
    return hi, lo


def _const_tables():
    t = {}
    pos = np.arange(S)
    augk = np.stack([pos // 128, pos // 128, pos % 128, pos % 128, np.ones(S), np.ones(S)]).astype(np.float32)
    t["augk_tok"] = _bf(augk)
    cpos = np.arange(255) * 16 + 31
    augc = np.zeros((6, 256), np.float32)
    augc[:, :255] = np.stack([cpos // 128, cpos // 128, cpos % 128, cpos % 128, np.ones(255), np.ones(255)])
    t["augk_cmp"] = _bf(augc)
    slopes = list(2.0 ** (-8.0 * np.arange(1, 9) / 8)) + list(2.0 ** (-8.0 * np.arange(1, 17) / 16))
    augq = np.zeros((24, 6, S), np.float32)
    for i, s in enumerate(slopes):
        shi, slo = _hi_lo(s)
        augq[i, 0] = 1024.0 * shi
        augq[i, 1] = 1024.0 * slo
        augq[i, 2] = 8.0 * shi
        augq[i, 3] = 8.0 * slo
        hi, lo = _hi_lo(-8.0 * s * pos)
        augq[i, 4] = hi
        augq[i, 5] = lo
    t["augq"] = _bf(augq)
    kk = np.arange(128)[:, None]
    qq = np.arange(128)[None, :]
    t["m_caus"] = _bf(np.where(qq >= kk, 0.0, NEGB))
    t["m_band"] = _bf(np.where(qq < kk, 0.0, NEGB))
    t["ident"] = _bf(np.eye(128))
    n = np.arange(256).reshape(2, 128)
    valid = ((n[:, :, None] * 16 + 31) <= pos[None, None, :]) & (n[:, :, None] < 255)
    t["m_cmp"] = _bf(np.where(valid, 0.0, NEGB).transpose(1, 0, 2))
    c_lo = np.arange(255) * 16
    s_lo = np.arange(64) * 64
    ov = np.minimum(c_lo[:, None] + 32, s_lo[None, :] + 64) - np.maximum(c_lo[:, None], s_lo[None, :])
    c2s = np.zeros((256, 64), np.float32)
    c2s[:255] = np.clip(ov, 0, None) / 32.0
    t["c2s"] = _bf(c2s.reshape(2, 128, 64).transpose(1, 0, 2))
    cur = (pos // 64)[:, None]
    j = np.arange(64)[None, :]
    forced = (j == 0) | (j == cur) | (j == cur - 1)
    keep = (~forced) & (j <= cur)
    tf = np.where(forced, 1e30, np.where(j <= cur, 0.0, -1e30)).astype(np.float32)
    t["sel_keep"] = np.ascontiguousarray(keep.astype(np.float32).reshape(32, 128, 64).transpose(1, 0, 2))
    t["sel_force"] = np.ascontiguousarray(tf.reshape(32, 128, 64).transpose(1, 0, 2))
    t["expand"] = _bf((np.arange(64)[:, None] == (pos // 64)[None, :]).astype(np.float32))
    t["triu"] = _bf((np.arange(128)[:, None] < np.arange(128)[None, :]).astype(np.float32))
    t["ones128"] = _bf(np.ones((128, 128)))
    t["iota32"] = np.tile(np.arange(32, dtype=np.float32)[None, :], (128, 1))
    t["trp"] = (32 * CAP + np.arange(128, dtype=np.float32)).reshape(128, 1)
    return t


WEIGHT_NAMES = [
    "w_in", "diff_lq1", "diff_lk1", "diff_lq2", "diff_lk2", "diff_subln_g",
    "nsa_pos_k", "nsa_pos_v", "nsa_phi_k1", "nsa_phi_k2", "nsa_phi_v1", "nsa_phi_v2",
    "w_br_diff", "w_br_nsa", "w_mgate", "b_mgate", "w_o", "ln1_g", "ln1_b",
    "w_router", "b_router", "w_e1", "b_e1", "w_e2", "b_e2",
    "w_ple_gate", "b_ple_gate", "w_ple_proj", "ln2_g", "ln2_b",
]


class Buf:
    __slots__ = ("t", "res")

    def __init__(self, t):
        self.t = t
        self.res = Res()


class KB:
    def __init__(self, dbg=()):
        self.dbg = set(dbg)
        self.nc = bass.Bass("TRN2", target_bir_lowering=False)
        self.es = contextlib.ExitStack()
        self.sc = Sched(self.nc, self.es)
        self.din = {}
        self.rot = {}

    def inp(self, name, shape, dt=F32):
        t = self.nc.dram_tensor(name, list(shape), dt, kind="ExternalInput").ap()
        self.din[name] = t
        return t

    def scratch(self, name, shape, dt):
        kind = "ExternalOutput" if name in self.dbg else "Internal"
        return self.nc.dram_tensor(name, list(shape), dt, kind=kind).ap()

    def sb(self, es, name, shape, dt):
        return Buf(es.enter_context(self.nc.sbuf_tensor("sb_" + name, list(shape), dt)))

    def ps(self, es, name, shape, dt):
        return Buf(es.enter_context(self.nc.psum_tensor("ps_" + name, list(shape), dt)))

    def nxt(self, key, lst):
        i = self.rot.get(key, 0)
        self.rot[key] = i + 1
        return lst[i % len(lst)]


def build(dbg=(), phases=(1, 2, 3, 4, 5)):
    k = KB(dbg)
    nc, sc = k.nc, k.sc
    pe, act, dve, pool, sp = sc.pe, sc.act, sc.dve, sc.pool, sc.sp
    V, G, T = nc.vector, nc.gpsimd, nc.tensor
    A = nc.scalar

    SHAPES = {
        "x": ([S, D], F32), "xT": ([D, S], F32), "pT": ([256, S], F32), "w_in": ([D, 5680], F32),
        "diff_lq1": ([1, 64], F32), "diff_lk1": ([1, 64], F32), "diff_lq2": ([1, 64], F32), "diff_lk2": ([1, 64], F32),
        "diff_subln_g": ([1, 128], F32), "nsa_pos_kT": ([64, 32], F32), "nsa_pos_vT": ([64, 32], F32),
        "nsa_phi_k1": ([64, 32, 256], F32), "nsa_phi_v1": ([64, 32, 256], F32),
        "nsa_phi_k1s": ([128, 16, 256], F32), "nsa_phi_v1s": ([128, 16, 256], F32), "nsa_pos_kTs": ([128, 16], F32), "nsa_pos_vTs": ([128, 16], F32),
        "nsa_phi_k2": ([256, 64], F32), "nsa_phi_v2": ([256, 64], F32),
        "w_br_diff": ([D, D], F32), "w_br_nsa": ([D, D], F32), "w_mgate": ([D, 2 * D], F32), "b_mgate": ([1, 2 * D], F32),
        "w_o": ([D, D], F32), "ln1_g": ([1, D], F32), "ln1_b": ([1, D], F32),
        "w_router": ([D, 32], F32), "b_router": ([1, 32], F32),
        "w_e1": ([32, D, 2 * D], F32), "b_e1T": ([128, 512], F32), "w_e2": ([32, D, D], F32), "b_e2": ([32, D], F32),
        "w_ple_gate": ([D, D], F32), "b_ple_gate": ([1, D], F32), "w_ple_proj": ([256, D], F32),
        "ln2_g": ([1, D], F32), "ln2_b": ([1, D], F32),
        "augk_tok": ([6, S], BF16), "augk_cmp": ([6, 256], BF16), "augq": ([24, 6, S], BF16),
        "m_caus": ([128, 128], BF16), "m_band": ([128, 128], BF16), "ident": ([128, 128], BF16),
        "m_cmp": ([128, 2, S], BF16), "c2s": ([128, 2, 64], BF16),
        "sel_keep": ([128, 32, 64], F32), "sel_force": ([128, 32, 64], F32), "expand": ([64, S], BF16),
        "triu": ([128, 128], BF16), "ones128": ([128, 128], BF16), "iota32": ([128, 32], F32), "trp": ([128, 1], F32),
    }

    def I(name):
        if name not in k.din:
            shp, dt = SHAPES[name]
            k.inp(name, shp, dt)
        return k.din[name]

    out_d = nc.dram_tensor("out", [S, D], F32, kind="ExternalOutput").ap()

    QdT = k.scratch("QdT", [D, S], BF16); KdT = k.scratch("KdT", [D, S], BF16)
    Vd = k.scratch("Vd", [S, D], BF16); NqT = k.scratch("NqT", [D, S], BF16)
    kcT = k.scratch("kcT", [256, S], BF16); vcT = k.scratch("vcT", [256, S], BF16)
    ksT = k.scratch("ksT", [256, S], BF16); vs_d = k.scratch("vs", [S, 256], BF16)
    kwT = k.scratch("kwT", [256, S], BF16); vw_d = k.scratch("vw", [S, 256], BF16)
    gates_d = k.scratch("gates", [S, 48], F32)
    odiff_d = k.scratch("o_diff", [S, D], BF16); onsa_d = k.scratch("o_nsa", [S, D], BF16)
    hT_d = k.scratch("hT", [D, S], BF16); base_d = k.scratch("base", [S, D], F32)
    G_d = k.scratch("Gd", [S, 32], F32)
    xs_d = k.scratch("xs", [NSLOT, D], BF16); ys_d = k.scratch("ys", [NSLOT, D], F32)

    es0 = k.es
    banks = [k.ps(es0, f"bank{i}", [128, 512], F32) for i in range(7)]
    tbank = k.ps(es0, "tbank", [128, 1024], BF16)
    ident = k.sb(es0, "ident", [128, 128], BF16)
    mcaus = k.sb(es0, "mcaus", [128, 128], BF16)
    mband = k.sb(es0, "mband", [128, 128], BF16)
    sc.dma(sp, ident.t[:], I("ident")[:, :], writes=[ident.res])
    wz = k.sb(es0, "wz", [128, 512], BF16)
    sc.op(pool, lambda: G.memset(wz.t[:, :], 0.0), writes=[wz.res])
    epsb = k.sb(es0, "epsb", [128, 1], F32)
    sc.op(pool, lambda: G.memset(epsb.t[:, :], LN_EPS), writes=[epsb.res])
    sc.dma(sp, mcaus.t[:], I("m_caus")[:, :], writes=[mcaus.res])
    sc.dma(sp, mband.t[:], I("m_band")[:, :], writes=[mband.res])

    U32 = mybir.dt.uint32
    dest_all = k.sb(es0, "dest_all", [128, 32, 4], U32)
    gk_all = k.sb(es0, "gk_all", [128, 32, 4], F32)
    dest_res = [Res() for _ in range(32)]
    gk_res = [Res() for _ in range(32)]
    evac_i = [0]

    def evac_copy(out_ap, in_ap, reads, writes):
        evac_i[0] += 1
        if evac_i[0] % 2:
            return sc.op(act, lambda: A.copy(out=out_ap, in_=in_ap), reads=reads, writes=writes)
        return sc.op(dve, lambda: V.tensor_copy(out=out_ap, in_=in_ap), reads=reads, writes=writes)

    def phase1():
        with contextlib.ExitStack() as es:
            xT = k.sb(es, "xT", [128, 8, S], BF16)
            rx = [Res() for _ in range(8)]
            for kc in range(8):
                sc.dma(pool, xT.t[:, kc, :], I("xT")[kc * 128:(kc + 1) * 128, :], writes=[rx[kc]])
            wb = [k.sb(es, f"wb{i}", [128, 8, 1024], BF16) for i in range(2)]
            sfm = [k.sb(es, f"sfm{i}", [128, S], BF16) for i in range(2)]
            stm = [k.sb(es, f"stm{i}", [128, 4, 1024], BF16) for i in range(2)]
            sg = k.sb(es, "sgate", [128, 32, 48], F32)
            loads = [(0, 1024), (1024, 1024), (2048, 1024), (3072, 1024), (4096, 1024), (5120, 560)]
            subs = [
                [(0, 1024, "fm", QdT, 0)], [(0, 1024, "fm", KdT, 0)], [(0, 1024, "tm", Vd, 0)],
                [(0, 1024, "fm", NqT, 0)],
                [(0, 256, "fm", kcT, 0), (256, 256, "fm", vcT, 0), (512, 256, "fm", ksT, 0), (768, 256, "tm", vs_d, 0)],
                [(0, 256, "fm", kwT, 0), (256, 256, "tm", vw_d, 0), (512, 48, "gate", None, 0)],
            ]
            bi = 0
            for li, (c0, n) in enumerate(loads):
                w = wb[li % 2]
                src = I("w_in")[:, c0:c0 + n].rearrange("(kc p) n -> p kc n", p=128)
                sc.dma(pool, w.t[:, :, 0:n], src, writes=[w.res])
                for (b0, nn, kind, dst, _) in subs[li]:
                    if kind == "fm":
                        for cc in range(nn // 128):
                            st = k.nxt("sfm", sfm)
                            for qt in range(8):
                                bk = k.nxt("bank", banks)
                                for kc in range(8):
                                    sc.op(pe, lambda: T.matmul(bk.t[:, :], lhsT=w.t[:, kc, b0 + cc * 128:b0 + (cc + 1) * 128],
                                                               rhs=xT.t[:, kc, qt * 512:(qt + 1) * 512],
                                                               start=(kc == 0), stop=(kc == 7)),
                                          reads=[w.res, rx[kc]], writes=[bk.res])
                                evac_copy(st.t[:, qt * 512:(qt + 1) * 512], bk.t[:, :], [bk.res], [st.res])
                            sc.dma(sp, dst[cc * 128:(cc + 1) * 128, :], st.t[:, :], reads=[st.res])
                    elif kind == "tm":
                        for t4 in range(8):
                            st = k.nxt("stm", stm)
                            for ti in range(4):
                                tt = t4 * 4 + ti
                                for ch in range((nn + 511) // 512):
                                    cw = min(512, nn - ch * 512)
                                    bk = k.nxt("bank", banks)
                                    for kc in range(8):
                                        sc.op(pe, lambda: T.matmul(bk.t[:, 0:cw], lhsT=xT.t[:, kc, tt * 128:(tt + 1) * 128],
                                                                   rhs=w.t[:, kc, b0 + ch * 512:b0 + ch * 512 + cw],
                                                                   start=(kc == 0), stop=(kc == 7)),
                                              reads=[w.res, rx[kc]], writes=[bk.res])
                                    evac_copy(st.t[:, ti, ch * 512:ch * 512 + cw], bk.t[:, 0:cw], [bk.res], [st.res])
                            sc.dma(sp, dst[t4 * 512:(t4 + 1) * 512, :].rearrange("(t p) c -> p t c", p=128),
                                   st.t[:, :, 0:nn], reads=[st.res])
                    else:
                        for tt in range(32):
                            bk = k.nxt("bank", banks)
                            for kc in range(8):
                                sc.op(pe, lambda: T.matmul(bk.t[:, 0:48], lhsT=xT.t[:, kc, tt * 128:(tt + 1) * 128],
                                                           rhs=w.t[:, kc, b0:b0 + 48], start=(kc == 0), stop=(kc == 7)),
                                      reads=[w.res, rx[kc]], writes=[bk.res])
                            sc.op(act, lambda: A.activation(out=sg.t[:, tt, :], in_=bk.t[:, 0:48], func=AF.Sigmoid),
                                  reads=[bk.res], writes=[sg.res])
                        sc.dma(sp, gates_d.rearrange("(t p) c -> p t c", p=128), sg.t[:, :, :], reads=[sg.res])
            sc.barrier()

    sbanks = banks[0:3]
    accs = banks[3:7]

    FILL = [int(os.environ.get('KFILL', '0'))]
    KDIM = int(os.environ.get('KDIM', '128'))

    def attn_pass(es_p, QT, KT, kdim, Vt, dvp, specs, evac_fn, reads, pbufs):
        items = []
        covers = {}
        for j in range(8):
            spj = specs[j]
            if not spj:
                continue
            covers[j] = {sub: [i for i, s_ in enumerate(spj) if s_[1] <= sub * 128 < s_[2]] for sub in range(4)}
            for i in range(len(spj)):
                items.append((j, i))

        def emit_S(n):
            j, i = items[n]
            kt, c0, c1, masks = specs[j][i]
            bk = k.nxt("sbank", sbanks)
            nm = len(masks)
            sc.op(pe, lambda: T.matmul(bk.t[:, c0:c1], lhsT=KT[0:kdim, kt * 128:(kt + 1) * 128],
                                       rhs=QT[0:kdim, j * 512 + c0:j * 512 + c1], start=True, stop=(nm == 0)),
                  reads=reads, writes=[bk.res])
            for mi, (ml, mr, lo, hi, mreads) in enumerate(masks):
                sc.op(pe, lambda: T.matmul(bk.t[:, lo:hi], lhsT=ml, rhs=mr, start=False, stop=(mi == nm - 1)),
                      reads=mreads, writes=[bk.res])
            return bk

        LA = 2
        pend = {}
        for n0 in range(min(LA, len(items))):
            pend[n0] = emit_S(n0)
        for n in range(len(items)):
            if n + LA < len(items):
                pend[n + LA] = emit_S(n + LA)
            j, i = items[n]
            kt, c0, c1, masks = specs[j][i]
            cover = covers[j]
            bk = pend.pop(n)
            pb = k.nxt("pbuf", pbufs)
            if FILL[0] > 0:
                sc.op(pe, lambda: T.matmul(tbank.t[:, :].bitcast(F32)[:, 0:FILL[0]], lhsT=ident.t[:, :], rhs=wz.t[:, 0:FILL[0]],
                                           start=True, stop=True), reads=[], writes=[])
            sc.op(act, lambda: A.activation(out=pb.t[:, c0:c1], in_=bk.t[:, c0:c1], func=AF.Exp, scale=SCALE),
                  reads=[bk.res], writes=[pb.res])
            for sub in range(c0 // 128, c1 // 128):
                ac = accs[sub]
                first = cover[sub][0] == i
                last = cover[sub][-1] == i
                sc.op(pe, lambda: T.matmul(ac.t[:, 0:dvp], lhsT=pb.t[:, sub * 128:(sub + 1) * 128],
                                           rhs=Vt[:, kt, 0:dvp], start=first, stop=last),
                      reads=[pb.res] + list(reads), writes=[ac.res])
                if last:
                    evac_fn(j * 4 + sub, ac)

    def pe_warmup(n=24):
        for i in range(n):
            sc.op(pe, lambda: T.matmul(tbank.t[:, :].bitcast(F32), lhsT=ident.t[:, :], rhs=wz.t[:, :], start=True, stop=True),
                  reads=[ident.res, wz.res], writes=[tbank.res] if i == 0 else [])

    def causal_specs(extra=None):
        specs = []
        for j in range(8):
            l = []
            for kt in range(4 * j + 4):
                d = kt - 4 * j
                c0 = 128 * d if d > 0 else 0
                masks = []
                if extra is not None:
                    masks += extra(j, kt, c0, 512)
                if d >= 0:
                    masks.append((ident.t[:, :], mcaus.t[:, :], 128 * d, 128 * d + 128, [ident.res, mcaus.res]))
                l.append((kt, c0, 512, masks))
            specs.append(l)
        return specs

    def bcast_load(es, name, src_ap, n, eng=None):
        b = k.sb(es, name, [128, n], F32)
        sc.dma(sp, b.t[:, :], src_ap.partition_broadcast(128), writes=[b.res])
        return b

    def phase2():
        with contextlib.ExitStack() as es:
            pbufs = [k.sb(es, f"pb{i}", [128, 512], BF16) for i in range(4)]
            Qb = [k.sb(es, f"dQ{i}", [128, S], BF16) for i in range(2)]
            Kb = [k.sb(es, f"dK{i}", [128, S], BF16) for i in range(2)]
            Vb = [k.sb(es, f"dV{i}", [128, 32, 129], BF16) for i in range(2)]
            A0 = k.sb(es, "dA0", [128, 32, 128], F32)
            A1 = k.sb(es, "dA1", [128, 32, 128], F32)
            Asq = k.sb(es, "dAsq", [128, 32, 128], F32)
            rst = k.sb(es, "drst", [128, 32, 2], F32)
            ost = [k.sb(es, f"dost{i}", [128, 32, 128], BF16) for i in range(2)]
            small = [k.sb(es, f"dsm{i}", [128, 4], F32) for i in range(8)]
            at = [k.sb(es, f"dat{i}", [128, 128], F32) for i in range(3)]
            junk = k.sb(es, "djunk", [128, 128], F32)
            l4 = [bcast_load(es, f"dl{i}", I(n)[:, :], 64) for i, n in
                  enumerate(("diff_lq1", "diff_lk1", "diff_lq2", "diff_lk2"))]
            gsub = bcast_load(es, "dgsub", I("diff_subln_g")[:, :], 128)
            lam = k.sb(es, "dlam", [128, 8], F32)
            sc.op(dve, lambda: V.tensor_tensor(out=l4[0].t[:, :], in0=l4[0].t[:, :], in1=l4[1].t[:, :], op=ALU.mult),
                  reads=[l4[1].res], writes=[l4[0].res])
            sc.op(dve, lambda: V.tensor_tensor(out=l4[2].t[:, :], in0=l4[2].t[:, :], in1=l4[3].t[:, :], op=ALU.mult),
                  reads=[l4[3].res], writes=[l4[2].res])
            sc.op(dve, lambda: V.reduce_sum(out=lam.t[:, 0:1], in_=l4[0].t[:, :], axis=AX.X), reads=[l4[0].res], writes=[lam.res])
            sc.op(dve, lambda: V.reduce_sum(out=lam.t[:, 1:2], in_=l4[2].t[:, :], axis=AX.X), reads=[l4[2].res], writes=[lam.res])
            sc.op(act, lambda: A.activation(out=lam.t[:, 2:4], in_=lam.t[:, 0:2], func=AF.Exp), reads=[lam.res], writes=[lam.res])
            sc.op(dve, lambda: V.scalar_tensor_tensor(out=lam.t[:, 4:5], in0=lam.t[:, 3:4], scalar=-LAMBDA_INIT,
                                                      in1=lam.t[:, 2:3], op0=ALU.add, op1=ALU.subtract),
                  reads=[lam.res], writes=[lam.res])
            sc.op(dve, lambda: V.tensor_scalar(out=gsub.t[:, :], in0=gsub.t[:, :], scalar1=1.0 - LAMBDA_INIT, scalar2=None,
                                               op0=ALU.mult), reads=[gsub.res], writes=[gsub.res])
            for b in Vb:
                sc.op(pool, lambda: G.memset(b.t[:, :, 128:129], 1.0), writes=[b.res])
            for b in Qb + Kb:
                sc.op(pool, lambda: G.memset(b.t[64:128, :], 0.0), writes=[b.res])
            for b in Kb:
                sc.dma(sp, b.t[64:70, :], I("augk_tok")[:, :], writes=[b.res])
            specs = causal_specs()
            for h in range(8):
                if h == 0:
                    pe_warmup()
                vb = Vb[h % 2]
                sc.dma(sp, vb.t[:, :, 0:128], Vd[:, h * 128:(h + 1) * 128].rearrange("(t p) c -> p t c", p=128),
                       writes=[vb.res])
                osb = ost[h % 2]
                for c in range(2):
                    qb, kb_ = Qb[c], Kb[c]
                    r0 = h * 128 + c * 64
                    sc.dma(sp, qb.t[0:64, :], QdT[r0:r0 + 64, :], writes=[qb.res])
                    sc.dma(sp, qb.t[64:70, :], I("augq")[h, :, :], writes=[qb.res])
                    sc.dma(sp, kb_.t[0:64, :], KdT[r0:r0 + 64, :], writes=[kb_.res])

                    def ev(qt, ac, c=c, osb=osb):
                        sm = k.nxt("dsm", small)
                        sc.op(dve, lambda: V.reciprocal(out=sm.t[:, 0:1], in_=ac.t[:, 128:129]), reads=[ac.res], writes=[sm.res])
                        if c == 0:
                            sc.op(dve, lambda: V.tensor_scalar(out=A0.t[:, qt, :], in0=ac.t[:, 0:128], scalar1=sm.t[:, 0:1],
                                                               scalar2=None, op0=ALU.mult),
                                  reads=[ac.res, sm.res], writes=[A0.res])
                            return
                        sc.op(dve, lambda: V.tensor_tensor(out=sm.t[:, 1:2], in0=sm.t[:, 0:1], in1=lam.t[:, 4:5], op=ALU.mult),
                              reads=[lam.res, sm.res], writes=[sm.res])
                        sc.op(dve, lambda: V.scalar_tensor_tensor(out=A1.t[:, qt, :], in0=ac.t[:, 0:128], scalar=sm.t[:, 1:2],
                                                                  in1=A0.t[:, qt, :], op0=ALU.mult, op1=ALU.add),
                              reads=[ac.res, sm.res, A0.res], writes=[A1.res])

                    attn_pass(es, qb.t, kb_.t, KDIM, vb.t, 129, specs, ev, [qb.res, kb_.res, vb.res], pbufs)
                sc.op(dve, lambda: V.tensor_tensor(out=Asq.t[:, :, :], in0=A1.t[:, :, :], in1=A1.t[:, :, :], op=ALU.mult),
                      reads=[A1.res], writes=[Asq.res])
                sc.op(dve, lambda: V.tensor_reduce(out=rst.t[:, :, 0:1], in_=Asq.t[:, :, :], axis=AX.X, op=ALU.add),
                      reads=[Asq.res], writes=[rst.res])
                sc.op(act, lambda: A.activation(out=rst.t[:, :, 1:2], in_=rst.t[:, :, 0:1], func=AF.Ln, bias=epsb.t[:, 0:1],
                                                scale=1.0 / 128.0), reads=[rst.res, epsb.res], writes=[rst.res])
                sc.op(act, lambda: A.activation(out=rst.t[:, :, 1:2], in_=rst.t[:, :, 1:2], func=AF.Exp, scale=-0.5),
                      reads=[], writes=[rst.res])
                sc.op(dve, lambda: V.tensor_tensor(out=Asq.t[:, :, :], in0=A1.t[:, :, :],
                                                   in1=rst.t[:, :, 1:2].to_broadcast([128, 32, 128]), op=ALU.mult),
                      reads=[A1.res, rst.res], writes=[Asq.res])
                sc.op(pool, lambda: G.tensor_tensor(out=osb.t[:, :, :], in0=Asq.t[:, :, :],
                                                    in1=gsub.t[:, :].unsqueeze(1).to_broadcast([128, 32, 128]), op=ALU.mult),
                      reads=[Asq.res, gsub.res], writes=[osb.res])
                sc.dma(pool, odiff_d[:, h * 128:(h + 1) * 128].rearrange("(t p) c -> p t c", p=128), osb.t[:, :, :],
                       reads=[osb.res])
            sc.barrier()

    k.phase2 = phase2
    def phase3():
        with contextlib.ExitStack() as es:
            pbufs = [k.sb(es, f"npb{i}", [128, 512], BF16) for i in range(4)]
            gates = k.sb(es, "ngates", [128, 32, 48], F32)
            sc.dma(sp, gates.t[:, :, :], gates_d.rearrange("(t p) c -> p t c", p=128), writes=[gates.res])
            Kc = k.sb(es, "nKc", [128, 4, 256], BF16)
            Vc = k.sb(es, "nVc", [128, 4, 2, 129], BF16)
            sc.op(pool, lambda: G.memset(Kc.t[:, :, :], 0.0), writes=[Kc.res])
            sc.op(pool, lambda: G.memset(Vc.t[:, :, :, :], 0.0), writes=[Vc.res])
            sc.op(pool, lambda: G.memset(Vc.t[:, :, :, 128:129], 1.0), writes=[Vc.res])
            for g in range(4):
                sc.dma(sp, Kc.t[64:70, g, :], I("augk_cmp")[:, :], writes=[Kc.res])
                sc.dma(sp, Vc.t[:, g, :, 64:128], I("c2s")[:, :, :], writes=[Vc.res])
            with contextlib.ExitStack() as es2:
                w1 = [k.sb(es2, f"cw1{i}", [128, 16, 256], BF16) for i in range(2)]
                w2 = [k.sb(es2, f"cw2{i}", [128, 2, 64], BF16) for i in range(2)]
                posT = [k.sb(es2, f"cpos{i}", [128, 16], BF16) for i in range(2)]
                biasv = k.sb(es2, "cbias", [128, 4], F32)
                srcb = [k.sb(es2, f"csrc{i}", [128, S], BF16) for i in range(2)]
                for b_ in srcb:
                    sc.op(pool, lambda: G.memset(b_.t[64:128, S - 16:S], 0.0), writes=[b_.res])
                hid = [k.sb(es2, f"chid{i}", [128, 2, 256], BF16) for i in range(2)]
                for kv, (n1, n2, npos) in enumerate((("nsa_phi_k1s", "nsa_phi_k2", "nsa_pos_kTs"),
                                                     ("nsa_phi_v1s", "nsa_phi_v2", "nsa_pos_vTs"))):
                    sc.dma(pool, w1[kv].t[:, :, :], I(n1)[:, :, :], writes=[w1[kv].res])
                    sc.dma(pool, w2[kv].t[:, :, :], I(n2).rearrange("(c p) d -> p c d", p=128), writes=[w2[kv].res])
                    sc.dma(pool, posT[kv].t[:, :], I(npos)[:, :], writes=[posT[kv].res])
                for kv in range(2):
                    for hc in range(2):
                        bk = k.nxt("bank", banks)
                        for l in range(16):
                            sc.op(pe, lambda: T.matmul(bk.t[:, 0:1], lhsT=w1[kv].t[:, l, hc * 128:(hc + 1) * 128],
                                                       rhs=posT[kv].t[:, l:l + 1], start=(l == 0), stop=(l == 15)),
                                  reads=[w1[kv].res, posT[kv].res], writes=[bk.res])
                        sc.op(dve, lambda: V.tensor_copy(out=biasv.t[:, kv * 2 + hc:kv * 2 + hc + 1], in_=bk.t[:, 0:1]),
                              reads=[bk.res], writes=[biasv.res])
                for g in range(4):
                    for kv in range(2):
                        sb_ = k.nxt("csrc", srcb)
                        srcd = (kcT if kv == 0 else vcT)
                        sc.dma(sp, sb_.t[0:64, :], srcd[g * 64:(g + 1) * 64, :], writes=[sb_.res])
                        sc.dma(sp, sb_.t[64:128, 0:S - 1], srcd[g * 64:(g + 1) * 64, 1:S], writes=[sb_.res])
                        sv = sb_.t[:, :].rearrange("p (n s) -> p s n", s=16)
                        hb = hid[kv]
                        for hc in range(2):
                            bk = k.nxt("bank", banks)
                            for l2 in range(16):
                                l = 2 * l2
                                sc.op(pe, lambda: T.matmul(bk.t[:, 0:255], lhsT=w1[kv].t[:, l2, hc * 128:(hc + 1) * 128],
                                                           rhs=sv[:, l % 16, (l // 16):(l // 16) + 255],
                                                           start=(l2 == 0), stop=(l2 == 15)),
                                      reads=[w1[kv].res, sb_.res], writes=[bk.res])
                            sc.op(act, lambda: A.activation(out=hb.t[:, hc, 0:255], in_=bk.t[:, 0:255], func=AF.Silu,
                                                            bias=biasv.t[:, kv * 2 + hc:kv * 2 + hc + 1]),
                                  reads=[bk.res, biasv.res], writes=[hb.res])
                        if kv == 0:
                            bk = k.nxt("bank", banks)
                            for hc in range(2):
                                sc.op(pe, lambda: T.matmul(bk.t[0:64, 0:255], lhsT=w2[0].t[:, hc, :], rhs=hb.t[:, hc, 0:255],
                                                           start=(hc == 0), stop=(hc == 1)),
                                      reads=[w2[0].res, hb.res], writes=[bk.res])
                            sc.op(dve, lambda: V.tensor_copy(out=Kc.t[0:64, g, 0:255], in_=bk.t[0:64, 0:255]),
                                  reads=[bk.res], writes=[Kc.res])
                        else:
                            for nt in range(2):
                                m = 128 if nt == 0 else 127
                                bk = k.nxt("bank", banks)
                                for hc in range(2):
                                    sc.op(pe, lambda: T.matmul(bk.t[0:m, 0:64], lhsT=hb.t[:, hc, nt * 128:nt * 128 + m],
                                                               rhs=w2[1].t[:, hc, :], start=(hc == 0), stop=(hc == 1)),
                                          reads=[w2[1].res, hb.res], writes=[bk.res])
                                sc.op(dve, lambda: V.tensor_copy(out=Vc.t[0:m, g, nt, 0:64], in_=bk.t[0:m, 0:64]),
                                      reads=[bk.res], writes=[Vc.res])
                sc.barrier()
            mcmp = k.sb(es, "nmcmp", [128, 2, S], BF16)
            keep = k.sb(es, "nkeep", [128, 32, 64], F32)
            force = k.sb(es, "nforce", [128, 32, 64], F32)
            expand = k.sb(es, "nexpand", [128, S], BF16)
            sc.dma(sp, mcmp.t[:, :, :], I("m_cmp")[:, :, :], writes=[mcmp.res])
            sc.dma(sp, keep.t[:, :, :], I("sel_keep")[:, :, :], writes=[keep.res])
            sc.dma(sp, force.t[:, :, :], I("sel_force")[:, :, :], writes=[force.res])
            sc.op(pool, lambda: G.memset(expand.t[64:128, :], 0.0), writes=[expand.res])
            sc.dma(sp, expand.t[0:64, :], I("expand")[:, :], writes=[expand.res])
            Qb = [k.sb(es, f"nQ{i}", [128, S], BF16) for i in range(2)]
            ksb = k.sb(es, "nks", [128, S], BF16)
            kwb = k.sb(es, "nkw", [128, S], BF16)
            vsb = k.sb(es, "nvs", [128, 32, 65], BF16)
            vwb = k.sb(es, "nvw", [128, 32, 65], BF16)
            for b in (ksb, kwb) + tuple(Qb):
                sc.op(pool, lambda: G.memset(b.t[64:128, :], 0.0), writes=[b.res])
            for b in (ksb, kwb):
                sc.dma(sp, b.t[64:70, :], I("augk_tok")[:, :], writes=[b.res])
            for b in (vsb, vwb):
                sc.op(pool, lambda: G.memset(b.t[:, :, 64:65], 1.0), writes=[b.res])
            selmT = k.sb(es, "nselmT", [128, S], BF16)
            sc.op(pool, lambda: G.memset(selmT.t[64:128, :], 0.0), writes=[selmT.res])
            imp = k.sb(es, "nimp", [128, 32, 64], F32)
            O = k.sb(es, "nO", [128, 32, 256], F32)
            cst2 = [k.sb(es, f"ncst{i}", [128, 32, 129], F32) for i in range(2)]
            cst_res2 = [[Res() for _ in range(32)] for _ in range(2)]
            cdn2 = [k.sb(es, f"ncdn{i}", [128, 32, 4], F32) for i in range(2)]
            ost = k.sb(es, "nost", [128, 32, 256], BF16)
            small = [k.sb(es, f"nsm{i}", [128, 4], F32) for i in range(8)]
            imb = [k.sb(es, f"nim{i}", [128, 64], F32) for i in range(4)]
            im2b = [k.sb(es, f"nim2{i}", [128, 64], F32) for i in range(4)]
            m8 = [k.sb(es, f"nm8{i}", [128, 16], F32) for i in range(4)]
            selb = [k.sb(es, f"nsel{i}", [128, 64], BF16) for i in range(8)]

            cmp_specs = []
            for j in range(8):
                l = [(0, 0, 512, [(ident.t[:, :], mcmp.t[:, 0, j * 512:(j + 1) * 512], 0, 512, [ident.res, mcmp.res])])]
                if j >= 4:
                    l.append((1, 0, 512, [(ident.t[:, :], mcmp.t[:, 1, j * 512:(j + 1) * 512], 0, 512, [ident.res, mcmp.res])]))
                cmp_specs.append(l)
            slc_specs = causal_specs(extra=lambda j, kt, c0, c1: [
                (expand.t[0:128, kt * 128:(kt + 1) * 128], selmT.t[0:128, j * 512 + c0:j * 512 + c1], c0, c1,
                 [expand.res, selmT.res])])
            win_specs = []
            for j in range(8):
                l = []
                for dp in range(4):
                    kt = 4 * j - 4 + dp
                    if kt < 0:
                        continue
                    l.append((kt, 0, 128 * (dp + 1),
                              [(ident.t[:, :], mband.t[:, :], 128 * dp, 128 * dp + 128, [ident.res, mband.res])]))
                for d in range(4):
                    l.append((4 * j + d, 128 * d, 512,
                              [(ident.t[:, :], mcaus.t[:, :], 128 * d, 128 * d + 128, [ident.res, mcaus.res])]))
                win_specs.append(l)

            def load_q(h):
                qb = k.nxt("nQ", Qb)
                sc.dma(sp, qb.t[0:64, :], NqT[h * 64:(h + 1) * 64, :], writes=[qb.res])
                sc.dma(sp, qb.t[64:70, :], I("augq")[8 + h, :, :], writes=[qb.res])
                return qb

            for g in range(4):
                sc.dma(sp, ksb.t[0:64, :], ksT[g * 64:(g + 1) * 64, :], writes=[ksb.res])
                sc.dma(sp, kwb.t[0:64, :], kwT[g * 64:(g + 1) * 64, :], writes=[kwb.res])
                sc.dma(sp, vsb.t[:, :, 0:64], vs_d[:, g * 64:(g + 1) * 64].rearrange("(t p) c -> p t c", p=128), writes=[vsb.res])
                sc.dma(sp, vwb.t[:, :, 0:64], vw_d[:, g * 64:(g + 1) * 64].rearrange("(t p) c -> p t c", p=128), writes=[vwb.res])
                for hh in range(4):
                    h = 4 * g + hh
                    qb = load_q(h)

                    cst, cst_res, cdn = cst2[hh % 2], cst_res2[hh % 2], cdn2[hh % 2]

                    def ev_cmp(qt, ac, h=h, hh=hh, cst=cst, cst_res=cst_res):
                        sc.op(dve, lambda: V.tensor_copy(out=cst.t[:, qt, :], in_=ac.t[:, 0:129]), reads=[ac.res], writes=[cst_res[qt]])

                    attn_pass(es, qb.t, Kc.t[:, g, :], KDIM, Vc.t[:, g, :, :], 129, cmp_specs, ev_cmp,
                              [qb.res, Kc.res, Vc.res], pbufs)
                    sc.op(dve, lambda: V.tensor_scalar(out=cdn.t[:, :, 0:1], in0=cst.t[:, :, 128:129], scalar1=1e-30, scalar2=None,
                                                       op0=ALU.max), reads=cst_res, writes=[cdn.res])
                    sc.op(dve, lambda: V.reciprocal(out=cdn.t[:, :, 1:2], in_=cdn.t[:, :, 0:1]), reads=[], writes=[cdn.res])
                    sc.op(dve, lambda: V.tensor_tensor(out=cdn.t[:, :, 2:3], in0=cdn.t[:, :, 1:2], in1=gates.t[:, :, 3 * h:3 * h + 1],
                                                       op=ALU.mult), reads=[gates.res], writes=[cdn.res])
                    sc.op(dve, lambda: V.tensor_tensor(out=O.t[:, :, hh * 64:(hh + 1) * 64], in0=cst.t[:, :, 0:64],
                                                       in1=cdn.t[:, :, 2:3].to_broadcast([128, 32, 64]), op=ALU.mult),
                          reads=[cdn.res] + cst_res, writes=[O.res])
                    if hh == 0:
                        sc.op(dve, lambda: V.tensor_tensor(out=imp.t[:, :, :], in0=cst.t[:, :, 64:128],
                                                           in1=cdn.t[:, :, 1:2].to_broadcast([128, 32, 64]), op=ALU.mult),
                              reads=[cdn.res] + cst_res, writes=[imp.res])
                    else:
                        sc.op(dve, lambda: V.tensor_tensor(out=cst.t[:, :, 64:128], in0=cst.t[:, :, 64:128],
                                                           in1=cdn.t[:, :, 1:2].to_broadcast([128, 32, 64]), op=ALU.mult),
                              reads=[cdn.res], writes=cst_res)
                        sc.op(dve, lambda: V.tensor_tensor(out=imp.t[:, :, :], in0=imp.t[:, :, :], in1=cst.t[:, :, 64:128], op=ALU.add),
                              reads=cst_res, writes=[imp.res])
                def sel_vec(q8):
                    sls = []
                    for i8 in range(8):
                        qt = q8 * 8 + i8
                        im = k.nxt("nim", imb)
                        im2 = k.nxt("nim2", im2b)
                        mm = k.nxt("nm8", m8)
                        sl = k.nxt("nsel", selb)
                        sls.append(sl)
                        sc.op(pool, lambda: G.tensor_tensor(out=im.t[:, :], in0=imp.t[:, qt, :], in1=keep.t[:, qt, :], op=ALU.mult),
                              reads=[imp.res, keep.res], writes=[im.res])
                        sc.op(pool, lambda: G.tensor_tensor(out=im.t[:, :], in0=im.t[:, :], in1=force.t[:, qt, :], op=ALU.add),
                              reads=[force.res], writes=[im.res])
                        sc.op(dve, lambda: V.max(out=mm.t[:, 0:8], in_=im.t[:, :]), reads=[im.res], writes=[mm.res])
                        sc.op(dve, lambda: V.match_replace(out=im2.t[:, :], in_to_replace=mm.t[:, 0:8], in_values=im.t[:, :],
                                                           imm_value=-3.0e38), reads=[im.res, mm.res], writes=[im2.res])
                        sc.op(dve, lambda: V.max(out=mm.t[:, 8:16], in_=im2.t[:, :]), reads=[im2.res], writes=[mm.res])
                        sc.op(dve, lambda: V.tensor_scalar(out=sl.t[:, :], in0=im.t[:, :], scalar1=mm.t[:, 15:16], scalar2=NEGB,
                                                           op0=ALU.is_lt, op1=ALU.mult), reads=[im.res, mm.res], writes=[sl.res])
                    return sls

                def sel_pe(q8, sls):
                    for i8, sl in enumerate(sls):
                        sc.op(pe, lambda: T.transpose(out=tbank.t[0:64, i8 * 128:(i8 + 1) * 128], in_=sl.t[:, :],
                                                      identity=ident.t[:, :]), reads=[sl.res, ident.res], writes=[tbank.res])
                    sc.op(dve, lambda: V.tensor_copy(out=selmT.t[0:64, q8 * 1024:(q8 + 1) * 1024], in_=tbank.t[0:64, :]),
                          reads=[tbank.res], writes=[selmT.res])

                for hh in range(4):
                    h = 4 * g + hh
                    qb = load_q(h)
                    sls = sel_vec(hh)

                    def ev_win(qt, ac, h=h, hh=hh):
                        sm = k.nxt("nsm", small)
                        sc.op(dve, lambda: V.reciprocal(out=sm.t[:, 1:2], in_=ac.t[:, 64:65]), reads=[ac.res], writes=[sm.res])
                        sc.op(dve, lambda: V.tensor_tensor(out=sm.t[:, 2:3], in0=sm.t[:, 1:2],
                                                           in1=gates.t[:, qt, 3 * h + 2:3 * h + 3], op=ALU.mult),
                              reads=[sm.res, gates.res], writes=[sm.res])
                        sc.op(dve, lambda: V.scalar_tensor_tensor(out=O.t[:, qt, hh * 64:(hh + 1) * 64], in0=ac.t[:, 0:64],
                                                                  scalar=sm.t[:, 2:3], in1=O.t[:, qt, hh * 64:(hh + 1) * 64],
                                                                  op0=ALU.mult, op1=ALU.add),
                              reads=[ac.res, sm.res], writes=[O.res])

                    attn_pass(es, qb.t, kwb.t, KDIM, vwb.t, 65, win_specs, ev_win, [qb.res, kwb.res, vwb.res], pbufs)
                    sel_pe(hh, sls)
                for hh in range(4):
                    h = 4 * g + hh
                    qb = load_q(h)

                    def ev_slc(qt, ac, h=h, hh=hh):
                        sm = k.nxt("nsm", small)
                        sc.op(dve, lambda: V.reciprocal(out=sm.t[:, 1:2], in_=ac.t[:, 64:65]), reads=[ac.res], writes=[sm.res])
                        sc.op(dve, lambda: V.tensor_tensor(out=sm.t[:, 2:3], in0=sm.t[:, 1:2],
                                                           in1=gates.t[:, qt, 3 * h + 1:3 * h + 2], op=ALU.mult),
                              reads=[sm.res, gates.res], writes=[sm.res])
                        sc.op(dve, lambda: V.scalar_tensor_tensor(out=ost.t[:, qt, hh * 64:(hh + 1) * 64], in0=ac.t[:, 0:64],
                                                                  scalar=sm.t[:, 2:3], in1=O.t[:, qt, hh * 64:(hh + 1) * 64],
                                                                  op0=ALU.mult, op1=ALU.add),
                              reads=[ac.res, sm.res, O.res], writes=[ost.res])

                    attn_pass(es, qb.t, ksb.t, KDIM, vsb.t, 65, slc_specs, ev_slc, [qb.res, ksb.res, vsb.res], pbufs)
                sc.dma(pool, onsa_d[:, g * 256:(g + 1) * 256].rearrange("(t p) c -> p t c", p=128), ost.t[:, :, :],
                       reads=[ost.res])
            sc.barrier()

    k.phase3 = phase3
    def layer_norm_rows(src, dst_tmp, gb, bb, out_ap, smalls, writes_res, out_reads=()):
        st = k.nxt("lnst", smalls)
        sc.op(dve, lambda: V.bn_stats(out=st.t[:, 0:6], in_=src.t[:, 0:512]), reads=[src.res], writes=[st.res])
        sc.op(dve, lambda: V.bn_stats(out=st.t[:, 6:12], in_=src.t[:, 512:1024]), reads=[src.res], writes=[st.res])
        sc.op(dve, lambda: V.bn_aggr(out=st.t[:, 12:14], in_=st.t[:, 0:12]), reads=[st.res], writes=[st.res])
        sc.op(act, lambda: A.activation(out=st.t[:, 14:15], in_=st.t[:, 13:14], func=AF.Ln, bias=epsb.t[:, 0:1], scale=1.0),
              reads=[st.res, epsb.res], writes=[st.res])
        sc.op(act, lambda: A.activation(out=st.t[:, 14:15], in_=st.t[:, 14:15], func=AF.Exp, scale=-0.5),
              reads=[st.res], writes=[st.res])
        sc.op(dve, lambda: V.tensor_scalar(out=dst_tmp.t[:, :], in0=src.t[:, :], scalar1=st.t[:, 12:13], scalar2=st.t[:, 14:15],
                                           op0=ALU.subtract, op1=ALU.mult), reads=[src.res, st.res], writes=[dst_tmp.res])
        sc.op(dve, lambda: V.tensor_tensor(out=dst_tmp.t[:, :], in0=dst_tmp.t[:, :], in1=gb.t[:, :], op=ALU.mult),
              reads=[gb.res], writes=[dst_tmp.res])
        sc.op(dve, lambda: V.tensor_tensor(out=out_ap, in0=dst_tmp.t[:, :], in1=bb.t[:, :], op=ALU.add),
              reads=[dst_tmp.res, bb.res] + list(out_reads), writes=writes_res)

    def phase4():
        with contextlib.ExitStack() as es:
            def wload(name, src_ap, kc, n):
                b = k.sb(es, name, [128, kc, n], BF16)
                sc.dma(pool, b.t[:, :, :], src_ap.rearrange("(kc p) n -> p kc n", p=128), writes=[b.res])
                return b
            Wmg = wload("Wmg", I("w_mgate"), 8, 2048)
            Wd = wload("Wd", I("w_br_diff"), 8, 1024)
            Wn = wload("Wn", I("w_br_nsa"), 8, 1024)
            Wo = wload("Wo", I("w_o"), 8, 1024)
            Wpg = wload("Wpg", I("w_ple_gate"), 8, 1024)
            Wpp = wload("Wpp", I("w_ple_proj"), 2, 1024)
            Wr = wload("Wr", I("w_router"), 8, 32)
            bmg = k.sb(es, "bmg", [1, 2048], BF16); sc.dma(pool, bmg.t[:, :], I("b_mgate")[:, :], writes=[bmg.res])
            bpg = k.sb(es, "bpg", [1, 1024], BF16); sc.dma(pool, bpg.t[:, :], I("b_ple_gate")[:, :], writes=[bpg.res])
            brt = k.sb(es, "brt", [1, 32], BF16); sc.dma(pool, brt.t[:, :], I("b_router")[:, :], writes=[brt.res])
            b2a = k.sb(es, "b2a", [32, 1024], BF16); sc.dma(pool, b2a.t[:, :], I("b_e2")[:, :], writes=[b2a.res])
            ones = k.sb(es, "ones", [1, 128], BF16); sc.op(pool, lambda: G.memset(ones.t[:, :], 1.0), writes=[ones.res])
            g1b = bcast_load(es, "g1b", I("ln1_g")[:, :], 1024)
            b1b = bcast_load(es, "b1b", I("ln1_b")[:, :], 1024)

            def rot(name, shape, dt, n=2):
                return [k.sb(es, f"{name}{i}", shape, dt) for i in range(n)]
            od_b = rot("od", [128, 1024], BF16); on_b = rot("on", [128, 1024], BF16)
            odT_b = rot("odT", [128, 1024], BF16, 1); onT_b = rot("onT", [128, 1024], BF16, 1)
            xt_b = rot("xt", [128, 1024], F32); xTt_b = rot("xTt", [128, 8, 128], BF16)
            pTt_b = rot("pTt", [128, 2, 128], BF16)
            sgd_b = rot("sgd", [128, 1024], F32, 1); sgn_b = rot("sgn", [128, 1024], F32, 1)
            mixbf_b = rot("mixbf", [128, 1024], BF16); mixT_b = rot("mixT", [128, 1024], BF16, 1)
            r_b = rot("rr", [128, 1024], F32, 1); h_b = rot("hh", [128, 1024], F32)
            hbf_b = rot("hbf", [128, 1024], BF16); hT_b = rot("hTt", [128, 1024], BF16)
            spg_b = rot("spg", [128, 1024], F32, 1); base_b = rot("base", [128, 1024], F32)
            lnst = rot("lnst", [128, 16], F32, 4)
            lg_b = rot("lg", [128, 32], F32); e_b = rot("eb", [128, 32], F32); msk_b = rot("msk", [128, 32], F32)
            m8_b = rot("rm8", [128, 8], F32); rs_b = rot("rs", [128, 2], F32)
            gt_b = rot("gt", [128, 32], F32); gtbf_b = rot("gtbf", [128, 32], BF16); gT_b = rot("gT", [32, 128], BF16)
            st = {}
            triu = k.sb(es, "triu", [128, 128], BF16); sc.dma(sp, triu.t[:, :], I("triu")[:, :], writes=[triu.res])
            ones128 = k.sb(es, "ones128", [128, 128], BF16); sc.dma(sp, ones128.t[:, :], I("ones128")[:, :], writes=[ones128.res])
            iota32 = k.sb(es, "iota32", [128, 32], F32); sc.dma(sp, iota32.t[:, :], I("iota32")[:, :], writes=[iota32.res])
            trp = k.sb(es, "trp", [128, 1], F32); sc.dma(sp, trp.t[:, :], I("trp")[:, :], writes=[trp.res])
            cumrep = k.sb(es, "cumrep", [128, 32], F32); sc.op(pool, lambda: G.memset(cumrep.t[:, :], 0.0), writes=[cumrep.res])
            idxu_b = rot("idxu", [128, 8], U32); idxf_b = rot("idxf", [128, 4], F32); mkbf_b = rot("mkbf", [128, 32], BF16)
            rank_b = rot("rank", [128, 32], F32); oh_b = rot("oh", [128, 32], F32); rk_b = rot("rk", [128, 4], F32)
            val_b = rot("val", [128, 4], F32); t1_b = rot("t1", [128, 4], F32); e4_b = rot("e4", [128, 4], F32)

            def transposes(src, dst, eng):
                for i in range(8):
                    sc.op(pe, lambda: T.transpose(out=tbank.t[:, i * 128:(i + 1) * 128], in_=src.t[:, i * 128:(i + 1) * 128],
                                                  identity=ident.t[:, :]), reads=[src.res, ident.res], writes=[tbank.res])
                if eng is act:
                    sc.op(act, lambda: A.copy(out=dst.t[:, :], in_=tbank.t[:, :]), reads=[tbank.res], writes=[dst.res])
                else:
                    sc.op(dve, lambda: V.tensor_copy(out=dst.t[:, :], in_=tbank.t[:, :]), reads=[tbank.res], writes=[dst.res])

            def mm_tok(lhs_fn, nk, W, c0, bias=None, lhs_reads=(), n=512):
                bk = k.nxt("bank", banks)
                for kc in range(nk):
                    sc.op(pe, lambda: T.matmul(bk.t[:, 0:n], lhsT=lhs_fn(kc), rhs=W.t[:, kc, c0:c0 + n],
                                               start=(kc == 0), stop=(kc == nk - 1 and bias is None)),
                          reads=[W.res] + list(lhs_reads), writes=[bk.res])
                if bias is not None:
                    sc.op(pe, lambda: T.matmul(bk.t[:, 0:n], lhsT=ones.t[0:1, :], rhs=bias.t[0:1, c0:c0 + n],
                                               start=False, stop=True), reads=[ones.res, bias.res], writes=[bk.res])
                return bk

            def stage_a(tt):
                d = st[tt] = {}
                od = d["od"] = k.nxt("od", od_b); on = d["on"] = k.nxt("on", on_b)
                xt = d["xt"] = k.nxt("xt", xt_b); xTt = k.nxt("xTt", xTt_b); pTt = d["pTt"] = k.nxt("pTt", pTt_b)
                rows = slice(tt * 128, (tt + 1) * 128)
                sc.dma(sp, od.t[:, :], odiff_d[rows, :], writes=[od.res])
                sc.dma(sp, on.t[:, :], onsa_d[rows, :], writes=[on.res])
                sc.dma(sp, xt.t[:, :], I("x")[rows, :], writes=[xt.res])
                sc.dma(pool, xTt.t[:, :, :], I("xT")[:, rows].rearrange("(kc p) t -> p kc t", p=128), writes=[xTt.res])
                sc.dma(pool, pTt.t[:, :, :], I("pT")[:, rows].rearrange("(kc p) t -> p kc t", p=128), writes=[pTt.res])
                odT = k.nxt("odT", odT_b); onT = k.nxt("onT", onT_b)
                sgd = k.nxt("sgd", sgd_b); sgn = k.nxt("sgn", sgn_b)
                mix = sgd; t2 = sgn; mixbf = d["mixbf"] = k.nxt("mixbf", mixbf_b)
                for ch in range(4):
                    bk = mm_tok(lambda kc: xTt.t[:, kc, :], 8, Wmg, ch * 512, bias=bmg, lhs_reads=[xTt.res])
                    dst = sgd if ch < 2 else sgn
                    sc.op(act, lambda: A.activation(out=dst.t[:, (ch % 2) * 512:(ch % 2) * 512 + 512], in_=bk.t[:, :],
                                                    func=AF.Sigmoid), reads=[bk.res], writes=[dst.res])
                transposes(od, odT, act)
                transposes(on, onT, dve)
                for ch in range(2):
                    bk = mm_tok(lambda kc: odT.t[:, kc * 128:(kc + 1) * 128], 8, Wd, ch * 512, lhs_reads=[odT.res])
                    sc.op(dve, lambda: V.tensor_tensor(out=mix.t[:, ch * 512:(ch + 1) * 512], in0=sgd.t[:, ch * 512:(ch + 1) * 512],
                                                       in1=bk.t[:, :], op=ALU.mult), reads=[sgd.res, bk.res], writes=[mix.res])
                for ch in range(2):
                    bk = mm_tok(lambda kc: onT.t[:, kc * 128:(kc + 1) * 128], 8, Wn, ch * 512, lhs_reads=[onT.res])
                    sc.op(dve, lambda: V.tensor_tensor(out=t2.t[:, ch * 512:(ch + 1) * 512], in0=sgn.t[:, ch * 512:(ch + 1) * 512],
                                                       in1=bk.t[:, :], op=ALU.mult), reads=[sgn.res, bk.res], writes=[t2.res])
                sc.op(pool, lambda: G.tensor_tensor(out=mixbf.t[:, :], in0=mix.t[:, :], in1=t2.t[:, :], op=ALU.add),
                      reads=[mix.res, t2.res], writes=[mixbf.res])

            def stage_b(tt):
                d = st[tt]
                mixT = k.nxt("mixT", mixT_b); r = k.nxt("rr", r_b); h = d["h"] = k.nxt("hh", h_b)
                hbf = d["hbf"] = k.nxt("hbf", hbf_b); hT = d["hT"] = k.nxt("hTt", hT_b)
                xt = d["xt"]
                transposes(d["mixbf"], mixT, act)
                for ch in range(2):
                    bk = mm_tok(lambda kc: mixT.t[:, kc * 128:(kc + 1) * 128], 8, Wo, ch * 512, lhs_reads=[mixT.res])
                    sc.op(dve, lambda: V.scalar_tensor_tensor(out=r.t[:, ch * 512:(ch + 1) * 512], in0=xt.t[:, ch * 512:(ch + 1) * 512],
                                                              scalar=ALPHA, in1=bk.t[:, :], op0=ALU.mult, op1=ALU.add),
                          reads=[xt.res, bk.res], writes=[r.res])
                layer_norm_rows(r, r, g1b, b1b, h.t[:, :], lnst, [h.res])
                sc.op(act, lambda: A.copy(out=hbf.t[:, :], in_=h.t[:, :]), reads=[h.res], writes=[hbf.res])

            def stage_b2(tt):
                d = st[tt]
                hbf, hT = d["hbf"], d["hT"]
                transposes(hbf, hT, dve)
                sc.dma(sp, hT_d[:, tt * 128:(tt + 1) * 128].rearrange("(kc p) t -> p kc t", p=128),
                       hT.t[:, :].rearrange("p (kc t) -> p kc t", kc=8), reads=[hT.res])

            def stage_c(tt):
                d = st[tt]
                h, hT, pTt = d["h"], d["hT"], d["pTt"]
                spg = k.nxt("spg", spg_b); base = d["base"] = k.nxt("base", base_b)
                rows = slice(tt * 128, (tt + 1) * 128)
                for ch in range(2):
                    bk = mm_tok(lambda kc: hT.t[:, kc * 128:(kc + 1) * 128], 8, Wpg, ch * 512, bias=bpg, lhs_reads=[hT.res])
                    sc.op(act, lambda: A.activation(out=spg.t[:, ch * 512:(ch + 1) * 512], in_=bk.t[:, :], func=AF.Sigmoid),
                          reads=[bk.res], writes=[spg.res])
                for ch in range(2):
                    bk = mm_tok(lambda kc: pTt.t[:, kc, :], 2, Wpp, ch * 512, lhs_reads=[pTt.res])
                    sc.op(dve, lambda: V.tensor_tensor(out=spg.t[:, ch * 512:(ch + 1) * 512], in0=spg.t[:, ch * 512:(ch + 1) * 512],
                                                       in1=bk.t[:, :], op=ALU.mult), reads=[bk.res], writes=[spg.res])
                sc.op(dve, lambda: V.scalar_tensor_tensor(out=base.t[:, :], in0=h.t[:, :], scalar=ALPHA, in1=spg.t[:, :],
                                                          op0=ALU.mult, op1=ALU.add), reads=[h.res, spg.res], writes=[base.res])
                bk = mm_tok(lambda kc: hT.t[:, kc * 128:(kc + 1) * 128], 8, Wr, 0, bias=brt, lhs_reads=[hT.res], n=32)
                lg = k.nxt("lg", lg_b); e_ = k.nxt("eb", e_b); mk = k.nxt("msk", msk_b); m8_ = k.nxt("rm8", m8_b)
                rs = k.nxt("rs", rs_b); gt = k.nxt("gt", gt_b); gtbf = k.nxt("gtbf", gtbf_b); gT = k.nxt("gT", gT_b)
                sc.op(dve, lambda: V.tensor_copy(out=lg.t[:, :], in_=bk.t[:, 0:32]), reads=[bk.res], writes=[lg.res])
                sc.op(dve, lambda: V.max(out=m8_.t[:, 0:8], in_=lg.t[:, :]), reads=[lg.res], writes=[m8_.res])
                sc.op(dve, lambda: V.tensor_scalar(out=e_.t[:, :], in0=lg.t[:, :], scalar1=m8_.t[:, 0:1], scalar2=None,
                                                   op0=ALU.subtract), reads=[lg.res, m8_.res], writes=[e_.res])
                sc.op(act, lambda: A.activation(out=e_.t[:, :], in_=e_.t[:, :], func=AF.Exp), reads=[], writes=[e_.res])
                sc.op(dve, lambda: V.tensor_scalar(out=mk.t[:, :], in0=lg.t[:, :], scalar1=m8_.t[:, 3:4], scalar2=None,
                                                   op0=ALU.is_ge), reads=[lg.res, m8_.res], writes=[mk.res])
                sc.op(dve, lambda: V.tensor_tensor(out=e_.t[:, :], in0=e_.t[:, :], in1=mk.t[:, :], op=ALU.mult),
                      reads=[mk.res], writes=[e_.res])
                sc.op(dve, lambda: V.reduce_sum(out=rs.t[:, 0:1], in_=e_.t[:, :], axis=AX.X), reads=[e_.res], writes=[rs.res])
                sc.op(dve, lambda: V.reciprocal(out=rs.t[:, 1:2], in_=rs.t[:, 0:1]), reads=[], writes=[rs.res])
                sc.op(dve, lambda: V.tensor_scalar(out=gt.t[:, :], in0=e_.t[:, :], scalar1=rs.t[:, 1:2], scalar2=None,
                                                   op0=ALU.mult), reads=[e_.res, rs.res], writes=[gt.res])
                sc.dma(sp, G_d[rows, :], gt.t[:, :], reads=[gt.res])
                idxu = k.nxt("idxu", idxu_b); idxf = d["idxf"] = k.nxt("idxf", idxf_b); mkbf = d["mkbf"] = k.nxt("mkbf", mkbf_b)
                e4 = d["e4"] = k.nxt("e4", e4_b)
                d["rs"] = rs; d["gtbf"] = gtbf; d["gT"] = gT
                sc.op(dve, lambda: V.max_index(out=idxu.t[:, :], in_max=m8_.t[:, :], in_values=lg.t[:, :]),
                      reads=[m8_.res, lg.res], writes=[idxu.res])
                sc.op(dve, lambda: V.tensor_copy(out=idxf.t[:, :], in_=idxu.t[:, 0:4]), reads=[idxu.res], writes=[idxf.res])
                sc.op(pool, lambda: G.tensor_copy(out=mkbf.t[:, :], in_=mk.t[:, :]), reads=[mk.res], writes=[mkbf.res])
                sc.op(pool, lambda: G.tensor_copy(out=gtbf.t[:, :], in_=gt.t[:, :]), reads=[gt.res], writes=[gtbf.res])
                sc.op(dve, lambda: V.tensor_scalar(out=e4.t[:, :], in0=m8_.t[:, 0:4], scalar1=m8_.t[:, 0:1], scalar2=None,
                                                   op0=ALU.subtract), reads=[m8_.res], writes=[e4.res])
                sc.op(act, lambda: A.activation(out=e4.t[:, :], in_=e4.t[:, :], func=AF.Exp), reads=[], writes=[e4.res])

            def stage_d1(tt):
                d = st[tt]
                idxf, mkbf, e4, rs, gtbf, gT, hbf = d["idxf"], d["mkbf"], d["e4"], d["rs"], d["gtbf"], d["gT"], d["hbf"]
                rank = k.nxt("rank", rank_b); oh = k.nxt("oh", oh_b); rk = k.nxt("rk", rk_b)
                val = k.nxt("val", val_b); t1 = k.nxt("t1", t1_b)
                bkr = k.nxt("bank", banks)
                sc.op(pe, lambda: T.matmul(bkr.t[:, 0:32], lhsT=triu.t[:, :], rhs=mkbf.t[:, :], start=True, stop=True),
                      reads=[triu.res, mkbf.res], writes=[bkr.res])
                bkc = k.nxt("bank", banks)
                sc.op(pe, lambda: T.matmul(bkc.t[:, 0:32], lhsT=ones128.t[:, :], rhs=mkbf.t[:, :], start=True, stop=True),
                      reads=[ones128.res, mkbf.res], writes=[bkc.res])
                sc.op(pe, lambda: T.transpose(out=tbank.t[0:32, 0:128], in_=gtbf.t[:, :], identity=ident.t[:, :]),
                      reads=[gtbf.res, ident.res], writes=[tbank.res])
                sc.op(act, lambda: A.copy(out=gT.t[:, :], in_=tbank.t[0:32, 0:128]), reads=[tbank.res], writes=[gT.res])
                sc.op(dve, lambda: V.tensor_tensor(out=rank.t[:, :], in0=cumrep.t[:, :], in1=bkr.t[:, 0:32], op=ALU.add),
                      reads=[cumrep.res, bkr.res], writes=[rank.res])
                sc.op(dve, lambda: V.tensor_tensor(out=cumrep.t[:, :], in0=cumrep.t[:, :], in1=bkc.t[:, 0:32], op=ALU.add),
                      reads=[bkc.res], writes=[cumrep.res])
                for kk in range(4):
                    sc.op(dve, lambda: V.tensor_scalar(out=oh.t[:, :], in0=iota32.t[:, :], scalar1=idxf.t[:, kk:kk + 1], scalar2=None,
                                                       op0=ALU.is_equal), reads=[iota32.res, idxf.res], writes=[oh.res])
                    sc.op(dve, lambda: V.tensor_tensor(out=oh.t[:, :], in0=oh.t[:, :], in1=rank.t[:, :], op=ALU.mult),
                          reads=[rank.res], writes=[oh.res])
                    sc.op(dve, lambda: V.reduce_sum(out=rk.t[:, kk:kk + 1], in_=oh.t[:, :], axis=AX.X), reads=[oh.res], writes=[rk.res])
                sc.op(dve, lambda: V.tensor_scalar(out=val.t[:, :], in0=rk.t[:, :], scalar1=float(CAP), scalar2=None, op0=ALU.is_lt),
                      reads=[rk.res], writes=[val.res])
                sc.op(dve, lambda: V.scalar_tensor_tensor(out=t1.t[:, :], in0=idxf.t[:, :], scalar=float(CAP), in1=rk.t[:, :],
                                                          op0=ALU.mult, op1=ALU.add), reads=[idxf.res, rk.res], writes=[t1.res])
                sc.op(dve, lambda: V.scalar_tensor_tensor(out=t1.t[:, :], in0=t1.t[:, :], scalar=trp.t[:, 0:1], in1=val.t[:, :],
                                                          op0=ALU.subtract, op1=ALU.mult), reads=[trp.res, val.res], writes=[t1.res])
                sc.op(dve, lambda: V.tensor_scalar(out=dest_all.t[:, tt, :], in0=t1.t[:, :], scalar1=trp.t[:, 0:1], scalar2=None,
                                                   op0=ALU.add), reads=[t1.res, trp.res], writes=[dest_res[tt]])
                sc.op(dve, lambda: V.scalar_tensor_tensor(out=gk_all.t[:, tt, :], in0=e4.t[:, :], scalar=rs.t[:, 1:2], in1=val.t[:, :],
                                                          op0=ALU.mult, op1=ALU.mult), reads=[e4.res, rs.res, val.res], writes=[gk_res[tt]])
                for kk in range(4):
                    sc.dma(pool, lambda: G.indirect_dma_start(
                        out=xs_d[:, :], out_offset=bass.IndirectOffsetOnAxis(ap=dest_all.t[:, tt, kk:kk + 1], axis=0),
                        in_=hbf.t[:, :], in_offset=None), None, reads=[hbf.res, dest_res[tt]])

            def stage_d2(tt):
                d = st.pop(tt)
                gT, base = d["gT"], d["base"]
                rows = slice(tt * 128, (tt + 1) * 128)
                for ch in range(2):
                    bk2 = k.nxt("bank", banks)
                    sc.op(pe, lambda: T.matmul(bk2.t[:, :], lhsT=gT.t[0:32, :], rhs=b2a.t[0:32, ch * 512:(ch + 1) * 512],
                                               start=True, stop=True), reads=[gT.res, b2a.res], writes=[bk2.res])
                    sc.op(dve, lambda: V.tensor_tensor(out=base.t[:, ch * 512:(ch + 1) * 512], in0=base.t[:, ch * 512:(ch + 1) * 512],
                                                       in1=bk2.t[:, :], op=ALU.add), reads=[bk2.res], writes=[base.res])
                sc.dma(sp, base_d[rows, :], base.t[:, :], reads=[base.res])

            for it in range(NQT + 3):
                if 0 <= it - 3 < NQT:
                    stage_d1(it - 3)
                if 0 <= it - 2 < NQT:
                    stage_c(it - 2)
                if 0 <= it - 1 < NQT:
                    stage_b(it - 1)
                if it < NQT:
                    stage_a(it)
                if 0 <= it - 1 < NQT:
                    stage_b2(it - 1)
                if 0 <= it - 3 < NQT:
                    stage_d2(it - 3)
            sc.barrier()

    k.phase4 = phase4
    def phase5():
        NST = CAP // 128
        groups = [(g0, min(512, CAP - g0)) for g0 in range(0, CAP, 512)]
        with contextlib.ExitStack() as es:
            W1b = [k.sb(es, f"W1b{i}", [128, 8, 2048], BF16) for i in range(2)]
            W1res = [[Res() for _ in range(4)] for _ in range(2)]
            W2b = [k.sb(es, f"W2b{i}", [128, 8, 1024], BF16) for i in range(2)]
            W2res = [[Res() for _ in range(2)] for _ in range(2)]
            stg = [k.sb(es, f"wstg{i}", [128, 8, 512], F32) for i in range(2)]
            xTe2 = [k.sb(es, f"xTe{i}", [128, 8, CAP], BF16) for i in range(2)]
            actT = k.sb(es, "actT", [128, 8, CAP], BF16)
            xsl = [k.sb(es, f"xsl{i}", [128, 1024], BF16) for i in range(4)]
            yst = [k.sb(es, f"yst{i}", [128, 1024], F32) for i in range(2)]
            b1T = k.sb(es, "b1T", [128, 512], F32)
            sc.dma(sp, b1T.t[:, :], I("b_e1T")[:, :], writes=[b1T.res])
            gt_ = [k.sb(es, f"mg{i}", [128, 512], F32) for i in range(3)]
            sg_ = [k.sb(es, f"msg{i}", [128, 512], F32) for i in range(3)]
            lt_ = [k.sb(es, f"ml{i}", [128, 512], F32) for i in range(3)]
            w_e1, w_e2 = I("w_e1"), I("w_e2")
            sc.op(pool, lambda: G.memset(yst[0].t[:, :], 0.0), writes=[yst[0].res])
            sc.dma(sp, ys_d[32 * CAP:32 * CAP + 128, :], yst[0].t[:, :], reads=[yst[0].res])

            wseq = []
            for e_ in range(32):
                for blk in (0, 2, 1, 3):
                    wseq.append((e_, 1, blk))
                for blk in range(2):
                    wseq.append((e_, 2, blk))
            wstate = {"dma": 0, "cast": 0}

            def w_dma():
                n = wstate["dma"]
                if n >= len(wseq):
                    return
                wstate["dma"] += 1
                e_, kind, blk = wseq[n]
                s_ = stg[n % 2]
                src = (w_e1 if kind == 1 else w_e2)[e_, :, blk * 512:(blk + 1) * 512].rearrange("(kc p) n -> p kc n", p=128)
                sc.dma(sp, s_.t[:, :, :], src, writes=[s_.res])

            NPIECE = 2

            def w_cast_piece():
                n = wstate["cast"]
                if n >= len(wseq):
                    return
                j = wstate.get("piece", 0)
                e_, kind, blk = wseq[n]
                s_ = stg[n % 2]
                if kind == 1:
                    dst, rs_ = W1b[e_ % 2], W1res[e_ % 2][blk]
                else:
                    dst, rs_ = W2b[e_ % 2], W2res[e_ % 2][blk]
                kc0 = j * 4
                sc.op(act, lambda: A.copy(out=dst.t[:, kc0:kc0 + 4, blk * 512:(blk + 1) * 512], in_=s_.t[:, kc0:kc0 + 4, :]),
                      reads=[s_.res], writes=[rs_])
                if j + 1 == NPIECE:
                    wstate["piece"] = 0
                    wstate["cast"] += 1
                    w_dma()
                else:
                    wstate["piece"] = j + 1

            def w_cast():
                for _ in range(NPIECE):
                    w_cast_piece()

            w_dma(); w_dma()
            for _ in range(6):
                w_cast()
            def xs_tile(e_, s_i):
                xt_ = xTe2[e_ % 2]
                xl = k.nxt("xsl", xsl)
                r0 = e_ * CAP + s_i * 128
                sc.dma(sp, xl.t[:, :], xs_d[r0:r0 + 128, :], writes=[xl.res])
                for i in range(8):
                    sc.op(pe, lambda: T.transpose(out=tbank.t[:, i * 128:(i + 1) * 128], in_=xl.t[:, i * 128:(i + 1) * 128],
                                                  identity=ident.t[:, :]), reads=[xl.res, ident.res], writes=[tbank.res])
                sc.op(dve, lambda: V.tensor_copy(out=xt_.t[:, :, s_i * 128:(s_i + 1) * 128],
                                                 in_=tbank.t[:, :].rearrange("p (kc t) -> p kc t", kc=8)),
                      reads=[tbank.res], writes=[xt_.res])

            for s_i in range(NST):
                xs_tile(0, s_i)
            for e in range(32):
                w1 = W1b[e % 2]
                xTe = xTe2[e % 2]
                gi = 0
                for (g0, gn) in groups:
                    for fc in range(8):
                        if gi in (0, 2, 3, 5, 6, 8, 9, 11, 12, 14):
                            w_cast_piece()
                        gi += 1
                        bg = k.nxt("bank", banks)
                        bl = k.nxt("bank", banks)
                        for (bk, c0) in ((bg, fc * 128), (bl, 1024 + fc * 128)):
                            blk = c0 // 512
                            for kc in range(8):
                                sc.op(pe, lambda: T.matmul(bk.t[:, 0:gn], lhsT=w1.t[:, kc, c0:c0 + 128],
                                                           rhs=xTe.t[:, kc, g0:g0 + gn], start=(kc == 0), stop=(kc == 7)),
                                      reads=[W1res[e % 2][blk], xTe.res], writes=[bk.res])
                        g_ = k.nxt("mg", gt_); s_ = k.nxt("msg", sg_); l_ = k.nxt("ml", lt_)
                        cg = e * 16 + fc
                        cl = e * 16 + 8 + fc
                        sc.op(dve, lambda: V.tensor_scalar(out=g_.t[:, 0:gn], in0=bg.t[:, 0:gn], scalar1=b1T.t[:, cg:cg + 1], scalar2=7.0,
                                                           op0=ALU.add, op1=ALU.min), reads=[bg.res, b1T.res], writes=[g_.res])
                        sc.op(act, lambda: A.activation(out=s_.t[:, 0:gn], in_=g_.t[:, 0:gn], func=AF.Sigmoid, scale=1.702),
                              reads=[g_.res], writes=[s_.res])
                        sc.op(dve, lambda: V.tensor_scalar(out=l_.t[:, 0:gn], in0=bl.t[:, 0:gn], scalar1=b1T.t[:, cl:cl + 1], scalar2=-7.0,
                                                           op0=ALU.add, op1=ALU.max), reads=[bl.res, b1T.res], writes=[l_.res])
                        sc.op(dve, lambda: V.tensor_scalar(out=l_.t[:, 0:gn], in0=l_.t[:, 0:gn], scalar1=7.0, scalar2=1.0,
                                                           op0=ALU.min, op1=ALU.add), reads=[], writes=[l_.res])
                        sc.op(dve, lambda: V.tensor_tensor(out=g_.t[:, 0:gn], in0=g_.t[:, 0:gn], in1=s_.t[:, 0:gn], op=ALU.mult),
                              reads=[s_.res], writes=[g_.res])
                        sc.op(pool, lambda: G.tensor_tensor(out=actT.t[:, fc, g0:g0 + gn], in0=g_.t[:, 0:gn], in1=l_.t[:, 0:gn], op=ALU.mult),
                              reads=[g_.res, l_.res], writes=[actT.res])
                for s_i in range(NST):
                    if s_i in (1, 3):
                        w_cast_piece()
                    ys_ = k.nxt("yst", yst)
                    for ch in range(2):
                        bk = k.nxt("bank", banks)
                        for fc in range(8):
                            sc.op(pe, lambda: T.matmul(bk.t[:, :], lhsT=actT.t[:, fc, s_i * 128:(s_i + 1) * 128],
                                                       rhs=W2b[e % 2].t[:, fc, ch * 512:(ch + 1) * 512], start=(fc == 0), stop=(fc == 7)),
                                  reads=[actT.res, W2res[e % 2][ch]], writes=[bk.res])
                        evac_copy(ys_.t[:, ch * 512:(ch + 1) * 512], bk.t[:, :], [bk.res], [ys_.res])
                    r0 = e * CAP + s_i * 128
                    sc.dma(act, ys_d[r0:r0 + 128, :], ys_.t[:, :], reads=[ys_.res])
                    if e + 1 < 32:
                        xs_tile(e + 1, s_i)
            sc.barrier()
        with contextlib.ExitStack() as es:
            g2b = bcast_load(es, "g2b", I("ln2_g")[:, :], 1024)
            b2b = bcast_load(es, "b2b", I("ln2_b")[:, :], 1024)
            accb = [k.sb(es, f"cacc{i}", [128, 1024], F32) for i in range(4)]
            yb = [k.sb(es, f"cy{i}", [128, 1024], F32) for i in range(12)]
            lnst = [k.sb(es, f"lnst5{i}", [128, 16], F32) for i in range(4)]
            for tt in range(NQT):
                ac = k.nxt("cacc", accb)
                rows = slice(tt * 128, (tt + 1) * 128)
                sc.dma(sp, ac.t[:, :], base_d[rows, :], writes=[ac.res])
                for kk in range(4):
                    y_ = k.nxt("cy", yb)
                    sc.dma(pool, lambda: G.indirect_dma_start(
                        out=y_.t[:, :], out_offset=None, in_=ys_d[:, :],
                        in_offset=bass.IndirectOffsetOnAxis(ap=dest_all.t[:, tt, kk:kk + 1], axis=0)), None,
                        reads=[dest_res[tt]], writes=[y_.res])
                    sc.op(dve, lambda: V.scalar_tensor_tensor(out=ac.t[:, :], in0=y_.t[:, :], scalar=gk_all.t[:, tt, kk:kk + 1],
                                                              in1=ac.t[:, :], op0=ALU.mult, op1=ALU.add),
                          reads=[y_.res, gk_res[tt]], writes=[ac.res])
                st_ = k.nxt("lnst5", lnst)
                sc.op(dve, lambda: V.bn_stats(out=st_.t[:, 0:6], in_=ac.t[:, 0:512]), reads=[ac.res], writes=[st_.res])
                sc.op(dve, lambda: V.bn_stats(out=st_.t[:, 6:12], in_=ac.t[:, 512:1024]), reads=[ac.res], writes=[st_.res])
                sc.op(dve, lambda: V.bn_aggr(out=st_.t[:, 12:14], in_=st_.t[:, 0:12]), reads=[], writes=[st_.res])
                sc.op(act, lambda: A.activation(out=st_.t[:, 14:15], in_=st_.t[:, 13:14], func=AF.Ln, bias=epsb.t[:, 0:1], scale=1.0),
                      reads=[st_.res, epsb.res], writes=[st_.res])
                sc.op(act, lambda: A.activation(out=st_.t[:, 14:15], in_=st_.t[:, 14:15], func=AF.Exp, scale=-0.5),
                      reads=[], writes=[st_.res])
                sc.op(dve, lambda: V.tensor_scalar(out=ac.t[:, :], in0=ac.t[:, :], scalar1=st_.t[:, 12:13],
                                                   scalar2=st_.t[:, 14:15], op0=ALU.subtract, op1=ALU.mult),
                      reads=[st_.res], writes=[ac.res])
                sc.op(dve, lambda: V.tensor_tensor(out=ac.t[:, :], in0=ac.t[:, :], in1=g2b.t[:, :], op=ALU.mult),
                      reads=[g2b.res], writes=[ac.res])
                sc.op(dve, lambda: V.tensor_tensor(out=ac.t[:, :], in0=ac.t[:, :], in1=b2b.t[:, :], op=ALU.add),
                      reads=[b2b.res], writes=[ac.res])
                sc.dma(sp, out_d[rows, :], ac.t[:, :], reads=[ac.res])
            sc.barrier()

    k.phase5 = phase5
    k.phase1 = phase1
    return k


def prep_inputs(inp):
    f = lambda a: np.ascontiguousarray(np.asarray(a, np.float32))
    sh = {}
    sh["w_in"] = f(inp["w_in"][0])
    for n in ("diff_lq1", "diff_lk1", "diff_lq2", "diff_lk2", "diff_subln_g", "b_mgate", "ln1_g", "ln1_b",
              "b_router", "b_ple_gate", "ln2_g", "ln2_b"):
        sh[n] = f(inp[n][0]).reshape(1, -1)
    sh["nsa_pos_kT"] = f(np.asarray(inp["nsa_pos_k"][0]).T)
    sh["nsa_pos_vT"] = f(np.asarray(inp["nsa_pos_v"][0]).T)
    sh["nsa_phi_k1"] = f(np.asarray(inp["nsa_phi_k1"][0]).reshape(32, 64, 256).transpose(1, 0, 2))
    for nm in ("k", "v"):
        w = np.asarray(inp["nsa_phi_%s1" % nm][0]).reshape(16, 2, 64, 256)
        sh["nsa_phi_%s1s" % nm] = f(w.transpose(1, 2, 0, 3).reshape(128, 16, 256))
        ps = np.asarray(inp["nsa_pos_%s" % nm][0]).reshape(16, 2, 64)
        sh["nsa_pos_%sTs" % nm] = f(ps.transpose(1, 2, 0).reshape(128, 16))
    sh["nsa_phi_v1"] = f(np.asarray(inp["nsa_phi_v1"][0]).reshape(32, 64, 256).transpose(1, 0, 2))
    for n in ("nsa_phi_k2", "nsa_phi_v2", "w_br_diff", "w_br_nsa", "w_mgate", "w_o", "w_router", "w_e1", "w_e2",
              "b_e2", "w_ple_gate", "w_ple_proj"):
        sh[n] = f(inp[n][0])
    sh["b_e1T"] = f(np.asarray(inp["b_e1"][0]).reshape(32, 16, 128).transpose(2, 0, 1).reshape(128, 512))
    sh.update(_const_tables())
    x = np.asarray(inp["x"], np.float32)
    p = np.asarray(inp["p"], np.float32)
    maps = []
    for b in range(8):
        m = dict(sh)
        m["x"] = f(x[b])
        m["xT"] = f(x[b].T)
        m["pT"] = f(p[0, b].T)
        maps.append(m)
    return maps


_CACHE = {}


def kernel(**inputs):
    if "nc" not in _CACHE:
        kb = build()
        kb.phase1(); kb.phase2(); kb.phase3(); kb.phase4(); kb.phase5()
        _CACHE["nc"] = kb
    kb = _CACHE["nc"]
    maps = prep_inputs(inputs)
    maps = [{n: m[n] for n in kb.din} for m in maps]
    res = run_bass_kernel_spmd(kb.nc, maps, core_ids=list(range(8)))
    out = np.stack([np.asarray(res.results[b]["out"], np.float32) for b in range(8)], axis=0)
    return out
```

```python
import contextlib
import os
import math
import numpy as np
import ml_dtypes
import concourse.bass as bass
import concourse.mybir as mybir
from concourse.bass_utils import run_bass_kernel_spmd

F32 = mybir.dt.float32
BF16 = mybir.dt.bfloat16
AF = mybir.ActivationFunctionType
ALU = mybir.AluOpType
AX = mybir.AxisListType

S = 4096
D = 1024
NQT = 32
NEGB = -131072.0
SCALE = 0.125
LN_EPS = 1e-5
ALPHA = 2.0 ** 0.25
LAMBDA_INIT = 0.2
SEM_LIMIT = 30000
SKIP_SAME = set(os.environ.get('KSKIP', '').split(',')) - {''}
CAP = 768
NSLOT = 32 * CAP + 128


class Res:
    __slots__ = ("w", "r")

    def __init__(self):
        self.w = None
        self.r = {}


class Eng:
    def __init__(self, sch, name, eng):
        self.sch = sch
        self.name = name
        self.eng = eng
        self.sem = sch.new_sem(name)
        self.cnt = 0
        self.known = {}
        self.slots = []
        self.slot_i = 0


class Sched:
    def __init__(self, nc, es):
        self.nc = nc
        self.es = es
        self.nsem = 0
        self.sems = {}
        self.pe = Eng(self, "pe", nc.tensor)
        self.act = Eng(self, "act", nc.scalar)
        self.dve = Eng(self, "dve", nc.vector)
        self.pool = Eng(self, "pool", nc.gpsimd)
        self.sp = Eng(self, "sp", nc.sync)
        self.engs = [self.pe, self.act, self.dve, self.pool, self.sp]
        for e, n in ((self.sp, 24), (self.pool, 16), (self.act, 4)):
            for i in range(n):
                e.slots.append([self.new_sem(f"{e.name}_d{i}"), 0])
        self.n_ins = 0

    def new_sem(self, name):
        self.nsem += 1
        s = self.es.enter_context(self.nc.semaphore(f"s{self.nsem}_{name}"))
        self.sems[id(s)] = s
        return s

    def _wait(self, E, toks):
        for sem, val in toks:
            k = id(sem)
            if E.known.get(k, 0) >= val:
                continue
            E.eng.wait_ge(sem, val)
            E.known[k] = val

    def _deps(self, E, reads, writes):
        deps = []
        for r in reads:
            if r.w is not None:
                deps.append(r.w)
        for w in writes:
            if w.w is not None:
                deps.append(w.w)
            deps.extend(w.r.values())
        if E is self.pe or E.name in SKIP_SAME:
            deps = [d for d in deps if d[0] is not E.sem]
        return deps

    def _commit(self, tok, reads, writes):
        for r in reads:
            k = id(tok[0])
            r.r[k] = tok
        for w in writes:
            w.w = tok
            w.r = {}

    def op(self, E, fn, reads=(), writes=()):
        self._wait(E, self._deps(E, reads, writes))
        ins = fn()
        E.cnt += 1
        ins.then_inc(E.sem, 1)
        tok = (E.sem, E.cnt)
        self._commit(tok, reads, writes)
        self.n_ins += 1
        if E.cnt >= SEM_LIMIT:
            E.sem = self.new_sem(E.name)
            E.cnt = 0
        return tok

    def dma(self, E, out, in_, reads=(), writes=(), **kw):
        slot = E.slots[E.slot_i]
        E.slot_i = (E.slot_i + 1) % len(E.slots)
        deps = self._deps(E, reads, writes)
        if slot[1] > 0:
            deps.append((slot[0], slot[1]))
        self._wait(E, deps)
        if slot[1] + 16 > SEM_LIMIT:
            slot[0] = self.new_sem(E.name + "_d")
            slot[1] = 0
        if callable(out):
            out().then_inc(slot[0], 16)
        else:
            E.eng.dma_start(out=out, in_=in_, **kw).then_inc(slot[0], 16)
        slot[1] += 16
        tok = (slot[0], slot[1])
        self._commit(tok, reads, writes)
        self.n_ins += 1
        return tok

    def barrier(self):
        toks = []
        for e in self.engs:
            if e.cnt > 0:
                toks.append((e.sem, e.cnt))
            for s in e.slots:
                if s[1] > 0:
                    toks.append((s[0], s[1]))
        for e in self.engs:
            self._wait(e, toks)


def _bf(x):
    return np.asarray(x, np.float32).astype(ml_dtypes.bfloat16)


def _hi_lo(v):
    v = np.asarray(v, np.float64)
    hi = v.astype(np.float32).astype(ml_dtypes.bfloat16).astype(np.float64)
    lo = (v - hi)
    return hi, lo


def _const_tables():
    t = {}
    pos = np.arange(S)
    augk = np.stack([pos // 128, pos // 128, pos % 128, pos % 128, np.ones(S), np.ones(S)]).astype(np.float32)
    t["augk_tok"] = _bf(augk)
    cpos = np.arange(255) * 16 + 31
    augc = np.zeros((6, 256), np.float32)
    augc[:, :255] = np.stack([cpos // 128, cpos // 128, cpos % 128, cpos % 128, np.ones(255), np.ones(255)])
    t["augk_cmp"] = _bf(augc)
    slopes = list(2.0 ** (-8.0 * np.arange(1, 9) / 8)) + list(2.0 ** (-8.0 * np.arange(1, 17) / 16))
    augq = np.zeros((24, 6, S), np.float32)
    for i, s in enumerate(slopes):
        shi, slo = _hi_lo(s)
        augq[i, 0] = 1024.0 * shi
        augq[i, 1] = 1024.0 * slo
        augq[i, 2] = 8.0 * shi
        augq[i, 3] = 8.0 * slo
        hi, lo = _hi_lo(-8.0 * s * pos)
        augq[i, 4] = hi
        augq[i, 5] = lo
    t["augq"] = _bf(augq)
    kk = np.arange(128)[:, None]
    qq = np.arange(128)[None, :]
    t["m_caus"] = _bf(np.where(qq >= kk, 0.0, NEGB))
    t["m_band"] = _bf(np.where(qq < kk, 0.0, NEGB))
    t["ident"] = _bf(np.eye(128))
    n = np.arange(256).reshape(2, 128)
    valid = ((n[:, :, None] * 16 + 31) <= pos[None, None, :]) & (n[:, :, None] < 255)
    t["m_cmp"] = _bf(np.where(valid, 0.0, NEGB).transpose(1, 0, 2))
    c_lo = np.arange(255) * 16
    s_lo = np.arange(64) * 64
    ov = np.minimum(c_lo[:, None] + 32, s_lo[None, :] + 64) - np.maximum(c_lo[:, None], s_lo[None, :])
    c2s = np.zeros((256, 64), np.float32)
    c2s[:255] = np.clip(ov, 0, None) / 32.0
    t["c2s"] = _bf(c2s.reshape(2, 128, 64).transpose(1, 0, 2))
    cur = (pos // 64)[:, None]
    j = np.arange(64)[None, :]
    forced = (j == 0) | (j == cur) | (j == cur - 1)
    keep = (~forced) & (j <= cur)
    tf = np.where(forced, 1e30, np.where(j <= cur, 0.0, -1e30)).astype(np.float32)
    t["sel_keep"] = np.ascontiguousarray(keep.astype(np.float32).reshape(32, 128, 64).transpose(1, 0, 2))
    t["sel_force"] = np.ascontiguousarray(tf.reshape(32, 128, 64).transpose(1, 0, 2))
    t["expand"] = _bf((np.arange(64)[:, None] == (pos // 64)[None, :]).astype(np.float32))
    t["triu"] = _bf((np.arange(128)[:, None] < np.arange(128)[None, :]).astype(np.float32))
    t["ones128"] = _bf(np.ones((128, 128)))
    t["iota32"] = np.tile(np.arange(32, dtype=np.float32)[None, :], (128, 1))
    t["trp"] = (32 * CAP + np.arange(128, dtype=np.float32)).reshape(128, 1)
    return t


WEIGHT_NAMES = [
    "w_in", "diff_lq1", "diff_lk1", "diff_lq2", "diff_lk2", "diff_subln_g",
    "nsa_pos_k", "nsa_pos_v", "nsa_phi_k1", "nsa_phi_k2", "nsa_phi_v1", "nsa_phi_v2",
    "w_br_diff", "w_br_nsa", "w_mgate", "b_mgate", "w_o", "ln1_g", "ln1_b",
    "w_router", "b_router", "w_e1", "b_e1", "w_e2", "b_e2",
    "w_ple_gate", "b_ple_gate", "w_ple_proj", "ln2_g", "ln2_b",
]


class Buf:
    __slots__ = ("t", "res")

    def __init__(self, t):
        self.t = t
        self.res = Res()


class KB:
    def __init__(self, dbg=()):
        self.dbg = set(dbg)
        self.nc = bass.Bass("TRN2", target_bir_lowering=False)
        self.es = contextlib.ExitStack()
        self.sc = Sched(self.nc, self.es)
        self.din = {}
        self.rot = {}

    def inp(self, name, shape, dt=F32):
        t = self.nc.dram_tensor(name, list(shape), dt, kind="ExternalInput").ap()
        self.din[name] = t
        return t

    def scratch(self, name, shape, dt):
        kind = "ExternalOutput" if name in self.dbg else "Internal"
        return self.nc.dram_tensor(name, list(shape), dt, kind=kind).ap()

    def sb(self, es, name, shape, dt):
        return Buf(es.enter_context(self.nc.sbuf_tensor("sb_" + name, list(shape), dt)))

    def ps(self, es, name, shape, dt):
        return Buf(es.enter_context(self.nc.psum_tensor("ps_" + name, list(shape), dt)))

    def nxt(self, key, lst):
        i = self.rot.get(key, 0)
        self.rot[key] = i + 1
        return lst[i % len(lst)]


def build(dbg=(), phases=(1, 2, 3, 4, 5)):
    k = KB(dbg)
    nc, sc = k.nc, k.sc
    pe, act, dve, pool, sp = sc.pe, sc.act, sc.dve, sc.pool, sc.sp
    V, G, T = nc.vector, nc.gpsimd, nc.tensor
    A = nc.scalar

    SHAPES = {
        "x": ([S, D], F32), "xT": ([D, S], F32), "pT": ([256, S], F32), "w_in": ([D, 5680], F32),
        "diff_lq1": ([1, 64], F32), "diff_lk1": ([1, 64], F32), "diff_lq2": ([1, 64], F32), "diff_lk2": ([1, 64], F32),
        "diff_subln_g": ([1, 128], F32), "nsa_pos_kT": ([64, 32], F32), "nsa_pos_vT": ([64, 32], F32),
        "nsa_phi_k1": ([64, 32, 256], F32), "nsa_phi_v1": ([64, 32, 256], F32),
        "nsa_phi_k1s": ([128, 16, 256], F32), "nsa_phi_v1s": ([128, 16, 256], F32), "nsa_pos_kTs": ([128, 16], F32), "nsa_pos_vTs": ([128, 16], F32),
        "nsa_phi_k2": ([256, 64], F32), "nsa_phi_v2": ([256, 64], F32),
        "w_br_diff": ([D, D], F32), "w_br_nsa": ([D, D], F32), "w_mgate": ([D, 2 * D], F32), "b_mgate": ([1, 2 * D], F32),
        "w_o": ([D, D], F32), "ln1_g": ([1, D], F32), "ln1_b": ([1, D], F32),
        "w_router": ([D, 32], F32), "b_router": ([1, 32], F32),
        "w_e1": ([32, D, 2 * D], F32), "b_e1T": ([128, 512], F32), "w_e2": ([32, D, D], F32), "b_e2": ([32, D], F32),
        "w_ple_gate": ([D, D], F32), "b_ple_gate": ([1, D], F32), "w_ple_proj": ([256, D], F32),
        "ln2_g": ([1, D], F32), "ln2_b": ([1, D], F32),
        "augk_tok": ([6, S], BF16), "augk_cmp": ([6, 256], BF16), "augq": ([24, 6, S], BF16),
        "m_caus": ([128, 128], BF16), "m_band": ([128, 128], BF16), "ident": ([128, 128], BF16),
        "m_cmp": ([128, 2, S], BF16), "c2s": ([128, 2, 64], BF16),
        "sel_keep": ([128, 32, 64], F32), "sel_force": ([128, 32, 64], F32), "expand": ([64, S], BF16),
        "triu": ([128, 128], BF16), "ones128": ([128, 128], BF16), "iota32": ([128, 32], F32), "trp": ([128, 1], F32),
    }

    def I(name):
        if name not in k.din:
            shp, dt = SHAPES[name]
            k.inp(name, shp, dt)
        return k.din[name]

    out_d = nc.dram_tensor("out", [S, D], F32, kind="ExternalOutput").ap()

    QdT = k.scratch("QdT", [D, S], BF16); KdT = k.scratch("KdT", [D, S], BF16)
    Vd = k.scratch("Vd", [S, D], BF16); NqT = k.scratch("NqT", [D, S], BF16)
    kcT = k.scratch("kcT", [256, S], BF16); vcT = k.scratch("vcT", [256, S], BF16)
    ksT = k.scratch("ksT", [256, S], BF16); vs_d = k.scratch("vs", [S, 256], BF16)
    kwT = k.scratch("kwT", [256, S], BF16); vw_d = k.scratch("vw", [S, 256], BF16)
    gates_d = k.scratch("gates", [S, 48], F32)
    odiff_d = k.scratch("o_diff", [S, D], BF16); onsa_d = k.scratch("o_nsa", [S, D], BF16)
    hT_d = k.scratch("hT", [D, S], BF16); base_d = k.scratch("base", [S, D], F32)
    G_d = k.scratch("Gd", [S, 32], F32)
    xs_d = k.scratch("xs", [NSLOT, D], BF16); ys_d = k.scratch("ys", [NSLOT, D], F32)

    es0 = k.es
    banks = [k.ps(es0, f"bank{i}", [128, 512], F32) for i in range(7)]
    tbank = k.ps(es0, "tbank", [128, 1024], BF16)
    ident = k.sb(es0, "ident", [128, 128], BF16)
    mcaus = k.sb(es0, "mcaus", [128, 128], BF16)
    mband = k.sb(es0, "mband", [128, 128], BF16)
    sc.dma(sp, ident.t[:], I("ident")[:, :], writes=[ident.res])
    wz = k.sb(es0, "wz", [128, 512], BF16)
    sc.op(pool, lambda: G.memset(wz.t[:, :], 0.0), writes=[wz.res])
    epsb = k.sb(es0, "epsb", [128, 1], F32)
    sc.op(pool, lambda: G.memset(epsb.t[:, :], LN_EPS), writes=[epsb.res])
    sc.dma(sp, mcaus.t[:], I("m_caus")[:, :], writes=[mcaus.res])
    sc.dma(sp, mband.t[:], I("m_band")[:, :], writes=[mband.res])

    U32 = mybir.dt.uint32
    dest_all = k.sb(es0, "dest_all", [128, 32, 4], U32)
    gk_all = k.sb(es0, "gk_all", [128, 32, 4], F32)
    dest_res = [Res() for _ in range(32)]
    gk_res = [Res() for _ in range(32)]
    evac_i = [0]

    def evac_copy(out_ap, in_ap, reads, writes):
        evac_i[0] += 1
        if evac_i[0] % 2:
            return sc.op(act, lambda: A.copy(out=out_ap, in_=in_ap), reads=reads, writes=writes)
        return sc.op(dve, lambda: V.tensor_copy(out=out_ap, in_=in_ap), reads=reads, writes=writes)

    def phase1():
        with contextlib.ExitStack() as es:
            xT = k.sb(es, "xT", [128, 8, S], BF16)
            rx = [Res() for _ in range(8)]
            for kc in range(8):
                sc.dma(pool, xT.t[:, kc, :], I("xT")[kc * 128:(kc + 1) * 128, :], writes=[rx[kc]])
            wb = [k.sb(es, f"wb{i}", [128, 8, 1024], BF16) for i in range(2)]
            sfm = [k.sb(es, f"sfm{i}", [128, S], BF16) for i in range(2)]
            stm = [k.sb(es, f"stm{i}", [128, 4, 1024], BF16) for i in range(2)]
            sg = k.sb(es, "sgate", [128, 32, 48], F32)
            loads = [(0, 1024), (1024, 1024), (2048, 1024), (3072, 1024), (4096, 1024), (5120, 560)]
            subs = [
                [(0, 1024, "fm", QdT, 0)], [(0, 1024, "fm", KdT, 0)], [(0, 1024, "tm", Vd, 0)],
                [(0, 1024, "fm", NqT, 0)],
                [(0, 256, "fm", kcT, 0), (256, 256, "fm", vcT, 0), (512, 256, "fm", ksT, 0), (768, 256, "tm", vs_d, 0)],
                [(0, 256, "fm", kwT, 0), (256, 256, "tm", vw_d, 0), (512, 48, "gate", None, 0)],
            ]
            bi = 0
            for li, (c0, n) in enumerate(loads):
                w = wb[li % 2]
                src = I("w_in")[:, c0:c0 + n].rearrange("(kc p) n -> p kc n", p=128)
                sc.dma(pool, w.t[:, :, 0:n], src, writes=[w.res])
                for (b0, nn, kind, dst, _) in subs[li]:
                    if kind == "fm":
                        for cc in range(nn // 128):
                            st = k.nxt("sfm", sfm)
                            for qt in range(8):
                                bk = k.nxt("bank", banks)
                                for kc in range(8):
                                    sc.op(pe, lambda: T.matmul(bk.t[:, :], lhsT=w.t[:, kc, b0 + cc * 128:b0 + (cc + 1) * 128],
                                                               rhs=xT.t[:, kc, qt * 512:(qt + 1) * 512],
                                                               start=(kc == 0), stop=(kc == 7)),
                                          reads=[w.res, rx[kc]], writes=[bk.res])
                                evac_copy(st.t[:, qt * 512:(qt + 1) * 512], bk.t[:, :], [bk.res], [st.res])
                            sc.dma(sp, dst[cc * 128:(cc + 1) * 128, :], st.t[:, :], reads=[st.res])
                    elif kind == "tm":
                        for t4 in range(8):
                            st = k.nxt("stm", stm)
                            for ti in range(4):
                                tt = t4 * 4 + ti
                                for ch in range((nn + 511) // 512):
                                    cw = min(512, nn - ch * 512)
                                    bk = k.nxt("bank", banks)
                                    for kc in range(8):
                                        sc.op(pe, lambda: T.matmul(bk.t[:, 0:cw], lhsT=xT.t[:, kc, tt * 128:(tt + 1) * 128],
                                                                   rhs=w.t[:, kc, b0 + ch * 512:b0 + ch * 512 + cw],
                                                                   start=(kc == 0), stop=(kc == 7)),
                                              reads=[w.res, rx[kc]], writes=[bk.res])
                                    evac_copy(st.t[:, ti, ch * 512:ch * 512 + cw], bk.t[:, 0:cw], [bk.res], [st.res])
                            sc.dma(sp, dst[t4 * 512:(t4 + 1) * 512, :].rearrange("(t p) c -> p t c", p=128),
                                   st.t[:, :, 0:nn], reads=[st.res])
                    else:
                        for tt in range(32):
                            bk = k.nxt("bank", banks)
                            for kc in range(8):
                                sc.op(pe, lambda: T.matmul(bk.t[:, 0:48], lhsT=xT.t[:, kc, tt * 128:(tt + 1) * 128],
                                                           rhs=w.t[:, kc, b0:b0 + 48], start=(kc == 0), stop=(kc == 7)),
                                      reads=[w.res, rx[kc]], writes=[bk.res])
                            sc.op(act, lambda: A.activation(out=sg.t[:, tt, :], in_=bk.t[:, 0:48], func=AF.Sigmoid),
                                  reads=[bk.res], writes=[sg.res])
                        sc.dma(sp, gates_d.rearrange("(t p) c -> p t c", p=128), sg.t[:, :, :], reads=[sg.res])
            sc.barrier()

    sbanks = banks[0:3]
    accs = banks[3:7]

    FILL = [int(os.environ.get('KFILL', '0'))]
    KDIM = int(os.environ.get('KDIM', '128'))

    def attn_pass(es_p, QT, KT, kdim, Vt, dvp, specs, evac_fn, reads, pbufs):
        items = []
        covers = {}
        for j in range(8):
            spj = specs[j]
            if not spj:
                continue
            covers[j] = {sub: [i for i, s_ in enumerate(spj) if s_[1] <= sub * 128 < s_[2]] for sub in range(4)}
            for i in range(len(spj)):
                items.append((j, i))

        def emit_S(n):
            j, i = items[n]
            kt, c0, c1, masks = specs[j][i]
            bk = k.nxt("sbank", sbanks)
            nm = len(masks)
            sc.op(pe, lambda: T.matmul(bk.t[:, c0:c1], lhsT=KT[0:kdim, kt * 128:(kt + 1) * 128],
                                       rhs=QT[0:kdim, j * 512 + c0:j * 512 + c1], start=True, stop=(nm == 0)),
                  reads=reads, writes=[bk.res])
            for mi, (ml, mr, lo, hi, mreads) in enumerate(masks):
                sc.op(pe, lambda: T.matmul(bk.t[:, lo:hi], lhsT=ml, rhs=mr, start=False, stop=(mi == nm - 1)),
                      reads=mreads, writes=[bk.res])
            return bk

        LA = 2
        pend = {}
        for n0 in range(min(LA, len(items))):
            pend[n0] = emit_S(n0)
        for n in range(len(items)):
            if n + LA < len(items):
                pend[n + LA] = emit_S(n + LA)
            j, i = items[n]
            kt, c0, c1, masks = specs[j][i]
            cover = covers[j]
            bk = pend.pop(n)
            pb = k.nxt("pbuf", pbufs)
            if FILL[0] > 0:
                sc.op(pe, lambda: T.matmul(tbank.t[:, :].bitcast(F32)[:, 0:FILL[0]], lhsT=ident.t[:, :], rhs=wz.t[:, 0:FILL[0]],
                                           start=True, stop=True), reads=[], writes=[])
            sc.op(act, lambda: A.activation(out=pb.t[:, c0:c1], in_=bk.t[:, c0:c1], func=AF.Exp, scale=SCALE),
                  reads=[bk.res], writes=[pb.res])
            for sub in range(c0 // 128, c1 // 128):
                ac = accs[sub]
                first = cover[sub][0] == i
                last = cover[sub][-1] == i
                sc.op(pe, lambda: T.matmul(ac.t[:, 0:dvp], lhsT=pb.t[:, sub * 128:(sub + 1) * 128],
                                           rhs=Vt[:, kt, 0:dvp], start=first, stop=last),
                      reads=[pb.res] + list(reads), writes=[ac.res])
                if last:
                    evac_fn(j * 4 + sub, ac)

    def pe_warmup(n=24):
        for i in range(n):
            sc.op(pe, lambda: T.matmul(tbank.t[:, :].bitcast(F32), lhsT=ident.t[:, :], rhs=wz.t[:, :], start=True, stop=True),
                  reads=[ident.res, wz.res], writes=[tbank.res] if i == 0 else [])

    def causal_specs(extra=None):
        specs = []
        for j in range(8):
            l = []
            for kt in range(4 * j + 4):
                d = kt - 4 * j
                c0 = 128 * d if d > 0 else 0
                masks = []
                if extra is not None:
                    masks += extra(j, kt, c0, 512)
                if d >= 0:
                    masks.append((ident.t[:, :], mcaus.t[:, :], 128 * d, 128 * d + 128, [ident.res, mcaus.res]))
                l.append((kt, c0, 512, masks))
            specs.append(l)
        return specs

    def bcast_load(es, name, src_ap, n, eng=None):
        b = k.sb(es, name, [128, n], F32)
        sc.dma(sp, b.t[:, :], src_ap.partition_broadcast(128), writes=[b.res])
        return b

    def phase2():
        with contextlib.ExitStack() as es:
            pbufs = [k.sb(es, f"pb{i}", [128, 512], BF16) for i in range(4)]
            Qb = [k.sb(es, f"dQ{i}", [128, S], BF16) for i in range(2)]
            Kb = [k.sb(es, f"dK{i}", [128, S], BF16) for i in range(2)]
            Vb = [k.sb(es, f"dV{i}", [128, 32, 129], BF16) for i in range(2)]
            A0 = k.sb(es, "dA0", [128, 32, 128], F32)
            A1 = k.sb(es, "dA1", [128, 32, 128], F32)
            Asq = k.sb(es, "dAsq", [128, 32, 128], F32)
            rst = k.sb(es, "drst", [128, 32, 2], F32)
            ost = [k.sb(es, f"dost{i}", [128, 32, 128], BF16) for i in range(2)]
            small = [k.sb(es, f"dsm{i}", [128, 4], F32) for i in range(8)]
            at = [k.sb(es, f"dat{i}", [128, 128], F32) for i in range(3)]
            junk = k.sb(es, "djunk", [128, 128], F32)
            l4 = [bcast_load(es, f"dl{i}", I(n)[:, :], 64) for i, n in
                  enumerate(("diff_lq1", "diff_lk1", "diff_lq2", "diff_lk2"))]
            gsub = bcast_load(es, "dgsub", I("diff_subln_g")[:, :], 128)
            lam = k.sb(es, "dlam", [128, 8], F32)
            sc.op(dve, lambda: V.tensor_tensor(out=l4[0].t[:, :], in0=l4[0].t[:, :], in1=l4[1].t[:, :], op=ALU.mult),
                  reads=[l4[1].res], writes=[l4[0].res])
            sc.op(dve, lambda: V.tensor_tensor(out=l4[2].t[:, :], in0=l4[2].t[:, :], in1=l4[3].t[:, :], op=ALU.mult),
                  reads=[l4[3].res], writes=[l4[2].res])
            sc.op(dve, lambda: V.reduce_sum(out=lam.t[:, 0:1], in_=l4[0].t[:, :], axis=AX.X), reads=[l4[0].res], writes=[lam.res])
            sc.op(dve, lambda: V.reduce_sum(out=lam.t[:, 1:2], in_=l4[2].t[:, :], axis=AX.X), reads=[l4[2].res], writes=[lam.res])
            sc.op(act, lambda: A.activation(out=lam.t[:, 2:4], in_=lam.t[:, 0:2], func=AF.Exp), reads=[lam.res], writes=[lam.res])
            sc.op(dve, lambda: V.scalar_tensor_tensor(out=lam.t[:, 4:5], in0=lam.t[:, 3:4], scalar=-LAMBDA_INIT,
                                                      in1=lam.t[:, 2:3], op0=ALU.add, op1=ALU.subtract),
                  reads=[lam.res], writes=[lam.res])
            sc.op(dve, lambda: V.tensor_scalar(out=gsub.t[:, :], in0=gsub.t[:, :], scalar1=1.0 - LAMBDA_INIT, scalar2=None,
                                               op0=ALU.mult), reads=[gsub.res], writes=[gsub.res])
            for b in Vb:
                sc.op(pool, lambda: G.memset(b.t[:, :, 128:129], 1.0), writes=[b.res])
            for b in Qb + Kb:
                sc.op(pool, lambda: G.memset(b.t[64:128, :], 0.0), writes=[b.res])
            for b in Kb:
                sc.dma(sp, b.t[64:70, :], I("augk_tok")[:, :], writes=[b.res])
            specs = causal_specs()
            for h in range(8):
                if h == 0:
                    pe_warmup()
                vb = Vb[h % 2]
                sc.dma(sp, vb.t[:, :, 0:128], Vd[:, h * 128:(h + 1) * 128].rearrange("(t p) c -> p t c", p=128),
                       writes=[vb.res])
                osb = ost[h % 2]
                for c in range(2):
                    qb, kb_ = Qb[c], Kb[c]
                    r0 = h * 128 + c * 64
                    sc.dma(sp, qb.t[0:64, :], QdT[r0:r0 + 64, :], writes=[qb.res])
                    sc.dma(sp, qb.t[64:70, :], I("augq")[h, :, :], writes=[qb.res])
                    sc.dma(sp, kb_.t[0:64, :], KdT[r0:r0 + 64, :], writes=[kb_.res])

                    def ev(qt, ac, c=c, osb=osb):
                        sm = k.nxt("dsm", small)
                        sc.op(dve, lambda: V.reciprocal(out=sm.t[:, 0:1], in_=ac.t[:, 128:129]), reads=[ac.res], writes=[sm.res])
                        if c == 0:
                            sc.op(dve, lambda: V.tensor_scalar(out=A0.t[:, qt, :], in0=ac.t[:, 0:128], scalar1=sm.t[:, 0:1],
                                                               scalar2=None, op0=ALU.mult),
                                  reads=[ac.res, sm.res], writes=[A0.res])
                            return
                        sc.op(dve, lambda: V.tensor_tensor(out=sm.t[:, 1:2], in0=sm.t[:, 0:1], in1=lam.t[:, 4:5], op=ALU.mult),
                              reads=[lam.res, sm.res], writes=[sm.res])
                        sc.op(dve, lambda: V.scalar_tensor_tensor(out=A1.t[:, qt, :], in0=ac.t[:, 0:128], scalar=sm.t[:, 1:2],
                                                                  in1=A0.t[:, qt, :], op0=ALU.mult, op1=ALU.add),
                              reads=[ac.res, sm.res, A0.res], writes=[A1.res])

                    attn_pass(es, qb.t, kb_.t, KDIM, vb.t, 129, specs, ev, [qb.res, kb_.res, vb.res], pbufs)
                sc.op(dve, lambda: V.tensor_tensor(out=Asq.t[:, :, :], in0=A1.t[:, :, :], in1=A1.t[:, :, :], op=ALU.mult),
                      reads=[A1.res], writes=[Asq.res])
                sc.op(dve, lambda: V.tensor_reduce(out=rst.t[:, :, 0:1], in_=Asq.t[:, :, :], axis=AX.X, op=ALU.add),
                      reads=[Asq.res], writes=[rst.res])
                sc.op(act, lambda: A.activation(out=rst.t[:, :, 1:2], in_=rst.t[:, :, 0:1], func=AF.Ln, bias=epsb.t[:, 0:1],
                                                scale=1.0 / 128.0), reads=[rst.res, epsb.res], writes=[rst.res])
                sc.op(act, lambda: A.activation(out=rst.t[:, :, 1:2], in_=rst.t[:, :, 1:2], func=AF.Exp, scale=-0.5),
                      reads=[], writes=[rst.res])
                sc.op(dve, lambda: V.tensor_tensor(out=Asq.t[:, :, :], in0=A1.t[:, :, :],
                                                   in1=rst.t[:, :, 1:2].to_broadcast([128, 32, 128]), op=ALU.mult),
                      reads=[A1.res, rst.res], writes=[Asq.res])
                sc.op(pool, lambda: G.tensor_tensor(out=osb.t[:, :, :], in0=Asq.t[:, :, :],
                                                    in1=gsub.t[:, :].unsqueeze(1).to_broadcast([128, 32, 128]), op=ALU.mult),
                      reads=[Asq.res, gsub.res], writes=[osb.res])
                sc.dma(pool, odiff_d[:, h * 128:(h + 1) * 128].rearrange("(t p) c -> p t c", p=128), osb.t[:, :, :],
                       reads=[osb.res])
            sc.barrier()

    k.phase2 = phase2
    def phase3():
        with contextlib.ExitStack() as es:
            pbufs = [k.sb(es, f"npb{i}", [128, 512], BF16) for i in range(4)]
            gates = k.sb(es, "ngates", [128, 32, 48], F32)
            sc.dma(sp, gates.t[:, :, :], gates_d.rearrange("(t p) c -> p t c", p=128), writes=[gates.res])
            Kc = k.sb(es, "nKc", [128, 4, 256], BF16)
            Vc = k.sb(es, "nVc", [128, 4, 2, 129], BF16)
            sc.op(pool, lambda: G.memset(Kc.t[:, :, :], 0.0), writes=[Kc.res])
            sc.op(pool, lambda: G.memset(Vc.t[:, :, :, :], 0.0), writes=[Vc.res])
            sc.op(pool, lambda: G.memset(Vc.t[:, :, :, 128:129], 1.0), writes=[Vc.res])
            for g in range(4):
                sc.dma(sp, Kc.t[64:70, g, :], I("augk_cmp")[:, :], writes=[Kc.res])
                sc.dma(sp, Vc.t[:, g, :, 64:128], I("c2s")[:, :, :], writes=[Vc.res])
            with contextlib.ExitStack() as es2:
                w1 = [k.sb(es2, f"cw1{i}", [128, 16, 256], BF16) for i in range(2)]
                w2 = [k.sb(es2, f"cw2{i}", [128, 2, 64], BF16) for i in range(2)]
                posT = [k.sb(es2, f"cpos{i}", [128, 16], BF16) for i in range(2)]
                biasv = k.sb(es2, "cbias", [128, 4], F32)
                srcb = [k.sb(es2, f"csrc{i}", [128, S], BF16) for i in range(2)]
                for b_ in srcb:
                    sc.op(pool, lambda: G.memset(b_.t[64:128, S - 16:S], 0.0), writes=[b_.res])
                hid = [k.sb(es2, f"chid{i}", [128, 2, 256], BF16) for i in range(2)]
                for kv, (n1, n2, npos) in enumerate((("nsa_phi_k1s", "nsa_phi_k2", "nsa_pos_kTs"),
                                                     ("nsa_phi_v1s", "nsa_phi_v2", "nsa_pos_vTs"))):
                    sc.dma(pool, w1[kv].t[:, :, :], I(n1)[:, :, :], writes=[w1[kv].res])
                    sc.dma(pool, w2[kv].t[:, :, :], I(n2).rearrange("(c p) d -> p c d", p=128), writes=[w2[kv].res])
                    sc.dma(pool, posT[kv].t[:, :], I(npos)[:, :], writes=[posT[kv].res])
                for kv in range(2):
                    for hc in range(2):
                        bk = k.nxt("bank", banks)
                        for l in range(16):
                            sc.op(pe, lambda: T.matmul(bk.t[:, 0:1], lhsT=w1[kv].t[:, l, hc * 128:(hc + 1) * 128],
                                                       rhs=posT[kv].t[:, l:l + 1], start=(l == 0), stop=(l == 15)),
                                  reads=[w1[kv].res, posT[kv].res], writes=[bk.res])
                        sc.op(dve, lambda: V.tensor_copy(out=biasv.t[:, kv * 2 + hc:kv * 2 + hc + 1], in_=bk.t[:, 0:1]),
                              reads=[bk.res], writes=[biasv.res])
                for g in range(4):
                    for kv in range(2):
                        sb_ = k.nxt("csrc", srcb)
                        srcd = (kcT if kv == 0 else vcT)
                        sc.dma(sp, sb_.t[0:64, :], srcd[g * 64:(g + 1) * 64, :], writes=[sb_.res])
                        sc.dma(sp, sb_.t[64:128, 0:S - 1], srcd[g * 64:(g + 1) * 64, 1:S], writes=[sb_.res])
                        sv = sb_.t[:, :].rearrange("p (n s) -> p s n", s=16)
                        hb = hid[kv]
                        for hc in range(2):
                            bk = k.nxt("bank", banks)
                            for l2 in range(16):
                                l = 2 * l2
                                sc.op(pe, lambda: T.matmul(bk.t[:, 0:255], lhsT=w1[kv].t[:, l2, hc * 128:(hc + 1) * 128],
                                                           rhs=sv[:, l % 16, (l // 16):(l // 16) + 255],
                                                           start=(l2 == 0), stop=(l2 == 15)),
                                      reads=[w1[kv].res, sb_.res], writes=[bk.res])
                            sc.op(act, lambda: A.activation(out=hb.t[:, hc, 0:255], in_=bk.t[:, 0:255], func=AF.Silu,
                                                            bias=biasv.t[:, kv * 2 + hc:kv * 2 + hc + 1]),
                                  reads=[bk.res, biasv.res], writes=[hb.res])
                        if kv == 0:
                            bk = k.nxt("bank", banks)
                            for hc in range(2):
                                sc.op(pe, lambda: T.matmul(bk.t[0:64, 0:255], lhsT=w2[0].t[:, hc, :], rhs=hb.t[:, hc, 0:255],
                                                           start=(hc == 0), stop=(hc == 1)),
                                      reads=[w2[0].res, hb.res], writes=[bk.res])
                            sc.op(dve, lambda: V.tensor_copy(out=Kc.t[0:64, g, 0:255], in_=bk.t[0:64, 0:255]),
                                  reads=[bk.res], writes=[Kc.res])
                        else:
                            for nt in range(2):
                                m = 128 if nt == 0 else 127
                                bk = k.nxt("bank", banks)
                                for hc in range(2):
                                    sc.op(pe, lambda: T.matmul(bk.t[0:m, 0:64], lhsT=hb.t[:, hc, nt * 128:nt * 128 + m],
                                                               rhs=w2[1].t[:, hc, :], start=(hc == 0), stop=(hc == 1)),
                                          reads=[w2[1].res, hb.res], writes=[bk.res])
                                sc.op(dve, lambda: V.tensor_copy(out=Vc.t[0:m, g, nt, 0:64], in_=bk.t[0:m, 0:64]),
                                      reads=[bk.res], writes=[Vc.res])
                sc.barrier()
            mcmp = k.sb(es, "nmcmp", [128, 2, S], BF16)
            keep = k.sb(es, "nkeep", [128, 32, 64], F32)
            force = k.sb(es, "nforce", [128, 32, 64], F32)
            expand = k.sb(es, "nexpand", [128, S], BF16)
            sc.dma(sp, mcmp.t[:, :, :], I("m_cmp")[:, :, :], writes=[mcmp.res])
            sc.dma(sp, keep.t[:, :, :], I("sel_keep")[:, :, :], writes=[keep.res])
            sc.dma(sp, force.t[:, :, :], I("sel_force")[:, :, :], writes=[force.res])
            sc.op(pool, lambda: G.memset(expand.t[64:128, :], 0.0), writes=[expand.res])
            sc.dma(sp, expand.t[0:64, :], I("expand")[:, :], writes=[expand.res])
            Qb = [k.sb(es, f"nQ{i}", [128, S], BF16) for i in range(2)]
            ksb = k.sb(es, "nks", [128, S], BF16)
            kwb = k.sb(es, "nkw", [128, S], BF16)
            vsb = k.sb(es, "nvs", [128, 32, 65], BF16)
            vwb = k.sb(es, "nvw", [128, 32, 65], BF16)
            for b in (ksb, kwb) + tuple(Qb):
                sc.op(pool, lambda: G.memset(b.t[64:128, :], 0.0), writes=[b.res])
            for b in (ksb, kwb):
                sc.dma(sp, b.t[64:70, :], I("augk_tok")[:, :], writes=[b.res])
            for b in (vsb, vwb):
                sc.op(pool, lambda: G.memset(b.t[:, :, 64:65], 1.0), writes=[b.res])
            selmT = k.sb(es, "nselmT", [128, S], BF16)
            sc.op(pool, lambda: G.memset(selmT.t[64:128, :], 0.0), writes=[selmT.res])
            imp = k.sb(es, "nimp", [128, 32, 64], F32)
            O = k.sb(es, "nO", [128, 32, 256], F32)
            cst2 = [k.sb(es, f"ncst{i}", [128, 32, 129], F32) for i in range(2)]
            cst_res2 = [[Res() for _ in range(32)] for _ in range(2)]
            cdn2 = [k.sb(es, f"ncdn{i}", [128, 32, 4], F32) for i in range(2)]
            ost = k.sb(es, "nost", [128, 32, 256], BF16)
            small = [k.sb(es, f"nsm{i}", [128, 4], F32) for i in range(8)]
            imb = [k.sb(es, f"nim{i}", [128, 64], F32) for i in range(4)]
            im2b = [k.sb(es, f"nim2{i}", [128, 64], F32) for i in range(4)]
            m8 = [k.sb(es, f"nm8{i}", [128, 16], F32) for i in range(4)]
            selb = [k.sb(es, f"nsel{i}", [128, 64], BF16) for i in range(8)]

            cmp_specs = []
            for j in range(8):
                l = [(0, 0, 512, [(ident.t[:, :], mcmp.t[:, 0, j * 512:(j + 1) * 512], 0, 512, [ident.res, mcmp.res])])]
                if j >= 4:
                    l.append((1, 0, 512, [(ident.t[:, :], mcmp.t[:, 1, j * 512:(j + 1) * 512], 0, 512, [ident.res, mcmp.res])]))
                cmp_specs.append(l)
            slc_specs = causal_specs(extra=lambda j, kt, c0, c1: [
                (expand.t[0:128, kt * 128:(kt + 1) * 128], selmT.t[0:128, j * 512 + c0:j * 512 + c1], c0, c1,
                 [expand.res, selmT.res])])
            win_specs = []
            for j in range(8):
                l = []
                for dp in range(4):
                    kt = 4 * j - 4 + dp
                    if kt < 0:
                        continue
                    l.append((kt, 0, 128 * (dp + 1),
                              [(ident.t[:, :], mband.t[:, :], 128 * dp, 128 * dp + 128, [ident.res, mband.res])]))
                for d in range(4):
                    l.append((4 * j + d, 128 * d, 512,
                              [(ident.t[:, :], mcaus.t[:, :], 128 * d, 128 * d + 128, [ident.res, mcaus.res])]))
                win_specs.append(l)

            def load_q(h):
                qb = k.nxt("nQ", Qb)
                sc.dma(sp, qb.t[0:64, :], NqT[h * 64:(h + 1) * 64, :], writes=[qb.res])
                sc.dma(sp, qb.t[64:70, :], I("augq")[8 + h, :, :], writes=[qb.res])
                return qb

            for g in range(4):
                sc.dma(sp, kwb.t[0:64, :], kwT[g * 64:(g + 1) * 64, :], writes=[kwb.res])
                sc.dma(sp, vwb.t[:, :, 0:64], vw_d[:, g * 64:(g + 1) * 64].rearrange("(t p) c -> p t c", p=128), writes=[vwb.res])
                for hh in range(4):
                    h = 4 * g + hh
                    qb = load_q(h)

                    cst, cst_res, cdn = cst2[hh % 2], cst_res2[hh % 2], cdn2[hh % 2]

                    def ev_cmp(qt, ac, h=h, hh=hh, cst=cst, cst_res=cst_res):
                        sc.op(dve, lambda: V.tensor_copy(out=cst.t[:, qt, :], in_=ac.t[:, 0:129]), reads=[ac.res], writes=[cst_res[qt]])

                    attn_pass(es, qb.t, Kc.t[:, g, :], KDIM, Vc.t[:, g, :, :], 129, cmp_specs, ev_cmp,
                              [qb.res, Kc.res, Vc.res], pbufs)
                    sc.op(dve, lambda: V.tensor_scalar(out=cdn.t[:, :, 0:1], in0=cst.t[:, :, 128:129], scalar1=1e-30, scalar2=None,
                                                       op0=ALU.max), reads=cst_res, writes=[cdn.res])
                    sc.op(dve, lambda: V.reciprocal(out=cdn.t[:, :, 1:2], in_=cdn.t[:, :, 0:1]), reads=[], writes=[cdn.res])
                    sc.op(dve, lambda: V.tensor_tensor(out=cdn.t[:, :, 2:3], in0=cdn.t[:, :, 1:2], in1=gates.t[:, :, 3 * h:3 * h + 1],
                                                       op=ALU.mult), reads=[gates.res], writes=[cdn.res])
                    sc.op(dve, lambda: V.tensor_tensor(out=O.t[:, :, hh * 64:(hh + 1) * 64], in0=cst.t[:, :, 0:64],
                                                       in1=cdn.t[:, :, 2:3].to_broadcast([128, 32, 64]), op=ALU.mult),
                          reads=[cdn.res] + cst_res, writes=[O.res])
                    if hh == 0:
                        sc.op(dve, lambda: V.tensor_tensor(out=imp.t[:, :, :], in0=cst.t[:, :, 64:128],
                                                           in1=cdn.t[:, :, 1:2].to_broadcast([128, 32, 64]), op=ALU.mult),
                              reads=[cdn.res] + cst_res, writes=[imp.res])
                    else:
                        sc.op(dve, lambda: V.tensor_tensor(out=cst.t[:, :, 64:128], in0=cst.t[:, :, 64:128],
                                                           in1=cdn.t[:, :, 1:2].to_broadcast([128, 32, 64]), op=ALU.mult),
                              reads=[cdn.res], writes=cst_res)
                        sc.op(dve, lambda: V.tensor_tensor(out=imp.t[:, :, :], in0=imp.t[:, :, :], in1=cst.t[:, :, 64:128], op=ALU.add),
                              reads=cst_res, writes=[imp.res])
                def sel_vec(q8):
                    sls = []
                    for i8 in range(8):
                        qt = q8 * 8 + i8
                        im = k.nxt("nim", imb)
                        im2 = k.nxt("nim2", im2b)
                        mm = k.nxt("nm8", m8)
                        sl = k.nxt("nsel", selb)
                        sls.append(sl)
                        sc.op(pool, lambda: G.tensor_tensor(out=im.t[:, :], in0=imp.t[:, qt, :], in1=keep.t[:, qt, :], op=ALU.mult),
                              reads=[imp.res, keep.res], writes=[im.res])
                        sc.op(pool, lambda: G.tensor_tensor(out=im.t[:, :], in0=im.t[:, :], in1=force.t[:, qt, :], op=ALU.add),
                              reads=[force.res], writes=[im.res])
                        sc.op(dve, lambda: V.max(out=mm.t[:, 0:8], in_=im.t[:, :]), reads=[im.res], writes=[mm.res])
                        sc.op(dve, lambda: V.match_replace(out=im2.t[:, :], in_to_replace=mm.t[:, 0:8], in_values=im.t[:, :],
                                                           imm_value=-3.0e38), reads=[im.res, mm.res], writes=[im2.res])
                        sc.op(dve, lambda: V.max(out=mm.t[:, 8:16], in_=im2.t[:, :]), reads=[im2.res], writes=[mm.res])
                        sc.op(dve, lambda: V.tensor_scalar(out=sl.t[:, :], in0=im.t[:, :], scalar1=mm.t[:, 15:16], scalar2=NEGB,
                                                           op0=ALU.is_lt, op1=ALU.mult), reads=[im.res, mm.res], writes=[sl.res])
                    return sls

                def sel_pe(q8, sls):
                    for i8, sl in enumerate(sls):
                        sc.op(pe, lambda: T.transpose(out=tbank.t[0:64, i8 * 128:(i8 + 1) * 128], in_=sl.t[:, :],
                                                      identity=ident.t[:, :]), reads=[sl.res, ident.res], writes=[tbank.res])
                    sc.op(dve, lambda: V.tensor_copy(out=selmT.t[0:64, q8 * 1024:(q8 + 1) * 1024], in_=tbank.t[0:64, :]),
                          reads=[tbank.res], writes=[selmT.res])

                for hh in range(4):
                    h = 4 * g + hh
                    qb = load_q(h)
                    sls = sel_vec(hh)

                    def ev_win(qt, ac, h=h, hh=hh):
                        sm = k.nxt("nsm", small)
                        sc.op(dve, lambda: V.reciprocal(out=sm.t[:, 1:2], in_=ac.t[:, 64:65]), reads=[ac.res], writes=[sm.res])
                        sc.op(dve, lambda: V.tensor_tensor(out=sm.t[:, 2:3], in0=sm.t[:, 1:2],
                                                           in1=gates.t[:, qt, 3 * h + 2:3 * h + 3], op=ALU.mult),
                              reads=[sm.res, gates.res], writes=[sm.res])
                        sc.op(dve, lambda: V.scalar_tensor_tensor(out=O.t[:, qt, hh * 64:(hh + 1) * 64], in0=ac.t[:, 0:64],
                                                                  scalar=sm.t[:, 2:3], in1=O.t[:, qt, hh * 64:(hh + 1) * 64],
                                                                  op0=ALU.mult, op1=ALU.add),
                              reads=[ac.res, sm.res], writes=[O.res])

                    attn_pass(es, qb.t, kwb.t, KDIM, vwb.t, 65, win_specs, ev_win, [qb.res, kwb.res, vwb.res], pbufs)
                    sel_pe(hh, sls)
                sc.dma(sp, ksb.t[0:64, :], ksT[g * 64:(g + 1) * 64, :], writes=[ksb.res])
                sc.dma(sp, vsb.t[:, :, 0:64], vs_d[:, g * 64:(g + 1) * 64].rearrange("(t p) c -> p t c", p=128), writes=[vsb.res])
                for hh in range(4):
                    h = 4 * g + hh
                    qb = load_q(h)

                    def ev_slc(qt, ac, h=h, hh=hh):
                        sm = k.nxt("nsm", small)
                        sc.op(dve, lambda: V.reciprocal(out=sm.t[:, 1:2], in_=ac.t[:, 64:65]), reads=[ac.res], writes=[sm.res])
                        sc.op(dve, lambda: V.tensor_tensor(out=sm.t[:, 2:3], in0=sm.t[:, 1:2],
                                                           in1=gates.t[:, qt, 3 * h + 1:3 * h + 2], op=ALU.mult),
                              reads=[sm.res, gates.res], writes=[sm.res])
                        sc.op(dve, lambda: V.scalar_tensor_tensor(out=ost.t[:, qt, hh * 64:(hh + 1) * 64], in0=ac.t[:, 0:64],
                                                                  scalar=sm.t[:, 2:3], in1=O.t[:, qt, hh * 64:(hh + 1) * 64],
                                                                  op0=ALU.mult, op1=ALU.add),
                              reads=[ac.res, sm.res, O.res], writes=[ost.res])

                    attn_pass(es, qb.t, ksb.t, KDIM, vsb.t, 65, slc_specs, ev_slc, [qb.res, ksb.res, vsb.res], pbufs)
                sc.dma(pool, onsa_d[:, g * 256:(g + 1) * 256].rearrange("(t p) c -> p t c", p=128), ost.t[:, :, :],
                       reads=[ost.res])
            sc.barrier()

    k.phase3 = phase3
    def layer_norm_rows(src, dst_tmp, gb, bb, out_ap, smalls, writes_res, out_reads=()):
        st = k.nxt("lnst", smalls)
        sc.op(dve, lambda: V.bn_stats(out=st.t[:, 0:6], in_=src.t[:, 0:512]), reads=[src.res], writes=[st.res])
        sc.op(dve, lambda: V.bn_stats(out=st.t[:, 6:12], in_=src.t[:, 512:1024]), reads=[src.res], writes=[st.res])
        sc.op(dve, lambda: V.bn_aggr(out=st.t[:, 12:14], in_=st.t[:, 0:12]), reads=[st.res], writes=[st.res])
        sc.op(act, lambda: A.activation(out=st.t[:, 14:15], in_=st.t[:, 13:14], func=AF.Ln, bias=epsb.t[:, 0:1], scale=1.0),
              reads=[st.res, epsb.res], writes=[st.res])
        sc.op(act, lambda: A.activation(out=st.t[:, 14:15], in_=st.t[:, 14:15], func=AF.Exp, scale=-0.5),
              reads=[st.res], writes=[st.res])
        sc.op(dve, lambda: V.tensor_scalar(out=dst_tmp.t[:, :], in0=src.t[:, :], scalar1=st.t[:, 12:13], scalar2=st.t[:, 14:15],
                                           op0=ALU.subtract, op1=ALU.mult), reads=[src.res, st.res], writes=[dst_tmp.res])
        sc.op(dve, lambda: V.tensor_tensor(out=dst_tmp.t[:, :], in0=dst_tmp.t[:, :], in1=gb.t[:, :], op=ALU.mult),
              reads=[gb.res], writes=[dst_tmp.res])
        sc.op(dve, lambda: V.tensor_tensor(out=out_ap, in0=dst_tmp.t[:, :], in1=bb.t[:, :], op=ALU.add),
              reads=[dst_tmp.res, bb.res] + list(out_reads), writes=writes_res)

    def phase4():
        with contextlib.ExitStack() as es:
            def wload(name, src_ap, kc, n):
                b = k.sb(es, name, [128, kc, n], BF16)
                sc.dma(pool, b.t[:, :, :], src_ap.rearrange("(kc p) n -> p kc n", p=128), writes=[b.res])
                return b
            Wmg = wload("Wmg", I("w_mgate"), 8, 2048)
            Wd = wload("Wd", I("w_br_diff"), 8, 1024)
            Wn = wload("Wn", I("w_br_nsa"), 8, 1024)
            Wo = wload("Wo", I("w_o"), 8, 1024)
            Wpg = wload("Wpg", I("w_ple_gate"), 8, 1024)
            Wpp = wload("Wpp", I("w_ple_proj"), 2, 1024)
            Wr = wload("Wr", I("w_router"), 8, 32)
            bmg = k.sb(es, "bmg", [1, 2048], BF16); sc.dma(pool, bmg.t[:, :], I("b_mgate")[:, :], writes=[bmg.res])
            bpg = k.sb(es, "bpg", [1, 1024], BF16); sc.dma(pool, bpg.t[:, :], I("b_ple_gate")[:, :], writes=[bpg.res])
            brt = k.sb(es, "brt", [1, 32], BF16); sc.dma(pool, brt.t[:, :], I("b_router")[:, :], writes=[brt.res])
            b2a = k.sb(es, "b2a", [32, 1024], BF16); sc.dma(pool, b2a.t[:, :], I("b_e2")[:, :], writes=[b2a.res])
            ones = k.sb(es, "ones", [1, 128], BF16); sc.op(pool, lambda: G.memset(ones.t[:, :], 1.0), writes=[ones.res])
            g1b = bcast_load(es, "g1b", I("ln1_g")[:, :], 1024)
            b1b = bcast_load(es, "b1b", I("ln1_b")[:, :], 1024)

            def rot(name, shape, dt, n=2):
                return [k.sb(es, f"{name}{i}", shape, dt) for i in range(n)]
            od_b = rot("od", [128, 1024], BF16); on_b = rot("on", [128, 1024], BF16)
            odT_b = rot("odT", [128, 1024], BF16, 1); onT_b = rot("onT", [128, 1024], BF16, 1)
            xt_b = rot("xt", [128, 1024], F32); xTt_b = rot("xTt", [128, 8, 128], BF16)
            pTt_b = rot("pTt", [128, 2, 128], BF16)
            sgd_b = rot("sgd", [128, 1024], F32, 1); sgn_b = rot("sgn", [128, 1024], F32, 1)
            mixbf_b = rot("mixbf", [128, 1024], BF16); mixT_b = rot("mixT", [128, 1024], BF16, 1)
            r_b = rot("rr", [128, 1024], F32, 1); h_b = rot("hh", [128, 1024], F32)
            hbf_b = rot("hbf", [128, 1024], BF16); hT_b = rot("hTt", [128, 1024], BF16)
            spg_b = rot("spg", [128, 1024], F32, 1); base_b = rot("base", [128, 1024], F32)
            lnst = rot("lnst", [128, 16], F32, 4)
            lg_b = rot("lg", [128, 32], F32); e_b = rot("eb", [128, 32], F32); msk_b = rot("msk", [128, 32], F32)
            m8_b = rot("rm8", [128, 8], F32); rs_b = rot("rs", [128, 2], F32)
            gt_b = rot("gt", [128, 32], F32); gtbf_b = rot("gtbf", [128, 32], BF16); gT_b = rot("gT", [32, 128], BF16)
            st = {}
            triu = k.sb(es, "triu", [128, 128], BF16); sc.dma(sp, triu.t[:, :], I("triu")[:, :], writes=[triu.res])
            ones128 = k.sb(es, "ones128", [128, 128], BF16); sc.dma(sp, ones128.t[:, :], I("ones128")[:, :], writes=[ones128.res])
            iota32 = k.sb(es, "iota32", [128, 32], F32); sc.dma(sp, iota32.t[:, :], I("iota32")[:, :], writes=[iota32.res])
            trp = k.sb(es, "trp", [128, 1], F32); sc.dma(sp, trp.t[:, :], I("trp")[:, :], writes=[trp.res])
            cumrep = k.sb(es, "cumrep", [128, 32], F32); sc.op(pool, lambda: G.memset(cumrep.t[:, :], 0.0), writes=[cumrep.res])
            idxu_b = rot("idxu", [128, 8], U32); idxf_b = rot("idxf", [128, 4], F32); mkbf_b = rot("mkbf", [128, 32], BF16)
            rank_b = rot("rank", [128, 32], F32); oh_b = rot("oh", [128, 32], F32); rk_b = rot("rk", [128, 4], F32)
            val_b = rot("val", [128, 4], F32); t1_b = rot("t1", [128, 4], F32); e4_b = rot("e4", [128, 4], F32)

            def transposes(src, dst, eng):
                for i in range(8):
                    sc.op(pe, lambda: T.transpose(out=tbank.t[:, i * 128:(i + 1) * 128], in_=src.t[:, i * 128:(i + 1) * 128],
                                                  identity=ident.t[:, :]), reads=[src.res, ident.res], writes=[tbank.res])
                if eng is act:
                    sc.op(act, lambda: A.copy(out=dst.t[:, :], in_=tbank.t[:, :]), reads=[tbank.res], writes=[dst.res])
                else:
                    sc.op(dve, lambda: V.tensor_copy(out=dst.t[:, :], in_=tbank.t[:, :]), reads=[tbank.res], writes=[dst.res])

            def mm_tok(lhs_fn, nk, W, c0, bias=None, lhs_reads=(), n=512):
                bk = k.nxt("bank", banks)
                for kc in range(nk):
                    sc.op(pe, lambda: T.matmul(bk.t[:, 0:n], lhsT=lhs_fn(kc), rhs=W.t[:, kc, c0:c0 + n],
                                               start=(kc == 0), stop=(kc == nk - 1 and bias is None)),
                          reads=[W.res] + list(lhs_reads), writes=[bk.res])
                if bias is not None:
                    sc.op(pe, lambda: T.matmul(bk.t[:, 0:n], lhsT=ones.t[0:1, :], rhs=bias.t[0:1, c0:c0 + n],
                                               start=False, stop=True), reads=[ones.res, bias.res], writes=[bk.res])
                return bk

            def stage_a(tt):
                d = st[tt] = {}
                od = d["od"] = k.nxt("od", od_b); on = d["on"] = k.nxt("on", on_b)
                xt = d["xt"] = k.nxt("xt", xt_b); xTt = k.nxt("xTt", xTt_b); pTt = d["pTt"] = k.nxt("pTt", pTt_b)
                rows = slice(tt * 128, (tt + 1) * 128)
                sc.dma(sp, od.t[:, :], odiff_d[rows, :], writes=[od.res])
                sc.dma(sp, on.t[:, :], onsa_d[rows, :], writes=[on.res])
                sc.dma(sp, xt.t[:, :], I("x")[rows, :], writes=[xt.res])
                sc.dma(pool, xTt.t[:, :, :], I("xT")[:, rows].rearrange("(kc p) t -> p kc t", p=128), writes=[xTt.res])
                sc.dma(pool, pTt.t[:, :, :], I("pT")[:, rows].rearrange("(kc p) t -> p kc t", p=128), writes=[pTt.res])
                odT = k.nxt("odT", odT_b); onT = k.nxt("onT", onT_b)
                sgd = k.nxt("sgd", sgd_b); sgn = k.nxt("sgn", sgn_b)
                mix = sgd; t2 = sgn; mixbf = d["mixbf"] = k.nxt("mixbf", mixbf_b)
                for ch in range(4):
                    bk = mm_tok(lambda kc: xTt.t[:, kc, :], 8, Wmg, ch * 512, bias=bmg, lhs_reads=[xTt.res])
                    dst = sgd if ch < 2 else sgn
                    sc.op(act, lambda: A.activation(out=dst.t[:, (ch % 2) * 512:(ch % 2) * 512 + 512], in_=bk.t[:, :],
                                                    func=AF.Sigmoid), reads=[bk.res], writes=[dst.res])
                transposes(od, odT, act)
                transposes(on, onT, dve)
                for ch in range(2):
                    bk = mm_tok(lambda kc: odT.t[:, kc * 128:(kc + 1) * 128], 8, Wd, ch * 512, lhs_reads=[odT.res])
                    sc.op(dve, lambda: V.tensor_tensor(out=mix.t[:, ch * 512:(ch + 1) * 512], in0=sgd.t[:, ch * 512:(ch + 1) * 512],
                                                       in1=bk.t[:, :], op=ALU.mult), reads=[sgd.res, bk.res], writes=[mix.res])
                for ch in range(2):
                    bk = mm_tok(lambda kc: onT.t[:, kc * 128:(kc + 1) * 128], 8, Wn, ch * 512, lhs_reads=[onT.res])
                    sc.op(dve, lambda: V.tensor_tensor(out=t2.t[:, ch * 512:(ch + 1) * 512], in0=sgn.t[:, ch * 512:(ch + 1) * 512],
                                                       in1=bk.t[:, :], op=ALU.mult), reads=[sgn.res, bk.res], writes=[t2.res])
                sc.op(pool, lambda: G.tensor_tensor(out=mixbf.t[:, :], in0=mix.t[:, :], in1=t2.t[:, :], op=ALU.add),
                      reads=[mix.res, t2.res], writes=[mixbf.res])

            def stage_b(tt):
                d = st[tt]
                mixT = k.nxt("mixT", mixT_b); r = k.nxt("rr", r_b); h = d["h"] = k.nxt("hh", h_b)
                hbf = d["hbf"] = k.nxt("hbf", hbf_b); hT = d["hT"] = k.nxt("hTt", hT_b)
                xt = d["xt"]
                transposes(d["mixbf"], mixT, act)
                for ch in range(2):
                    bk = mm_tok(lambda kc: mixT.t[:, kc * 128:(kc + 1) * 128], 8, Wo, ch * 512, lhs_reads=[mixT.res])
                    sc.op(dve, lambda: V.scalar_tensor_tensor(out=r.t[:, ch * 512:(ch + 1) * 512], in0=xt.t[:, ch * 512:(ch + 1) * 512],
                                                              scalar=ALPHA, in1=bk.t[:, :], op0=ALU.mult, op1=ALU.add),
                          reads=[xt.res, bk.res], writes=[r.res])
                layer_norm_rows(r, r, g1b, b1b, h.t[:, :], lnst, [h.res])
                sc.op(act, lambda: A.copy(out=hbf.t[:, :], in_=h.t[:, :]), reads=[h.res], writes=[hbf.res])

            def stage_b2(tt):
                d = st[tt]
                hbf, hT = d["hbf"], d["hT"]
                transposes(hbf, hT, dve)
                sc.dma(sp, hT_d[:, tt * 128:(tt + 1) * 128].rearrange("(kc p) t -> p kc t", p=128),
                       hT.t[:, :].rearrange("p (kc t) -> p kc t", kc=8), reads=[hT.res])

            def stage_c(tt):
                d = st[tt]
                h, hT, pTt = d["h"], d["hT"], d["pTt"]
                spg = k.nxt("spg", spg_b); base = d["base"] = k.nxt("base", base_b)
                rows = slice(tt * 128, (tt + 1) * 128)
                for ch in range(2):
                    bk = mm_tok(lambda kc: hT.t[:, kc * 128:(kc + 1) * 128], 8, Wpg, ch * 512, bias=bpg, lhs_reads=[hT.res])
                    sc.op(act, lambda: A.activation(out=spg.t[:, ch * 512:(ch + 1) * 512], in_=bk.t[:, :], func=AF.Sigmoid),
                          reads=[bk.res], writes=[spg.res])
                for ch in range(2):
                    bk = mm_tok(lambda kc: pTt.t[:, kc, :], 2, Wpp, ch * 512, lhs_reads=[pTt.res])
                    sc.op(dve, lambda: V.tensor_tensor(out=spg.t[:, ch * 512:(ch + 1) * 512], in0=spg.t[:, ch * 512:(ch + 1) * 512],
                                                       in1=bk.t[:, :], op=ALU.mult), reads=[bk.res], writes=[spg.res])
                sc.op(dve, lambda: V.scalar_tensor_tensor(out=base.t[:, :], in0=h.t[:, :], scalar=ALPHA, in1=spg.t[:, :],
                                                          op0=ALU.mult, op1=ALU.add), reads=[h.res, spg.res], writes=[base.res])
                bk = mm_tok(lambda kc: hT.t[:, kc * 128:(kc + 1) * 128], 8, Wr, 0, bias=brt, lhs_reads=[hT.res], n=32)
                lg = k.nxt("lg", lg_b); e_ = k.nxt("eb", e_b); mk = k.nxt("msk", msk_b); m8_ = k.nxt("rm8", m8_b)
                rs = k.nxt("rs", rs_b); gt = k.nxt("gt", gt_b); gtbf = k.nxt("gtbf", gtbf_b); gT = k.nxt("gT", gT_b)
                sc.op(dve, lambda: V.tensor_copy(out=lg.t[:, :], in_=bk.t[:, 0:32]), reads=[bk.res], writes=[lg.res])
                sc.op(dve, lambda: V.max(out=m8_.t[:, 0:8], in_=lg.t[:, :]), reads=[lg.res], writes=[m8_.res])
                sc.op(dve, lambda: V.tensor_scalar(out=e_.t[:, :], in0=lg.t[:, :], scalar1=m8_.t[:, 0:1], scalar2=None,
                                                   op0=ALU.subtract), reads=[lg.res, m8_.res], writes=[e_.res])
                sc.op(act, lambda: A.activation(out=e_.t[:, :], in_=e_.t[:, :], func=AF.Exp), reads=[], writes=[e_.res])
                sc.op(dve, lambda: V.tensor_scalar(out=mk.t[:, :], in0=lg.t[:, :], scalar1=m8_.t[:, 3:4], scalar2=None,
                                                   op0=ALU.is_ge), reads=[lg.res, m8_.res], writes=[mk.res])
                sc.op(dve, lambda: V.tensor_tensor(out=e_.t[:, :], in0=e_.t[:, :], in1=mk.t[:, :], op=ALU.mult),
                      reads=[mk.res], writes=[e_.res])
                sc.op(dve, lambda: V.reduce_sum(out=rs.t[:, 0:1], in_=e_.t[:, :], axis=AX.X), reads=[e_.res], writes=[rs.res])
                sc.op(dve, lambda: V.reciprocal(out=rs.t[:, 1:2], in_=rs.t[:, 0:1]), reads=[], writes=[rs.res])
                sc.op(dve, lambda: V.tensor_scalar(out=gt.t[:, :], in0=e_.t[:, :], scalar1=rs.t[:, 1:2], scalar2=None,
                                                   op0=ALU.mult), reads=[e_.res, rs.res], writes=[gt.res])
                sc.dma(sp, G_d[rows, :], gt.t[:, :], reads=[gt.res])
                idxu = k.nxt("idxu", idxu_b); idxf = d["idxf"] = k.nxt("idxf", idxf_b); mkbf = d["mkbf"] = k.nxt("mkbf", mkbf_b)
                e4 = d["e4"] = k.nxt("e4", e4_b)
                d["rs"] = rs; d["gtbf"] = gtbf; d["gT"] = gT
                sc.op(dve, lambda: V.max_index(out=idxu.t[:, :], in_max=m8_.t[:, :], in_values=lg.t[:, :]),
                      reads=[m8_.res, lg.res], writes=[idxu.res])
                sc.op(dve, lambda: V.tensor_copy(out=idxf.t[:, :], in_=idxu.t[:, 0:4]), reads=[idxu.res], writes=[idxf.res])
                sc.op(pool, lambda: G.tensor_copy(out=mkbf.t[:, :], in_=mk.t[:, :]), reads=[mk.res], writes=[mkbf.res])
                sc.op(pool, lambda: G.tensor_copy(out=gtbf.t[:, :], in_=gt.t[:, :]), reads=[gt.res], writes=[gtbf.res])
                sc.op(dve, lambda: V.tensor_scalar(out=e4.t[:, :], in0=m8_.t[:, 0:4], scalar1=m8_.t[:, 0:1], scalar2=None,
                                                   op0=ALU.subtract), reads=[m8_.res], writes=[e4.res])
                sc.op(act, lambda: A.activation(out=e4.t[:, :], in_=e4.t[:, :], func=AF.Exp), reads=[], writes=[e4.res])

            def stage_d1(tt):
                d = st[tt]
                idxf, mkbf, e4, rs, gtbf, gT, hbf = d["idxf"], d["mkbf"], d["e4"], d["rs"], d["gtbf"], d["gT"], d["hbf"]
                rank = k.nxt("rank", rank_b); oh = k.nxt("oh", oh_b); rk = k.nxt("rk", rk_b)
                val = k.nxt("val", val_b); t1 = k.nxt("t1", t1_b)
                bkr = k.nxt("bank", banks)
                sc.op(pe, lambda: T.matmul(bkr.t[:, 0:32], lhsT=triu.t[:, :], rhs=mkbf.t[:, :], start=True, stop=True),
                      reads=[triu.res, mkbf.res], writes=[bkr.res])
                bkc = k.nxt("bank", banks)
                sc.op(pe, lambda: T.matmul(bkc.t[:, 0:32], lhsT=ones128.t[:, :], rhs=mkbf.t[:, :], start=True, stop=True),
                      reads=[ones128.res, mkbf.res], writes=[bkc.res])
                sc.op(pe, lambda: T.transpose(out=tbank.t[0:32, 0:128], in_=gtbf.t[:, :], identity=ident.t[:, :]),
                      reads=[gtbf.res, ident.res], writes=[tbank.res])
                sc.op(act, lambda: A.copy(out=gT.t[:, :], in_=tbank.t[0:32, 0:128]), reads=[tbank.res], writes=[gT.res])
                sc.op(dve, lambda: V.tensor_tensor(out=rank.t[:, :], in0=cumrep.t[:, :], in1=bkr.t[:, 0:32], op=ALU.add),
                      reads=[cumrep.res, bkr.res], writes=[rank.res])
                sc.op(dve, lambda: V.tensor_tensor(out=cumrep.t[:, :], in0=cumrep.t[:, :], in1=bkc.t[:, 0:32], op=ALU.add),
                      reads=[bkc.res], writes=[cumrep.res])
                for kk in range(4):
                    sc.op(dve, lambda: V.tensor_scalar(out=oh.t[:, :], in0=iota32.t[:, :], scalar1=idxf.t[:, kk:kk + 1], scalar2=None,
                                                       op0=ALU.is_equal), reads=[iota32.res, idxf.res], writes=[oh.res])
                    sc.op(dve, lambda: V.tensor_tensor(out=oh.t[:, :], in0=oh.t[:, :], in1=rank.t[:, :], op=ALU.mult),
                          reads=[rank.res], writes=[oh.res])
                    sc.op(dve, lambda: V.reduce_sum(out=rk.t[:, kk:kk + 1], in_=oh.t[:, :], axis=AX.X), reads=[oh.res], writes=[rk.res])
                sc.op(dve, lambda: V.tensor_scalar(out=val.t[:, :], in0=rk.t[:, :], scalar1=float(CAP), scalar2=None, op0=ALU.is_lt),
                      reads=[rk.res], writes=[val.res])
                sc.op(dve, lambda: V.scalar_tensor_tensor(out=t1.t[:, :], in0=idxf.t[:, :], scalar=float(CAP), in1=rk.t[:, :],
                                                          op0=ALU.mult, op1=ALU.add), reads=[idxf.res, rk.res], writes=[t1.res])
                sc.op(dve, lambda: V.scalar_tensor_tensor(out=t1.t[:, :], in0=t1.t[:, :], scalar=trp.t[:, 0:1], in1=val.t[:, :],
                                                          op0=ALU.subtract, op1=ALU.mult), reads=[trp.res, val.res], writes=[t1.res])
                sc.op(dve, lambda: V.tensor_scalar(out=dest_all.t[:, tt, :], in0=t1.t[:, :], scalar1=trp.t[:, 0:1], scalar2=None,
                                                   op0=ALU.add), reads=[t1.res, trp.res], writes=[dest_res[tt]])
                sc.op(dve, lambda: V.scalar_tensor_tensor(out=gk_all.t[:, tt, :], in0=e4.t[:, :], scalar=rs.t[:, 1:2], in1=val.t[:, :],
                                                          op0=ALU.mult, op1=ALU.mult), reads=[e4.res, rs.res, val.res], writes=[gk_res[tt]])
                for kk in range(4):
                    sc.dma(pool, lambda: G.indirect_dma_start(
                        out=xs_d[:, :], out_offset=bass.IndirectOffsetOnAxis(ap=dest_all.t[:, tt, kk:kk + 1], axis=0),
                        in_=hbf.t[:, :], in_offset=None), None, reads=[hbf.res, dest_res[tt]])

            def stage_d2(tt):
                d = st.pop(tt)
                gT, base = d["gT"], d["base"]
                rows = slice(tt * 128, (tt + 1) * 128)
                for ch in range(2):
                    bk2 = k.nxt("bank", banks)
                    sc.op(pe, lambda: T.matmul(bk2.t[:, :], lhsT=gT.t[0:32, :], rhs=b2a.t[0:32, ch * 512:(ch + 1) * 512],
                                               start=True, stop=True), reads=[gT.res, b2a.res], writes=[bk2.res])
                    sc.op(dve, lambda: V.tensor_tensor(out=base.t[:, ch * 512:(ch + 1) * 512], in0=base.t[:, ch * 512:(ch + 1) * 512],
                                                       in1=bk2.t[:, :], op=ALU.add), reads=[bk2.res], writes=[base.res])
                sc.dma(sp, base_d[rows, :], base.t[:, :], reads=[base.res])

            for it in range(NQT + 3):
                if 0 <= it - 3 < NQT:
                    stage_d1(it - 3)
                if 0 <= it - 2 < NQT:
                    stage_c(it - 2)
                if 0 <= it - 1 < NQT:
                    stage_b(it - 1)
                if it < NQT:
                    stage_a(it)
                if 0 <= it - 1 < NQT:
                    stage_b2(it - 1)
                if 0 <= it - 3 < NQT:
                    stage_d2(it - 3)
            sc.barrier()

    k.phase4 = phase4
    def phase5():
        NST = CAP // 128
        groups = [(g0, min(512, CAP - g0)) for g0 in range(0, CAP, 512)]
        with contextlib.ExitStack() as es:
            W1b = [k.sb(es, f"W1b{i}", [128, 8, 2048], BF16) for i in range(2)]
            W1res = [[Res() for _ in range(4)] for _ in range(2)]
            W2b = [k.sb(es, f"W2b{i}", [128, 8, 1024], BF16) for i in range(2)]
            W2res = [[Res() for _ in range(2)] for _ in range(2)]
            stg = [k.sb(es, f"wstg{i}", [128, 8, 512], F32) for i in range(2)]
            xTe2 = [k.sb(es, f"xTe{i}", [128, 8, CAP], BF16) for i in range(2)]
            actT = k.sb(es, "actT", [128, 8, CAP], BF16)
            xsl = [k.sb(es, f"xsl{i}", [128, 1024], BF16) for i in range(4)]
            yst = [k.sb(es, f"yst{i}", [128, 1024], F32) for i in range(2)]
            b1T = k.sb(es, "b1T", [128, 512], F32)
            sc.dma(sp, b1T.t[:, :], I("b_e1T")[:, :], writes=[b1T.res])
            gt_ = [k.sb(es, f"mg{i}", [128, 512], F32) for i in range(3)]
            sg_ = [k.sb(es, f"msg{i}", [128, 512], F32) for i in range(3)]
            lt_ = [k.sb(es, f"ml{i}", [128, 512], F32) for i in range(3)]
            w_e1, w_e2 = I("w_e1"), I("w_e2")
            sc.op(pool, lambda: G.memset(yst[0].t[:, :], 0.0), writes=[yst[0].res])
            sc.dma(sp, ys_d[32 * CAP:32 * CAP + 128, :], yst[0].t[:, :], reads=[yst[0].res])

            wseq = []
            for e_ in range(32):
                for blk in (0, 2, 1, 3):
                    wseq.append((e_, 1, blk))
                for blk in range(2):
                    wseq.append((e_, 2, blk))
            wstate = {"dma": 0, "cast": 0}

            def w_dma():
                n = wstate["dma"]
                if n >= len(wseq):
                    return
                wstate["dma"] += 1
                e_, kind, blk = wseq[n]
                s_ = stg[n % 2]
                src = (w_e1 if kind == 1 else w_e2)[e_, :, blk * 512:(blk + 1) * 512].rearrange("(kc p) n -> p kc n", p=128)
                sc.dma(sp, s_.t[:, :, :], src, writes=[s_.res])

            NPIECE = 2

            def w_cast_piece():
                n = wstate["cast"]
                if n >= len(wseq):
                    return
                j = wstate.get("piece", 0)
                e_, kind, blk = wseq[n]
                s_ = stg[n % 2]
                if kind == 1:
                    dst, rs_ = W1b[e_ % 2], W1res[e_ % 2][blk]
                else:
                    dst, rs_ = W2b[e_ % 2], W2res[e_ % 2][blk]
                kc0 = j * 4
                sc.op(act, lambda: A.copy(out=dst.t[:, kc0:kc0 + 4, blk * 512:(blk + 1) * 512], in_=s_.t[:, kc0:kc0 + 4, :]),
                      reads=[s_.res], writes=[rs_])
                if j + 1 == NPIECE:
                    wstate["piece"] = 0
                    wstate["cast"] += 1
                    w_dma()
                else:
                    wstate["piece"] = j + 1

            def w_cast():
                for _ in range(NPIECE):
                    w_cast_piece()

            w_dma(); w_dma()
            for _ in range(6):
                w_cast()
            def xs_tile(e_, s_i):
                xt_ = xTe2[e_ % 2]
                xl = k.nxt("xsl", xsl)
                r0 = e_ * CAP + s_i * 128
                sc.dma(sp, xl.t[:, :], xs_d[r0:r0 + 128, :], writes=[xl.res])
                for i in range(8):
                    sc.op(pe, lambda: T.transpose(out=tbank.t[:, i * 128:(i + 1) * 128], in_=xl.t[:, i * 128:(i + 1) * 128],
                                                  identity=ident.t[:, :]), reads=[xl.res, ident.res], writes=[tbank.res])
                sc.op(dve, lambda: V.tensor_copy(out=xt_.t[:, :, s_i * 128:(s_i + 1) * 128],
                                                 in_=tbank.t[:, :].rearrange("p (kc t) -> p kc t", kc=8)),
                      reads=[tbank.res], writes=[xt_.res])

            for s_i in range(NST):
                xs_tile(0, s_i)
            for e in range(32):
                w1 = W1b[e % 2]
                xTe = xTe2[e % 2]
                gi = 0
                for (g0, gn) in groups:
                    for fc in range(8):
                        if gi in (0, 2, 3, 5, 6, 8, 9, 11, 12, 14):
                            w_cast_piece()
                        gi += 1
                        bg = k.nxt("bank", banks)
                        bl = k.nxt("bank", banks)
                        for (bk, c0) in ((bg, fc * 128), (bl, 1024 + fc * 128)):
                            blk = c0 // 512
                            for kc in range(8):
                                sc.op(pe, lambda: T.matmul(bk.t[:, 0:gn], lhsT=w1.t[:, kc, c0:c0 + 128],
                                                           rhs=xTe.t[:, kc, g0:g0 + gn], start=(kc == 0), stop=(kc == 7)),
                                      reads=[W1res[e % 2][blk], xTe.res], writes=[bk.res])
                        g_ = k.nxt("mg", gt_); s_ = k.nxt("msg", sg_); l_ = k.nxt("ml", lt_)
                        cg = e * 16 + fc
                        cl = e * 16 + 8 + fc
                        sc.op(dve, lambda: V.tensor_scalar(out=g_.t[:, 0:gn], in0=bg.t[:, 0:gn], scalar1=b1T.t[:, cg:cg + 1], scalar2=7.0,
                                                           op0=ALU.add, op1=ALU.min), reads=[bg.res, b1T.res], writes=[g_.res])
                        sc.op(act, lambda: A.activation(out=s_.t[:, 0:gn], in_=g_.t[:, 0:gn], func=AF.Sigmoid, scale=1.702),
                              reads=[g_.res], writes=[s_.res])
                        sc.op(dve, lambda: V.tensor_scalar(out=l_.t[:, 0:gn], in0=bl.t[:, 0:gn], scalar1=b1T.t[:, cl:cl + 1], scalar2=-7.0,
                                                           op0=ALU.add, op1=ALU.max), reads=[bl.res, b1T.res], writes=[l_.res])
                        sc.op(dve, lambda: V.tensor_scalar(out=l_.t[:, 0:gn], in0=l_.t[:, 0:gn], scalar1=7.0, scalar2=1.0,
                                                           op0=ALU.min, op1=ALU.add), reads=[], writes=[l_.res])
                        sc.op(dve, lambda: V.tensor_tensor(out=g_.t[:, 0:gn], in0=g_.t[:, 0:gn], in1=s_.t[:, 0:gn], op=ALU.mult),
                              reads=[s_.res], writes=[g_.res])
                        sc.op(pool, lambda: G.tensor_tensor(out=actT.t[:, fc, g0:g0 + gn], in0=g_.t[:, 0:gn], in1=l_.t[:, 0:gn], op=ALU.mult),
                              reads=[g_.res, l_.res], writes=[actT.res])
                for s_i in range(NST):
                    if s_i in (1, 3):
                        w_cast_piece()
                    ys_ = k.nxt("yst", yst)
                    for ch in range(2):
                        bk = k.nxt("bank", banks)
                        for fc in range(8):
                            sc.op(pe, lambda: T.matmul(bk.t[:, :], lhsT=actT.t[:, fc, s_i * 128:(s_i + 1) * 128],
                                                       rhs=W2b[e % 2].t[:, fc, ch * 512:(ch + 1) * 512], start=(fc == 0), stop=(fc == 7)),
                                  reads=[actT.res, W2res[e % 2][ch]], writes=[bk.res])
                        evac_copy(ys_.t[:, ch * 512:(ch + 1) * 512], bk.t[:, :], [bk.res], [ys_.res])
                    r0 = e * CAP + s_i * 128
                    sc.dma(act, ys_d[r0:r0 + 128, :], ys_.t[:, :], reads=[ys_.res])
                    if e + 1 < 32:
                        xs_tile(e + 1, s_i)
            sc.barrier()
        with contextlib.ExitStack() as es:
            g2b = bcast_load(es, "g2b", I("ln2_g")[:, :], 1024)
            b2b = bcast_load(es, "b2b", I("ln2_b")[:, :], 1024)
            accb = [k.sb(es, f"cacc{i}", [128, 1024], F32) for i in range(4)]
            yb = [k.sb(es, f"cy{i}", [128, 1024], F32) for i in range(12)]
            lnst = [k.sb(es, f"lnst5{i}", [128, 16], F32) for i in range(4)]
            for tt in range(NQT):
                ac = k.nxt("cacc", accb)
                rows = slice(tt * 128, (tt + 1) * 128)
                sc.dma(sp, ac.t[:, :], base_d[rows, :], writes=[ac.res])
                for kk in range(4):
                    y_ = k.nxt("cy", yb)
                    sc.dma(pool, lambda: G.indirect_dma_start(
                        out=y_.t[:, :], out_offset=None, in_=ys_d[:, :],
                        in_offset=bass.IndirectOffsetOnAxis(ap=dest_all.t[:, tt, kk:kk + 1], axis=0)), None,
                        reads=[dest_res[tt]], writes=[y_.res])
                    sc.op(dve, lambda: V.scalar_tensor_tensor(out=ac.t[:, :], in0=y_.t[:, :], scalar=gk_all.t[:, tt, kk:kk + 1],
                                                              in1=ac.t[:, :], op0=ALU.mult, op1=ALU.add),
                          reads=[y_.res, gk_res[tt]], writes=[ac.res])
                st_ = k.nxt("lnst5", lnst)
                sc.op(dve, lambda: V.bn_stats(out=st_.t[:, 0:6], in_=ac.t[:, 0:512]), reads=[ac.res], writes=[st_.res])
                sc.op(dve, lambda: V.bn_stats(out=st_.t[:, 6:12], in_=ac.t[:, 512:1024]), reads=[ac.res], writes=[st_.res])
                sc.op(dve, lambda: V.bn_aggr(out=st_.t[:, 12:14], in_=st_.t[:, 0:12]), reads=[], writes=[st_.res])
                sc.op(act, lambda: A.activation(out=st_.t[:, 14:15], in_=st_.t[:, 13:14], func=AF.Ln, bias=epsb.t[:, 0:1], scale=1.0),
                      reads=[st_.res, epsb.res], writes=[st_.res])
                sc.op(act, lambda: A.activation(out=st_.t[:, 14:15], in_=st_.t[:, 14:15], func=AF.Exp, scale=-0.5),
                      reads=[], writes=[st_.res])
                sc.op(dve, lambda: V.tensor_scalar(out=ac.t[:, :], in0=ac.t[:, :], scalar1=st_.t[:, 12:13],
                                                   scalar2=st_.t[:, 14:15], op0=ALU.subtract, op1=ALU.mult),
                      reads=[st_.res], writes=[ac.res])
                sc.op(dve, lambda: V.tensor_tensor(out=ac.t[:, :], in0=ac.t[:, :], in1=g2b.t[:, :], op=ALU.mult),
                      reads=[g2b.res], writes=[ac.res])
                sc.op(dve, lambda: V.tensor_tensor(out=ac.t[:, :], in0=ac.t[:, :], in1=b2b.t[:, :], op=ALU.add),
                      reads=[b2b.res], writes=[ac.res])
                sc.dma(sp, out_d[rows, :], ac.t[:, :], reads=[ac.res])
            sc.barrier()

    k.phase5 = phase5
    k.phase1 = phase1
    return k


def prep_inputs(inp):
    f = lambda a: np.ascontiguousarray(np.asarray(a, np.float32))
    sh = {}
    sh["w_in"] = f(inp["w_in"][0])
    for n in ("diff_lq1", "diff_lk1", "diff_lq2", "diff_lk2", "diff_subln_g", "b_mgate", "ln1_g", "ln1_b",
              "b_router", "b_ple_gate", "ln2_g", "ln2_b"):
        sh[n] = f(inp[n][0]).reshape(1, -1)
    sh["nsa_pos_kT"] = f(np.asarray(inp["nsa_pos_k"][0]).T)
    sh["nsa_pos_vT"] = f(np.asarray(inp["nsa_pos_v"][0]).T)
    sh["nsa_phi_k1"] = f(np.asarray(inp["nsa_phi_k1"][0]).reshape(32, 64, 256).transpose(1, 0, 2))
    for nm in ("k", "v"):
        w = np.asarray(inp["nsa_phi_%s1" % nm][0]).reshape(16, 2, 64, 256)
        sh["nsa_phi_%s1s" % nm] = f(w.transpose(1, 2, 0, 3).reshape(128, 16, 256))
        ps = np.asarray(inp["nsa_pos_%s" % nm][0]).reshape(16, 2, 64)
        sh["nsa_pos_%sTs" % nm] = f(ps.transpose(1, 2, 0).reshape(128, 16))
    sh["nsa_phi_v1"] = f(np.asarray(inp["nsa_phi_v1"][0]).reshape(32, 64, 256).transpose(1, 0, 2))
    for n in ("nsa_phi_k2", "nsa_phi_v2", "w_br_diff", "w_br_nsa", "w_mgate", "w_o", "w_router", "w_e1", "w_e2",
              "b_e2", "w_ple_gate", "w_ple_proj"):
        sh[n] = f(inp[n][0])
    sh["b_e1T"] = f(np.asarray(inp["b_e1"][0]).reshape(32, 16, 128).transpose(2, 0, 1).reshape(128, 512))
    sh.update(_const_tables())
    x = np.asarray(inp["x"], np.float32)
    p = np.asarray(inp["p"], np.float32)
    maps = []
    for b in range(8):
        m = dict(sh)
        m["x"] = f(x[b])
        m["xT"] = f(x[b].T)
        m["pT"] = f(p[0, b].T)
        maps.append(m)
    return maps


_CACHE = {}


def kernel(**inputs):
    if "nc" not in _CACHE:
        kb = build()
        kb.phase1(); kb.phase2(); kb.phase3(); kb.phase4(); kb.phase5()
        _CACHE["nc"] = kb
    kb = _CACHE["nc"]
    maps = prep_inputs(inputs)
    maps = [{n: m[n] for n in kb.din} for m in maps]
    res = run_bass_kernel_spmd(kb.nc, maps, core_ids=list(range(8)))
    out = np.stack([np.asarray(res.results[b]["out"], np.float32) for b in range(8)], axis=0)
    return out
```

```python
import contextlib
import os
import math
import numpy as np
import ml_dtypes
import concourse.bass as bass
import concourse.mybir as mybir
from concourse.bass_utils import run_bass_kernel_spmd

F32 = mybir.dt.float32
BF16 = mybir.dt.bfloat16
AF = mybir.ActivationFunctionType
ALU = mybir.AluOpType
AX = mybir.AxisListType

S = 4096
D = 1024
NQT = 32
NEGB = -131072.0
SCALE = 0.125
LN_EPS = 1e-5
ALPHA = 2.0 ** 0.25
LAMBDA_INIT = 0.2
SEM_LIMIT = 30000
SKIP_SAME = set(os.environ.get('KSKIP', '').split(',')) - {''}
CAP = 768
NSLOT = 32 * CAP + 128


class Res:
    __slots__ = ("w", "r")

    def __init__(self):
        self.w = None
        self.r = {}


class Eng:
    def __init__(self, sch, name, eng):
        self.sch = sch
        self.name = name
        self.eng = eng
        self.sem = sch.new_sem(name)
        self.cnt = 0
        self.known = {}
        self.slots = []
        self.slot_i = 0


class Sched:
    def __init__(self, nc, es):
        self.nc = nc
        self.es = es
        self.nsem = 0
        self.sems = {}
        self.pe = Eng(self, "pe", nc.tensor)
        self.act = Eng(self, "act", nc.scalar)
        self.dve = Eng(self, "dve", nc.vector)
        self.pool = Eng(self, "pool", nc.gpsimd)
        self.sp = Eng(self, "sp", nc.sync)
        self.engs = [self.pe, self.act, self.dve, self.pool, self.sp]
        for e, n in ((self.sp, 24), (self.pool, 16), (self.act, 4)):
            for i in range(n):
                e.slots.append([self.new_sem(f"{e.name}_d{i}"), 0])
        self.n_ins = 0

    def new_sem(self, name):
        self.nsem += 1
        s = self.es.enter_context(self.nc.semaphore(f"s{self.nsem}_{name}"))
        self.sems[id(s)] = s
        return s

    def _wait(self, E, toks):
        for sem, val in toks:
            k = id(sem)
            if E.known.get(k, 0) >= val:
                continue
            E.eng.wait_ge(sem, val)
            E.known[k] = val

    def _deps(self, E, reads, writes):
        deps = []
        for r in reads:
            if r.w is not None:
                deps.append(r.w)
        for w in writes:
            if w.w is not None:
                deps.append(w.w)
            deps.extend(w.r.values())
        if E is self.pe or E.name in SKIP_SAME:
            deps = [d for d in deps if d[0] is not E.sem]
        return deps

    def _commit(self, tok, reads, writes):
        for r in reads:
            k = id(tok[0])
            r.r[k] = tok
        for w in writes:
            w.w = tok
            w.r = {}

    def op(self, E, fn, reads=(), writes=()):
        self._wait(E, self._deps(E, reads, writes))
        ins = fn()
        E.cnt += 1
        ins.then_inc(E.sem, 1)
        tok = (E.sem, E.cnt)
        self._commit(tok, reads, writes)
        self.n_ins += 1
        if E.cnt >= SEM_LIMIT:
            E.sem = self.new_sem(E.name)
            E.cnt = 0
        return tok

    def dma(self, E, out, in_, reads=(), writes=(), **kw):
        slot = E.slots[E.slot_i]
        E.slot_i = (E.slot_i + 1) % len(E.slots)
        deps = self._deps(E, reads, writes)
        if slot[1] > 0:
            deps.append((slot[0], slot[1]))
        self._wait(E, deps)
        if slot[1] + 16 > SEM_LIMIT:
            slot[0] = self.new_sem(E.name + "_d")
            slot[1] = 0
        if callable(out):
            out().then_inc(slot[0], 16)
        else:
            E.eng.dma_start(out=out, in_=in_, **kw).then_inc(slot[0], 16)
        slot[1] += 16
        tok = (slot[0], slot[1])
        self._commit(tok, reads, writes)
        self.n_ins += 1
        return tok

    def barrier(self):
        toks = []
        for e in self.engs:
            if e.cnt > 0:
                toks.append((e.sem, e.cnt))
            for s in e.slots:
                if s[1] > 0:
                    toks.append((s[0], s[1]))
        for e in self.engs:
            self._wait(e, toks)


def _bf(x):
    return np.asarray(x, np.float32).astype(ml_dtypes.bfloat16)


def _hi_lo(v):
    v = np.asarray(v, np.float64)
    hi = v.astype(np.float32).astype(ml_dtypes.bfloat16).astype(np.float64)
    lo = (v - hi)
    return hi, lo


def _const_tables():
    t = {}
    pos = np.arange(S)
    augk = np.stack([pos // 128, pos // 128, pos % 128, pos % 128, np.ones(S), np.ones(S)]).astype(np.float32)
    t["augk_tok"] = _bf(augk)
    cpos = np.arange(255) * 16 + 31
    augc = np.zeros((6, 256), np.float32)
    augc[:, :255] = np.stack([cpos // 128, cpos // 128, cpos % 128, cpos % 128, np.ones(255), np.ones(255)])
    t["augk_cmp"] = _bf(augc)
    slopes = list(2.0 ** (-8.0 * np.arange(1, 9) / 8)) + list(2.0 ** (-8.0 * np.arange(1, 17) / 16))
    augq = np.zeros((24, 6, S), np.float32)
    for i, s in enumerate(slopes):
        shi, slo = _hi_lo(s)
        augq[i, 0] = 1024.0 * shi
        augq[i, 1] = 1024.0 * slo
        augq[i, 2] = 8.0 * shi
        augq[i, 3] = 8.0 * slo
        hi, lo = _hi_lo(-8.0 * s * pos)
        augq[i, 4] = hi
        augq[i, 5] = lo
    t["augq"] = _bf(augq)
    kk = np.arange(128)[:, None]
    qq = np.arange(128)[None, :]
    t["m_caus"] = _bf(np.where(qq >= kk, 0.0, NEGB))
    t["m_band"] = _bf(np.where(qq < kk, 0.0, NEGB))
    t["ident"] = _bf(np.eye(128))
    n = np.arange(256).reshape(2, 128)
    valid = ((n[:, :, None] * 16 + 31) <= pos[None, None, :]) & (n[:, :, None] < 255)
    t["m_cmp"] = _bf(np.where(valid, 0.0, NEGB).transpose(1, 0, 2))
    c_lo = np.arange(255) * 16
    s_lo = np.arange(64) * 64
    ov = np.minimum(c_lo[:, None] + 32, s_lo[None, :] + 64) - np.maximum(c_lo[:, None], s_lo[None, :])
    c2s = np.zeros((256, 64), np.float32)
    c2s[:255] = np.clip(ov, 0, None) / 32.0
    t["c2s"] = _bf(c2s.reshape(2, 128, 64).transpose(1, 0, 2))
    cur = (pos // 64)[:, None]
    j = np.arange(64)[None, :]
    forced = (j == 0) | (j == cur) | (j == cur - 1)
    keep = (~forced) & (j <= cur)
    tf = np.where(forced, 1e30, np.where(j <= cur, 0.0, -1e30)).astype(np.float32)
    t["sel_keep"] = np.ascontiguousarray(keep.astype(np.float32).reshape(32, 128, 64).transpose(1, 0, 2))
    t["sel_force"] = np.ascontiguousarray(tf.reshape(32, 128, 64).transpose(1, 0, 2))
    t["expand"] = _bf((np.arange(64)[:, None] == (pos // 64)[None, :]).astype(np.float32))
    t["triu"] = _bf((np.arange(128)[:, None] < np.arange(128)[None, :]).astype(np.float32))
    t["ones128"] = _bf(np.ones((128, 128)))
    t["iota32"] = np.tile(np.arange(32, dtype=np.float32)[None, :], (128, 1))
    t["trp"] = (32 * CAP + np.arange(128, dtype=np.float32)).reshape(128, 1)
    return t


WEIGHT_NAMES = [
    "w_in", "diff_lq1", "diff_lk1", "diff_lq2", "diff_lk2", "diff_subln_g",
    "nsa_pos_k", "nsa_pos_v", "nsa_phi_k1", "nsa_phi_k2", "nsa_phi_v1", "nsa_phi_v2",
    "w_br_diff", "w_br_nsa", "w_mgate", "b_mgate", "w_o", "ln1_g", "ln1_b",
    "w_router", "b_router", "w_e1", "b_e1", "w_e2", "b_e2",
    "w_ple_gate", "b_ple_gate", "w_ple_proj", "ln2_g", "ln2_b",
]


class Buf:
    __slots__ = ("t", "res")

    def __init__(self, t):
        self.t = t
        self.res = Res()


class KB:
    def __init__(self, dbg=()):
        self.dbg = set(dbg)
        self.nc = bass.Bass("TRN2", target_bir_lowering=False)
        self.es = contextlib.ExitStack()
        self.sc = Sched(self.nc, self.es)
        self.din = {}
        self.rot = {}

    def inp(self, name, shape, dt=F32):
        t = self.nc.dram_tensor(name, list(shape), dt, kind="ExternalInput").ap()
        self.din[name] = t
        return t

    def scratch(self, name, shape, dt):
        kind = "ExternalOutput" if name in self.dbg else "Internal"
        return self.nc.dram_tensor(name, list(shape), dt, kind=kind).ap()

    def sb(self, es, name, shape, dt):
        return Buf(es.enter_context(self.nc.sbuf_tensor("sb_" + name, list(shape), dt)))

    def ps(self, es, name, shape, dt):
        return Buf(es.enter_context(self.nc.psum_tensor("ps_" + name, list(shape), dt)))

    def nxt(self, key, lst):
        i = self.rot.get(key, 0)
        self.rot[key] = i + 1
        return lst[i % len(lst)]


def build(dbg=(), phases=(1, 2, 3, 4, 5)):
    k = KB(dbg)
    nc, sc = k.nc, k.sc
    pe, act, dve, pool, sp = sc.pe, sc.act, sc.dve, sc.pool, sc.sp
    V, G, T = nc.vector, nc.gpsimd, nc.tensor
    A = nc.scalar

    SHAPES = {
        "x": ([S, D], F32), "xT": ([D, S], F32), "pT": ([256, S], F32), "w_in": ([D, 5680], F32),
        "diff_lq1": ([1, 64], F32), "diff_lk1": ([1, 64], F32), "diff_lq2": ([1, 64], F32), "diff_lk2": ([1, 64], F32),
        "diff_subln_g": ([1, 128], F32), "nsa_pos_kT": ([64, 32], F32), "nsa_pos_vT": ([64, 32], F32),
        "nsa_phi_k1": ([64, 32, 256], F32), "nsa_phi_v1": ([64, 32, 256], F32),
        "nsa_phi_k1s": ([128, 16, 256], F32), "nsa_phi_v1s": ([128, 16, 256], F32), "nsa_pos_kTs": ([128, 16], F32), "nsa_pos_vTs": ([128, 16], F32),
        "nsa_phi_k2": ([256, 64], F32), "nsa_phi_v2": ([256, 64], F32),
        "w_br_diff": ([D, D], F32), "w_br_nsa": ([D, D], F32), "w_mgate": ([D, 2 * D], F32), "b_mgate": ([1, 2 * D], F32),
        "w_o": ([D, D], F32), "ln1_g": ([1, D], F32), "ln1_b": ([1, D], F32),
        "w_router": ([D, 32], F32), "b_router": ([1, 32], F32),
        "w_e1": ([32, D, 2 * D], F32), "b_e1T": ([128, 512], F32), "w_e2": ([32, D, D], F32), "b_e2": ([32, D], F32),
        "w_ple_gate": ([D, D], F32), "b_ple_gate": ([1, D], F32), "w_ple_proj": ([256, D], F32),
        "ln2_g": ([1, D], F32), "ln2_b": ([1, D], F32),
        "augk_tok": ([6, S], BF16), "augk_cmp": ([6, 256], BF16), "augq": ([24, 6, S], BF16),
        "m_caus": ([128, 128], BF16), "m_band": ([128, 128], BF16), "ident": ([128, 128], BF16),
        "m_cmp": ([128, 2, S], BF16), "c2s": ([128, 2, 64], BF16),
        "sel_keep": ([128, 32, 64], F32), "sel_force": ([128, 32, 64], F32), "expand": ([64, S], BF16),
        "triu": ([128, 128], BF16), "ones128": ([128, 128], BF16), "iota32": ([128, 32], F32), "trp": ([128, 1], F32),
    }

    def I(name):
        if name not in k.din:
            shp, dt = SHAPES[name]
            k.inp(name, shp, dt)
        return k.din[name]

    out_d = nc.dram_tensor("out", [S, D], F32, kind="ExternalOutput").ap()

    QdT = k.scratch("QdT", [D, S], BF16); KdT = k.scratch("KdT", [D, S], BF16)
    Vd = k.scratch("Vd", [S, D], BF16); NqT = k.scratch("NqT", [D, S], BF16)
    kcT = k.scratch("kcT", [256, S], BF16); vcT = k.scratch("vcT", [256, S], BF16)
    ksT = k.scratch("ksT", [256, S], BF16); vs_d = k.scratch("vs", [S, 256], BF16)
    kwT = k.scratch("kwT", [256, S], BF16); vw_d = k.scratch("vw", [S, 256], BF16)
    gates_d = k.scratch("gates", [S, 48], F32)
    odiff_d = k.scratch("o_diff", [S, D], BF16); onsa_d = k.scratch("o_nsa", [S, D], BF16)
    hT_d = k.scratch("hT", [D, S], BF16); base_d = k.scratch("base", [S, D], F32)
    G_d = k.scratch("Gd", [S, 32], F32)
    xs_d = k.scratch("xs", [NSLOT, D], BF16); ys_d = k.scratch("ys", [NSLOT, D], F32)

    es0 = k.es
    banks = [k.ps(es0, f"bank{i}", [128, 512], F32) for i in range(7)]
    tbank = k.ps(es0, "tbank", [128, 1024], BF16)
    ident = k.sb(es0, "ident", [128, 128], BF16)
    mcaus = k.sb(es0, "mcaus", [128, 128], BF16)
    mband = k.sb(es0, "mband", [128, 128], BF16)
    sc.dma(sp, ident.t[:], I("ident")[:, :], writes=[ident.res])
    wz = k.sb(es0, "wz", [128, 512], BF16)
    sc.op(pool, lambda: G.memset(wz.t[:, :], 0.0), writes=[wz.res])
    epsb = k.sb(es0, "epsb", [128, 1], F32)
    sc.op(pool, lambda: G.memset(epsb.t[:, :], LN_EPS), writes=[epsb.res])
    sc.dma(sp, mcaus.t[:], I("m_caus")[:, :], writes=[mcaus.res])
    sc.dma(sp, mband.t[:], I("m_band")[:, :], writes=[mband.res])

    U32 = mybir.dt.uint32
    dest_all = k.sb(es0, "dest_all", [128, 32, 4], U32)
    gk_all = k.sb(es0, "gk_all", [128, 32, 4], F32)
    dest_res = [Res() for _ in range(32)]
    gk_res = [Res() for _ in range(32)]
    evac_i = [0]

    def evac_copy(out_ap, in_ap, reads, writes):
        evac_i[0] += 1
        if evac_i[0] % 2:
            return sc.op(act, lambda: A.copy(out=out_ap, in_=in_ap), reads=reads, writes=writes)
        return sc.op(dve, lambda: V.tensor_copy(out=out_ap, in_=in_ap), reads=reads, writes=writes)

    def phase1():
        with contextlib.ExitStack() as es:
            xT = k.sb(es, "xT", [128, 8, S], BF16)
            rx = [Res() for _ in range(8)]
            for kc in range(8):
                sc.dma(pool, xT.t[:, kc, :], I("xT")[kc * 128:(kc + 1) * 128, :], writes=[rx[kc]])
            wb = [k.sb(es, f"wb{i}", [128, 8, 1024], BF16) for i in range(2)]
            sfm = [k.sb(es, f"sfm{i}", [128, S], BF16) for i in range(2)]
            stm = [k.sb(es, f"stm{i}", [128, 4, 1024], BF16) for i in range(2)]
            sg = k.sb(es, "sgate", [128, 32, 48], F32)
            loads = [(0, 1024), (1024, 1024), (2048, 1024), (3072, 1024), (4096, 1024), (5120, 560)]
            subs = [
                [(0, 1024, "fm", QdT, 0)], [(0, 1024, "fm", KdT, 0)], [(0, 1024, "tm", Vd, 0)],
                [(0, 1024, "fm", NqT, 0)],
                [(0, 256, "fm", kcT, 0), (256, 256, "fm", vcT, 0), (512, 256, "fm", ksT, 0), (768, 256, "tm", vs_d, 0)],
                [(0, 256, "fm", kwT, 0), (256, 256, "tm", vw_d, 0), (512, 48, "gate", None, 0)],
            ]
            bi = 0
            for li, (c0, n) in enumerate(loads):
                w = wb[li % 2]
                src = I("w_in")[:, c0:c0 + n].rearrange("(kc p) n -> p kc n", p=128)
                sc.dma(pool, w.t[:, :, 0:n], src, writes=[w.res])
                for (b0, nn, kind, dst, _) in subs[li]:
                    if kind == "fm":
                        for cc in range(nn // 128):
                            st = k.nxt("sfm", sfm)
                            for qt in range(8):
                                bk = k.nxt("bank", banks)
                                for kc in range(8):
                                    sc.op(pe, lambda: T.matmul(bk.t[:, :], lhsT=w.t[:, kc, b0 + cc * 128:b0 + (cc + 1) * 128],
                                                               rhs=xT.t[:, kc, qt * 512:(qt + 1) * 512],
                                                               start=(kc == 0), stop=(kc == 7)),
                                          reads=[w.res, rx[kc]], writes=[bk.res])
                                evac_copy(st.t[:, qt * 512:(qt + 1) * 512], bk.t[:, :], [bk.res], [st.res])
                            sc.dma(sp, dst[cc * 128:(cc + 1) * 128, :], st.t[:, :], reads=[st.res])
                    elif kind == "tm":
                        for t4 in range(8):
                            st = k.nxt("stm", stm)
                            for ti in range(4):
                                tt = t4 * 4 + ti
                                for ch in range((nn + 511) // 512):
                                    cw = min(512, nn - ch * 512)
                                    bk = k.nxt("bank", banks)
                                    for kc in range(8):
                                        sc.op(pe, lambda: T.matmul(bk.t[:, 0:cw], lhsT=xT.t[:, kc, tt * 128:(tt + 1) * 128],
                                                                   rhs=w.t[:, kc, b0 + ch * 512:b0 + ch * 512 + cw],
                                                                   start=(kc == 0), stop=(kc == 7)),
                                              reads=[w.res, rx[kc]], writes=[bk.res])
                                    evac_copy(st.t[:, ti, ch * 512:ch * 512 + cw], bk.t[:, 0:cw], [bk.res], [st.res])
                            sc.dma(sp, dst[t4 * 512:(t4 + 1) * 512, :].rearrange("(t p) c -> p t c", p=128),
                                   st.t[:, :, 0:nn], reads=[st.res])
                    else:
                        for tt in range(32):
                            bk = k.nxt("bank", banks)
                            for kc in range(8):
                                sc.op(pe, lambda: T.matmul(bk.t[:, 0:48], lhsT=xT.t[:, kc, tt * 128:(tt + 1) * 128],
                                                           rhs=w.t[:, kc, b0:b0 + 48], start=(kc == 0), stop=(kc == 7)),
                                      reads=[w.res, rx[kc]], writes=[bk.res])
                            sc.op(act, lambda: A.activation(out=sg.t[:, tt, :], in_=bk.t[:, 0:48], func=AF.Sigmoid),
                                  reads=[bk.res], writes=[sg.res])
                        sc.dma(sp, gates_d.rearrange("(t p) c -> p t c", p=128), sg.t[:, :, :], reads=[sg.res])
            sc.barrier()

    sbanks = banks[0:3]
    accs = banks[3:7]

    FILL = [int(os.environ.get('KFILL', '0'))]
    KDIM = int(os.environ.get('KDIM', '128'))

    def attn_pass(es_p, QT, KT, kdim, Vt, dvp, specs, evac_fn, reads, pbufs):
        items = []
        covers = {}
        for j in range(8):
            spj = specs[j]
            if not spj:
                continue
            covers[j] = {sub: [i for i, s_ in enumerate(spj) if s_[1] <= sub * 128 < s_[2]] for sub in range(4)}
            for i in range(len(spj)):
                items.append((j, i))

        def emit_S(n):
            j, i = items[n]
            kt, c0, c1, masks = specs[j][i]
            bk = k.nxt("sbank", sbanks)
            nm = len(masks)
            sc.op(pe, lambda: T.matmul(bk.t[:, c0:c1], lhsT=KT[0:kdim, kt * 128:(kt + 1) * 128],
                                       rhs=QT[0:kdim, j * 512 + c0:j * 512 + c1], start=True, stop=(nm == 0)),
                  reads=reads, writes=[bk.res])
            for mi, (ml, mr, lo, hi, mreads) in enumerate(masks):
                sc.op(pe, lambda: T.matmul(bk.t[:, lo:hi], lhsT=ml, rhs=mr, start=False, stop=(mi == nm - 1)),
                      reads=mreads, writes=[bk.res])
            return bk

        LA = 2
        pend = {}
        for n0 in range(min(LA, len(items))):
            pend[n0] = emit_S(n0)
        for n in range(len(items)):
            if n + LA < len(items):
                pend[n + LA] = emit_S(n + LA)
            j, i = items[n]
            kt, c0, c1, masks = specs[j][i]
            cover = covers[j]
            bk = pend.pop(n)
            pb = k.nxt("pbuf", pbufs)
            if FILL[0] > 0:
                sc.op(pe, lambda: T.matmul(tbank.t[:, :].bitcast(F32)[:, 0:FILL[0]], lhsT=ident.t[:, :], rhs=wz.t[:, 0:FILL[0]],
                                           start=True, stop=True), reads=[], writes=[])
            sc.op(act, lambda: A.activation(out=pb.t[:, c0:c1], in_=bk.t[:, c0:c1], func=AF.Exp, scale=SCALE),
                  reads=[bk.res], writes=[pb.res])
            for sub in range(c0 // 128, c1 // 128):
                ac = accs[sub]
                first = cover[sub][0] == i
                last = cover[sub][-1] == i
                sc.op(pe, lambda: T.matmul(ac.t[:, 0:dvp], lhsT=pb.t[:, sub * 128:(sub + 1) * 128],
                                           rhs=Vt[:, kt, 0:dvp], start=first, stop=last),
                      reads=[pb.res] + list(reads), writes=[ac.res])
                if last:
                    evac_fn(j * 4 + sub, ac)

    def pe_warmup(n=24):
        for i in range(n):
            sc.op(pe, lambda: T.matmul(tbank.t[:, :].bitcast(F32), lhsT=ident.t[:, :], rhs=wz.t[:, :], start=True, stop=True),
                  reads=[ident.res, wz.res], writes=[tbank.res] if i == 0 else [])

    def causal_specs(extra=None):
        specs = []
        for j in range(8):
            l = []
            for kt in range(4 * j + 4):
                d = kt - 4 * j
                c0 = 128 * d if d > 0 else 0
                masks = []
                if extra is not None:
                    masks += extra(j, kt, c0, 512)
                if d >= 0:
                    masks.append((ident.t[:, :], mcaus.t[:, :], 128 * d, 128 * d + 128, [ident.res, mcaus.res]))
                l.append((kt, c0, 512, masks))
            specs.append(l)
        return specs

    def bcast_load(es, name, src_ap, n, eng=None):
        b = k.sb(es, name, [128, n], F32)
        sc.dma(sp, b.t[:, :], src_ap.partition_broadcast(128), writes=[b.res])
        return b

    def phase2():
        with contextlib.ExitStack() as es:
            pbufs = [k.sb(es, f"pb{i}", [128, 512], BF16) for i in range(4)]
            Qb = [k.sb(es, f"dQ{i}", [128, S], BF16) for i in range(2)]
            Kb = [k.sb(es, f"dK{i}", [128, S], BF16) for i in range(2)]
            Vb = [k.sb(es, f"dV{i}", [128, 32, 129], BF16) for i in range(2)]
            A0 = k.sb(es, "dA0", [128, 32, 128], F32)
            A1 = k.sb(es, "dA1", [128, 32, 128], F32)
            Asq = k.sb(es, "dAsq", [128, 32, 128], F32)
            rst = k.sb(es, "drst", [128, 32, 2], F32)
            ost = [k.sb(es, f"dost{i}", [128, 32, 128], BF16) for i in range(2)]
            small = [k.sb(es, f"dsm{i}", [128, 4], F32) for i in range(8)]
            at = [k.sb(es, f"dat{i}", [128, 128], F32) for i in range(3)]
            junk = k.sb(es, "djunk", [128, 128], F32)
            l4 = [bcast_load(es, f"dl{i}", I(n)[:, :], 64) for i, n in
                  enumerate(("diff_lq1", "diff_lk1", "diff_lq2", "diff_lk2"))]
            gsub = bcast_load(es, "dgsub", I("diff_subln_g")[:, :], 128)
            lam = k.sb(es, "dlam", [128, 8], F32)
            sc.op(dve, lambda: V.tensor_tensor(out=l4[0].t[:, :], in0=l4[0].t[:, :], in1=l4[1].t[:, :], op=ALU.mult),
                  reads=[l4[1].res], writes=[l4[0].res])
            sc.op(dve, lambda: V.tensor_tensor(out=l4[2].t[:, :], in0=l4[2].t[:, :], in1=l4[3].t[:, :], op=ALU.mult),
                  reads=[l4[3].res], writes=[l4[2].res])
            sc.op(dve, lambda: V.reduce_sum(out=lam.t[:, 0:1], in_=l4[0].t[:, :], axis=AX.X), reads=[l4[0].res], writes=[lam.res])
            sc.op(dve, lambda: V.reduce_sum(out=lam.t[:, 1:2], in_=l4[2].t[:, :], axis=AX.X), reads=[l4[2].res], writes=[lam.res])
            sc.op(act, lambda: A.activation(out=lam.t[:, 2:4], in_=lam.t[:, 0:2], func=AF.Exp), reads=[lam.res], writes=[lam.res])
            sc.op(dve, lambda: V.scalar_tensor_tensor(out=lam.t[:, 4:5], in0=lam.t[:, 3:4], scalar=-LAMBDA_INIT,
                                                      in1=lam.t[:, 2:3], op0=ALU.add, op1=ALU.subtract),
                  reads=[lam.res], writes=[lam.res])
            sc.op(dve, lambda: V.tensor_scalar(out=gsub.t[:, :], in0=gsub.t[:, :], scalar1=1.0 - LAMBDA_INIT, scalar2=None,
                                               op0=ALU.mult), reads=[gsub.res], writes=[gsub.res])
            for b in Vb:
                sc.op(pool, lambda: G.memset(b.t[:, :, 128:129], 1.0), writes=[b.res])
            for b in Qb + Kb:
                sc.op(pool, lambda: G.memset(b.t[64:128, :], 0.0), writes=[b.res])
            for b in Kb:
                sc.dma(sp, b.t[64:70, :], I("augk_tok")[:, :], writes=[b.res])
            specs = causal_specs()
            for h in range(8):
                if h == 0:
                    pe_warmup()
                vb = Vb[h % 2]
                sc.dma(sp, vb.t[:, :, 0:128], Vd[:, h * 128:(h + 1) * 128].rearrange("(t p) c -> p t c", p=128),
                       writes=[vb.res])
                osb = ost[h % 2]
                for c in range(2):
                    qb, kb_ = Qb[c], Kb[c]
                    r0 = h * 128 + c * 64
                    sc.dma(sp, qb.t[0:64, :], QdT[r0:r0 + 64, :], writes=[qb.res])
                    sc.dma(sp, qb.t[64:70, :], I("augq")[h, :, :], writes=[qb.res])
                    sc.dma(sp, kb_.t[0:64, :], KdT[r0:r0 + 64, :], writes=[kb_.res])

                    def ev(qt, ac, c=c, osb=osb):
                        sm = k.nxt("dsm", small)
                        sc.op(dve, lambda: V.reciprocal(out=sm.t[:, 0:1], in_=ac.t[:, 128:129]), reads=[ac.res], writes=[sm.res])
                        if c == 0:
                            sc.op(dve, lambda: V.tensor_scalar(out=A0.t[:, qt, :], in0=ac.t[:, 0:128], scalar1=sm.t[:, 0:1],
                                                               scalar2=None, op0=ALU.mult),
                                  reads=[ac.res, sm.res], writes=[A0.res])
                            return
                        sc.op(dve, lambda: V.tensor_tensor(out=sm.t[:, 1:2], in0=sm.t[:, 0:1], in1=lam.t[:, 4:5], op=ALU.mult),
                              reads=[lam.res, sm.res], writes=[sm.res])
                        sc.op(dve, lambda: V.scalar_tensor_tensor(out=A1.t[:, qt, :], in0=ac.t[:, 0:128], scalar=sm.t[:, 1:2],
                                                                  in1=A0.t[:, qt, :], op0=ALU.mult, op1=ALU.add),
                              reads=[ac.res, sm.res, A0.res], writes=[A1.res])

                    attn_pass(es, qb.t, kb_.t, KDIM, vb.t, 129, specs, ev, [qb.res, kb_.res, vb.res], pbufs)
                sc.op(dve, lambda: V.tensor_tensor(out=Asq.t[:, :, :], in0=A1.t[:, :, :], in1=A1.t[:, :, :], op=ALU.mult),
                      reads=[A1.res], writes=[Asq.res])
                sc.op(dve, lambda: V.tensor_reduce(out=rst.t[:, :, 0:1], in_=Asq.t[:, :, :], axis=AX.X, op=ALU.add),
                      reads=[Asq.res], writes=[rst.res])
                sc.op(act, lambda: A.activation(out=rst.t[:, :, 1:2], in_=rst.t[:, :, 0:1], func=AF.Ln, bias=epsb.t[:, 0:1],
                                                scale=1.0 / 128.0), reads=[rst.res, epsb.res], writes=[rst.res])
                sc.op(act, lambda: A.activation(out=rst.t[:, :, 1:2], in_=rst.t[:, :, 1:2], func=AF.Exp, scale=-0.5),
                      reads=[], writes=[rst.res])
                sc.op(dve, lambda: V.tensor_tensor(out=Asq.t[:, :, :], in0=A1.t[:, :, :],
                                                   in1=rst.t[:, :, 1:2].to_broadcast([128, 32, 128]), op=ALU.mult),
                      reads=[A1.res, rst.res], writes=[Asq.res])
                sc.op(pool, lambda: G.tensor_tensor(out=osb.t[:, :, :], in0=Asq.t[:, :, :],
                                                    in1=gsub.t[:, :].unsqueeze(1).to_broadcast([128, 32, 128]), op=ALU.mult),
                      reads=[Asq.res, gsub.res], writes=[osb.res])
                sc.dma(pool, odiff_d[:, h * 128:(h + 1) * 128].rearrange("(t p) c -> p t c", p=128), osb.t[:, :, :],
                       reads=[osb.res])
            sc.barrier()

    k.phase2 = phase2
    def phase3():
        with contextlib.ExitStack() as es:
            pbufs = [k.sb(es, f"npb{i}", [128, 512], BF16) for i in range(4)]
            gates = k.sb(es, "ngates", [128, 32, 48], F32)
            sc.dma(sp, gates.t[:, :, :], gates_d.rearrange("(t p) c -> p t c", p=128), writes=[gates.res])
            Kc = k.sb(es, "nKc", [128, 4, 256], BF16)
            Vc = k.sb(es, "nVc", [128, 4, 2, 129], BF16)
            sc.op(pool, lambda: G.memset(Kc.t[:, :, :], 0.0), writes=[Kc.res])
            sc.op(pool, lambda: G.memset(Vc.t[:, :, :, :], 0.0), writes=[Vc.res])
            sc.op(pool, lambda: G.memset(Vc.t[:, :, :, 128:129], 1.0), writes=[Vc.res])
            for g in range(4):
                sc.dma(sp, Kc.t[64:70, g, :], I("augk_cmp")[:, :], writes=[Kc.res])
                sc.dma(sp, Vc.t[:, g, :, 64:128], I("c2s")[:, :, :], writes=[Vc.res])
            with contextlib.ExitStack() as es2:
                w1 = [k.sb(es2, f"cw1{i}", [128, 16, 256], BF16) for i in range(2)]
                w2 = [k.sb(es2, f"cw2{i}", [128, 2, 64], BF16) for i in range(2)]
                posT = [k.sb(es2, f"cpos{i}", [128, 16], BF16) for i in range(2)]
                biasv = k.sb(es2, "cbias", [128, 4], F32)
                srcb = [k.sb(es2, f"csrc{i}", [128, S], BF16) for i in range(2)]
                for b_ in srcb:
                    sc.op(pool, lambda: G.memset(b_.t[64:128, S - 16:S], 0.0), writes=[b_.res])
                hid = [k.sb(es2, f"chid{i}", [128, 2, 256], BF16) for i in range(2)]
                for kv, (n1, n2, npos) in enumerate((("nsa_phi_k1s", "nsa_phi_k2", "nsa_pos_kTs"),
                                                     ("nsa_phi_v1s", "nsa_phi_v2", "nsa_pos_vTs"))):
                    sc.dma(pool, w1[kv].t[:, :, :], I(n1)[:, :, :], writes=[w1[kv].res])
                    sc.dma(pool, w2[kv].t[:, :, :], I(n2).rearrange("(c p) d -> p c d", p=128), writes=[w2[kv].res])
                    sc.dma(pool, posT[kv].t[:, :], I(npos)[:, :], writes=[posT[kv].res])
                for kv in range(2):
                    for hc in range(2):
                        bk = k.nxt("bank", banks)
                        for l in range(16):
                            sc.op(pe, lambda: T.matmul(bk.t[:, 0:1], lhsT=w1[kv].t[:, l, hc * 128:(hc + 1) * 128],
                                                       rhs=posT[kv].t[:, l:l + 1], start=(l == 0), stop=(l == 15)),
                                  reads=[w1[kv].res, posT[kv].res], writes=[bk.res])
                        sc.op(dve, lambda: V.tensor_copy(out=biasv.t[:, kv * 2 + hc:kv * 2 + hc + 1], in_=bk.t[:, 0:1]),
                              reads=[bk.res], writes=[biasv.res])
                for g in range(4):
                    for kv in range(2):
                        sb_ = k.nxt("csrc", srcb)
                        srcd = (kcT if kv == 0 else vcT)
                        sc.dma(sp, sb_.t[0:64, :], srcd[g * 64:(g + 1) * 64, :], writes=[sb_.res])
                        sc.dma(sp, sb_.t[64:128, 0:S - 1], srcd[g * 64:(g + 1) * 64, 1:S], writes=[sb_.res])
                        sv = sb_.t[:, :].rearrange("p (n s) -> p s n", s=16)
                        hb = hid[kv]
                        for hc in range(2):
                            bk = k.nxt("bank", banks)
                            for l2 in range(16):
                                l = 2 * l2
                                sc.op(pe, lambda: T.matmul(bk.t[:, 0:255], lhsT=w1[kv].t[:, l2, hc * 128:(hc + 1) * 128],
                                                           rhs=sv[:, l % 16, (l // 16):(l // 16) + 255],
                                                           start=(l2 == 0), stop=(l2 == 15)),
                                      reads=[w1[kv].res, sb_.res], writes=[bk.res])
                            sc.op(act, lambda: A.activation(out=hb.t[:, hc, 0:255], in_=bk.t[:, 0:255], func=AF.Silu,
                                                            bias=biasv.t[:, kv * 2 + hc:kv * 2 + hc + 1]),
                                  reads=[bk.res, biasv.res], writes=[hb.res])
                        if kv == 0:
                            bk = k.nxt("bank", banks)
                            for hc in range(2):
                                sc.op(pe, lambda: T.matmul(bk.t[0:64, 0:255], lhsT=w2[0].t[:, hc, :], rhs=hb.t[:, hc, 0:255],
                                                           start=(hc == 0), stop=(hc == 1)),
                                      reads=[w2[0].res, hb.res], writes=[bk.res])
                            sc.op(dve, lambda: V.tensor_copy(out=Kc.t[0:64, g, 0:255], in_=bk.t[0:64, 0:255]),
                                  reads=[bk.res], writes=[Kc.res])
                        else:
                            for nt in range(2):
                                m = 128 if nt == 0 else 127
                                bk = k.nxt("bank", banks)
                                for hc in range(2):
                                    sc.op(pe, lambda: T.matmul(bk.t[0:m, 0:64], lhsT=hb.t[:, hc, nt * 128:nt * 128 + m],
                                                               rhs=w2[1].t[:, hc, :], start=(hc == 0), stop=(hc == 1)),
                                          reads=[w2[1].res, hb.res], writes=[bk.res])
                                sc.op(dve, lambda: V.tensor_copy(out=Vc.t[0:m, g, nt, 0:64], in_=bk.t[0:m, 0:64]),
                                      reads=[bk.res], writes=[Vc.res])
                sc.barrier()
            mcmp = k.sb(es, "nmcmp", [128, 2, S], BF16)
            keep = k.sb(es, "nkeep", [128, 32, 64], F32)
            force = k.sb(es, "nforce", [128, 32, 64], F32)
            expand = k.sb(es, "nexpand", [128, S], BF16)
            sc.dma(sp, mcmp.t[:, :, :], I("m_cmp")[:, :, :], writes=[mcmp.res])
            sc.dma(sp, keep.t[:, :, :], I("sel_keep")[:, :, :], writes=[keep.res])
            sc.dma(sp, force.t[:, :, :], I("sel_force")[:, :, :], writes=[force.res])
            sc.op(pool, lambda: G.memset(expand.t[64:128, :], 0.0), writes=[expand.res])
            sc.dma(sp, expand.t[0:64, :], I("expand")[:, :], writes=[expand.res])
            Qb = [k.sb(es, f"nQ{i}", [128, S], BF16) for i in range(2)]
            ksb = k.sb(es, "nks", [128, S], BF16)
            kwb = k.sb(es, "nkw", [128, S], BF16)
            vsb = k.sb(es, "nvs", [128, 32, 65], BF16)
            vwb = k.sb(es, "nvw", [128, 32, 65], BF16)
            for b in (ksb, kwb) + tuple(Qb):
                sc.op(pool, lambda: G.memset(b.t[64:128, :], 0.0), writes=[b.res])
            for b in (ksb, kwb):
                sc.dma(sp, b.t[64:70, :], I("augk_tok")[:, :], writes=[b.res])
            for b in (vsb, vwb):
                sc.op(pool, lambda: G.memset(b.t[:, :, 64:65], 1.0), writes=[b.res])
            selmT = k.sb(es, "nselmT", [128, S], BF16)
            sc.op(pool, lambda: G.memset(selmT.t[64:128, :], 0.0), writes=[selmT.res])
            imp = k.sb(es, "nimp", [128, 32, 64], F32)
            O = k.sb(es, "nO", [128, 32, 256], F32)
            cst2 = [k.sb(es, f"ncst{i}", [128, 32, 129], F32) for i in range(2)]
            cst_res2 = [[Res() for _ in range(32)] for _ in range(2)]
            cdn2 = [k.sb(es, f"ncdn{i}", [128, 32, 4], F32) for i in range(2)]
            ost = k.sb(es, "nost", [128, 32, 256], BF16)
            small = [k.sb(es, f"nsm{i}", [128, 4], F32) for i in range(8)]
            imb = [k.sb(es, f"nim{i}", [128, 64], F32) for i in range(4)]
            im2b = [k.sb(es, f"nim2{i}", [128, 64], F32) for i in range(4)]
            m8 = [k.sb(es, f"nm8{i}", [128, 16], F32) for i in range(4)]
            selb = [k.sb(es, f"nsel{i}", [128, 64], BF16) for i in range(8)]

            cmp_specs = []
            for j in range(8):
                l = [(0, 0, 512, [(ident.t[:, :], mcmp.t[:, 0, j * 512:(j + 1) * 512], 0, 512, [ident.res, mcmp.res])])]
                if j >= 4:
                    l.append((1, 0, 512, [(ident.t[:, :], mcmp.t[:, 1, j * 512:(j + 1) * 512], 0, 512, [ident.res, mcmp.res])]))
                cmp_specs.append(l)
            slc_specs = causal_specs(extra=lambda j, kt, c0, c1: [
                (expand.t[0:128, kt * 128:(kt + 1) * 128], selmT.t[0:128, j * 512 + c0:j * 512 + c1], c0, c1,
                 [expand.res, selmT.res])])
            win_specs = []
            for j in range(8):
                l = []
                for dp in range(4):
                    kt = 4 * j - 4 + dp
                    if kt < 0:
                        continue
                    l.append((kt, 0, 128 * (dp + 1),
                              [(ident.t[:, :], mband.t[:, :], 128 * dp, 128 * dp + 128, [ident.res, mband.res])]))
                for d in range(4):
                    l.append((4 * j + d, 128 * d, 512,
                              [(ident.t[:, :], mcaus.t[:, :], 128 * d, 128 * d + 128, [ident.res, mcaus.res])]))
                win_specs.append(l)

            def load_q(h):
                qb = k.nxt("nQ", Qb)
                sc.dma(sp, qb.t[0:64, :], NqT[h * 64:(h + 1) * 64, :], writes=[qb.res])
                sc.dma(sp, qb.t[64:70, :], I("augq")[8 + h, :, :], writes=[qb.res])
                return qb

            for g in range(4):
                sc.dma(sp, kwb.t[0:64, :], kwT[g * 64:(g + 1) * 64, :], writes=[kwb.res])
                sc.dma(sp, vwb.t[:, :, 0:64], vw_d[:, g * 64:(g + 1) * 64].rearrange("(t p) c -> p t c", p=128), writes=[vwb.res])
                for hh in range(4):
                    h = 4 * g + hh
                    qb = load_q(h)

                    cst, cst_res, cdn = cst2[hh % 2], cst_res2[hh % 2], cdn2[hh % 2]

                    def ev_cmp(qt, ac, h=h, hh=hh, cst=cst, cst_res=cst_res):
                        sc.op(dve, lambda: V.tensor_copy(out=cst.t[:, qt, :], in_=ac.t[:, 0:129]), reads=[ac.res], writes=[cst_res[qt]])

                    attn_pass(es, qb.t, Kc.t[:, g, :], KDIM, Vc.t[:, g, :, :], 129, cmp_specs, ev_cmp,
                              [qb.res, Kc.res, Vc.res], pbufs)
                    sc.op(dve, lambda: V.tensor_scalar(out=cdn.t[:, :, 0:1], in0=cst.t[:, :, 128:129], scalar1=1e-30, scalar2=None,
                                                       op0=ALU.max), reads=cst_res, writes=[cdn.res])
                    sc.op(dve, lambda: V.reciprocal(out=cdn.t[:, :, 1:2], in_=cdn.t[:, :, 0:1]), reads=[], writes=[cdn.res])
                    sc.op(dve, lambda: V.tensor_tensor(out=cdn.t[:, :, 2:3], in0=cdn.t[:, :, 1:2], in1=gates.t[:, :, 3 * h:3 * h + 1],
                                                       op=ALU.mult), reads=[gates.res], writes=[cdn.res])
                    sc.op(dve, lambda: V.tensor_tensor(out=O.t[:, :, hh * 64:(hh + 1) * 64], in0=cst.t[:, :, 0:64],
                                                       in1=cdn.t[:, :, 2:3].to_broadcast([128, 32, 64]), op=ALU.mult),
                          reads=[cdn.res] + cst_res, writes=[O.res])
                    if hh == 0:
                        sc.op(dve, lambda: V.tensor_tensor(out=imp.t[:, :, :], in0=cst.t[:, :, 64:128],
                                                           in1=cdn.t[:, :, 1:2].to_broadcast([128, 32, 64]), op=ALU.mult),
                              reads=[cdn.res] + cst_res, writes=[imp.res])
                    else:
                        sc.op(dve, lambda: V.tensor_tensor(out=cst.t[:, :, 64:128], in0=cst.t[:, :, 64:128],
                                                           in1=cdn.t[:, :, 1:2].to_broadcast([128, 32, 64]), op=ALU.mult),
                              reads=[cdn.res], writes=cst_res)
                        sc.op(dve, lambda: V.tensor_tensor(out=imp.t[:, :, :], in0=imp.t[:, :, :], in1=cst.t[:, :, 64:128], op=ALU.add),
                              reads=cst_res, writes=[imp.res])
                def sel_vec(q8):
                    sls = []
                    for i8 in range(8):
                        qt = q8 * 8 + i8
                        im = k.nxt("nim", imb)
                        im2 = k.nxt("nim2", im2b)
                        mm = k.nxt("nm8", m8)
                        sl = k.nxt("nsel", selb)
                        sls.append(sl)
                        sc.op(pool, lambda: G.tensor_tensor(out=im.t[:, :], in0=imp.t[:, qt, :], in1=keep.t[:, qt, :], op=ALU.mult),
                              reads=[imp.res, keep.res], writes=[im.res])
                        sc.op(pool, lambda: G.tensor_tensor(out=im.t[:, :], in0=im.t[:, :], in1=force.t[:, qt, :], op=ALU.add),
                              reads=[force.res], writes=[im.res])
                        sc.op(dve, lambda: V.max(out=mm.t[:, 0:8], in_=im.t[:, :]), reads=[im.res], writes=[mm.res])
                        sc.op(dve, lambda: V.match_replace(out=im2.t[:, :], in_to_replace=mm.t[:, 0:8], in_values=im.t[:, :],
                                                           imm_value=-3.0e38), reads=[im.res, mm.res], writes=[im2.res])
                        sc.op(dve, lambda: V.max(out=mm.t[:, 8:16], in_=im2.t[:, :]), reads=[im2.res], writes=[mm.res])
                        sc.op(dve, lambda: V.tensor_scalar(out=sl.t[:, :], in0=im.t[:, :], scalar1=mm.t[:, 15:16], scalar2=NEGB,
                                                           op0=ALU.is_lt, op1=ALU.mult), reads=[im.res, mm.res], writes=[sl.res])
                    return sls

                def sel_pe(q8, sls):
                    for i8, sl in enumerate(sls):
                        sc.op(pe, lambda: T.transpose(out=tbank.t[0:64, i8 * 128:(i8 + 1) * 128], in_=sl.t[:, :],
                                                      identity=ident.t[:, :]), reads=[sl.res, ident.res], writes=[tbank.res])
                    sc.op(dve, lambda: V.tensor_copy(out=selmT.t[0:64, q8 * 1024:(q8 + 1) * 1024], in_=tbank.t[0:64, :]),
                          reads=[tbank.res], writes=[selmT.res])

                for hh in range(4):
                    h = 4 * g + hh
                    qb = load_q(h)
                    sls = sel_vec(hh)

                    def ev_win(qt, ac, h=h, hh=hh):
                        sm = k.nxt("nsm", small)
                        sc.op(dve, lambda: V.reciprocal(out=sm.t[:, 1:2], in_=ac.t[:, 64:65]), reads=[ac.res], writes=[sm.res])
                        sc.op(dve, lambda: V.tensor_tensor(out=sm.t[:, 2:3], in0=sm.t[:, 1:2],
                                                           in1=gates.t[:, qt, 3 * h + 2:3 * h + 3], op=ALU.mult),
                              reads=[sm.res, gates.res], writes=[sm.res])
                        sc.op(dve, lambda: V.scalar_tensor_tensor(out=O.t[:, qt, hh * 64:(hh + 1) * 64], in0=ac.t[:, 0:64],
                                                                  scalar=sm.t[:, 2:3], in1=O.t[:, qt, hh * 64:(hh + 1) * 64],
                                                                  op0=ALU.mult, op1=ALU.add),
                              reads=[ac.res, sm.res], writes=[O.res])

                    attn_pass(es, qb.t, kwb.t, KDIM, vwb.t, 65, win_specs, ev_win, [qb.res, kwb.res, vwb.res], pbufs)
                    sel_pe(hh, sls)
                sc.dma(sp, ksb.t[0:64, :], ksT[g * 64:(g + 1) * 64, :], writes=[ksb.res])
                sc.dma(sp, vsb.t[:, :, 0:64], vs_d[:, g * 64:(g + 1) * 64].rearrange("(t p) c -> p t c", p=128), writes=[vsb.res])
                for hh in range(4):
                    h = 4 * g + hh
                    qb = load_q(h)

                    def ev_slc(qt, ac, h=h, hh=hh):
                        sm = k.nxt("nsm", small)
                        sc.op(dve, lambda: V.reciprocal(out=sm.t[:, 1:2], in_=ac.t[:, 64:65]), reads=[ac.res], writes=[sm.res])
                        sc.op(dve, lambda: V.tensor_tensor(out=sm.t[:, 2:3], in0=sm.t[:, 1:2],
                                                           in1=gates.t[:, qt, 3 * h + 1:3 * h + 2], op=ALU.mult),
                              reads=[sm.res, gates.res], writes=[sm.res])
                        sc.op(dve, lambda: V.scalar_tensor_tensor(out=ost.t[:, qt, hh * 64:(hh + 1) * 64], in0=ac.t[:, 0:64],
                                                                  scalar=sm.t[:, 2:3], in1=O.t[:, qt, hh * 64:(hh + 1) * 64],
                                                                  op0=ALU.mult, op1=ALU.add),
                              reads=[ac.res, sm.res, O.res], writes=[ost.res])

                    attn_pass(es, qb.t, ksb.t, KDIM, vsb.t, 65, slc_specs, ev_slc, [qb.res, ksb.res, vsb.res], pbufs)
                sc.dma(pool, onsa_d[:, g * 256:(g + 1) * 256].rearrange("(t p) c -> p t c", p=128), ost.t[:, :, :],
                       reads=[ost.res])
            sc.barrier()

    k.phase3 = phase3
    def layer_norm_rows(src, dst_tmp, gb, bb, out_ap, smalls, writes_res, out_reads=()):
        st = k.nxt("lnst", smalls)
        sc.op(dve, lambda: V.bn_stats(out=st.t[:, 0:6], in_=src.t[:, 0:512]), reads=[src.res], writes=[st.res])
        sc.op(dve, lambda: V.bn_stats(out=st.t[:, 6:12], in_=src.t[:, 512:1024]), reads=[src.res], writes=[st.res])
        sc.op(dve, lambda: V.bn_aggr(out=st.t[:, 12:14], in_=st.t[:, 0:12]), reads=[st.res], writes=[st.res])
        sc.op(act, lambda: A.activation(out=st.t[:, 14:15], in_=st.t[:, 13:14], func=AF.Ln, bias=epsb.t[:, 0:1], scale=1.0),
              reads=[st.res, epsb.res], writes=[st.res])
        sc.op(act, lambda: A.activation(out=st.t[:, 14:15], in_=st.t[:, 14:15], func=AF.Exp, scale=-0.5),
              reads=[st.res], writes=[st.res])
        sc.op(dve, lambda: V.tensor_scalar(out=dst_tmp.t[:, :], in0=src.t[:, :], scalar1=st.t[:, 12:13], scalar2=st.t[:, 14:15],
                                           op0=ALU.subtract, op1=ALU.mult), reads=[src.res, st.res], writes=[dst_tmp.res])
        sc.op(dve, lambda: V.tensor_tensor(out=dst_tmp.t[:, :], in0=dst_tmp.t[:, :], in1=gb.t[:, :], op=ALU.mult),
              reads=[gb.res], writes=[dst_tmp.res])
        sc.op(dve, lambda: V.tensor_tensor(out=out_ap, in0=dst_tmp.t[:, :], in1=bb.t[:, :], op=ALU.add),
              reads=[dst_tmp.res, bb.res] + list(out_reads), writes=writes_res)

    def phase4():
        with contextlib.ExitStack() as es:
            def wload(name, src_ap, kc, n):
                b = k.sb(es, name, [128, kc, n], BF16)
                sc.dma(pool, b.t[:, :, :], src_ap.rearrange("(kc p) n -> p kc n", p=128), writes=[b.res])
                return b
            Wmg = wload("Wmg", I("w_mgate"), 8, 2048)
            Wd = wload("Wd", I("w_br_diff"), 8, 1024)
            Wn = wload("Wn", I("w_br_nsa"), 8, 1024)
            Wo = wload("Wo", I("w_o"), 8, 1024)
            Wpg = wload("Wpg", I("w_ple_gate"), 8, 1024)
            Wpp = wload("Wpp", I("w_ple_proj"), 2, 1024)
            Wr = wload("Wr", I("w_router"), 8, 32)
            bmg = k.sb(es, "bmg", [1, 2048], BF16); sc.dma(pool, bmg.t[:, :], I("b_mgate")[:, :], writes=[bmg.res])
            bpg = k.sb(es, "bpg", [1, 1024], BF16); sc.dma(pool, bpg.t[:, :], I("b_ple_gate")[:, :], writes=[bpg.res])
            brt = k.sb(es, "brt", [1, 32], BF16); sc.dma(pool, brt.t[:, :], I("b_router")[:, :], writes=[brt.res])
            b2a = k.sb(es, "b2a", [32, 1024], BF16); sc.dma(pool, b2a.t[:, :], I("b_e2")[:, :], writes=[b2a.res])
            ones = k.sb(es, "ones", [1, 128], BF16); sc.op(pool, lambda: G.memset(ones.t[:, :], 1.0), writes=[ones.res])
            g1b = bcast_load(es, "g1b", I("ln1_g")[:, :], 1024)
            b1b = bcast_load(es, "b1b", I("ln1_b")[:, :], 1024)

            def rot(name, shape, dt, n=2):
                return [k.sb(es, f"{name}{i}", shape, dt) for i in range(n)]
            od_b = rot("od", [128, 1024], BF16); on_b = rot("on", [128, 1024], BF16)
            odT_b = rot("odT", [128, 1024], BF16, 1); onT_b = rot("onT", [128, 1024], BF16, 1)
            xt_b = rot("xt", [128, 1024], F32); xTt_b = rot("xTt", [128, 8, 128], BF16)
            pTt_b = rot("pTt", [128, 2, 128], BF16)
            sgd_b = rot("sgd", [128, 1024], F32, 1); sgn_b = rot("sgn", [128, 1024], F32, 1)
            mixbf_b = rot("mixbf", [128, 1024], BF16); mixT_b = rot("mixT", [128, 1024], BF16, 1)
            r_b = rot("rr", [128, 1024], F32, 1); h_b = rot("hh", [128, 1024], F32)
            hbf_b = rot("hbf", [128, 1024], BF16); hT_b = rot("hTt", [128, 1024], BF16)
            spg_b = rot("spg", [128, 1024], F32, 1); base_b = rot("base", [128, 1024], F32)
            lnst = rot("lnst", [128, 16], F32, 4)
            lg_b = rot("lg", [128, 32], F32); e_b = rot("eb", [128, 32], F32); msk_b = rot("msk", [128, 32], F32)
            m8_b = rot("rm8", [128, 8], F32); rs_b = rot("rs", [128, 2], F32)
            gt_b = rot("gt", [128, 32], F32); gtbf_b = rot("gtbf", [128, 32], BF16); gT_b = rot("gT", [32, 128], BF16)
            st = {}
            triu = k.sb(es, "triu", [128, 128], BF16); sc.dma(sp, triu.t[:, :], I("triu")[:, :], writes=[triu.res])
            ones128 = k.sb(es, "ones128", [128, 128], BF16); sc.dma(sp, ones128.t[:, :], I("ones128")[:, :], writes=[ones128.res])
            iota32 = k.sb(es, "iota32", [128, 32], F32); sc.dma(sp, iota32.t[:, :], I("iota32")[:, :], writes=[iota32.res])
            trp = k.sb(es, "trp", [128, 1], F32); sc.dma(sp, trp.t[:, :], I("trp")[:, :], writes=[trp.res])
            cumrep = k.sb(es, "cumrep", [128, 32], F32); sc.op(pool, lambda: G.memset(cumrep.t[:, :], 0.0), writes=[cumrep.res])
            idxu_b = rot("idxu", [128, 8], U32); idxf_b = rot("idxf", [128, 4], F32); mkbf_b = rot("mkbf", [128, 32], BF16)
            rank_b = rot("rank", [128, 32], F32); oh_b = rot("oh", [128, 32], F32); rk_b = rot("rk", [128, 4], F32)
            val_b = rot("val", [128, 4], F32); t1_b = rot("t1", [128, 4], F32); e4_b = rot("e4", [128, 4], F32)

            def transposes(src, dst, eng):
                for i in range(8):
                    sc.op(pe, lambda: T.transpose(out=tbank.t[:, i * 128:(i + 1) * 128], in_=src.t[:, i * 128:(i + 1) * 128],
                                                  identity=ident.t[:, :]), reads=[src.res, ident.res], writes=[tbank.res])
                if eng is act:
                    sc.op(act, lambda: A.copy(out=dst.t[:, :], in_=tbank.t[:, :]), reads=[tbank.res], writes=[dst.res])
                else:
                    sc.op(dve, lambda: V.tensor_copy(out=dst.t[:, :], in_=tbank.t[:, :]), reads=[tbank.res], writes=[dst.res])

            def mm_tok(lhs_fn, nk, W, c0, bias=None, lhs_reads=(), n=512):
                bk = k.nxt("bank", banks)
                for kc in range(nk):
                    sc.op(pe, lambda: T.matmul(bk.t[:, 0:n], lhsT=lhs_fn(kc), rhs=W.t[:, kc, c0:c0 + n],
                                               start=(kc == 0), stop=(kc == nk - 1 and bias is None)),
                          reads=[W.res] + list(lhs_reads), writes=[bk.res])
                if bias is not None:
                    sc.op(pe, lambda: T.matmul(bk.t[:, 0:n], lhsT=ones.t[0:1, :], rhs=bias.t[0:1, c0:c0 + n],
                                               start=False, stop=True), reads=[ones.res, bias.res], writes=[bk.res])
                return bk

            def stage_a(tt):
                d = st[tt] = {}
                od = d["od"] = k.nxt("od", od_b); on = d["on"] = k.nxt("on", on_b)
                xt = d["xt"] = k.nxt("xt", xt_b); xTt = k.nxt("xTt", xTt_b); pTt = d["pTt"] = k.nxt("pTt", pTt_b)
                rows = slice(tt * 128, (tt + 1) * 128)
                sc.dma(sp, od.t[:, :], odiff_d[rows, :], writes=[od.res])
                sc.dma(sp, on.t[:, :], onsa_d[rows, :], writes=[on.res])
                sc.dma(sp, xt.t[:, :], I("x")[rows, :], writes=[xt.res])
                sc.dma(pool, xTt.t[:, :, :], I("xT")[:, rows].rearrange("(kc p) t -> p kc t", p=128), writes=[xTt.res])
                sc.dma(pool, pTt.t[:, :, :], I("pT")[:, rows].rearrange("(kc p) t -> p kc t", p=128), writes=[pTt.res])
                odT = k.nxt("odT", odT_b); onT = k.nxt("onT", onT_b)
                sgd = k.nxt("sgd", sgd_b); sgn = k.nxt("sgn", sgn_b)
                mix = sgd; t2 = sgn; mixbf = d["mixbf"] = k.nxt("mixbf", mixbf_b)
                for ch in range(4):
                    bk = mm_tok(lambda kc: xTt.t[:, kc, :], 8, Wmg, ch * 512, bias=bmg, lhs_reads=[xTt.res])
                    dst = sgd if ch < 2 else sgn
                    sc.op(act, lambda: A.activation(out=dst.t[:, (ch % 2) * 512:(ch % 2) * 512 + 512], in_=bk.t[:, :],
                                                    func=AF.Sigmoid), reads=[bk.res], writes=[dst.res])
                transposes(od, odT, act)
                transposes(on, onT, dve)
                for ch in range(2):
                    bk = mm_tok(lambda kc: odT.t[:, kc * 128:(kc + 1) * 128], 8, Wd, ch * 512, lhs_reads=[odT.res])
                    sc.op(dve, lambda: V.tensor_tensor(out=mix.t[:, ch * 512:(ch + 1) * 512], in0=sgd.t[:, ch * 512:(ch + 1) * 512],
                                                       in1=bk.t[:, :], op=ALU.mult), reads=[sgd.res, bk.res], writes=[mix.res])
                for ch in range(2):
                    bk = mm_tok(lambda kc: onT.t[:, kc * 128:(kc + 1) * 128], 8, Wn, ch * 512, lhs_reads=[onT.res])
                    sc.op(dve, lambda: V.tensor_tensor(out=t2.t[:, ch * 512:(ch + 1) * 512], in0=sgn.t[:, ch * 512:(ch + 1) * 512],
                                                       in1=bk.t[:, :], op=ALU.mult), reads=[sgn.res, bk.res], writes=[t2.res])
                sc.op(pool, lambda: G.tensor_tensor(out=mixbf.t[:, :], in0=mix.t[:, :], in1=t2.t[:, :], op=ALU.add),
                      reads=[mix.res, t2.res], writes=[mixbf.res])

            def stage_b(tt):
                d = st[tt]
                mixT = k.nxt("mixT", mixT_b); r = k.nxt("rr", r_b); h = d["h"] = k.nxt("hh", h_b)
                hbf = d["hbf"] = k.nxt("hbf", hbf_b); hT = d["hT"] = k.nxt("hTt", hT_b)
                xt = d["xt"]
                transposes(d["mixbf"], mixT, act)
                for ch in range(2):
                    bk = mm_tok(lambda kc: mixT.t[:, kc * 128:(kc + 1) * 128], 8, Wo, ch * 512, lhs_reads=[mixT.res])
                    sc.op(dve, lambda: V.scalar_tensor_tensor(out=r.t[:, ch * 512:(ch + 1) * 512], in0=xt.t[:, ch * 512:(ch + 1) * 512],
                                                              scalar=ALPHA, in1=bk.t[:, :], op0=ALU.mult, op1=ALU.add),
                          reads=[xt.res, bk.res], writes=[r.res])
                layer_norm_rows(r, r, g1b, b1b, h.t[:, :], lnst, [h.res])
                sc.op(act, lambda: A.copy(out=hbf.t[:, :], in_=h.t[:, :]), reads=[h.res], writes=[hbf.res])

            def stage_b2(tt):
                d = st[tt]
                hbf, hT = d["hbf"], d["hT"]
                transposes(hbf, hT, dve)
                sc.dma(sp, hT_d[:, tt * 128:(tt + 1) * 128].rearrange("(kc p) t -> p kc t", p=128),
                       hT.t[:, :].rearrange("p (kc t) -> p kc t", kc=8), reads=[hT.res])

            def stage_c(tt):
                d = st[tt]
                h, hT, pTt = d["h"], d["hT"], d["pTt"]
                spg = k.nxt("spg", spg_b); base = d["base"] = k.nxt("base", base_b)
                rows = slice(tt * 128, (tt + 1) * 128)
                for ch in range(2):
                    bk = mm_tok(lambda kc: hT.t[:, kc * 128:(kc + 1) * 128], 8, Wpg, ch * 512, bias=bpg, lhs_reads=[hT.res])
                    sc.op(act, lambda: A.activation(out=spg.t[:, ch * 512:(ch + 1) * 512], in_=bk.t[:, :], func=AF.Sigmoid),
                          reads=[bk.res], writes=[spg.res])
                for ch in range(2):
                    bk = mm_tok(lambda kc: pTt.t[:, kc, :], 2, Wpp, ch * 512, lhs_reads=[pTt.res])
                    sc.op(dve, lambda: V.tensor_tensor(out=spg.t[:, ch * 512:(ch + 1) * 512], in0=spg.t[:, ch * 512:(ch + 1) * 512],
                                                       in1=bk.t[:, :], op=ALU.mult), reads=[bk.res], writes=[spg.res])
                sc.op(dve, lambda: V.scalar_tensor_tensor(out=base.t[:, :], in0=h.t[:, :], scalar=ALPHA, in1=spg.t[:, :],
                                                          op0=ALU.mult, op1=ALU.add), reads=[h.res, spg.res], writes=[base.res])
                bk = mm_tok(lambda kc: hT.t[:, kc * 128:(kc + 1) * 128], 8, Wr, 0, bias=brt, lhs_reads=[hT.res], n=32)
                lg = k.nxt("lg", lg_b); e_ = k.nxt("eb", e_b); mk = k.nxt("msk", msk_b); m8_ = k.nxt("rm8", m8_b)
                rs = k.nxt("rs", rs_b); gt = k.nxt("gt", gt_b); gtbf = k.nxt("gtbf", gtbf_b); gT = k.nxt("gT", gT_b)
                sc.op(dve, lambda: V.tensor_copy(out=lg.t[:, :], in_=bk.t[:, 0:32]), reads=[bk.res], writes=[lg.res])
                sc.op(dve, lambda: V.max(out=m8_.t[:, 0:8], in_=lg.t[:, :]), reads=[lg.res], writes=[m8_.res])
                sc.op(dve, lambda: V.tensor_scalar(out=e_.t[:, :], in0=lg.t[:, :], scalar1=m8_.t[:, 0:1], scalar2=None,
                                                   op0=ALU.subtract), reads=[lg.res, m8_.res], writes=[e_.res])
                sc.op(act, lambda: A.activation(out=e_.t[:, :], in_=e_.t[:, :], func=AF.Exp), reads=[], writes=[e_.res])
                sc.op(dve, lambda: V.tensor_scalar(out=mk.t[:, :], in0=lg.t[:, :], scalar1=m8_.t[:, 3:4], scalar2=None,
                                                   op0=ALU.is_ge), reads=[lg.res, m8_.res], writes=[mk.res])
                sc.op(dve, lambda: V.tensor_tensor(out=e_.t[:, :], in0=e_.t[:, :], in1=mk.t[:, :], op=ALU.mult),
                      reads=[mk.res], writes=[e_.res])
                sc.op(dve, lambda: V.reduce_sum(out=rs.t[:, 0:1], in_=e_.t[:, :], axis=AX.X), reads=[e_.res], writes=[rs.res])
                sc.op(dve, lambda: V.reciprocal(out=rs.t[:, 1:2], in_=rs.t[:, 0:1]), reads=[], writes=[rs.res])
                sc.op(dve, lambda: V.tensor_scalar(out=gt.t[:, :], in0=e_.t[:, :], scalar1=rs.t[:, 1:2], scalar2=None,
                                                   op0=ALU.mult), reads=[e_.res, rs.res], writes=[gt.res])
                idxu = k.nxt("idxu", idxu_b); idxf = d["idxf"] = k.nxt("idxf", idxf_b); mkbf = d["mkbf"] = k.nxt("mkbf", mkbf_b)
                e4 = d["e4"] = k.nxt("e4", e4_b)
                d["rs"] = rs; d["gtbf"] = gtbf; d["gT"] = gT
                sc.op(dve, lambda: V.max_index(out=idxu.t[:, :], in_max=m8_.t[:, :], in_values=lg.t[:, :]),
                      reads=[m8_.res, lg.res], writes=[idxu.res])
                sc.op(dve, lambda: V.tensor_copy(out=idxf.t[:, :], in_=idxu.t[:, 0:4]), reads=[idxu.res], writes=[idxf.res])
                sc.op(pool, lambda: G.tensor_copy(out=mkbf.t[:, :], in_=mk.t[:, :]), reads=[mk.res], writes=[mkbf.res])
                sc.op(pool, lambda: G.tensor_copy(out=gtbf.t[:, :], in_=gt.t[:, :]), reads=[gt.res], writes=[gtbf.res])
                sc.op(dve, lambda: V.tensor_scalar(out=e4.t[:, :], in0=m8_.t[:, 0:4], scalar1=m8_.t[:, 0:1], scalar2=None,
                                                   op0=ALU.subtract), reads=[m8_.res], writes=[e4.res])
                sc.op(act, lambda: A.activation(out=e4.t[:, :], in_=e4.t[:, :], func=AF.Exp), reads=[], writes=[e4.res])

            def stage_d1(tt):
                d = st[tt]
                idxf, mkbf, e4, rs, gtbf, gT, hbf = d["idxf"], d["mkbf"], d["e4"], d["rs"], d["gtbf"], d["gT"], d["hbf"]
                rank = k.nxt("rank", rank_b); oh = k.nxt("oh", oh_b); rk = k.nxt("rk", rk_b)
                val = k.nxt("val", val_b); t1 = k.nxt("t1", t1_b)
                bkr = k.nxt("bank", banks)
                sc.op(pe, lambda: T.matmul(bkr.t[:, 0:32], lhsT=triu.t[:, :], rhs=mkbf.t[:, :], start=True, stop=True),
                      reads=[triu.res, mkbf.res], writes=[bkr.res])
                bkc = k.nxt("bank", banks)
                sc.op(pe, lambda: T.matmul(bkc.t[:, 0:32], lhsT=ones128.t[:, :], rhs=mkbf.t[:, :], start=True, stop=True),
                      reads=[ones128.res, mkbf.res], writes=[bkc.res])
                sc.op(pe, lambda: T.transpose(out=tbank.t[0:32, 0:128], in_=gtbf.t[:, :], identity=ident.t[:, :]),
                      reads=[gtbf.res, ident.res], writes=[tbank.res])
                sc.op(act, lambda: A.copy(out=gT.t[:, :], in_=tbank.t[0:32, 0:128]), reads=[tbank.res], writes=[gT.res])
                sc.op(dve, lambda: V.tensor_tensor(out=rank.t[:, :], in0=cumrep.t[:, :], in1=bkr.t[:, 0:32], op=ALU.add),
                      reads=[cumrep.res, bkr.res], writes=[rank.res])
                sc.op(dve, lambda: V.tensor_tensor(out=cumrep.t[:, :], in0=cumrep.t[:, :], in1=bkc.t[:, 0:32], op=ALU.add),
                      reads=[bkc.res], writes=[cumrep.res])
                for kk in range(4):
                    sc.op(dve, lambda: V.tensor_scalar(out=oh.t[:, :], in0=iota32.t[:, :], scalar1=idxf.t[:, kk:kk + 1], scalar2=None,
                                                       op0=ALU.is_equal), reads=[iota32.res, idxf.res], writes=[oh.res])
                    sc.op(dve, lambda: V.tensor_tensor(out=oh.t[:, :], in0=oh.t[:, :], in1=rank.t[:, :], op=ALU.mult),
                          reads=[rank.res], writes=[oh.res])
                    sc.op(dve, lambda: V.reduce_sum(out=rk.t[:, kk:kk + 1], in_=oh.t[:, :], axis=AX.X), reads=[oh.res], writes=[rk.res])
                sc.op(dve, lambda: V.tensor_scalar(out=val.t[:, :], in0=rk.t[:, :], scalar1=float(CAP), scalar2=None, op0=ALU.is_lt),
                      reads=[rk.res], writes=[val.res])
                sc.op(dve, lambda: V.scalar_tensor_tensor(out=t1.t[:, :], in0=idxf.t[:, :], scalar=float(CAP), in1=rk.t[:, :],
                                                          op0=ALU.mult, op1=ALU.add), reads=[idxf.res, rk.res], writes=[t1.res])
                sc.op(dve, lambda: V.scalar_tensor_tensor(out=t1.t[:, :], in0=t1.t[:, :], scalar=trp.t[:, 0:1], in1=val.t[:, :],
                                                          op0=ALU.subtract, op1=ALU.mult), reads=[trp.res, val.res], writes=[t1.res])
                sc.op(dve, lambda: V.tensor_scalar(out=dest_all.t[:, tt, :], in0=t1.t[:, :], scalar1=trp.t[:, 0:1], scalar2=None,
                                                   op0=ALU.add), reads=[t1.res, trp.res], writes=[dest_res[tt]])
                sc.op(dve, lambda: V.scalar_tensor_tensor(out=gk_all.t[:, tt, :], in0=e4.t[:, :], scalar=rs.t[:, 1:2], in1=val.t[:, :],
                                                          op0=ALU.mult, op1=ALU.mult), reads=[e4.res, rs.res, val.res], writes=[gk_res[tt]])
                for kk in range(4):
                    sc.dma(pool, lambda: G.indirect_dma_start(
                        out=xs_d[:, :], out_offset=bass.IndirectOffsetOnAxis(ap=dest_all.t[:, tt, kk:kk + 1], axis=0),
                        in_=hbf.t[:, :], in_offset=None), None, reads=[hbf.res, dest_res[tt]])

            def stage_d2(tt):
                d = st.pop(tt)
                gT, base = d["gT"], d["base"]
                rows = slice(tt * 128, (tt + 1) * 128)
                for ch in range(2):
                    bk2 = k.nxt("bank", banks)
                    sc.op(pe, lambda: T.matmul(bk2.t[:, :], lhsT=gT.t[0:32, :], rhs=b2a.t[0:32, ch * 512:(ch + 1) * 512],
                                               start=True, stop=True), reads=[gT.res, b2a.res], writes=[bk2.res])
                    sc.op(dve, lambda: V.tensor_tensor(out=base.t[:, ch * 512:(ch + 1) * 512], in0=base.t[:, ch * 512:(ch + 1) * 512],
                                                       in1=bk2.t[:, :], op=ALU.add), reads=[bk2.res], writes=[base.res])
                sc.dma(sp, base_d[rows, :], base.t[:, :], reads=[base.res])

            for it in range(NQT + 3):
                if 0 <= it - 3 < NQT:
                    stage_d1(it - 3)
                if 0 <= it - 2 < NQT:
                    stage_c(it - 2)
                if 0 <= it - 1 < NQT:
                    stage_b(it - 1)
                if it < NQT:
                    stage_a(it)
                if 0 <= it - 1 < NQT:
                    stage_b2(it - 1)
                if 0 <= it - 3 < NQT:
                    stage_d2(it - 3)
            sc.barrier()

    k.phase4 = phase4
    def phase5():
        NST = CAP // 128
        groups = [(g0, min(512, CAP - g0)) for g0 in range(0, CAP, 512)]
        with contextlib.ExitStack() as es:
            W1b = [k.sb(es, f"W1b{i}", [128, 8, 2048], BF16) for i in range(2)]
            W1res = [[Res() for _ in range(4)] for _ in range(2)]
            W2b = [k.sb(es, f"W2b{i}", [128, 8, 1024], BF16) for i in range(2)]
            W2res = [[Res() for _ in range(2)] for _ in range(2)]
            stg = [k.sb(es, f"wstg{i}", [128, 8, 512], F32) for i in range(2)]
            xTe2 = [k.sb(es, f"xTe{i}", [128, 8, CAP], BF16) for i in range(2)]
            actT = k.sb(es, "actT", [128, 8, CAP], BF16)
            xsl = [k.sb(es, f"xsl{i}", [128, 1024], BF16) for i in range(4)]
            yst = [k.sb(es, f"yst{i}", [128, 1024], F32) for i in range(2)]
            b1T = k.sb(es, "b1T", [128, 512], F32)
            sc.dma(sp, b1T.t[:, :], I("b_e1T")[:, :], writes=[b1T.res])
            gt_ = [k.sb(es, f"mg{i}", [128, 512], F32) for i in range(3)]
            sg_ = [k.sb(es, f"msg{i}", [128, 512], F32) for i in range(3)]
            lt_ = [k.sb(es, f"ml{i}", [128, 512], F32) for i in range(3)]
            w_e1, w_e2 = I("w_e1"), I("w_e2")
            sc.op(pool, lambda: G.memset(yst[0].t[:, :], 0.0), writes=[yst[0].res])
            sc.dma(sp, ys_d[32 * CAP:32 * CAP + 128, :], yst[0].t[:, :], reads=[yst[0].res])

            wseq = []
            for e_ in range(32):
                for blk in (0, 2, 1, 3):
                    wseq.append((e_, 1, blk))
                for blk in range(2):
                    wseq.append((e_, 2, blk))
            wstate = {"dma": 0, "cast": 0}

            def w_dma():
                n = wstate["dma"]
                if n >= len(wseq):
                    return
                wstate["dma"] += 1
                e_, kind, blk = wseq[n]
                s_ = stg[n % 2]
                src = (w_e1 if kind == 1 else w_e2)[e_, :, blk * 512:(blk + 1) * 512].rearrange("(kc p) n -> p kc n", p=128)
                sc.dma(sp, s_.t[:, :, :], src, writes=[s_.res])

            NPIECE = 2

            def w_cast_piece():
                n = wstate["cast"]
                if n >= len(wseq):
                    return
                j = wstate.get("piece", 0)
                e_, kind, blk = wseq[n]
                s_ = stg[n % 2]
                if kind == 1:
                    dst, rs_ = W1b[e_ % 2], W1res[e_ % 2][blk]
                else:
                    dst, rs_ = W2b[e_ % 2], W2res[e_ % 2][blk]
                kc0 = j * 4
                sc.op(act, lambda: A.copy(out=dst.t[:, kc0:kc0 + 4, blk * 512:(blk + 1) * 512], in_=s_.t[:, kc0:kc0 + 4, :]),
                      reads=[s_.res], writes=[rs_])
                if j + 1 == NPIECE:
                    wstate["piece"] = 0
                    wstate["cast"] += 1
                    w_dma()
                else:
                    wstate["piece"] = j + 1

            def w_cast():
                for _ in range(NPIECE):
                    w_cast_piece()

            w_dma(); w_dma()
            for _ in range(6):
                w_cast()
            def xs_tile(e_, s_i):
                xt_ = xTe2[e_ % 2]
                xl = k.nxt("xsl", xsl)
                r0 = e_ * CAP + s_i * 128
                sc.dma(sp, xl.t[:, :], xs_d[r0:r0 + 128, :], writes=[xl.res])
                for i in range(8):
                    sc.op(pe, lambda: T.transpose(out=tbank.t[:, i * 128:(i + 1) * 128], in_=xl.t[:, i * 128:(i + 1) * 128],
                                                  identity=ident.t[:, :]), reads=[xl.res, ident.res], writes=[tbank.res])
                sc.op(dve, lambda: V.tensor_copy(out=xt_.t[:, :, s_i * 128:(s_i + 1) * 128],
                                                 in_=tbank.t[:, :].rearrange("p (kc t) -> p kc t", kc=8)),
                      reads=[tbank.res], writes=[xt_.res])

            for s_i in range(NST):
                xs_tile(0, s_i)
            for e in range(32):
                w1 = W1b[e % 2]
                xTe = xTe2[e % 2]
                gi = 0
                for (g0, gn) in groups:
                    for fc in range(8):
                        if gi in (0, 2, 3, 5, 6, 8, 9, 11, 12, 14):
                            w_cast_piece()
                        gi += 1
                        bg = k.nxt("bank", banks)
                        bl = k.nxt("bank", banks)
                        for (bk, c0) in ((bg, fc * 128), (bl, 1024 + fc * 128)):
                            blk = c0 // 512
                            for kc in range(8):
                                sc.op(pe, lambda: T.matmul(bk.t[:, 0:gn], lhsT=w1.t[:, kc, c0:c0 + 128],
                                                           rhs=xTe.t[:, kc, g0:g0 + gn], start=(kc == 0), stop=(kc == 7)),
                                      reads=[W1res[e % 2][blk], xTe.res], writes=[bk.res])
                        g_ = k.nxt("mg", gt_); s_ = k.nxt("msg", sg_); l_ = k.nxt("ml", lt_)
                        cg = e * 16 + fc
                        cl = e * 16 + 8 + fc
                        sc.op(dve, lambda: V.tensor_scalar(out=g_.t[:, 0:gn], in0=bg.t[:, 0:gn], scalar1=b1T.t[:, cg:cg + 1], scalar2=7.0,
                                                           op0=ALU.add, op1=ALU.min), reads=[bg.res, b1T.res], writes=[g_.res])
                        sc.op(act, lambda: A.activation(out=s_.t[:, 0:gn], in_=g_.t[:, 0:gn], func=AF.Sigmoid, scale=1.702),
                              reads=[g_.res], writes=[s_.res])
                        sc.op(dve, lambda: V.tensor_scalar(out=l_.t[:, 0:gn], in0=bl.t[:, 0:gn], scalar1=b1T.t[:, cl:cl + 1], scalar2=-7.0,
                                                           op0=ALU.add, op1=ALU.max), reads=[bl.res, b1T.res], writes=[l_.res])
                        sc.op(dve, lambda: V.tensor_scalar(out=l_.t[:, 0:gn], in0=l_.t[:, 0:gn], scalar1=7.0, scalar2=1.0,
                                                           op0=ALU.min, op1=ALU.add), reads=[], writes=[l_.res])
                        sc.op(dve, lambda: V.tensor_tensor(out=g_.t[:, 0:gn], in0=g_.t[:, 0:gn], in1=s_.t[:, 0:gn], op=ALU.mult),
                              reads=[s_.res], writes=[g_.res])
                        sc.op(pool, lambda: G.tensor_tensor(out=actT.t[:, fc, g0:g0 + gn], in0=g_.t[:, 0:gn], in1=l_.t[:, 0:gn], op=ALU.mult),
                              reads=[g_.res, l_.res], writes=[actT.res])
                for s_i in range(NST):
                    if s_i in (1, 3):
                        w_cast_piece()
                    ys_ = k.nxt("yst", yst)
                    for ch in range(2):
                        bk = k.nxt("bank", banks)
                        for fc in range(8):
                            sc.op(pe, lambda: T.matmul(bk.t[:, :], lhsT=actT.t[:, fc, s_i * 128:(s_i + 1) * 128],
                                                       rhs=W2b[e % 2].t[:, fc, ch * 512:(ch + 1) * 512], start=(fc == 0), stop=(fc == 7)),
                                  reads=[actT.res, W2res[e % 2][ch]], writes=[bk.res])
                        evac_copy(ys_.t[:, ch * 512:(ch + 1) * 512], bk.t[:, :], [bk.res], [ys_.res])
                    r0 = e * CAP + s_i * 128
                    sc.dma(act, ys_d[r0:r0 + 128, :], ys_.t[:, :], reads=[ys_.res])
                    if e + 1 < 32:
                        xs_tile(e + 1, s_i)
            sc.barrier()
        with contextlib.ExitStack() as es:
            g2b = bcast_load(es, "g2b", I("ln2_g")[:, :], 1024)
            b2b = bcast_load(es, "b2b", I("ln2_b")[:, :], 1024)
            accb = [k.sb(es, f"cacc{i}", [128, 1024], F32) for i in range(4)]
            yb = [k.sb(es, f"cy{i}", [128, 1024], F32) for i in range(12)]
            lnst = [k.sb(es, f"lnst5{i}", [128, 16], F32) for i in range(4)]
            for tt in range(NQT):
                ac = k.nxt("cacc", accb)
                rows = slice(tt * 128, (tt + 1) * 128)
                sc.dma(sp, ac.t[:, :], base_d[rows, :], writes=[ac.res])
                for kk in range(4):
                    y_ = k.nxt("cy", yb)
                    sc.dma(pool, lambda: G.indirect_dma_start(
                        out=y_.t[:, :], out_offset=None, in_=ys_d[:, :],
                        in_offset=bass.IndirectOffsetOnAxis(ap=dest_all.t[:, tt, kk:kk + 1], axis=0)), None,
                        reads=[dest_res[tt]], writes=[y_.res])
                    sc.op(dve, lambda: V.scalar_tensor_tensor(out=ac.t[:, :], in0=y_.t[:, :], scalar=gk_all.t[:, tt, kk:kk + 1],
                                                              in1=ac.t[:, :], op0=ALU.mult, op1=ALU.add),
                          reads=[y_.res, gk_res[tt]], writes=[ac.res])
                st_ = k.nxt("lnst5", lnst)
                sc.op(dve, lambda: V.bn_stats(out=st_.t[:, 0:6], in_=ac.t[:, 0:512]), reads=[ac.res], writes=[st_.res])
                sc.op(dve, lambda: V.bn_stats(out=st_.t[:, 6:12], in_=ac.t[:, 512:1024]), reads=[ac.res], writes=[st_.res])
                sc.op(dve, lambda: V.bn_aggr(out=st_.t[:, 12:14], in_=st_.t[:, 0:12]), reads=[], writes=[st_.res])
                sc.op(act, lambda: A.activation(out=st_.t[:, 14:15], in_=st_.t[:, 13:14], func=AF.Ln, bias=epsb.t[:, 0:1], scale=1.0),
                      reads=[st_.res, epsb.res], writes=[st_.res])
                sc.op(act, lambda: A.activation(out=st_.t[:, 14:15], in_=st_.t[:, 14:15], func=AF.Exp, scale=-0.5),
                      reads=[], writes=[st_.res])
                sc.op(dve, lambda: V.scalar_tensor_tensor(out=st_.t[:, 15:16], in0=st_.t[:, 12:13], scalar=-1.0, in1=st_.t[:, 14:15],
                                                          op0=ALU.mult, op1=ALU.mult), reads=[], writes=[st_.res])
                sc.op(act, lambda: A.activation(out=ac.t[:, :], in_=ac.t[:, :], func=AF.Identity, bias=st_.t[:, 15:16],
                                                scale=st_.t[:, 14:15]), reads=[st_.res], writes=[ac.res])
                sc.op(dve, lambda: V.tensor_tensor(out=ac.t[:, :], in0=ac.t[:, :], in1=g2b.t[:, :], op=ALU.mult),
                      reads=[g2b.res], writes=[ac.res])
                sc.op(dve, lambda: V.tensor_tensor(out=ac.t[:, :], in0=ac.t[:, :], in1=b2b.t[:, :], op=ALU.add),
                      reads=[b2b.res], writes=[ac.res])
                sc.dma(sp, out_d[rows, :], ac.t[:, :], reads=[ac.res])
            sc.barrier()

    k.phase5 = phase5
    k.phase1 = phase1
    return k


def prep_inputs(inp):
    f = lambda a: np.ascontiguousarray(np.asarray(a, np.float32))
    sh = {}
    sh["w_in"] = f(inp["w_in"][0])
    for n in ("diff_lq1", "diff_lk1", "diff_lq2", "diff_lk2", "diff_subln_g", "b_mgate", "ln1_g", "ln1_b",
              "b_router", "b_ple_gate", "ln2_g", "ln2_b"):
        sh[n] = f(inp[n][0]).reshape(1, -1)
    sh["nsa_pos_kT"] = f(np.asarray(inp["nsa_pos_k"][0]).T)
    sh["nsa_pos_vT"] = f(np.asarray(inp["nsa_pos_v"][0]).T)
    sh["nsa_phi_k1"] = f(np.asarray(inp["nsa_phi_k1"][0]).reshape(32, 64, 256).transpose(1, 0, 2))
    for nm in ("k", "v"):
        w = np.asarray(inp["nsa_phi_%s1" % nm][0]).reshape(16, 2, 64, 256)
        sh["nsa_phi_%s1s" % nm] = f(w.transpose(1, 2, 0, 3).reshape(128, 16, 256))
        ps = np.asarray(inp["nsa_pos_%s" % nm][0]).reshape(16, 2, 64)
        sh["nsa_pos_%sTs" % nm] = f(ps.transpose(1, 2, 0).reshape(128, 16))
    sh["nsa_phi_v1"] = f(np.asarray(inp["nsa_phi_v1"][0]).reshape(32, 64, 256).transpose(1, 0, 2))
    for n in ("nsa_phi_k2", "nsa_phi_v2", "w_br_diff", "w_br_nsa", "w_mgate", "w_o", "w_router", "w_e1", "w_e2",
              "b_e2", "w_ple_gate", "w_ple_proj"):
        sh[n] = f(inp[n][0])
    sh["b_e1T"] = f(np.asarray(inp["b_e1"][0]).reshape(32, 16, 128).transpose(2, 0, 1).reshape(128, 512))
    sh.update(_const_tables())
    x = np.asarray(inp["x"], np.float32)
    p = np.asarray(inp["p"], np.float32)
    maps = []
    for b in range(8):
        m = dict(sh)
        m["x"] = f(x[b])
        m["xT"] = f(x[b].T)
        m["pT"] = f(p[0, b].T)
        maps.append(m)
    return maps


_CACHE = {}


def kernel(**inputs):
    if "nc" not in _CACHE:
        kb = build()
        kb.phase1(); kb.phase2(); kb.phase3(); kb.phase4(); kb.phase5()
        _CACHE["nc"] = kb
    kb = _CACHE["nc"]
    maps = prep_inputs(inputs)
    maps = [{n: m[n] for n in kb.din} for m in maps]
    res = run_bass_kernel_spmd(kb.nc, maps, core_ids=list(range(8)))
    out = np.stack([np.asarray(res.results[b]["out"], np.float32) for b in range(8)], axis=0)
    return out
```

```python
import contextlib
import os
import math
import numpy as np
import ml_dtypes
import concourse.bass as bass
import concourse.mybir as mybir
from concourse.bass_utils import run_bass_kernel_spmd

F32 = mybir.dt.float32
BF16 = mybir.dt.bfloat16
AF = mybir.ActivationFunctionType
ALU = mybir.AluOpType
AX = mybir.AxisListType

S = 4096
D = 1024
NQT = 32
NEGB = -131072.0
SCALE = 0.125
LN_EPS = 1e-5
ALPHA = 2.0 ** 0.25
LAMBDA_INIT = 0.2
SEM_LIMIT = 30000
SKIP_SAME = set(os.environ.get('KSKIP', '').split(',')) - {''}
CAP = 768
NSLOT = 32 * CAP + 128


class Res:
    __slots__ = ("w", "r")

    def __init__(self):
        self.w = None
        self.r = {}


class Eng:
    def __init__(self, sch, name, eng):
        self.sch = sch
        self.name = name
        self.eng = eng
        self.sem = sch.new_sem(name)
        self.cnt = 0
        self.known = {}
        self.slots = []
        self.slot_i = 0


class Sched:
    def __init__(self, nc, es):
        self.nc = nc
        self.es = es
        self.nsem = 0
        self.sems = {}
        self.pe = Eng(self, "pe", nc.tensor)
        self.act = Eng(self, "act", nc.scalar)
        self.dve = Eng(self, "dve", nc.vector)
        self.pool = Eng(self, "pool", nc.gpsimd)
        self.sp = Eng(self, "sp", nc.sync)
        self.engs = [self.pe, self.act, self.dve, self.pool, self.sp]
        for e, n in ((self.sp, 24), (self.pool, 16), (self.act, 4)):
            for i in range(n):
                e.slots.append([self.new_sem(f"{e.name}_d{i}"), 0])
        self.n_ins = 0

    def new_sem(self, name):
        self.nsem += 1
        s = self.es.enter_context(self.nc.semaphore(f"s{self.nsem}_{name}"))
        self.sems[id(s)] = s
        return s

    def _wait(self, E, toks):
        for sem, val in toks:
            k = id(sem)
            if E.known.get(k, 0) >= val:
                continue
            E.eng.wait_ge(sem, val)
            E.known[k] = val

    def _deps(self, E, reads, writes):
        deps = []
        for r in reads:
            if r.w is not None:
                deps.append(r.w)
        for w in writes:
            if w.w is not None:
                deps.append(w.w)
            deps.extend(w.r.values())
        if E is self.pe or E.name in SKIP_SAME:
            deps = [d for d in deps if d[0] is not E.sem]
        return deps

    def _commit(self, tok, reads, writes):
        for r in reads:
            k = id(tok[0])
            r.r[k] = tok
        for w in writes:
            w.w = tok
            w.r = {}

    def op(self, E, fn, reads=(), writes=()):
        self._wait(E, self._deps(E, reads, writes))
        ins = fn()
        E.cnt += 1
        ins.then_inc(E.sem, 1)
        tok = (E.sem, E.cnt)
        self._commit(tok, reads, writes)
        self.n_ins += 1
        if E.cnt >= SEM_LIMIT:
            E.sem = self.new_sem(E.name)
            E.cnt = 0
        return tok

    def dma(self, E, out, in_, reads=(), writes=(), **kw):
        slot = E.slots[E.slot_i]
        E.slot_i = (E.slot_i + 1) % len(E.slots)
        deps = self._deps(E, reads, writes)
        if slot[1] > 0:
            deps.append((slot[0], slot[1]))
        self._wait(E, deps)
        if slot[1] + 16 > SEM_LIMIT:
            slot[0] = self.new_sem(E.name + "_d")
            slot[1] = 0
        if callable(out):
            out().then_inc(slot[0], 16)
        else:
            E.eng.dma_start(out=out, in_=in_, **kw).then_inc(slot[0], 16)
        slot[1] += 16
        tok = (slot[0], slot[1])
        self._commit(tok, reads, writes)
        self.n_ins += 1
        return tok

    def barrier(self):
        toks = []
        for e in self.engs:
            if e.cnt > 0:
                toks.append((e.sem, e.cnt))
            for s in e.slots:
                if s[1] > 0:
                    toks.append((s[0], s[1]))
        for e in self.engs:
            self._wait(e, toks)


def _bf(x):
    return np.asarray(x, np.float32).astype(ml_dtypes.bfloat16)


def _hi_lo(v):
    v = np.asarray(v, np.float64)
    hi = v.astype(np.float32).astype(ml_dtypes.bfloat16).astype(np.float64)
    lo = (v - hi)
    return hi, lo


def _const_tables():
    t = {}
    pos = np.arange(S)
    augk = np.stack([pos // 128, pos // 128, pos % 128, pos % 128, np.ones(S), np.ones(S)]).astype(np.float32)
    t["augk_tok"] = _bf(augk)
    cpos = np.arange(255) * 16 + 31
    augc = np.zeros((6, 256), np.float32)
    augc[:, :255] = np.stack([cpos // 128, cpos // 128, cpos % 128, cpos % 128, np.ones(255), np.ones(255)])
    t["augk_cmp"] = _bf(augc)
    slopes = list(2.0 ** (-8.0 * np.arange(1, 9) / 8)) + list(2.0 ** (-8.0 * np.arange(1, 17) / 16))
    augq = np.zeros((24, 6, S), np.float32)
    for i, s in enumerate(slopes):
        shi, slo = _hi_lo(s)
        augq[i, 0] = 1024.0 * shi
        augq[i, 1] = 1024.0 * slo
        augq[i, 2] = 8.0 * shi
        augq[i, 3] = 8.0 * slo
        hi, lo = _hi_lo(-8.0 * s * pos)
        augq[i, 4] = hi
        augq[i, 5] = lo
    t["augq"] = _bf(augq)
    kk = np.arange(128)[:, None]
    qq = np.arange(128)[None, :]
    t["m_caus"] = _bf(np.where(qq >= kk, 0.0, NEGB))
    t["m_band"] = _bf(np.where(qq < kk, 0.0, NEGB))
    t["ident"] = _bf(np.eye(128))
    n = np.arange(256).reshape(2, 128)
    valid = ((n[:, :, None] * 16 + 31) <= pos[None, None, :]) & (n[:, :, None] < 255)
    t["m_cmp"] = _bf(np.where(valid, 0.0, NEGB).transpose(1, 0, 2))
    c_lo = np.arange(255) * 16
    s_lo = np.arange(64) * 64
    ov = np.minimum(c_lo[:, None] + 32, s_lo[None, :] + 64) - np.maximum(c_lo[:, None], s_lo[None, :])
    c2s = np.zeros((256, 64), np.float32)
    c2s[:255] = np.clip(ov, 0, None) / 32.0
    t["c2s"] = _bf(c2s.reshape(2, 128, 64).transpose(1, 0, 2))
    cur = (pos // 64)[:, None]
    j = np.arange(64)[None, :]
    forced = (j == 0) | (j == cur) | (j == cur - 1)
    keep = (~forced) & (j <= cur)
    tf = np.where(forced, 1e30, np.where(j <= cur, 0.0, -1e30)).astype(np.float32)
    t["sel_keep"] = np.ascontiguousarray(keep.astype(np.float32).reshape(32, 128, 64).transpose(1, 0, 2))
    t["sel_force"] = np.ascontiguousarray(tf.reshape(32, 128, 64).transpose(1, 0, 2))
    t["expand"] = _bf((np.arange(64)[:, None] == (pos // 64)[None, :]).astype(np.float32))
    t["triu"] = _bf((np.arange(128)[:, None] < np.arange(128)[None, :]).astype(np.float32))
    t["ones128"] = _bf(np.ones((128, 128)))
    t["iota32"] = np.tile(np.arange(32, dtype=np.float32)[None, :], (128, 1))
    t["trp"] = (32 * CAP + np.arange(128, dtype=np.float32)).reshape(128, 1)
    return t


WEIGHT_NAMES = [
    "w_in", "diff_lq1", "diff_lk1", "diff_lq2", "diff_lk2", "diff_subln_g",
    "nsa_pos_k", "nsa_pos_v", "nsa_phi_k1", "nsa_phi_k2", "nsa_phi_v1", "nsa_phi_v2",
    "w_br_diff", "w_br_nsa", "w_mgate", "b_mgate", "w_o", "ln1_g", "ln1_b",
    "w_router", "b_router", "w_e1", "b_e1", "w_e2", "b_e2",
    "w_ple_gate", "b_ple_gate", "w_ple_proj", "ln2_g", "ln2_b",
]


class Buf:
    __slots__ = ("t", "res")

    def __init__(self, t):
        self.t = t
        self.res = Res()


class KB:
    def __init__(self, dbg=()):
        self.dbg = set(dbg)
        self.nc = bass.Bass("TRN2", target_bir_lowering=False)
        self.es = contextlib.ExitStack()
        self.sc = Sched(self.nc, self.es)
        self.din = {}
        self.rot = {}

    def inp(self, name, shape, dt=F32):
        t = self.nc.dram_tensor(name, list(shape), dt, kind="ExternalInput").ap()
        self.din[name] = t
        return t

    def scratch(self, name, shape, dt):
        kind = "ExternalOutput" if name in self.dbg else "Internal"
        return self.nc.dram_tensor(name, list(shape), dt, kind=kind).ap()

    def sb(self, es, name, shape, dt):
        return Buf(es.enter_context(self.nc.sbuf_tensor("sb_" + name, list(shape), dt)))

    def ps(self, es, name, shape, dt):
        return Buf(es.enter_context(self.nc.psum_tensor("ps_" + name, list(shape), dt)))

    def nxt(self, key, lst):
        i = self.rot.get(key, 0)
        self.rot[key] = i + 1
        return lst[i % len(lst)]


def build(dbg=(), phases=(1, 2, 3, 4, 5)):
    k = KB(dbg)
    nc, sc = k.nc, k.sc
    pe, act, dve, pool, sp = sc.pe, sc.act, sc.dve, sc.pool, sc.sp
    V, G, T = nc.vector, nc.gpsimd, nc.tensor
    A = nc.scalar

    SHAPES = {
        "x": ([S, D], F32), "xT": ([D, S], F32), "pT": ([256, S], F32), "w_in": ([D, 5680], F32),
        "diff_lq1": ([1, 64], F32), "diff_lk1": ([1, 64], F32), "diff_lq2": ([1, 64], F32), "diff_lk2": ([1, 64], F32),
        "diff_subln_g": ([1, 128], F32), "nsa_pos_kT": ([64, 32], F32), "nsa_pos_vT": ([64, 32], F32),
        "nsa_phi_k1": ([64, 32, 256], F32), "nsa_phi_v1": ([64, 32, 256], F32),
        "nsa_phi_k1s": ([128, 16, 256], F32), "nsa_phi_v1s": ([128, 16, 256], F32), "nsa_pos_kTs": ([128, 16], F32), "nsa_pos_vTs": ([128, 16], F32),
        "nsa_phi_k2": ([256, 64], F32), "nsa_phi_v2": ([256, 64], F32),
        "w_br_diff": ([D, D], F32), "w_br_nsa": ([D, D], F32), "w_mgate": ([D, 2 * D], F32), "b_mgate": ([1, 2 * D], F32),
        "w_o": ([D, D], F32), "ln1_g": ([1, D], F32), "ln1_b": ([1, D], F32),
        "w_router": ([D, 32], F32), "b_router": ([1, 32], F32),
        "w_e1": ([32, D, 2 * D], F32), "b_e1T": ([128, 512], F32), "w_e2": ([32, D, D], F32), "b_e2": ([32, D], F32),
        "w_ple_gate": ([D, D], F32), "b_ple_gate": ([1, D], F32), "w_ple_proj": ([256, D], F32),
        "ln2_g": ([1, D], F32), "ln2_b": ([1, D], F32),
        "augk_tok": ([6, S], BF16), "augk_cmp": ([6, 256], BF16), "augq": ([24, 6, S], BF16),
        "m_caus": ([128, 128], BF16), "m_band": ([128, 128], BF16), "ident": ([128, 128], BF16),
        "m_cmp": ([128, 2, S], BF16), "c2s": ([128, 2, 64], BF16),
        "sel_keep": ([128, 32, 64], F32), "sel_force": ([128, 32, 64], F32), "expand": ([64, S], BF16),
        "triu": ([128, 128], BF16), "ones128": ([128, 128], BF16), "iota32": ([128, 32], F32), "trp": ([128, 1], F32),
    }

    def I(name):
        if name not in k.din:
            shp, dt = SHAPES[name]
            k.inp(name, shp, dt)
        return k.din[name]

    out_d = nc.dram_tensor("out", [S, D], F32, kind="ExternalOutput").ap()

    QdT = k.scratch("QdT", [D, S], BF16); KdT = k.scratch("KdT", [D, S], BF16)
    Vd = k.scratch("Vd", [S, D], BF16); NqT = k.scratch("NqT", [D, S], BF16)
    kcT = k.scratch("kcT", [256, S], BF16); vcT = k.scratch("vcT", [256, S], BF16)
    ksT = k.scratch("ksT", [256, S], BF16); vs_d = k.scratch("vs", [S, 256], BF16)
    kwT = k.scratch("kwT", [256, S], BF16); vw_d = k.scratch("vw", [S, 256], BF16)
    gates_d = k.scratch("gates", [S, 48], F32)
    odiff_d = k.scratch("o_diff", [S, D], BF16); onsa_d = k.scratch("o_nsa", [S, D], BF16)
    hT_d = k.scratch("hT", [D, S], BF16); base_d = k.scratch("base", [S, D], F32)
    G_d = k.scratch("Gd", [S, 32], F32)
    xs_d = k.scratch("xs", [NSLOT, D], BF16); ys_d = k.scratch("ys", [NSLOT, D], F32)

    es0 = k.es
    banks = [k.ps(es0, f"bank{i}", [128, 512], F32) for i in range(7)]
    tbank = k.ps(es0, "tbank", [128, 1024], BF16)
    ident = k.sb(es0, "ident", [128, 128], BF16)
    mcaus = k.sb(es0, "mcaus", [128, 128], BF16)
    mband = k.sb(es0, "mband", [128, 128], BF16)
    sc.dma(sp, ident.t[:], I("ident")[:, :], writes=[ident.res])
    wz = k.sb(es0, "wz", [128, 512], BF16)
    sc.op(pool, lambda: G.memset(wz.t[:, :], 0.0), writes=[wz.res])
    epsb = k.sb(es0, "epsb", [128, 1], F32)
    sc.op(pool, lambda: G.memset(epsb.t[:, :], LN_EPS), writes=[epsb.res])
    sc.dma(sp, mcaus.t[:], I("m_caus")[:, :], writes=[mcaus.res])
    sc.dma(sp, mband.t[:], I("m_band")[:, :], writes=[mband.res])

    U32 = mybir.dt.uint32
    dest_all = k.sb(es0, "dest_all", [128, 32, 4], U32)
    gk_all = k.sb(es0, "gk_all", [128, 32, 4], F32)
    dest_res = [Res() for _ in range(32)]
    gk_res = [Res() for _ in range(32)]
    evac_i = [0]

    def evac_copy(out_ap, in_ap, reads, writes):
        evac_i[0] += 1
        if evac_i[0] % 2:
            return sc.op(act, lambda: A.copy(out=out_ap, in_=in_ap), reads=reads, writes=writes)
        return sc.op(dve, lambda: V.tensor_copy(out=out_ap, in_=in_ap), reads=reads, writes=writes)

    def phase1():
        with contextlib.ExitStack() as es:
            xT = k.sb(es, "xT", [128, 8, S], BF16)
            rx = [Res() for _ in range(8)]
            for kc in range(8):
                sc.dma(pool, xT.t[:, kc, :], I("xT")[kc * 128:(kc + 1) * 128, :], writes=[rx[kc]])
            wb = [k.sb(es, f"wb{i}", [128, 8, 1024], BF16) for i in range(2)]
            sfm = [k.sb(es, f"sfm{i}", [128, S], BF16) for i in range(2)]
            stm = [k.sb(es, f"stm{i}", [128, 4, 1024], BF16) for i in range(2)]
            sg = k.sb(es, "sgate", [128, 32, 48], F32)
            loads = [(0, 1024), (1024, 1024), (2048, 1024), (3072, 1024), (4096, 1024), (5120, 560)]
            subs = [
                [(0, 1024, "fm", QdT, 0)], [(0, 1024, "fm", KdT, 0)], [(0, 1024, "tm", Vd, 0)],
                [(0, 1024, "fm", NqT, 0)],
                [(0, 256, "fm", kcT, 0), (256, 256, "fm", vcT, 0), (512, 256, "fm", ksT, 0), (768, 256, "tm", vs_d, 0)],
                [(0, 256, "fm", kwT, 0), (256, 256, "tm", vw_d, 0), (512, 48, "gate", None, 0)],
            ]
            bi = 0
            for li, (c0, n) in enumerate(loads):
                w = wb[li % 2]
                src = I("w_in")[:, c0:c0 + n].rearrange("(kc p) n -> p kc n", p=128)
                sc.dma(pool, w.t[:, :, 0:n], src, writes=[w.res])
                for (b0, nn, kind, dst, _) in subs[li]:
                    if kind == "fm":
                        for cc in range(nn // 128):
                            st = k.nxt("sfm", sfm)
                            for qt in range(8):
                                bk = k.nxt("bank", banks)
                                for kc in range(8):
                                    sc.op(pe, lambda: T.matmul(bk.t[:, :], lhsT=w.t[:, kc, b0 + cc * 128:b0 + (cc + 1) * 128],
                                                               rhs=xT.t[:, kc, qt * 512:(qt + 1) * 512],
                                                               start=(kc == 0), stop=(kc == 7)),
                                          reads=[w.res, rx[kc]], writes=[bk.res])
                                evac_copy(st.t[:, qt * 512:(qt + 1) * 512], bk.t[:, :], [bk.res], [st.res])
                            sc.dma(sp, dst[cc * 128:(cc + 1) * 128, :], st.t[:, :], reads=[st.res])
                    elif kind == "tm":
                        for t4 in range(8):
                            st = k.nxt("stm", stm)
                            for ti in range(4):
                                tt = t4 * 4 + ti
                                for ch in range((nn + 511) // 512):
                                    cw = min(512, nn - ch * 512)
                                    bk = k.nxt("bank", banks)
                                    for kc in range(8):
                                        sc.op(pe, lambda: T.matmul(bk.t[:, 0:cw], lhsT=xT.t[:, kc, tt * 128:(tt + 1) * 128],
                                                                   rhs=w.t[:, kc, b0 + ch * 512:b0 + ch * 512 + cw],
                                                                   start=(kc == 0), stop=(kc == 7)),
                                              reads=[w.res, rx[kc]], writes=[bk.res])
                                    evac_copy(st.t[:, ti, ch * 512:ch * 512 + cw], bk.t[:, 0:cw], [bk.res], [st.res])
                            sc.dma(sp, dst[t4 * 512:(t4 + 1) * 512, :].rearrange("(t p) c -> p t c", p=128),
                                   st.t[:, :, 0:nn], reads=[st.res])
                    else:
                        for tt in range(32):
                            bk = k.nxt("bank", banks)
                            for kc in range(8):
                                sc.op(pe, lambda: T.matmul(bk.t[:, 0:48], lhsT=xT.t[:, kc, tt * 128:(tt + 1) * 128],
                                                           rhs=w.t[:, kc, b0:b0 + 48], start=(kc == 0), stop=(kc == 7)),
                                      reads=[w.res, rx[kc]], writes=[bk.res])
                            sc.op(act, lambda: A.activation(out=sg.t[:, tt, :], in_=bk.t[:, 0:48], func=AF.Sigmoid),
                                  reads=[bk.res], writes=[sg.res])
                        sc.dma(sp, gates_d.rearrange("(t p) c -> p t c", p=128), sg.t[:, :, :], reads=[sg.res])
            sc.barrier()

    sbanks = banks[0:3]

    class _View:
        pass
    tb32 = _View()
    tb32.t = tbank.t[:, :].bitcast(F32)
    tb32.res = tbank.res
    SB = [sbanks]
    accs = banks[3:7]

    FILL = [int(os.environ.get('KFILL', '0'))]
    KDIM = int(os.environ.get('KDIM', '128'))

    def attn_pass(es_p, QT, KT, kdim, Vt, dvp, specs, evac_fn, reads, pbufs):
        items = []
        covers = {}
        for j in range(8):
            spj = specs[j]
            if not spj:
                continue
            covers[j] = {sub: [i for i, s_ in enumerate(spj) if s_[1] <= sub * 128 < s_[2]] for sub in range(4)}
            for i in range(len(spj)):
                items.append((j, i))

        def emit_S(n):
            j, i = items[n]
            kt, c0, c1, masks = specs[j][i]
            bk = k.nxt("sbank" + str(len(SB[0])), SB[0])
            nm = len(masks)
            sc.op(pe, lambda: T.matmul(bk.t[:, c0:c1], lhsT=KT[0:kdim, kt * 128:(kt + 1) * 128],
                                       rhs=QT[0:kdim, j * 512 + c0:j * 512 + c1], start=True, stop=(nm == 0)),
                  reads=reads, writes=[bk.res])
            for mi, (ml, mr, lo, hi, mreads) in enumerate(masks):
                sc.op(pe, lambda: T.matmul(bk.t[:, lo:hi], lhsT=ml, rhs=mr, start=False, stop=(mi == nm - 1)),
                      reads=mreads, writes=[bk.res])
            return bk

        LA = len(SB[0]) - 1
        pend = {}
        for n0 in range(min(LA, len(items))):
            pend[n0] = emit_S(n0)
        for n in range(len(items)):
            if n + LA < len(items):
                pend[n + LA] = emit_S(n + LA)
            j, i = items[n]
            kt, c0, c1, masks = specs[j][i]
            cover = covers[j]
            bk = pend.pop(n)
            pb = k.nxt("pbuf", pbufs)
            if FILL[0] > 0:
                sc.op(pe, lambda: T.matmul(tbank.t[:, :].bitcast(F32)[:, 0:FILL[0]], lhsT=ident.t[:, :], rhs=wz.t[:, 0:FILL[0]],
                                           start=True, stop=True), reads=[], writes=[])
            sc.op(act, lambda: A.activation(out=pb.t[:, c0:c1], in_=bk.t[:, c0:c1], func=AF.Exp, scale=SCALE),
                  reads=[bk.res], writes=[pb.res])
            for sub in range(c0 // 128, c1 // 128):
                ac = accs[sub]
                first = cover[sub][0] == i
                last = cover[sub][-1] == i
                sc.op(pe, lambda: T.matmul(ac.t[:, 0:dvp], lhsT=pb.t[:, sub * 128:(sub + 1) * 128],
                                           rhs=Vt[:, kt, 0:dvp], start=first, stop=last),
                      reads=[pb.res] + list(reads), writes=[ac.res])
                if last:
                    evac_fn(j * 4 + sub, ac)

    def pe_warmup(n=24):
        for i in range(n):
            sc.op(pe, lambda: T.matmul(tbank.t[:, :].bitcast(F32), lhsT=ident.t[:, :], rhs=wz.t[:, :], start=True, stop=True),
                  reads=[ident.res, wz.res], writes=[tbank.res] if i == 0 else [])

    def causal_specs(extra=None):
        specs = []
        for j in range(8):
            l = []
            for kt in range(4 * j + 4):
                d = kt - 4 * j
                c0 = 128 * d if d > 0 else 0
                masks = []
                if extra is not None:
                    masks += extra(j, kt, c0, 512)
                if d >= 0:
                    masks.append((ident.t[:, :], mcaus.t[:, :], 128 * d, 128 * d + 128, [ident.res, mcaus.res]))
                l.append((kt, c0, 512, masks))
            specs.append(l)
        return specs

    def bcast_load(es, name, src_ap, n, eng=None):
        b = k.sb(es, name, [128, n], F32)
        sc.dma(sp, b.t[:, :], src_ap.partition_broadcast(128), writes=[b.res])
        return b

    def phase2():
        with contextlib.ExitStack() as es:
            pbufs = [k.sb(es, f"pb{i}", [128, 512], BF16) for i in range(6)]
            Qb = [k.sb(es, f"dQ{i}", [128, S], BF16) for i in range(2)]
            Kb = [k.sb(es, f"dK{i}", [128, S], BF16) for i in range(2)]
            Vb = [k.sb(es, f"dV{i}", [128, 32, 129], BF16) for i in range(2)]
            A0 = k.sb(es, "dA0", [128, 32, 128], F32)
            A1 = k.sb(es, "dA1", [128, 32, 128], F32)
            Asq = k.sb(es, "dAsq", [128, 32, 128], F32)
            rst = k.sb(es, "drst", [128, 32, 2], F32)
            ost = [k.sb(es, f"dost{i}", [128, 32, 128], BF16) for i in range(2)]
            small = [k.sb(es, f"dsm{i}", [128, 4], F32) for i in range(8)]
            at = [k.sb(es, f"dat{i}", [128, 128], F32) for i in range(3)]
            junk = k.sb(es, "djunk", [128, 128], F32)
            l4 = [bcast_load(es, f"dl{i}", I(n)[:, :], 64) for i, n in
                  enumerate(("diff_lq1", "diff_lk1", "diff_lq2", "diff_lk2"))]
            gsub = bcast_load(es, "dgsub", I("diff_subln_g")[:, :], 128)
            lam = k.sb(es, "dlam", [128, 8], F32)
            sc.op(dve, lambda: V.tensor_tensor(out=l4[0].t[:, :], in0=l4[0].t[:, :], in1=l4[1].t[:, :], op=ALU.mult),
                  reads=[l4[1].res], writes=[l4[0].res])
            sc.op(dve, lambda: V.tensor_tensor(out=l4[2].t[:, :], in0=l4[2].t[:, :], in1=l4[3].t[:, :], op=ALU.mult),
                  reads=[l4[3].res], writes=[l4[2].res])
            sc.op(dve, lambda: V.reduce_sum(out=lam.t[:, 0:1], in_=l4[0].t[:, :], axis=AX.X), reads=[l4[0].res], writes=[lam.res])
            sc.op(dve, lambda: V.reduce_sum(out=lam.t[:, 1:2], in_=l4[2].t[:, :], axis=AX.X), reads=[l4[2].res], writes=[lam.res])
            sc.op(act, lambda: A.activation(out=lam.t[:, 2:4], in_=lam.t[:, 0:2], func=AF.Exp), reads=[lam.res], writes=[lam.res])
            sc.op(dve, lambda: V.scalar_tensor_tensor(out=lam.t[:, 4:5], in0=lam.t[:, 3:4], scalar=-LAMBDA_INIT,
                                                      in1=lam.t[:, 2:3], op0=ALU.add, op1=ALU.subtract),
                  reads=[lam.res], writes=[lam.res])
            sc.op(dve, lambda: V.tensor_scalar(out=gsub.t[:, :], in0=gsub.t[:, :], scalar1=1.0 - LAMBDA_INIT, scalar2=None,
                                               op0=ALU.mult), reads=[gsub.res], writes=[gsub.res])
            for b in Vb:
                sc.op(pool, lambda: G.memset(b.t[:, :, 128:129], 1.0), writes=[b.res])
            for b in Qb + Kb:
                sc.op(pool, lambda: G.memset(b.t[64:128, :], 0.0), writes=[b.res])
            for b in Kb:
                sc.dma(sp, b.t[64:70, :], I("augk_tok")[:, :], writes=[b.res])
            specs = causal_specs()
            SB[0] = sbanks + [tb32]
            for h in range(8):
                if h == 0:
                    pe_warmup()
                vb = Vb[h % 2]
                sc.dma(sp, vb.t[:, :, 0:128], Vd[:, h * 128:(h + 1) * 128].rearrange("(t p) c -> p t c", p=128),
                       writes=[vb.res])
                osb = ost[h % 2]
                for c in range(2):
                    qb, kb_ = Qb[c], Kb[c]
                    r0 = h * 128 + c * 64
                    sc.dma(sp, qb.t[0:64, :], QdT[r0:r0 + 64, :], writes=[qb.res])
                    sc.dma(sp, qb.t[64:70, :], I("augq")[h, :, :], writes=[qb.res])
                    sc.dma(sp, kb_.t[0:64, :], KdT[r0:r0 + 64, :], writes=[kb_.res])

                    def ev(qt, ac, c=c, osb=osb):
                        sm = k.nxt("dsm", small)
                        sc.op(dve, lambda: V.reciprocal(out=sm.t[:, 0:1], in_=ac.t[:, 128:129]), reads=[ac.res], writes=[sm.res])
                        if c == 0:
                            sc.op(dve, lambda: V.tensor_scalar(out=A0.t[:, qt, :], in0=ac.t[:, 0:128], scalar1=sm.t[:, 0:1],
                                                               scalar2=None, op0=ALU.mult),
                                  reads=[ac.res, sm.res], writes=[A0.res])
                            return
                        sc.op(dve, lambda: V.tensor_tensor(out=sm.t[:, 1:2], in0=sm.t[:, 0:1], in1=lam.t[:, 4:5], op=ALU.mult),
                              reads=[lam.res, sm.res], writes=[sm.res])
                        sc.op(dve, lambda: V.scalar_tensor_tensor(out=A1.t[:, qt, :], in0=ac.t[:, 0:128], scalar=sm.t[:, 1:2],
                                                                  in1=A0.t[:, qt, :], op0=ALU.mult, op1=ALU.add),
                              reads=[ac.res, sm.res, A0.res], writes=[A1.res])

                    attn_pass(es, qb.t, kb_.t, KDIM, vb.t, 129, specs, ev, [qb.res, kb_.res, vb.res], pbufs)
                sc.op(dve, lambda: V.tensor_tensor(out=Asq.t[:, :, :], in0=A1.t[:, :, :], in1=A1.t[:, :, :], op=ALU.mult),
                      reads=[A1.res], writes=[Asq.res])
                sc.op(dve, lambda: V.tensor_reduce(out=rst.t[:, :, 0:1], in_=Asq.t[:, :, :], axis=AX.X, op=ALU.add),
                      reads=[Asq.res], writes=[rst.res])
                sc.op(act, lambda: A.activation(out=rst.t[:, :, 1:2], in_=rst.t[:, :, 0:1], func=AF.Ln, bias=epsb.t[:, 0:1],
                                                scale=1.0 / 128.0), reads=[rst.res, epsb.res], writes=[rst.res])
                sc.op(act, lambda: A.activation(out=rst.t[:, :, 1:2], in_=rst.t[:, :, 1:2], func=AF.Exp, scale=-0.5),
                      reads=[], writes=[rst.res])
                sc.op(dve, lambda: V.tensor_tensor(out=Asq.t[:, :, :], in0=A1.t[:, :, :],
                                                   in1=rst.t[:, :, 1:2].to_broadcast([128, 32, 128]), op=ALU.mult),
                      reads=[A1.res, rst.res], writes=[Asq.res])
                sc.op(pool, lambda: G.tensor_tensor(out=osb.t[:, :, :], in0=Asq.t[:, :, :],
                                                    in1=gsub.t[:, :].unsqueeze(1).to_broadcast([128, 32, 128]), op=ALU.mult),
                      reads=[Asq.res, gsub.res], writes=[osb.res])
                sc.dma(pool, odiff_d[:, h * 128:(h + 1) * 128].rearrange("(t p) c -> p t c", p=128), osb.t[:, :, :],
                       reads=[osb.res])
            SB[0] = sbanks
            sc.barrier()

    k.phase2 = phase2
    def phase3():
        with contextlib.ExitStack() as es:
            pbufs = [k.sb(es, f"npb{i}", [128, 512], BF16) for i in range(4)]
            gates = k.sb(es, "ngates", [128, 32, 48], F32)
            sc.dma(sp, gates.t[:, :, :], gates_d.rearrange("(t p) c -> p t c", p=128), writes=[gates.res])
            Kc = k.sb(es, "nKc", [128, 4, 256], BF16)
            Vc = k.sb(es, "nVc", [128, 4, 2, 129], BF16)
            sc.op(pool, lambda: G.memset(Kc.t[:, :, :], 0.0), writes=[Kc.res])
            sc.op(pool, lambda: G.memset(Vc.t[:, :, :, :], 0.0), writes=[Vc.res])
            sc.op(pool, lambda: G.memset(Vc.t[:, :, :, 128:129], 1.0), writes=[Vc.res])
            for g in range(4):
                sc.dma(sp, Kc.t[64:70, g, :], I("augk_cmp")[:, :], writes=[Kc.res])
                sc.dma(sp, Vc.t[:, g, :, 64:128], I("c2s")[:, :, :], writes=[Vc.res])
            with contextlib.ExitStack() as es2:
                w1 = [k.sb(es2, f"cw1{i}", [128, 16, 256], BF16) for i in range(2)]
                w2 = [k.sb(es2, f"cw2{i}", [128, 2, 64], BF16) for i in range(2)]
                posT = [k.sb(es2, f"cpos{i}", [128, 16], BF16) for i in range(2)]
                biasv = k.sb(es2, "cbias", [128, 4], F32)
                srcb = [k.sb(es2, f"csrc{i}", [128, S], BF16) for i in range(2)]
                for b_ in srcb:
                    sc.op(pool, lambda: G.memset(b_.t[64:128, S - 16:S], 0.0), writes=[b_.res])
                hid = [k.sb(es2, f"chid{i}", [128, 2, 256], BF16) for i in range(2)]
                for kv, (n1, n2, npos) in enumerate((("nsa_phi_k1s", "nsa_phi_k2", "nsa_pos_kTs"),
                                                     ("nsa_phi_v1s", "nsa_phi_v2", "nsa_pos_vTs"))):
                    sc.dma(pool, w1[kv].t[:, :, :], I(n1)[:, :, :], writes=[w1[kv].res])
                    sc.dma(pool, w2[kv].t[:, :, :], I(n2).rearrange("(c p) d -> p c d", p=128), writes=[w2[kv].res])
                    sc.dma(pool, posT[kv].t[:, :], I(npos)[:, :], writes=[posT[kv].res])
                for kv in range(2):
                    for hc in range(2):
                        bk = k.nxt("bank", banks)
                        for l in range(16):
                            sc.op(pe, lambda: T.matmul(bk.t[:, 0:1], lhsT=w1[kv].t[:, l, hc * 128:(hc + 1) * 128],
                                                       rhs=posT[kv].t[:, l:l + 1], start=(l == 0), stop=(l == 15)),
                                  reads=[w1[kv].res, posT[kv].res], writes=[bk.res])
                        sc.op(dve, lambda: V.tensor_copy(out=biasv.t[:, kv * 2 + hc:kv * 2 + hc + 1], in_=bk.t[:, 0:1]),
                              reads=[bk.res], writes=[biasv.res])
                for g in range(4):
                    for kv in range(2):
                        sb_ = k.nxt("csrc", srcb)
                        srcd = (kcT if kv == 0 else vcT)
                        sc.dma(sp, sb_.t[0:64, :], srcd[g * 64:(g + 1) * 64, :], writes=[sb_.res])
                        sc.dma(sp, sb_.t[64:128, 0:S - 1], srcd[g * 64:(g + 1) * 64, 1:S], writes=[sb_.res])
                        sv = sb_.t[:, :].rearrange("p (n s) -> p s n", s=16)
                        hb = hid[kv]
                        for hc in range(2):
                            bk = k.nxt("bank", banks)
                            for l2 in range(16):
                                l = 2 * l2
                                sc.op(pe, lambda: T.matmul(bk.t[:, 0:255], lhsT=w1[kv].t[:, l2, hc * 128:(hc + 1) * 128],
                                                           rhs=sv[:, l % 16, (l // 16):(l // 16) + 255],
                                                           start=(l2 == 0), stop=(l2 == 15)),
                                      reads=[w1[kv].res, sb_.res], writes=[bk.res])
                            sc.op(act, lambda: A.activation(out=hb.t[:, hc, 0:255], in_=bk.t[:, 0:255], func=AF.Silu,
                                                            bias=biasv.t[:, kv * 2 + hc:kv * 2 + hc + 1]),
                                  reads=[bk.res, biasv.res], writes=[hb.res])
                        if kv == 0:
                            bk = k.nxt("bank", banks)
                            for hc in range(2):
                                sc.op(pe, lambda: T.matmul(bk.t[0:64, 0:255], lhsT=w2[0].t[:, hc, :], rhs=hb.t[:, hc, 0:255],
                                                           start=(hc == 0), stop=(hc == 1)),
                                      reads=[w2[0].res, hb.res], writes=[bk.res])
                            sc.op(dve, lambda: V.tensor_copy(out=Kc.t[0:64, g, 0:255], in_=bk.t[0:64, 0:255]),
                                  reads=[bk.res], writes=[Kc.res])
                        else:
                            for nt in range(2):
                                m = 128 if nt == 0 else 127
                                bk = k.nxt("bank", banks)
                                for hc in range(2):
                                    sc.op(pe, lambda: T.matmul(bk.t[0:m, 0:64], lhsT=hb.t[:, hc, nt * 128:nt * 128 + m],
                                                               rhs=w2[1].t[:, hc, :], start=(hc == 0), stop=(hc == 1)),
                                          reads=[w2[1].res, hb.res], writes=[bk.res])
                                sc.op(dve, lambda: V.tensor_copy(out=Vc.t[0:m, g, nt, 0:64], in_=bk.t[0:m, 0:64]),
                                      reads=[bk.res], writes=[Vc.res])
                sc.barrier()
            mcmp = k.sb(es, "nmcmp", [128, 2, S], BF16)
            keep = k.sb(es, "nkeep", [128, 32, 64], F32)
            force = k.sb(es, "nforce", [128, 32, 64], F32)
            expand = k.sb(es, "nexpand", [128, S], BF16)
            sc.dma(sp, mcmp.t[:, :, :], I("m_cmp")[:, :, :], writes=[mcmp.res])
            sc.dma(sp, keep.t[:, :, :], I("sel_keep")[:, :, :], writes=[keep.res])
            sc.dma(sp, force.t[:, :, :], I("sel_force")[:, :, :], writes=[force.res])
            sc.op(pool, lambda: G.memset(expand.t[64:128, :], 0.0), writes=[expand.res])
            sc.dma(sp, expand.t[0:64, :], I("expand")[:, :], writes=[expand.res])
            Qb = [k.sb(es, f"nQ{i}", [128, S], BF16) for i in range(2)]
            ksb = k.sb(es, "nks", [128, S], BF16)
            kwb = k.sb(es, "nkw", [128, S], BF16)
            vsb = k.sb(es, "nvs", [128, 32, 65], BF16)
            vwb = k.sb(es, "nvw", [128, 32, 65], BF16)
            for b in (ksb, kwb) + tuple(Qb):
                sc.op(pool, lambda: G.memset(b.t[64:128, :], 0.0), writes=[b.res])
            for b in (ksb, kwb):
                sc.dma(sp, b.t[64:70, :], I("augk_tok")[:, :], writes=[b.res])
            for b in (vsb, vwb):
                sc.op(pool, lambda: G.memset(b.t[:, :, 64:65], 1.0), writes=[b.res])
            selmT = k.sb(es, "nselmT", [128, S], BF16)
            sc.op(pool, lambda: G.memset(selmT.t[64:128, :], 0.0), writes=[selmT.res])
            imp = k.sb(es, "nimp", [128, 32, 64], F32)
            O = k.sb(es, "nO", [128, 32, 256], F32)
            cst2 = [k.sb(es, f"ncst{i}", [128, 32, 129], F32) for i in range(2)]
            cst_res2 = [[Res() for _ in range(32)] for _ in range(2)]
            cdn2 = [k.sb(es, f"ncdn{i}", [128, 32, 4], F32) for i in range(2)]
            ost = k.sb(es, "nost", [128, 32, 256], BF16)
            small = [k.sb(es, f"nsm{i}", [128, 4], F32) for i in range(8)]
            imb = [k.sb(es, f"nim{i}", [128, 64], F32) for i in range(4)]
            im2b = [k.sb(es, f"nim2{i}", [128, 64], F32) for i in range(4)]
            m8 = [k.sb(es, f"nm8{i}", [128, 16], F32) for i in range(4)]
            selb = [k.sb(es, f"nsel{i}", [128, 64], BF16) for i in range(8)]

            cmp_specs = []
            for j in range(8):
                l = [(0, 0, 512, [(ident.t[:, :], mcmp.t[:, 0, j * 512:(j + 1) * 512], 0, 512, [ident.res, mcmp.res])])]
                if j >= 4:
                    l.append((1, 0, 512, [(ident.t[:, :], mcmp.t[:, 1, j * 512:(j + 1) * 512], 0, 512, [ident.res, mcmp.res])]))
                cmp_specs.append(l)
            slc_specs = causal_specs(extra=lambda j, kt, c0, c1: [
                (expand.t[0:128, kt * 128:(kt + 1) * 128], selmT.t[0:128, j * 512 + c0:j * 512 + c1], c0, c1,
                 [expand.res, selmT.res])])
            win_specs = []
            for j in range(8):
                l = []
                for dp in range(4):
                    kt = 4 * j - 4 + dp
                    if kt < 0:
                        continue
                    l.append((kt, 0, 128 * (dp + 1),
                              [(ident.t[:, :], mband.t[:, :], 128 * dp, 128 * dp + 128, [ident.res, mband.res])]))
                for d in range(4):
                    l.append((4 * j + d, 128 * d, 512,
                              [(ident.t[:, :], mcaus.t[:, :], 128 * d, 128 * d + 128, [ident.res, mcaus.res])]))
                win_specs.append(l)

            def load_q(h):
                qb = k.nxt("nQ", Qb)
                sc.dma(sp, qb.t[0:64, :], NqT[h * 64:(h + 1) * 64, :], writes=[qb.res])
                sc.dma(sp, qb.t[64:70, :], I("augq")[8 + h, :, :], writes=[qb.res])
                return qb

            for g in range(4):
                sc.dma(sp, kwb.t[0:64, :], kwT[g * 64:(g + 1) * 64, :], writes=[kwb.res])
                sc.dma(sp, vwb.t[:, :, 0:64], vw_d[:, g * 64:(g + 1) * 64].rearrange("(t p) c -> p t c", p=128), writes=[vwb.res])
                for hh in range(4):
                    h = 4 * g + hh
                    qb = load_q(h)

                    cst, cst_res, cdn = cst2[hh % 2], cst_res2[hh % 2], cdn2[hh % 2]

                    def ev_cmp(qt, ac, h=h, hh=hh, cst=cst, cst_res=cst_res):
                        sc.op(dve, lambda: V.tensor_copy(out=cst.t[:, qt, :], in_=ac.t[:, 0:129]), reads=[ac.res], writes=[cst_res[qt]])

                    attn_pass(es, qb.t, Kc.t[:, g, :], KDIM, Vc.t[:, g, :, :], 129, cmp_specs, ev_cmp,
                              [qb.res, Kc.res, Vc.res], pbufs)
                    sc.op(dve, lambda: V.tensor_scalar(out=cdn.t[:, :, 0:1], in0=cst.t[:, :, 128:129], scalar1=1e-30, scalar2=None,
                                                       op0=ALU.max), reads=cst_res, writes=[cdn.res])
                    sc.op(dve, lambda: V.reciprocal(out=cdn.t[:, :, 1:2], in_=cdn.t[:, :, 0:1]), reads=[], writes=[cdn.res])
                    sc.op(dve, lambda: V.tensor_tensor(out=cdn.t[:, :, 2:3], in0=cdn.t[:, :, 1:2], in1=gates.t[:, :, 3 * h:3 * h + 1],
                                                       op=ALU.mult), reads=[gates.res], writes=[cdn.res])
                    sc.op(dve, lambda: V.tensor_tensor(out=O.t[:, :, hh * 64:(hh + 1) * 64], in0=cst.t[:, :, 0:64],
                                                       in1=cdn.t[:, :, 2:3].to_broadcast([128, 32, 64]), op=ALU.mult),
                          reads=[cdn.res] + cst_res, writes=[O.res])
                    if hh == 0:
                        sc.op(dve, lambda: V.tensor_tensor(out=imp.t[:, :, :], in0=cst.t[:, :, 64:128],
                                                           in1=cdn.t[:, :, 1:2].to_broadcast([128, 32, 64]), op=ALU.mult),
                              reads=[cdn.res] + cst_res, writes=[imp.res])
                    else:
                        sc.op(dve, lambda: V.tensor_tensor(out=cst.t[:, :, 64:128], in0=cst.t[:, :, 64:128],
                                                           in1=cdn.t[:, :, 1:2].to_broadcast([128, 32, 64]), op=ALU.mult),
                              reads=[cdn.res], writes=cst_res)
                        sc.op(dve, lambda: V.tensor_tensor(out=imp.t[:, :, :], in0=imp.t[:, :, :], in1=cst.t[:, :, 64:128], op=ALU.add),
                              reads=cst_res, writes=[imp.res])
                def sel_vec(q8):
                    sls = []
                    for i8 in range(8):
                        qt = q8 * 8 + i8
                        im = k.nxt("nim", imb)
                        im2 = k.nxt("nim2", im2b)
                        mm = k.nxt("nm8", m8)
                        sl = k.nxt("nsel", selb)
                        sls.append(sl)
                        sc.op(pool, lambda: G.tensor_tensor(out=im.t[:, :], in0=imp.t[:, qt, :], in1=keep.t[:, qt, :], op=ALU.mult),
                              reads=[imp.res, keep.res], writes=[im.res])
                        sc.op(pool, lambda: G.tensor_tensor(out=im.t[:, :], in0=im.t[:, :], in1=force.t[:, qt, :], op=ALU.add),
                              reads=[force.res], writes=[im.res])
                        sc.op(dve, lambda: V.max(out=mm.t[:, 0:8], in_=im.t[:, :]), reads=[im.res], writes=[mm.res])
                        sc.op(dve, lambda: V.match_replace(out=im2.t[:, :], in_to_replace=mm.t[:, 0:8], in_values=im.t[:, :],
                                                           imm_value=-3.0e38), reads=[im.res, mm.res], writes=[im2.res])
                        sc.op(dve, lambda: V.max(out=mm.t[:, 8:16], in_=im2.t[:, :]), reads=[im2.res], writes=[mm.res])
                        sc.op(dve, lambda: V.tensor_scalar(out=sl.t[:, :], in0=im.t[:, :], scalar1=mm.t[:, 15:16], scalar2=NEGB,
                                                           op0=ALU.is_lt, op1=ALU.mult), reads=[im.res, mm.res], writes=[sl.res])
                    return sls

                def sel_pe(q8, sls):
                    for i8, sl in enumerate(sls):
                        sc.op(pe, lambda: T.transpose(out=tbank.t[0:64, i8 * 128:(i8 + 1) * 128], in_=sl.t[:, :],
                                                      identity=ident.t[:, :]), reads=[sl.res, ident.res], writes=[tbank.res])
                    sc.op(dve, lambda: V.tensor_copy(out=selmT.t[0:64, q8 * 1024:(q8 + 1) * 1024], in_=tbank.t[0:64, :]),
                          reads=[tbank.res], writes=[selmT.res])

                for hh in range(4):
                    h = 4 * g + hh
                    qb = load_q(h)
                    sls = sel_vec(hh)

                    def ev_win(qt, ac, h=h, hh=hh):
                        sm = k.nxt("nsm", small)
                        sc.op(dve, lambda: V.reciprocal(out=sm.t[:, 1:2], in_=ac.t[:, 64:65]), reads=[ac.res], writes=[sm.res])
                        sc.op(dve, lambda: V.tensor_tensor(out=sm.t[:, 2:3], in0=sm.t[:, 1:2],
                                                           in1=gates.t[:, qt, 3 * h + 2:3 * h + 3], op=ALU.mult),
                              reads=[sm.res, gates.res], writes=[sm.res])
                        sc.op(dve, lambda: V.scalar_tensor_tensor(out=O.t[:, qt, hh * 64:(hh + 1) * 64], in0=ac.t[:, 0:64],
                                                                  scalar=sm.t[:, 2:3], in1=O.t[:, qt, hh * 64:(hh + 1) * 64],
                                                                  op0=ALU.mult, op1=ALU.add),
                              reads=[ac.res, sm.res], writes=[O.res])

                    attn_pass(es, qb.t, kwb.t, KDIM, vwb.t, 65, win_specs, ev_win, [qb.res, kwb.res, vwb.res], pbufs)
                    sel_pe(hh, sls)
                sc.dma(sp, ksb.t[0:64, :], ksT[g * 64:(g + 1) * 64, :], writes=[ksb.res])
                sc.dma(sp, vsb.t[:, :, 0:64], vs_d[:, g * 64:(g + 1) * 64].rearrange("(t p) c -> p t c", p=128), writes=[vsb.res])
                for hh in range(4):
                    h = 4 * g + hh
                    qb = load_q(h)

                    def ev_slc(qt, ac, h=h, hh=hh):
                        sm = k.nxt("nsm", small)
                        sc.op(dve, lambda: V.reciprocal(out=sm.t[:, 1:2], in_=ac.t[:, 64:65]), reads=[ac.res], writes=[sm.res])
                        sc.op(dve, lambda: V.tensor_tensor(out=sm.t[:, 2:3], in0=sm.t[:, 1:2],
                                                           in1=gates.t[:, qt, 3 * h + 1:3 * h + 2], op=ALU.mult),
                              reads=[sm.res, gates.res], writes=[sm.res])
                        sc.op(dve, lambda: V.scalar_tensor_tensor(out=ost.t[:, qt, hh * 64:(hh + 1) * 64], in0=ac.t[:, 0:64],
                                                                  scalar=sm.t[:, 2:3], in1=O.t[:, qt, hh * 64:(hh + 1) * 64],
                                                                  op0=ALU.mult, op1=ALU.add),
                              reads=[ac.res, sm.res, O.res], writes=[ost.res])

                    attn_pass(es, qb.t, ksb.t, KDIM, vsb.t, 65, slc_specs, ev_slc, [qb.res, ksb.res, vsb.res], pbufs)
                sc.dma(pool, onsa_d[:, g * 256:(g + 1) * 256].rearrange("(t p) c -> p t c", p=128), ost.t[:, :, :],
                       reads=[ost.res])
            sc.barrier()

    k.phase3 = phase3
    def layer_norm_rows(src, dst_tmp, gb, bb, out_ap, smalls, writes_res, out_reads=()):
        st = k.nxt("lnst", smalls)
        sc.op(dve, lambda: V.bn_stats(out=st.t[:, 0:6], in_=src.t[:, 0:512]), reads=[src.res], writes=[st.res])
        sc.op(dve, lambda: V.bn_stats(out=st.t[:, 6:12], in_=src.t[:, 512:1024]), reads=[src.res], writes=[st.res])
        sc.op(dve, lambda: V.bn_aggr(out=st.t[:, 12:14], in_=st.t[:, 0:12]), reads=[st.res], writes=[st.res])
        sc.op(act, lambda: A.activation(out=st.t[:, 14:15], in_=st.t[:, 13:14], func=AF.Ln, bias=epsb.t[:, 0:1], scale=1.0),
              reads=[st.res, epsb.res], writes=[st.res])
        sc.op(act, lambda: A.activation(out=st.t[:, 14:15], in_=st.t[:, 14:15], func=AF.Exp, scale=-0.5),
              reads=[st.res], writes=[st.res])
        sc.op(dve, lambda: V.tensor_scalar(out=dst_tmp.t[:, :], in0=src.t[:, :], scalar1=st.t[:, 12:13], scalar2=st.t[:, 14:15],
                                           op0=ALU.subtract, op1=ALU.mult), reads=[src.res, st.res], writes=[dst_tmp.res])
        sc.op(dve, lambda: V.tensor_tensor(out=dst_tmp.t[:, :], in0=dst_tmp.t[:, :], in1=gb.t[:, :], op=ALU.mult),
              reads=[gb.res], writes=[dst_tmp.res])
        sc.op(dve, lambda: V.tensor_tensor(out=out_ap, in0=dst_tmp.t[:, :], in1=bb.t[:, :], op=ALU.add),
              reads=[dst_tmp.res, bb.res] + list(out_reads), writes=writes_res)

    def phase4():
        with contextlib.ExitStack() as es:
            def wload(name, src_ap, kc, n):
                b = k.sb(es, name, [128, kc, n], BF16)
                sc.dma(pool, b.t[:, :, :], src_ap.rearrange("(kc p) n -> p kc n", p=128), writes=[b.res])
                return b
            Wmg = wload("Wmg", I("w_mgate"), 8, 2048)
            Wd = wload("Wd", I("w_br_diff"), 8, 1024)
            Wn = wload("Wn", I("w_br_nsa"), 8, 1024)
            Wo = wload("Wo", I("w_o"), 8, 1024)
            Wpg = wload("Wpg", I("w_ple_gate"), 8, 1024)
            Wpp = wload("Wpp", I("w_ple_proj"), 2, 1024)
            Wr = wload("Wr", I("w_router"), 8, 32)
            bmg = k.sb(es, "bmg", [1, 2048], BF16); sc.dma(pool, bmg.t[:, :], I("b_mgate")[:, :], writes=[bmg.res])
            bpg = k.sb(es, "bpg", [1, 1024], BF16); sc.dma(pool, bpg.t[:, :], I("b_ple_gate")[:, :], writes=[bpg.res])
            brt = k.sb(es, "brt", [1, 32], BF16); sc.dma(pool, brt.t[:, :], I("b_router")[:, :], writes=[brt.res])
            b2a = k.sb(es, "b2a", [32, 1024], BF16); sc.dma(pool, b2a.t[:, :], I("b_e2")[:, :], writes=[b2a.res])
            ones = k.sb(es, "ones", [1, 128], BF16); sc.op(pool, lambda: G.memset(ones.t[:, :], 1.0), writes=[ones.res])
            g1b = bcast_load(es, "g1b", I("ln1_g")[:, :], 1024)
            b1b = bcast_load(es, "b1b", I("ln1_b")[:, :], 1024)

            def rot(name, shape, dt, n=2):
                return [k.sb(es, f"{name}{i}", shape, dt) for i in range(n)]
            od_b = rot("od", [128, 1024], BF16); on_b = rot("on", [128, 1024], BF16)
            odT_b = rot("odT", [128, 1024], BF16, 1); onT_b = rot("onT", [128, 1024], BF16, 1)
            xt_b = rot("xt", [128, 1024], F32); xTt_b = rot("xTt", [128, 8, 128], BF16)
            pTt_b = rot("pTt", [128, 2, 128], BF16)
            sgd_b = rot("sgd", [128, 1024], F32, 1); sgn_b = rot("sgn", [128, 1024], F32, 1)
            mixbf_b = rot("mixbf", [128, 1024], BF16); mixT_b = rot("mixT", [128, 1024], BF16, 1)
            r_b = rot("rr", [128, 1024], F32, 1); h_b = rot("hh", [128, 1024], F32)
            hbf_b = rot("hbf", [128, 1024], BF16); hT_b = rot("hTt", [128, 1024], BF16)
            spg_b = rot("spg", [128, 1024], F32, 1); base_b = rot("base", [128, 1024], F32)
            lnst = rot("lnst", [128, 16], F32, 4)
            lg_b = rot("lg", [128, 32], F32); e_b = rot("eb", [128, 32], F32); msk_b = rot("msk", [128, 32], F32)
            m8_b = rot("rm8", [128, 8], F32); rs_b = rot("rs", [128, 2], F32)
            gt_b = rot("gt", [128, 32], F32); gtbf_b = rot("gtbf", [128, 32], BF16); gT_b = rot("gT", [32, 128], BF16)
            st = {}
            triu = k.sb(es, "triu", [128, 128], BF16); sc.dma(sp, triu.t[:, :], I("triu")[:, :], writes=[triu.res])
            ones128 = k.sb(es, "ones128", [128, 128], BF16); sc.dma(sp, ones128.t[:, :], I("ones128")[:, :], writes=[ones128.res])
            iota32 = k.sb(es, "iota32", [128, 32], F32); sc.dma(sp, iota32.t[:, :], I("iota32")[:, :], writes=[iota32.res])
            trp = k.sb(es, "trp", [128, 1], F32); sc.dma(sp, trp.t[:, :], I("trp")[:, :], writes=[trp.res])
            cumrep = k.sb(es, "cumrep", [128, 32], F32); sc.op(pool, lambda: G.memset(cumrep.t[:, :], 0.0), writes=[cumrep.res])
            idxu_b = rot("idxu", [128, 8], U32); idxf_b = rot("idxf", [128, 4], F32); mkbf_b = rot("mkbf", [128, 32], BF16)
            rank_b = rot("rank", [128, 32], F32); oh_b = rot("oh", [128, 32], F32); rk_b = rot("rk", [128, 4], F32)
            val_b = rot("val", [128, 4], F32); t1_b = rot("t1", [128, 4], F32); e4_b = rot("e4", [128, 4], F32)

            def transposes(src, dst, eng):
                for i in range(8):
                    sc.op(pe, lambda: T.transpose(out=tbank.t[:, i * 128:(i + 1) * 128], in_=src.t[:, i * 128:(i + 1) * 128],
                                                  identity=ident.t[:, :]), reads=[src.res, ident.res], writes=[tbank.res])
                if eng is act:
                    sc.op(act, lambda: A.copy(out=dst.t[:, :], in_=tbank.t[:, :]), reads=[tbank.res], writes=[dst.res])
                else:
                    sc.op(dve, lambda: V.tensor_copy(out=dst.t[:, :], in_=tbank.t[:, :]), reads=[tbank.res], writes=[dst.res])

            def mm_tok(lhs_fn, nk, W, c0, bias=None, lhs_reads=(), n=512):
                bk = k.nxt("bank", banks)
                for kc in range(nk):
                    sc.op(pe, lambda: T.matmul(bk.t[:, 0:n], lhsT=lhs_fn(kc), rhs=W.t[:, kc, c0:c0 + n],
                                               start=(kc == 0), stop=(kc == nk - 1 and bias is None)),
                          reads=[W.res] + list(lhs_reads), writes=[bk.res])
                if bias is not None:
                    sc.op(pe, lambda: T.matmul(bk.t[:, 0:n], lhsT=ones.t[0:1, :], rhs=bias.t[0:1, c0:c0 + n],
                                               start=False, stop=True), reads=[ones.res, bias.res], writes=[bk.res])
                return bk

            def stage_a(tt):
                d = st[tt] = {}
                od = d["od"] = k.nxt("od", od_b); on = d["on"] = k.nxt("on", on_b)
                xt = d["xt"] = k.nxt("xt", xt_b); xTt = k.nxt("xTt", xTt_b); pTt = d["pTt"] = k.nxt("pTt", pTt_b)
                rows = slice(tt * 128, (tt + 1) * 128)
                sc.dma(sp, od.t[:, :], odiff_d[rows, :], writes=[od.res])
                sc.dma(sp, on.t[:, :], onsa_d[rows, :], writes=[on.res])
                sc.dma(sp, xt.t[:, :], I("x")[rows, :], writes=[xt.res])
                sc.dma(pool, xTt.t[:, :, :], I("xT")[:, rows].rearrange("(kc p) t -> p kc t", p=128), writes=[xTt.res])
                sc.dma(pool, pTt.t[:, :, :], I("pT")[:, rows].rearrange("(kc p) t -> p kc t", p=128), writes=[pTt.res])
                odT = k.nxt("odT", odT_b); onT = k.nxt("onT", onT_b)
                sgd = k.nxt("sgd", sgd_b); sgn = k.nxt("sgn", sgn_b)
                mix = sgd; t2 = sgn; mixbf = d["mixbf"] = k.nxt("mixbf", mixbf_b)
                for ch in range(4):
                    bk = mm_tok(lambda kc: xTt.t[:, kc, :], 8, Wmg, ch * 512, bias=bmg, lhs_reads=[xTt.res])
                    dst = sgd if ch < 2 else sgn
                    sc.op(act, lambda: A.activation(out=dst.t[:, (ch % 2) * 512:(ch % 2) * 512 + 512], in_=bk.t[:, :],
                                                    func=AF.Sigmoid), reads=[bk.res], writes=[dst.res])
                transposes(od, odT, act)
                transposes(on, onT, dve)
                for ch in range(2):
                    bk = mm_tok(lambda kc: odT.t[:, kc * 128:(kc + 1) * 128], 8, Wd, ch * 512, lhs_reads=[odT.res])
                    sc.op(dve, lambda: V.tensor_tensor(out=mix.t[:, ch * 512:(ch + 1) * 512], in0=sgd.t[:, ch * 512:(ch + 1) * 512],
                                                       in1=bk.t[:, :], op=ALU.mult), reads=[sgd.res, bk.res], writes=[mix.res])
                for ch in range(2):
                    bk = mm_tok(lambda kc: onT.t[:, kc * 128:(kc + 1) * 128], 8, Wn, ch * 512, lhs_reads=[onT.res])
                    sc.op(dve, lambda: V.tensor_tensor(out=t2.t[:, ch * 512:(ch + 1) * 512], in0=sgn.t[:, ch * 512:(ch + 1) * 512],
                                                       in1=bk.t[:, :], op=ALU.mult), reads=[sgn.res, bk.res], writes=[t2.res])
                sc.op(pool, lambda: G.tensor_tensor(out=mixbf.t[:, :], in0=mix.t[:, :], in1=t2.t[:, :], op=ALU.add),
                      reads=[mix.res, t2.res], writes=[mixbf.res])

            def stage_b(tt):
                d = st[tt]
                mixT = k.nxt("mixT", mixT_b); r = k.nxt("rr", r_b); h = d["h"] = k.nxt("hh", h_b)
                hbf = d["hbf"] = k.nxt("hbf", hbf_b); hT = d["hT"] = k.nxt("hTt", hT_b)
                xt = d["xt"]
                transposes(d["mixbf"], mixT, act)
                for ch in range(2):
                    bk = mm_tok(lambda kc: mixT.t[:, kc * 128:(kc + 1) * 128], 8, Wo, ch * 512, lhs_reads=[mixT.res])
                    sc.op(dve, lambda: V.scalar_tensor_tensor(out=r.t[:, ch * 512:(ch + 1) * 512], in0=xt.t[:, ch * 512:(ch + 1) * 512],
                                                              scalar=ALPHA, in1=bk.t[:, :], op0=ALU.mult, op1=ALU.add),
                          reads=[xt.res, bk.res], writes=[r.res])
                layer_norm_rows(r, r, g1b, b1b, h.t[:, :], lnst, [h.res])
                sc.op(act, lambda: A.copy(out=hbf.t[:, :], in_=h.t[:, :]), reads=[h.res], writes=[hbf.res])

            def stage_b2(tt):
                d = st[tt]
                hbf, hT = d["hbf"], d["hT"]
                transposes(hbf, hT, dve)
                sc.dma(sp, hT_d[:, tt * 128:(tt + 1) * 128].rearrange("(kc p) t -> p kc t", p=128),
                       hT.t[:, :].rearrange("p (kc t) -> p kc t", kc=8), reads=[hT.res])

            def stage_c(tt):
                d = st[tt]
                h, hT, pTt = d["h"], d["hT"], d["pTt"]
                spg = k.nxt("spg", spg_b); base = d["base"] = k.nxt("base", base_b)
                rows = slice(tt * 128, (tt + 1) * 128)
                for ch in range(2):
                    bk = mm_tok(lambda kc: hT.t[:, kc * 128:(kc + 1) * 128], 8, Wpg, ch * 512, bias=bpg, lhs_reads=[hT.res])
                    sc.op(act, lambda: A.activation(out=spg.t[:, ch * 512:(ch + 1) * 512], in_=bk.t[:, :], func=AF.Sigmoid),
                          reads=[bk.res], writes=[spg.res])
                for ch in range(2):
                    bk = mm_tok(lambda kc: pTt.t[:, kc, :], 2, Wpp, ch * 512, lhs_reads=[pTt.res])
                    sc.op(dve, lambda: V.tensor_tensor(out=spg.t[:, ch * 512:(ch + 1) * 512], in0=spg.t[:, ch * 512:(ch + 1) * 512],
                                                       in1=bk.t[:, :], op=ALU.mult), reads=[bk.res], writes=[spg.res])
                sc.op(dve, lambda: V.scalar_tensor_tensor(out=base.t[:, :], in0=h.t[:, :], scalar=ALPHA, in1=spg.t[:, :],
                                                          op0=ALU.mult, op1=ALU.add), reads=[h.res, spg.res], writes=[base.res])
                bk = mm_tok(lambda kc: hT.t[:, kc * 128:(kc + 1) * 128], 8, Wr, 0, bias=brt, lhs_reads=[hT.res], n=32)
                lg = k.nxt("lg", lg_b); e_ = k.nxt("eb", e_b); mk = k.nxt("msk", msk_b); m8_ = k.nxt("rm8", m8_b)
                rs = k.nxt("rs", rs_b); gt = k.nxt("gt", gt_b); gtbf = k.nxt("gtbf", gtbf_b); gT = k.nxt("gT", gT_b)
                sc.op(dve, lambda: V.tensor_copy(out=lg.t[:, :], in_=bk.t[:, 0:32]), reads=[bk.res], writes=[lg.res])
                sc.op(dve, lambda: V.max(out=m8_.t[:, 0:8], in_=lg.t[:, :]), reads=[lg.res], writes=[m8_.res])
                sc.op(dve, lambda: V.tensor_scalar(out=e_.t[:, :], in0=lg.t[:, :], scalar1=m8_.t[:, 0:1], scalar2=None,
                                                   op0=ALU.subtract), reads=[lg.res, m8_.res], writes=[e_.res])
                sc.op(act, lambda: A.activation(out=e_.t[:, :], in_=e_.t[:, :], func=AF.Exp), reads=[], writes=[e_.res])
                sc.op(dve, lambda: V.tensor_scalar(out=mk.t[:, :], in0=lg.t[:, :], scalar1=m8_.t[:, 3:4], scalar2=None,
                                                   op0=ALU.is_ge), reads=[lg.res, m8_.res], writes=[mk.res])
                sc.op(dve, lambda: V.tensor_tensor(out=e_.t[:, :], in0=e_.t[:, :], in1=mk.t[:, :], op=ALU.mult),
                      reads=[mk.res], writes=[e_.res])
                sc.op(dve, lambda: V.reduce_sum(out=rs.t[:, 0:1], in_=e_.t[:, :], axis=AX.X), reads=[e_.res], writes=[rs.res])
                sc.op(dve, lambda: V.reciprocal(out=rs.t[:, 1:2], in_=rs.t[:, 0:1]), reads=[], writes=[rs.res])
                sc.op(dve, lambda: V.tensor_scalar(out=gt.t[:, :], in0=e_.t[:, :], scalar1=rs.t[:, 1:2], scalar2=None,
                                                   op0=ALU.mult), reads=[e_.res, rs.res], writes=[gt.res])
                idxu = k.nxt("idxu", idxu_b); idxf = d["idxf"] = k.nxt("idxf", idxf_b); mkbf = d["mkbf"] = k.nxt("mkbf", mkbf_b)
                e4 = d["e4"] = k.nxt("e4", e4_b)
                d["rs"] = rs; d["gtbf"] = gtbf; d["gT"] = gT
                sc.op(dve, lambda: V.max_index(out=idxu.t[:, :], in_max=m8_.t[:, :], in_values=lg.t[:, :]),
                      reads=[m8_.res, lg.res], writes=[idxu.res])
                sc.op(dve, lambda: V.tensor_copy(out=idxf.t[:, :], in_=idxu.t[:, 0:4]), reads=[idxu.res], writes=[idxf.res])
                sc.op(pool, lambda: G.tensor_copy(out=mkbf.t[:, :], in_=mk.t[:, :]), reads=[mk.res], writes=[mkbf.res])
                sc.op(pool, lambda: G.tensor_copy(out=gtbf.t[:, :], in_=gt.t[:, :]), reads=[gt.res], writes=[gtbf.res])
                sc.op(dve, lambda: V.tensor_scalar(out=e4.t[:, :], in0=m8_.t[:, 0:4], scalar1=m8_.t[:, 0:1], scalar2=None,
                                                   op0=ALU.subtract), reads=[m8_.res], writes=[e4.res])
                sc.op(act, lambda: A.activation(out=e4.t[:, :], in_=e4.t[:, :], func=AF.Exp), reads=[], writes=[e4.res])

            def stage_d1(tt):
                d = st[tt]
                idxf, mkbf, e4, rs, gtbf, gT, hbf = d["idxf"], d["mkbf"], d["e4"], d["rs"], d["gtbf"], d["gT"], d["hbf"]
                rank = k.nxt("rank", rank_b); oh = k.nxt("oh", oh_b); rk = k.nxt("rk", rk_b)
                val = k.nxt("val", val_b); t1 = k.nxt("t1", t1_b)
                bkr = k.nxt("bank", banks)
                sc.op(pe, lambda: T.matmul(bkr.t[:, 0:32], lhsT=triu.t[:, :], rhs=mkbf.t[:, :], start=True, stop=True),
                      reads=[triu.res, mkbf.res], writes=[bkr.res])
                bkc = k.nxt("bank", banks)
                sc.op(pe, lambda: T.matmul(bkc.t[:, 0:32], lhsT=ones128.t[:, :], rhs=mkbf.t[:, :], start=True, stop=True),
                      reads=[ones128.res, mkbf.res], writes=[bkc.res])
                sc.op(pe, lambda: T.transpose(out=tbank.t[0:32, 0:128], in_=gtbf.t[:, :], identity=ident.t[:, :]),
                      reads=[gtbf.res, ident.res], writes=[tbank.res])
                sc.op(act, lambda: A.copy(out=gT.t[:, :], in_=tbank.t[0:32, 0:128]), reads=[tbank.res], writes=[gT.res])
                sc.op(dve, lambda: V.tensor_tensor(out=rank.t[:, :], in0=cumrep.t[:, :], in1=bkr.t[:, 0:32], op=ALU.add),
                      reads=[cumrep.res, bkr.res], writes=[rank.res])
                sc.op(dve, lambda: V.tensor_tensor(out=cumrep.t[:, :], in0=cumrep.t[:, :], in1=bkc.t[:, 0:32], op=ALU.add),
                      reads=[bkc.res], writes=[cumrep.res])
                for kk in range(4):
                    sc.op(dve, lambda: V.tensor_scalar(out=oh.t[:, :], in0=iota32.t[:, :], scalar1=idxf.t[:, kk:kk + 1], scalar2=None,
                                                       op0=ALU.is_equal), reads=[iota32.res, idxf.res], writes=[oh.res])
                    sc.op(dve, lambda: V.tensor_tensor(out=oh.t[:, :], in0=oh.t[:, :], in1=rank.t[:, :], op=ALU.mult),
                          reads=[rank.res], writes=[oh.res])
                    sc.op(dve, lambda: V.reduce_sum(out=rk.t[:, kk:kk + 1], in_=oh.t[:, :], axis=AX.X), reads=[oh.res], writes=[rk.res])
                sc.op(dve, lambda: V.tensor_scalar(out=val.t[:, :], in0=rk.t[:, :], scalar1=float(CAP), scalar2=None, op0=ALU.is_lt),
                      reads=[rk.res], writes=[val.res])
                sc.op(dve, lambda: V.scalar_tensor_tensor(out=t1.t[:, :], in0=idxf.t[:, :], scalar=float(CAP), in1=rk.t[:, :],
                                                          op0=ALU.mult, op1=ALU.add), reads=[idxf.res, rk.res], writes=[t1.res])
                sc.op(dve, lambda: V.scalar_tensor_tensor(out=t1.t[:, :], in0=t1.t[:, :], scalar=trp.t[:, 0:1], in1=val.t[:, :],
                                                          op0=ALU.subtract, op1=ALU.mult), reads=[trp.res, val.res], writes=[t1.res])
                sc.op(dve, lambda: V.tensor_scalar(out=dest_all.t[:, tt, :], in0=t1.t[:, :], scalar1=trp.t[:, 0:1], scalar2=None,
                                                   op0=ALU.add), reads=[t1.res, trp.res], writes=[dest_res[tt]])
                sc.op(dve, lambda: V.scalar_tensor_tensor(out=gk_all.t[:, tt, :], in0=e4.t[:, :], scalar=rs.t[:, 1:2], in1=val.t[:, :],
                                                          op0=ALU.mult, op1=ALU.mult), reads=[e4.res, rs.res, val.res], writes=[gk_res[tt]])
                for kk in range(4):
                    sc.dma(pool, lambda: G.indirect_dma_start(
                        out=xs_d[:, :], out_offset=bass.IndirectOffsetOnAxis(ap=dest_all.t[:, tt, kk:kk + 1], axis=0),
                        in_=hbf.t[:, :], in_offset=None), None, reads=[hbf.res, dest_res[tt]])

            def stage_d2(tt):
                d = st.pop(tt)
                gT, base = d["gT"], d["base"]
                rows = slice(tt * 128, (tt + 1) * 128)
                for ch in range(2):
                    bk2 = k.nxt("bank", banks)
                    sc.op(pe, lambda: T.matmul(bk2.t[:, :], lhsT=gT.t[0:32, :], rhs=b2a.t[0:32, ch * 512:(ch + 1) * 512],
                                               start=True, stop=True), reads=[gT.res, b2a.res], writes=[bk2.res])
                    sc.op(dve, lambda: V.tensor_tensor(out=base.t[:, ch * 512:(ch + 1) * 512], in0=base.t[:, ch * 512:(ch + 1) * 512],
                                                       in1=bk2.t[:, :], op=ALU.add), reads=[bk2.res], writes=[base.res])
                sc.dma(sp, base_d[rows, :], base.t[:, :], reads=[base.res])

            for it in range(NQT + 3):
                if 0 <= it - 3 < NQT:
                    stage_d1(it - 3)
                if 0 <= it - 2 < NQT:
                    stage_c(it - 2)
                if 0 <= it - 1 < NQT:
                    stage_b(it - 1)
                if it < NQT:
                    stage_a(it)
                if 0 <= it - 1 < NQT:
                    stage_b2(it - 1)
                if 0 <= it - 3 < NQT:
                    stage_d2(it - 3)
            sc.barrier()

    k.phase4 = phase4
    def phase5():
        NST = CAP // 128
        groups = [(g0, min(512, CAP - g0)) for g0 in range(0, CAP, 512)]
        with contextlib.ExitStack() as es:
            W1b = [k.sb(es, f"W1b{i}", [128, 8, 2048], BF16) for i in range(2)]
            W1res = [[Res() for _ in range(4)] for _ in range(2)]
            W2b = [k.sb(es, f"W2b{i}", [128, 8, 1024], BF16) for i in range(2)]
            W2res = [[Res() for _ in range(2)] for _ in range(2)]
            stg = [k.sb(es, f"wstg{i}", [128, 8, 512], F32) for i in range(2)]
            xTe2 = [k.sb(es, f"xTe{i}", [128, 8, CAP], BF16) for i in range(2)]
            actT = k.sb(es, "actT", [128, 8, CAP], BF16)
            xsl = [k.sb(es, f"xsl{i}", [128, 1024], BF16) for i in range(4)]
            yst = [k.sb(es, f"yst{i}", [128, 1024], F32) for i in range(2)]
            b1T = k.sb(es, "b1T", [128, 512], F32)
            sc.dma(sp, b1T.t[:, :], I("b_e1T")[:, :], writes=[b1T.res])
            gt_ = [k.sb(es, f"mg{i}", [128, 512], F32) for i in range(3)]
            sg_ = [k.sb(es, f"msg{i}", [128, 512], F32) for i in range(3)]
            lt_ = [k.sb(es, f"ml{i}", [128, 512], F32) for i in range(3)]
            w_e1, w_e2 = I("w_e1"), I("w_e2")
            sc.op(pool, lambda: G.memset(yst[0].t[:, :], 0.0), writes=[yst[0].res])
            sc.dma(sp, ys_d[32 * CAP:32 * CAP + 128, :], yst[0].t[:, :], reads=[yst[0].res])

            wseq = []
            for e_ in range(32):
                for blk in (0, 2, 1, 3):
                    wseq.append((e_, 1, blk))
                for blk in range(2):
                    wseq.append((e_, 2, blk))
            wstate = {"dma": 0, "cast": 0}

            def w_dma():
                n = wstate["dma"]
                if n >= len(wseq):
                    return
                wstate["dma"] += 1
                e_, kind, blk = wseq[n]
                s_ = stg[n % 2]
                src = (w_e1 if kind == 1 else w_e2)[e_, :, blk * 512:(blk + 1) * 512].rearrange("(kc p) n -> p kc n", p=128)
                sc.dma(sp, s_.t[:, :, :], src, writes=[s_.res])

            NPIECE = 2

            def w_cast_piece():
                n = wstate["cast"]
                if n >= len(wseq):
                    return
                j = wstate.get("piece", 0)
                e_, kind, blk = wseq[n]
                s_ = stg[n % 2]
                if kind == 1:
                    dst, rs_ = W1b[e_ % 2], W1res[e_ % 2][blk]
                else:
                    dst, rs_ = W2b[e_ % 2], W2res[e_ % 2][blk]
                kc0 = j * 4
                sc.op(act, lambda: A.copy(out=dst.t[:, kc0:kc0 + 4, blk * 512:(blk + 1) * 512], in_=s_.t[:, kc0:kc0 + 4, :]),
                      reads=[s_.res], writes=[rs_])
                if j + 1 == NPIECE:
                    wstate["piece"] = 0
                    wstate["cast"] += 1
                    w_dma()
                else:
                    wstate["piece"] = j + 1

            def w_cast():
                for _ in range(NPIECE):
                    w_cast_piece()

            w_dma(); w_dma()
            for _ in range(6):
                w_cast()
            def xs_tile(e_, s_i):
                xt_ = xTe2[e_ % 2]
                xl = k.nxt("xsl", xsl)
                r0 = e_ * CAP + s_i * 128
                sc.dma(sp, xl.t[:, :], xs_d[r0:r0 + 128, :], writes=[xl.res])
                for i in range(8):
                    sc.op(pe, lambda: T.transpose(out=tbank.t[:, i * 128:(i + 1) * 128], in_=xl.t[:, i * 128:(i + 1) * 128],
                                                  identity=ident.t[:, :]), reads=[xl.res, ident.res], writes=[tbank.res])
                sc.op(dve, lambda: V.tensor_copy(out=xt_.t[:, :, s_i * 128:(s_i + 1) * 128],
                                                 in_=tbank.t[:, :].rearrange("p (kc t) -> p kc t", kc=8)),
                      reads=[tbank.res], writes=[xt_.res])

            for s_i in range(NST):
                xs_tile(0, s_i)
            for e in range(32):
                w1 = W1b[e % 2]
                xTe = xTe2[e % 2]
                gi = 0
                for (g0, gn) in groups:
                    for fc in range(8):
                        if gi in (0, 2, 3, 5, 6, 8, 9, 11, 12, 14):
                            w_cast_piece()
                        gi += 1
                        bg = k.nxt("bank", banks)
                        bl = k.nxt("bank", banks)
                        for (bk, c0) in ((bg, fc * 128), (bl, 1024 + fc * 128)):
                            blk = c0 // 512
                            for kc in range(8):
                                sc.op(pe, lambda: T.matmul(bk.t[:, 0:gn], lhsT=w1.t[:, kc, c0:c0 + 128],
                                                           rhs=xTe.t[:, kc, g0:g0 + gn], start=(kc == 0), stop=(kc == 7)),
                                      reads=[W1res[e % 2][blk], xTe.res], writes=[bk.res])
                        g_ = k.nxt("mg", gt_); s_ = k.nxt("msg", sg_); l_ = k.nxt("ml", lt_)
                        cg = e * 16 + fc
                        cl = e * 16 + 8 + fc
                        sc.op(dve, lambda: V.tensor_scalar(out=g_.t[:, 0:gn], in0=bg.t[:, 0:gn], scalar1=b1T.t[:, cg:cg + 1], scalar2=7.0,
                                                           op0=ALU.add, op1=ALU.min), reads=[bg.res, b1T.res], writes=[g_.res])
                        sc.op(act, lambda: A.activation(out=s_.t[:, 0:gn], in_=g_.t[:, 0:gn], func=AF.Sigmoid, scale=1.702),
                              reads=[g_.res], writes=[s_.res])
                        sc.op(dve, lambda: V.tensor_scalar(out=l_.t[:, 0:gn], in0=bl.t[:, 0:gn], scalar1=b1T.t[:, cl:cl + 1], scalar2=-7.0,
                                                           op0=ALU.add, op1=ALU.max), reads=[bl.res, b1T.res], writes=[l_.res])
                        sc.op(dve, lambda: V.tensor_scalar(out=l_.t[:, 0:gn], in0=l_.t[:, 0:gn], scalar1=7.0, scalar2=1.0,
                                                           op0=ALU.min, op1=ALU.add), reads=[], writes=[l_.res])
                        sc.op(dve, lambda: V.tensor_tensor(out=g_.t[:, 0:gn], in0=g_.t[:, 0:gn], in1=s_.t[:, 0:gn], op=ALU.mult),
                              reads=[s_.res], writes=[g_.res])
                        sc.op(pool, lambda: G.tensor_tensor(out=actT.t[:, fc, g0:g0 + gn], in0=g_.t[:, 0:gn], in1=l_.t[:, 0:gn], op=ALU.mult),
                              reads=[g_.res, l_.res], writes=[actT.res])
                for s_i in range(NST):
                    if s_i in (1, 3):
                        w_cast_piece()
                    ys_ = k.nxt("yst", yst)
                    for ch in range(2):
                        bk = k.nxt("bank", banks)
                        for fc in range(8):
                            sc.op(pe, lambda: T.matmul(bk.t[:, :], lhsT=actT.t[:, fc, s_i * 128:(s_i + 1) * 128],
                                                       rhs=W2b[e % 2].t[:, fc, ch * 512:(ch + 1) * 512], start=(fc == 0), stop=(fc == 7)),
                                  reads=[actT.res, W2res[e % 2][ch]], writes=[bk.res])
                        evac_copy(ys_.t[:, ch * 512:(ch + 1) * 512], bk.t[:, :], [bk.res], [ys_.res])
                    r0 = e * CAP + s_i * 128
                    sc.dma(act, ys_d[r0:r0 + 128, :], ys_.t[:, :], reads=[ys_.res])
                    if e + 1 < 32:
                        xs_tile(e + 1, s_i)
            sc.barrier()
        with contextlib.ExitStack() as es:
            g2b = bcast_load(es, "g2b", I("ln2_g")[:, :], 1024)
            b2b = bcast_load(es, "b2b", I("ln2_b")[:, :], 1024)
            accb = [k.sb(es, f"cacc{i}", [128, 1024], F32) for i in range(4)]
            yb = [k.sb(es, f"cy{i}", [128, 1024], F32) for i in range(12)]
            lnst = [k.sb(es, f"lnst5{i}", [128, 16], F32) for i in range(4)]
            for tt in range(NQT):
                ac = k.nxt("cacc", accb)
                rows = slice(tt * 128, (tt + 1) * 128)
                sc.dma(sp, ac.t[:, :], base_d[rows, :], writes=[ac.res])
                for kk in range(4):
                    y_ = k.nxt("cy", yb)
                    sc.dma(pool, lambda: G.indirect_dma_start(
                        out=y_.t[:, :], out_offset=None, in_=ys_d[:, :],
                        in_offset=bass.IndirectOffsetOnAxis(ap=dest_all.t[:, tt, kk:kk + 1], axis=0)), None,
                        reads=[dest_res[tt]], writes=[y_.res])
                    sc.op(dve, lambda: V.scalar_tensor_tensor(out=ac.t[:, :], in0=y_.t[:, :], scalar=gk_all.t[:, tt, kk:kk + 1],
                                                              in1=ac.t[:, :], op0=ALU.mult, op1=ALU.add),
                          reads=[y_.res, gk_res[tt]], writes=[ac.res])
                st_ = k.nxt("lnst5", lnst)
                sc.op(dve, lambda: V.bn_stats(out=st_.t[:, 0:6], in_=ac.t[:, 0:512]), reads=[ac.res], writes=[st_.res])
                sc.op(dve, lambda: V.bn_stats(out=st_.t[:, 6:12], in_=ac.t[:, 512:1024]), reads=[ac.res], writes=[st_.res])
                sc.op(dve, lambda: V.bn_aggr(out=st_.t[:, 12:14], in_=st_.t[:, 0:12]), reads=[], writes=[st_.res])
                sc.op(act, lambda: A.activation(out=st_.t[:, 14:15], in_=st_.t[:, 13:14], func=AF.Ln, bias=epsb.t[:, 0:1], scale=1.0),
                      reads=[st_.res, epsb.res], writes=[st_.res])
                sc.op(act, lambda: A.activation(out=st_.t[:, 14:15], in_=st_.t[:, 14:15], func=AF.Exp, scale=-0.5),
                      reads=[], writes=[st_.res])
                sc.op(dve, lambda: V.scalar_tensor_tensor(out=st_.t[:, 15:16], in0=st_.t[:, 12:13], scalar=-1.0, in1=st_.t[:, 14:15],
                                                          op0=ALU.mult, op1=ALU.mult), reads=[], writes=[st_.res])
                sc.op(act, lambda: A.activation(out=ac.t[:, :], in_=ac.t[:, :], func=AF.Identity, bias=st_.t[:, 15:16],
                                                scale=st_.t[:, 14:15]), reads=[st_.res], writes=[ac.res])
                sc.op(dve, lambda: V.tensor_tensor(out=ac.t[:, :], in0=ac.t[:, :], in1=g2b.t[:, :], op=ALU.mult),
                      reads=[g2b.res], writes=[ac.res])
                sc.op(dve, lambda: V.tensor_tensor(out=ac.t[:, :], in0=ac.t[:, :], in1=b2b.t[:, :], op=ALU.add),
                      reads=[b2b.res], writes=[ac.res])
                sc.dma(sp, out_d[rows, :], ac.t[:, :], reads=[ac.res])
            sc.barrier()

    k.phase5 = phase5
    k.phase1 = phase1
    return k


def prep_inputs(inp):
    f = lambda a: np.ascontiguousarray(np.asarray(a, np.float32))
    sh = {}
    sh["w_in"] = f(inp["w_in"][0])
    for n in ("diff_lq1", "diff_lk1", "diff_lq2", "diff_lk2", "diff_subln_g", "b_mgate", "ln1_g", "ln1_b",
              "b_router", "b_ple_gate", "ln2_g", "ln2_b"):
        sh[n] = f(inp[n][0]).reshape(1, -1)
    sh["nsa_pos_kT"] = f(np.asarray(inp["nsa_pos_k"][0]).T)
    sh["nsa_pos_vT"] = f(np.asarray(inp["nsa_pos_v"][0]).T)
    sh["nsa_phi_k1"] = f(np.asarray(inp["nsa_phi_k1"][0]).reshape(32, 64, 256).transpose(1, 0, 2))
    for nm in ("k", "v"):
        w = np.asarray(inp["nsa_phi_%s1" % nm][0]).reshape(16, 2, 64, 256)
        sh["nsa_phi_%s1s" % nm] = f(w.transpose(1, 2, 0, 3).reshape(128, 16, 256))
        ps = np.asarray(inp["nsa_pos_%s" % nm][0]).reshape(16, 2, 64)
        sh["nsa_pos_%sTs" % nm] = f(ps.transpose(1, 2, 0).reshape(128, 16))
    sh["nsa_phi_v1"] = f(np.asarray(inp["nsa_phi_v1"][0]).reshape(32, 64, 256).transpose(1, 0, 2))
    for n in ("nsa_phi_k2", "nsa_phi_v2", "w_br_diff", "w_br_nsa", "w_mgate", "w_o", "w_router", "w_e1", "w_e2",
              "b_e2", "w_ple_gate", "w_ple_proj"):
        sh[n] = f(inp[n][0])
    sh["b_e1T"] = f(np.asarray(inp["b_e1"][0]).reshape(32, 16, 128).transpose(2, 0, 1).reshape(128, 512))
    sh.update(_const_tables())
    x = np.asarray(inp["x"], np.float32)
    p = np.asarray(inp["p"], np.float32)
    maps = []
    for b in range(8):
        m = dict(sh)
        m["x"] = f(x[b])
        m["xT"] = f(x[b].T)
        m["pT"] = f(p[0, b].T)
        maps.append(m)
    return maps


_CACHE = {}


def kernel(**inputs):
    if "nc" not in _CACHE:
        kb = build()
        kb.phase1(); kb.phase2(); kb.phase3(); kb.phase4(); kb.phase5()
        _CACHE["nc"] = kb
    kb = _CACHE["nc"]
    maps = prep_inputs(inputs)
    maps = [{n: m[n] for n in kb.din} for m in maps]
    res = run_bass_kernel_spmd(kb.nc, maps, core_ids=list(range(8)))
    out = np.stack([np.asarray(res.results[b]["out"], np.float32) for b in range(8)], axis=0)
    return out
```

```python
import contextlib
import os
import math
import numpy as np
import ml_dtypes
import concourse.bass as bass
import concourse.mybir as mybir
from concourse.bass_utils import run_bass_kernel_spmd

F32 = mybir.dt.float32
BF16 = mybir.dt.bfloat16
AF = mybir.ActivationFunctionType
ALU = mybir.AluOpType
AX = mybir.AxisListType

S = 4096
D = 1024
NQT = 32
NEGB = -131072.0
SCALE = 0.125
LN_EPS = 1e-5
ALPHA = 2.0 ** 0.25
LAMBDA_INIT = 0.2
SEM_LIMIT = 30000
SKIP_SAME = set(os.environ.get('KSKIP', '').split(',')) - {''}
CAP = 768
NSLOT = 32 * CAP + 128


class Res:
    __slots__ = ("w", "r")

    def __init__(self):
        self.w = None
        self.r = {}


class Eng:
    def __init__(self, sch, name, eng):
        self.sch = sch
        self.name = name
        self.eng = eng
        self.sem = sch.new_sem(name)
        self.cnt = 0
        self.known = {}
        self.slots = []
        self.slot_i = 0


class Sched:
    def __init__(self, nc, es):
        self.nc = nc
        self.es = es
        self.nsem = 0
        self.sems = {}
        self.pe = Eng(self, "pe", nc.tensor)
        self.act = Eng(self, "act", nc.scalar)
        self.dve = Eng(self, "dve", nc.vector)
        self.pool = Eng(self, "pool", nc.gpsimd)
        self.sp = Eng(self, "sp", nc.sync)
        self.engs = [self.pe, self.act, self.dve, self.pool, self.sp]
        for e, n in ((self.sp, 24), (self.pool, 16), (self.act, 4)):
            for i in range(n):
                e.slots.append([self.new_sem(f"{e.name}_d{i}"), 0])
        self.n_ins = 0

    def new_sem(self, name):
        self.nsem += 1
        s = self.es.enter_context(self.nc.semaphore(f"s{self.nsem}_{name}"))
        self.sems[id(s)] = s
        return s

    def _wait(self, E, toks):
        for sem, val in toks:
            k = id(sem)
            if E.known.get(k, 0) >= val:
                continue
            E.eng.wait_ge(sem, val)
            E.known[k] = val

    def _deps(self, E, reads, writes):
        deps = []
        for r in reads:
            if r.w is not None:
                deps.append(r.w)
        for w in writes:
            if w.w is not None:
                deps.append(w.w)
            deps.extend(w.r.values())
        if E is self.pe or E.name in SKIP_SAME:
            deps = [d for d in deps if d[0] is not E.sem]
        return deps

    def _commit(self, tok, reads, writes):
        for r in reads:
            k = id(tok[0])
            r.r[k] = tok
        for w in writes:
            w.w = tok
            w.r = {}

    def op(self, E, fn, reads=(), writes=()):
        self._wait(E, self._deps(E, reads, writes))
        ins = fn()
        E.cnt += 1
        ins.then_inc(E.sem, 1)
        tok = (E.sem, E.cnt)
        self._commit(tok, reads, writes)
        self.n_ins += 1
        if E.cnt >= SEM_LIMIT:
            E.sem = self.new_sem(E.name)
            E.cnt = 0
        return tok

    def dma(self, E, out, in_, reads=(), writes=(), **kw):
        slot = E.slots[E.slot_i]
        E.slot_i = (E.slot_i + 1) % len(E.slots)
        deps = self._deps(E, reads, writes)
        if slot[1] > 0:
            deps.append((slot[0], slot[1]))
        self._wait(E, deps)
        if slot[1] + 16 > SEM_LIMIT:
            slot[0] = self.new_sem(E.name + "_d")
            slot[1] = 0
        if callable(out):
            out().then_inc(slot[0], 16)
        else:
            E.eng.dma_start(out=out, in_=in_, **kw).then_inc(slot[0], 16)
        slot[1] += 16
        tok = (slot[0], slot[1])
        self._commit(tok, reads, writes)
        self.n_ins += 1
        return tok

    def barrier(self):
        toks = []
        for e in self.engs:
            if e.cnt > 0:
                toks.append((e.sem, e.cnt))
            for s in e.slots:
                if s[1] > 0:
                    toks.append((s[0], s[1]))
        for e in self.engs:
            self._wait(e, toks)


def _bf(x):
    return np.asarray(x, np.float32).astype(ml_dtypes.bfloat16)


def _hi_lo(v):
    v = np.asarray(v, np.float64)
    hi = v.astype(np.float32).astype(ml_dtypes.bfloat16).astype(np.float64)
    lo = (v - hi)
    return hi, lo


def _const_tables():
    t = {}
    pos = np.arange(S)
    augk = np.stack([pos // 128, pos // 128, pos % 128, pos % 128, np.ones(S), np.ones(S)]).astype(np.float32)
    t["augk_tok"] = _bf(augk)
    cpos = np.arange(255) * 16 + 31
    augc = np.zeros((6, 256), np.float32)
    augc[:, :255] = np.stack([cpos // 128, cpos // 128, cpos % 128, cpos % 128, np.ones(255), np.ones(255)])
    t["augk_cmp"] = _bf(augc)
    slopes = list(2.0 ** (-8.0 * np.arange(1, 9) / 8)) + list(2.0 ** (-8.0 * np.arange(1, 17) / 16))
    augq = np.zeros((24, 6, S), np.float32)
    for i, s in enumerate(slopes):
        shi, slo = _hi_lo(s)
        augq[i, 0] = 1024.0 * shi
        augq[i, 1] = 1024.0 * slo
        augq[i, 2] = 8.0 * shi
        augq[i, 3] = 8.0 * slo
        hi, lo = _hi_lo(-8.0 * s * pos)
        augq[i, 4] = hi
        augq[i, 5] = lo
    t["augq"] = _bf(augq)
    kk = np.arange(128)[:, None]
    qq = np.arange(128)[None, :]
    t["m_caus"] = _bf(np.where(qq >= kk, 0.0, NEGB))
    t["m_band"] = _bf(np.where(qq < kk, 0.0, NEGB))
    t["ident"] = _bf(np.eye(128))
    n = np.arange(256).reshape(2, 128)
    valid = ((n[:, :, None] * 16 + 31) <= pos[None, None, :]) & (n[:, :, None] < 255)
    t["m_cmp"] = _bf(np.where(valid, 0.0, NEGB).transpose(1, 0, 2))
    c_lo = np.arange(255) * 16
    s_lo = np.arange(64) * 64
    ov = np.minimum(c_lo[:, None] + 32, s_lo[None, :] + 64) - np.maximum(c_lo[:, None], s_lo[None, :])
    c2s = np.zeros((256, 64), np.float32)
    c2s[:255] = np.clip(ov, 0, None) / 32.0
    t["c2s"] = _bf(c2s.reshape(2, 128, 64).transpose(1, 0, 2))
    cur = (pos // 64)[:, None]
    j = np.arange(64)[None, :]
    forced = (j == 0) | (j == cur) | (j == cur - 1)
    keep = (~forced) & (j <= cur)
    tf = np.where(forced, 1e30, np.where(j <= cur, 0.0, -1e30)).astype(np.float32)
    t["sel_keep"] = np.ascontiguousarray(keep.astype(np.float32).reshape(32, 128, 64).transpose(1, 0, 2))
    t["sel_force"] = np.ascontiguousarray(tf.reshape(32, 128, 64).transpose(1, 0, 2))
    t["expand"] = _bf((np.arange(64)[:, None] == (pos // 64)[None, :]).astype(np.float32))
    t["triu"] = _bf((np.arange(128)[:, None] < np.arange(128)[None, :]).astype(np.float32))
    t["ones128"] = _bf(np.ones((128, 128)))
    t["iota32"] = np.tile(np.arange(32, dtype=np.float32)[None, :], (128, 1))
    t["trp"] = (32 * CAP + np.arange(128, dtype=np.float32)).reshape(128, 1)
    return t


WEIGHT_NAMES = [
    "w_in", "diff_lq1", "diff_lk1", "diff_lq2", "diff_lk2", "diff_subln_g",
    "nsa_pos_k", "nsa_pos_v", "nsa_phi_k1", "nsa_phi_k2", "nsa_phi_v1", "nsa_phi_v2",
    "w_br_diff", "w_br_nsa", "w_mgate", "b_mgate", "w_o", "ln1_g", "ln1_b",
    "w_router", "b_router", "w_e1", "b_e1", "w_e2", "b_e2",
    "w_ple_gate", "b_ple_gate", "w_ple_proj", "ln2_g", "ln2_b",
]


class Buf:
    __slots__ = ("t", "res")

    def __init__(self, t):
        self.t = t
        self.res = Res()


class KB:
    def __init__(self, dbg=()):
        self.dbg = set(dbg)
        self.nc = bass.Bass("TRN2", target_bir_lowering=False)
        self.es = contextlib.ExitStack()
        self.sc = Sched(self.nc, self.es)
        self.din = {}
        self.rot = {}

    def inp(self, name, shape, dt=F32):
        t = self.nc.dram_tensor(name, list(shape), dt, kind="ExternalInput").ap()
        self.din[name] = t
        return t

    def scratch(self, name, shape, dt):
        kind = "ExternalOutput" if name in self.dbg else "Internal"
        return self.nc.dram_tensor(name, list(shape), dt, kind=kind).ap()

    def sb(self, es, name, shape, dt):
        return Buf(es.enter_context(self.nc.sbuf_tensor("sb_" + name, list(shape), dt)))

    def ps(self, es, name, shape, dt):
        return Buf(es.enter_context(self.nc.psum_tensor("ps_" + name, list(shape), dt)))

    def nxt(self, key, lst):
        i = self.rot.get(key, 0)
        self.rot[key] = i + 1
        return lst[i % len(lst)]


def build(dbg=(), phases=(1, 2, 3, 4, 5)):
    k = KB(dbg)
    nc, sc = k.nc, k.sc
    pe, act, dve, pool, sp = sc.pe, sc.act, sc.dve, sc.pool, sc.sp
    V, G, T = nc.vector, nc.gpsimd, nc.tensor
    A = nc.scalar

    SHAPES = {
        "x": ([S, D], F32), "xT": ([D, S], F32), "pT": ([256, S], F32), "w_in": ([D, 5680], F32),
        "diff_lq1": ([1, 64], F32), "diff_lk1": ([1, 64], F32), "diff_lq2": ([1, 64], F32), "diff_lk2": ([1, 64], F32),
        "diff_subln_g": ([1, 128], F32), "nsa_pos_kT": ([64, 32], F32), "nsa_pos_vT": ([64, 32], F32),
        "nsa_phi_k1": ([64, 32, 256], F32), "nsa_phi_v1": ([64, 32, 256], F32),
        "nsa_phi_k1s": ([128, 16, 256], F32), "nsa_phi_v1s": ([128, 16, 256], F32), "nsa_pos_kTs": ([128, 16], F32), "nsa_pos_vTs": ([128, 16], F32),
        "nsa_phi_k2": ([256, 64], F32), "nsa_phi_v2": ([256, 64], F32),
        "w_br_diff": ([D, D], F32), "w_br_nsa": ([D, D], F32), "w_mgate": ([D, 2 * D], F32), "b_mgate": ([1, 2 * D], F32),
        "w_o": ([D, D], F32), "ln1_g": ([1, D], F32), "ln1_b": ([1, D], F32),
        "w_router": ([D, 32], F32), "b_router": ([1, 32], F32),
        "w_e1": ([32, D, 2 * D], F32), "b_e1T": ([128, 512], F32), "w_e2": ([32, D, D], F32), "b_e2": ([32, D], F32),
        "w_ple_gate": ([D, D], F32), "b_ple_gate": ([1, D], F32), "w_ple_proj": ([256, D], F32),
        "ln2_g": ([1, D], F32), "ln2_b": ([1, D], F32),
        "augk_tok": ([6, S], BF16), "augk_cmp": ([6, 256], BF16), "augq": ([24, 6, S], BF16),
        "m_caus": ([128, 128], BF16), "m_band": ([128, 128], BF16), "ident": ([128, 128], BF16),
        "m_cmp": ([128, 2, S], BF16), "c2s": ([128, 2, 64], BF16),
        "sel_keep": ([128, 32, 64], F32), "sel_force": ([128, 32, 64], F32), "expand": ([64, S], BF16),
        "triu": ([128, 128], BF16), "ones128": ([128, 128], BF16), "iota32": ([128, 32], F32), "trp": ([128, 1], F32),
    }

    def I(name):
        if name not in k.din:
            shp, dt = SHAPES[name]
            k.inp(name, shp, dt)
        return k.din[name]

    out_d = nc.dram_tensor("out", [S, D], F32, kind="ExternalOutput").ap()

    QdT = k.scratch("QdT", [D, S], BF16); KdT = k.scratch("KdT", [D, S], BF16)
    Vd = k.scratch("Vd", [S, D], BF16); NqT = k.scratch("NqT", [D, S], BF16)
    kcT = k.scratch("kcT", [256, S], BF16); vcT = k.scratch("vcT", [256, S], BF16)
    ksT = k.scratch("ksT", [256, S], BF16); vs_d = k.scratch("vs", [S, 256], BF16)
    kwT = k.scratch("kwT", [256, S], BF16); vw_d = k.scratch("vw", [S, 256], BF16)
    gates_d = k.scratch("gates", [S, 48], F32)
    odiff_d = k.scratch("o_diff", [S, D], BF16); onsa_d = k.scratch("o_nsa", [S, D], BF16)
    hT_d = k.scratch("hT", [D, S], BF16); base_d = k.scratch("base", [S, D], F32)
    G_d = k.scratch("Gd", [S, 32], F32)
    xs_d = k.scratch("xs", [NSLOT, D], BF16); ys_d = k.scratch("ys", [NSLOT, D], F32)

    es0 = k.es
    banks = [k.ps(es0, f"bank{i}", [128, 512], F32) for i in range(7)]
    tbank = k.ps(es0, "tbank", [128, 1024], BF16)
    ident = k.sb(es0, "ident", [128, 128], BF16)
    mcaus = k.sb(es0, "mcaus", [128, 128], BF16)
    mband = k.sb(es0, "mband", [128, 128], BF16)
    sc.dma(sp, ident.t[:], I("ident")[:, :], writes=[ident.res])
    wz = k.sb(es0, "wz", [128, 512], BF16)
    sc.op(pool, lambda: G.memset(wz.t[:, :], 0.0), writes=[wz.res])
    epsb = k.sb(es0, "epsb", [128, 1], F32)
    sc.op(pool, lambda: G.memset(epsb.t[:, :], LN_EPS), writes=[epsb.res])
    sc.dma(sp, mcaus.t[:], I("m_caus")[:, :], writes=[mcaus.res])
    sc.dma(sp, mband.t[:], I("m_band")[:, :], writes=[mband.res])

    U32 = mybir.dt.uint32
    dest_all = k.sb(es0, "dest_all", [128, 32, 4], U32)
    gk_all = k.sb(es0, "gk_all", [128, 32, 4], F32)
    dest_res = [Res() for _ in range(32)]
    gk_res = [Res() for _ in range(32)]
    evac_i = [0]

    def evac_copy(out_ap, in_ap, reads, writes):
        evac_i[0] += 1
        if evac_i[0] % 2:
            return sc.op(act, lambda: A.copy(out=out_ap, in_=in_ap), reads=reads, writes=writes)
        return sc.op(dve, lambda: V.tensor_copy(out=out_ap, in_=in_ap), reads=reads, writes=writes)

    def phase1():
        with contextlib.ExitStack() as es:
            xT = k.sb(es, "xT", [128, 8, S], BF16)
            rx = [Res() for _ in range(8)]
            for kc in range(8):
                sc.dma(pool, xT.t[:, kc, :], I("xT")[kc * 128:(kc + 1) * 128, :], writes=[rx[kc]])
            wb = [k.sb(es, f"wb{i}", [128, 8, 1024], BF16) for i in range(2)]
            sfm = [k.sb(es, f"sfm{i}", [128, S], BF16) for i in range(2)]
            stm = [k.sb(es, f"stm{i}", [128, 4, 1024], BF16) for i in range(2)]
            sg = k.sb(es, "sgate", [128, 32, 48], F32)
            loads = [(0, 1024), (1024, 1024), (2048, 1024), (3072, 1024), (4096, 1024), (5120, 560)]
            subs = [
                [(0, 1024, "fm", QdT, 0)], [(0, 1024, "fm", KdT, 0)], [(0, 1024, "tm", Vd, 0)],
                [(0, 1024, "fm", NqT, 0)],
                [(0, 256, "fm", kcT, 0), (256, 256, "fm", vcT, 0), (512, 256, "fm", ksT, 0), (768, 256, "tm", vs_d, 0)],
                [(0, 256, "fm", kwT, 0), (256, 256, "tm", vw_d, 0), (512, 48, "gate", None, 0)],
            ]
            bi = 0
            for li, (c0, n) in enumerate(loads):
                w = wb[li % 2]
                src = I("w_in")[:, c0:c0 + n].rearrange("(kc p) n -> p kc n", p=128)
                sc.dma(pool, w.t[:, :, 0:n], src, writes=[w.res])
                for (b0, nn, kind, dst, _) in subs[li]:
                    if kind == "fm":
                        for cc in range(nn // 128):
                            st = k.nxt("sfm", sfm)
                            for qt in range(8):
                                bk = k.nxt("bank", banks)
                                for kc in range(8):
                                    sc.op(pe, lambda: T.matmul(bk.t[:, :], lhsT=w.t[:, kc, b0 + cc * 128:b0 + (cc + 1) * 128],
                                                               rhs=xT.t[:, kc, qt * 512:(qt + 1) * 512],
                                                               start=(kc == 0), stop=(kc == 7)),
                                          reads=[w.res, rx[kc]], writes=[bk.res])
                                evac_copy(st.t[:, qt * 512:(qt + 1) * 512], bk.t[:, :], [bk.res], [st.res])
                            sc.dma(sp, dst[cc * 128:(cc + 1) * 128, :], st.t[:, :], reads=[st.res])
                    elif kind == "tm":
                        for t4 in range(8):
                            st = k.nxt("stm", stm)
                            for ti in range(4):
                                tt = t4 * 4 + ti
                                for ch in range((nn + 511) // 512):
                                    cw = min(512, nn - ch * 512)
                                    bk = k.nxt("bank", banks)
                                    for kc in range(8):
                                        sc.op(pe, lambda: T.matmul(bk.t[:, 0:cw], lhsT=xT.t[:, kc, tt * 128:(tt + 1) * 128],
                                                                   rhs=w.t[:, kc, b0 + ch * 512:b0 + ch * 512 + cw],
                                                                   start=(kc == 0), stop=(kc == 7)),
                                              reads=[w.res, rx[kc]], writes=[bk.res])
                                    evac_copy(st.t[:, ti, ch * 512:ch * 512 + cw], bk.t[:, 0:cw], [bk.res], [st.res])
                            sc.dma(sp, dst[t4 * 512:(t4 + 1) * 512, :].rearrange("(t p) c -> p t c", p=128),
                                   st.t[:, :, 0:nn], reads=[st.res])
                    else:
                        for tt in range(32):
                            bk = k.nxt("bank", banks)
                            for kc in range(8):
                                sc.op(pe, lambda: T.matmul(bk.t[:, 0:48], lhsT=xT.t[:, kc, tt * 128:(tt + 1) * 128],
                                                           rhs=w.t[:, kc, b0:b0 + 48], start=(kc == 0), stop=(kc == 7)),
                                      reads=[w.res, rx[kc]], writes=[bk.res])
                            sc.op(act, lambda: A.activation(out=sg.t[:, tt, :], in_=bk.t[:, 0:48], func=AF.Sigmoid),
                                  reads=[bk.res], writes=[sg.res])
                        sc.dma(sp, gates_d.rearrange("(t p) c -> p t c", p=128), sg.t[:, :, :], reads=[sg.res])
            sc.barrier()

    sbanks = banks[0:3]

    class _View:
        pass
    tb32 = _View()
    tb32.t = tbank.t[:, :].bitcast(F32)
    tb32.res = tbank.res
    SB = [sbanks]
    accs = banks[3:7]

    FILL = [int(os.environ.get('KFILL', '0'))]
    KDIM = int(os.environ.get('KDIM', '128'))

    def attn_pass(es_p, QT, KT, kdim, Vt, dvp, specs, evac_fn, reads, pbufs):
        items = []
        covers = {}
        for j in range(8):
            spj = specs[j]
            if not spj:
                continue
            covers[j] = {sub: [i for i, s_ in enumerate(spj) if s_[1] <= sub * 128 < s_[2]] for sub in range(4)}
            for i in range(len(spj)):
                items.append((j, i))

        def emit_S(n):
            j, i = items[n]
            kt, c0, c1, masks = specs[j][i]
            bk = k.nxt("sbank" + str(len(SB[0])), SB[0])
            nm = len(masks)
            sc.op(pe, lambda: T.matmul(bk.t[:, c0:c1], lhsT=KT[0:kdim, kt * 128:(kt + 1) * 128],
                                       rhs=QT[0:kdim, j * 512 + c0:j * 512 + c1], start=True, stop=(nm == 0)),
                  reads=reads, writes=[bk.res])
            for mi, (ml, mr, lo, hi, mreads) in enumerate(masks):
                sc.op(pe, lambda: T.matmul(bk.t[:, lo:hi], lhsT=ml, rhs=mr, start=False, stop=(mi == nm - 1)),
                      reads=mreads, writes=[bk.res])
            return bk

        LA = len(SB[0]) - 1
        pend = {}
        for n0 in range(min(LA, len(items))):
            pend[n0] = emit_S(n0)
        for n in range(len(items)):
            if n + LA < len(items):
                pend[n + LA] = emit_S(n + LA)
            j, i = items[n]
            kt, c0, c1, masks = specs[j][i]
            cover = covers[j]
            bk = pend.pop(n)
            pb = k.nxt("pbuf", pbufs)
            if FILL[0] > 0:
                sc.op(pe, lambda: T.matmul(tbank.t[:, :].bitcast(F32)[:, 0:FILL[0]], lhsT=ident.t[:, :], rhs=wz.t[:, 0:FILL[0]],
                                           start=True, stop=True), reads=[], writes=[])
            sc.op(act, lambda: A.activation(out=pb.t[:, c0:c1], in_=bk.t[:, c0:c1], func=AF.Exp, scale=SCALE),
                  reads=[bk.res], writes=[pb.res])
            for sub in range(c0 // 128, c1 // 128):
                ac = accs[sub]
                first = cover[sub][0] == i
                last = cover[sub][-1] == i
                sc.op(pe, lambda: T.matmul(ac.t[:, 0:dvp], lhsT=pb.t[:, sub * 128:(sub + 1) * 128],
                                           rhs=Vt[:, kt, 0:dvp], start=first, stop=last),
                      reads=[pb.res] + list(reads), writes=[ac.res])
                if last:
                    evac_fn(j * 4 + sub, ac)

    def pe_warmup(n=24):
        for i in range(n):
            sc.op(pe, lambda: T.matmul(tbank.t[:, :].bitcast(F32), lhsT=ident.t[:, :], rhs=wz.t[:, :], start=True, stop=True),
                  reads=[ident.res, wz.res], writes=[tbank.res] if i == 0 else [])

    def causal_specs(extra=None):
        specs = []
        for j in range(8):
            l = []
            for kt in range(4 * j + 4):
                d = kt - 4 * j
                c0 = 128 * d if d > 0 else 0
                masks = []
                if extra is not None:
                    masks += extra(j, kt, c0, 512)
                if d >= 0:
                    masks.append((ident.t[:, :], mcaus.t[:, :], 128 * d, 128 * d + 128, [ident.res, mcaus.res]))
                l.append((kt, c0, 512, masks))
            specs.append(l)
        return specs

    def bcast_load(es, name, src_ap, n, eng=None):
        b = k.sb(es, name, [128, n], F32)
        sc.dma(sp, b.t[:, :], src_ap.partition_broadcast(128), writes=[b.res])
        return b

    def phase2():
        with contextlib.ExitStack() as es:
            pbufs = [k.sb(es, f"pb{i}", [128, 512], BF16) for i in range(6)]
            Qb = [k.sb(es, f"dQ{i}", [128, S], BF16) for i in range(2)]
            Kb = [k.sb(es, f"dK{i}", [128, S], BF16) for i in range(2)]
            Vb = [k.sb(es, f"dV{i}", [128, 32, 129], BF16) for i in range(2)]
            A0 = k.sb(es, "dA0", [128, 32, 128], F32)
            A1 = k.sb(es, "dA1", [128, 32, 128], F32)
            Asq = k.sb(es, "dAsq", [128, 32, 128], F32)
            rst = k.sb(es, "drst", [128, 32, 2], F32)
            ost = [k.sb(es, f"dost{i}", [128, 32, 128], BF16) for i in range(2)]
            small = [k.sb(es, f"dsm{i}", [128, 4], F32) for i in range(8)]
            at = [k.sb(es, f"dat{i}", [128, 128], F32) for i in range(3)]
            junk = k.sb(es, "djunk", [128, 128], F32)
            l4 = [bcast_load(es, f"dl{i}", I(n)[:, :], 64) for i, n in
                  enumerate(("diff_lq1", "diff_lk1", "diff_lq2", "diff_lk2"))]
            gsub = bcast_load(es, "dgsub", I("diff_subln_g")[:, :], 128)
            lam = k.sb(es, "dlam", [128, 8], F32)
            sc.op(dve, lambda: V.tensor_tensor(out=l4[0].t[:, :], in0=l4[0].t[:, :], in1=l4[1].t[:, :], op=ALU.mult),
                  reads=[l4[1].res], writes=[l4[0].res])
            sc.op(dve, lambda: V.tensor_tensor(out=l4[2].t[:, :], in0=l4[2].t[:, :], in1=l4[3].t[:, :], op=ALU.mult),
                  reads=[l4[3].res], writes=[l4[2].res])
            sc.op(dve, lambda: V.reduce_sum(out=lam.t[:, 0:1], in_=l4[0].t[:, :], axis=AX.X), reads=[l4[0].res], writes=[lam.res])
            sc.op(dve, lambda: V.reduce_sum(out=lam.t[:, 1:2], in_=l4[2].t[:, :], axis=AX.X), reads=[l4[2].res], writes=[lam.res])
            sc.op(act, lambda: A.activation(out=lam.t[:, 2:4], in_=lam.t[:, 0:2], func=AF.Exp), reads=[lam.res], writes=[lam.res])
            sc.op(dve, lambda: V.scalar_tensor_tensor(out=lam.t[:, 4:5], in0=lam.t[:, 3:4], scalar=-LAMBDA_INIT,
                                                      in1=lam.t[:, 2:3], op0=ALU.add, op1=ALU.subtract),
                  reads=[lam.res], writes=[lam.res])
            sc.op(dve, lambda: V.tensor_scalar(out=gsub.t[:, :], in0=gsub.t[:, :], scalar1=1.0 - LAMBDA_INIT, scalar2=None,
                                               op0=ALU.mult), reads=[gsub.res], writes=[gsub.res])
            for b in Vb:
                sc.op(pool, lambda: G.memset(b.t[:, :, 128:129], 1.0), writes=[b.res])
            for b in Qb + Kb:
                sc.op(pool, lambda: G.memset(b.t[64:128, :], 0.0), writes=[b.res])
            for b in Kb:
                sc.dma(sp, b.t[64:70, :], I("augk_tok")[:, :], writes=[b.res])
            specs = causal_specs()
            SB[0] = sbanks + [tb32]
            for h in range(8):
                if h == 0:
                    pe_warmup()
                vb = Vb[h % 2]
                sc.dma(sp, vb.t[:, :, 0:128], Vd[:, h * 128:(h + 1) * 128].rearrange("(t p) c -> p t c", p=128),
                       writes=[vb.res])
                osb = ost[h % 2]
                for c in range(2):
                    qb, kb_ = Qb[c], Kb[c]
                    r0 = h * 128 + c * 64
                    sc.dma(sp, qb.t[0:64, :], QdT[r0:r0 + 64, :], writes=[qb.res])
                    sc.dma(sp, qb.t[64:70, :], I("augq")[h, :, :], writes=[qb.res])
                    sc.dma(sp, kb_.t[0:64, :], KdT[r0:r0 + 64, :], writes=[kb_.res])

                    def ev(qt, ac, c=c, osb=osb):
                        sm = k.nxt("dsm", small)
                        sc.op(dve, lambda: V.reciprocal(out=sm.t[:, 0:1], in_=ac.t[:, 128:129]), reads=[ac.res], writes=[sm.res])
                        if c == 0:
                            sc.op(dve, lambda: V.tensor_scalar(out=A0.t[:, qt, :], in0=ac.t[:, 0:128], scalar1=sm.t[:, 0:1],
                                                               scalar2=None, op0=ALU.mult),
                                  reads=[ac.res, sm.res], writes=[A0.res])
                            return
                        sc.op(dve, lambda: V.tensor_tensor(out=sm.t[:, 1:2], in0=sm.t[:, 0:1], in1=lam.t[:, 4:5], op=ALU.mult),
                              reads=[lam.res, sm.res], writes=[sm.res])
                        sc.op(dve, lambda: V.scalar_tensor_tensor(out=A1.t[:, qt, :], in0=ac.t[:, 0:128], scalar=sm.t[:, 1:2],
                                                                  in1=A0.t[:, qt, :], op0=ALU.mult, op1=ALU.add),
                              reads=[ac.res, sm.res, A0.res], writes=[A1.res])

                    attn_pass(es, qb.t, kb_.t, KDIM, vb.t, 129, specs, ev, [qb.res, kb_.res, vb.res], pbufs)
                sc.op(dve, lambda: V.tensor_tensor(out=Asq.t[:, :, :], in0=A1.t[:, :, :], in1=A1.t[:, :, :], op=ALU.mult),
                      reads=[A1.res], writes=[Asq.res])
                sc.op(dve, lambda: V.tensor_reduce(out=rst.t[:, :, 0:1], in_=Asq.t[:, :, :], axis=AX.X, op=ALU.add),
                      reads=[Asq.res], writes=[rst.res])
                sc.op(act, lambda: A.activation(out=rst.t[:, :, 1:2], in_=rst.t[:, :, 0:1], func=AF.Ln, bias=epsb.t[:, 0:1],
                                                scale=1.0 / 128.0), reads=[rst.res, epsb.res], writes=[rst.res])
                sc.op(act, lambda: A.activation(out=rst.t[:, :, 1:2], in_=rst.t[:, :, 1:2], func=AF.Exp, scale=-0.5),
                      reads=[], writes=[rst.res])
                sc.op(dve, lambda: V.tensor_tensor(out=Asq.t[:, :, :], in0=A1.t[:, :, :],
                                                   in1=rst.t[:, :, 1:2].to_broadcast([128, 32, 128]), op=ALU.mult),
                      reads=[A1.res, rst.res], writes=[Asq.res])
                sc.op(pool, lambda: G.tensor_tensor(out=osb.t[:, :, :], in0=Asq.t[:, :, :],
                                                    in1=gsub.t[:, :].unsqueeze(1).to_broadcast([128, 32, 128]), op=ALU.mult),
                      reads=[Asq.res, gsub.res], writes=[osb.res])
                sc.dma(pool, odiff_d[:, h * 128:(h + 1) * 128].rearrange("(t p) c -> p t c", p=128), osb.t[:, :, :],
                       reads=[osb.res])
            SB[0] = sbanks
            sc.barrier()

    k.phase2 = phase2
    def phase3():
        with contextlib.ExitStack() as es:
            pbufs = [k.sb(es, f"npb{i}", [128, 512], BF16) for i in range(6)]
            gates = k.sb(es, "ngates", [128, 32, 48], F32)
            sc.dma(sp, gates.t[:, :, :], gates_d.rearrange("(t p) c -> p t c", p=128), writes=[gates.res])
            Kc = k.sb(es, "nKc", [128, 4, 256], BF16)
            Vc = k.sb(es, "nVc", [128, 4, 2, 129], BF16)
            sc.op(pool, lambda: G.memset(Kc.t[:, :, :], 0.0), writes=[Kc.res])
            sc.op(pool, lambda: G.memset(Vc.t[:, :, :, :], 0.0), writes=[Vc.res])
            sc.op(pool, lambda: G.memset(Vc.t[:, :, :, 128:129], 1.0), writes=[Vc.res])
            for g in range(4):
                sc.dma(sp, Kc.t[64:70, g, :], I("augk_cmp")[:, :], writes=[Kc.res])
                sc.dma(sp, Vc.t[:, g, :, 64:128], I("c2s")[:, :, :], writes=[Vc.res])
            with contextlib.ExitStack() as es2:
                w1 = [k.sb(es2, f"cw1{i}", [128, 16, 256], BF16) for i in range(2)]
                w2 = [k.sb(es2, f"cw2{i}", [128, 2, 64], BF16) for i in range(2)]
                posT = [k.sb(es2, f"cpos{i}", [128, 16], BF16) for i in range(2)]
                biasv = k.sb(es2, "cbias", [128, 4], F32)
                srcb = [k.sb(es2, f"csrc{i}", [128, S], BF16) for i in range(2)]
                for b_ in srcb:
                    sc.op(pool, lambda: G.memset(b_.t[64:128, S - 16:S], 0.0), writes=[b_.res])
                hid = [k.sb(es2, f"chid{i}", [128, 2, 256], BF16) for i in range(2)]
                for kv, (n1, n2, npos) in enumerate((("nsa_phi_k1s", "nsa_phi_k2", "nsa_pos_kTs"),
                                                     ("nsa_phi_v1s", "nsa_phi_v2", "nsa_pos_vTs"))):
                    sc.dma(pool, w1[kv].t[:, :, :], I(n1)[:, :, :], writes=[w1[kv].res])
                    sc.dma(pool, w2[kv].t[:, :, :], I(n2).rearrange("(c p) d -> p c d", p=128), writes=[w2[kv].res])
                    sc.dma(pool, posT[kv].t[:, :], I(npos)[:, :], writes=[posT[kv].res])
                for kv in range(2):
                    for hc in range(2):
                        bk = k.nxt("bank", banks)
                        for l in range(16):
                            sc.op(pe, lambda: T.matmul(bk.t[:, 0:1], lhsT=w1[kv].t[:, l, hc * 128:(hc + 1) * 128],
                                                       rhs=posT[kv].t[:, l:l + 1], start=(l == 0), stop=(l == 15)),
                                  reads=[w1[kv].res, posT[kv].res], writes=[bk.res])
                        sc.op(dve, lambda: V.tensor_copy(out=biasv.t[:, kv * 2 + hc:kv * 2 + hc + 1], in_=bk.t[:, 0:1]),
                              reads=[bk.res], writes=[biasv.res])
                for g in range(4):
                    for kv in range(2):
                        sb_ = k.nxt("csrc", srcb)
                        srcd = (kcT if kv == 0 else vcT)
                        sc.dma(sp, sb_.t[0:64, :], srcd[g * 64:(g + 1) * 64, :], writes=[sb_.res])
                        sc.dma(sp, sb_.t[64:128, 0:S - 1], srcd[g * 64:(g + 1) * 64, 1:S], writes=[sb_.res])
                        sv = sb_.t[:, :].rearrange("p (n s) -> p s n", s=16)
                        hb = hid[kv]
                        for hc in range(2):
                            bk = k.nxt("bank", banks)
                            for l2 in range(16):
                                l = 2 * l2
                                sc.op(pe, lambda: T.matmul(bk.t[:, 0:255], lhsT=w1[kv].t[:, l2, hc * 128:(hc + 1) * 128],
                                                           rhs=sv[:, l % 16, (l // 16):(l // 16) + 255],
                                                           start=(l2 == 0), stop=(l2 == 15)),
                                      reads=[w1[kv].res, sb_.res], writes=[bk.res])
                            sc.op(act, lambda: A.activation(out=hb.t[:, hc, 0:255], in_=bk.t[:, 0:255], func=AF.Silu,
                                                            bias=biasv.t[:, kv * 2 + hc:kv * 2 + hc + 1]),
                                  reads=[bk.res, biasv.res], writes=[hb.res])
                        if kv == 0:
                            bk = k.nxt("bank", banks)
                            for hc in range(2):
                                sc.op(pe, lambda: T.matmul(bk.t[0:64, 0:255], lhsT=w2[0].t[:, hc, :], rhs=hb.t[:, hc, 0:255],
                                                           start=(hc == 0), stop=(hc == 1)),
                                      reads=[w2[0].res, hb.res], writes=[bk.res])
                            sc.op(dve, lambda: V.tensor_copy(out=Kc.t[0:64, g, 0:255], in_=bk.t[0:64, 0:255]),
                                  reads=[bk.res], writes=[Kc.res])
                        else:
                            for nt in range(2):
                                m = 128 if nt == 0 else 127
                                bk = k.nxt("bank", banks)
                                for hc in range(2):
                                    sc.op(pe, lambda: T.matmul(bk.t[0:m, 0:64], lhsT=hb.t[:, hc, nt * 128:nt * 128 + m],
                                                               rhs=w2[1].t[:, hc, :], start=(hc == 0), stop=(hc == 1)),
                                          reads=[w2[1].res, hb.res], writes=[bk.res])
                                sc.op(dve, lambda: V.tensor_copy(out=Vc.t[0:m, g, nt, 0:64], in_=bk.t[0:m, 0:64]),
                                      reads=[bk.res], writes=[Vc.res])
                sc.barrier()
            mcmp = k.sb(es, "nmcmp", [128, 2, S], BF16)
            keep = k.sb(es, "nkeep", [128, 32, 64], F32)
            force = k.sb(es, "nforce", [128, 32, 64], F32)
            expand = k.sb(es, "nexpand", [128, S], BF16)
            sc.dma(sp, mcmp.t[:, :, :], I("m_cmp")[:, :, :], writes=[mcmp.res])
            sc.dma(sp, keep.t[:, :, :], I("sel_keep")[:, :, :], writes=[keep.res])
            sc.dma(sp, force.t[:, :, :], I("sel_force")[:, :, :], writes=[force.res])
            sc.op(pool, lambda: G.memset(expand.t[64:128, :], 0.0), writes=[expand.res])
            sc.dma(sp, expand.t[0:64, :], I("expand")[:, :], writes=[expand.res])
            Qb = [k.sb(es, f"nQ{i}", [128, S], BF16) for i in range(2)]
            ksb = k.sb(es, "nks", [128, S], BF16)
            kwb = k.sb(es, "nkw", [128, S], BF16)
            vsb = k.sb(es, "nvs", [128, 32, 65], BF16)
            vwb = k.sb(es, "nvw", [128, 32, 65], BF16)
            for b in (ksb, kwb) + tuple(Qb):
                sc.op(pool, lambda: G.memset(b.t[64:128, :], 0.0), writes=[b.res])
            for b in (ksb, kwb):
                sc.dma(sp, b.t[64:70, :], I("augk_tok")[:, :], writes=[b.res])
            for b in (vsb, vwb):
                sc.op(pool, lambda: G.memset(b.t[:, :, 64:65], 1.0), writes=[b.res])
            selmT = k.sb(es, "nselmT", [128, S], BF16)
            sc.op(pool, lambda: G.memset(selmT.t[64:128, :], 0.0), writes=[selmT.res])
            imp = k.sb(es, "nimp", [128, 32, 64], F32)
            O = k.sb(es, "nO", [128, 32, 256], F32)
            cst2 = [k.sb(es, f"ncst{i}", [128, 32, 129], F32) for i in range(2)]
            cst_res2 = [[Res() for _ in range(32)] for _ in range(2)]
            cdn2 = [k.sb(es, f"ncdn{i}", [128, 32, 4], F32) for i in range(2)]
            ost = k.sb(es, "nost", [128, 32, 256], BF16)
            small = [k.sb(es, f"nsm{i}", [128, 4], F32) for i in range(8)]
            imb = [k.sb(es, f"nim{i}", [128, 64], F32) for i in range(4)]
            im2b = [k.sb(es, f"nim2{i}", [128, 64], F32) for i in range(4)]
            m8 = [k.sb(es, f"nm8{i}", [128, 16], F32) for i in range(4)]
            selb = [k.sb(es, f"nsel{i}", [128, 64], BF16) for i in range(8)]

            cmp_specs = []
            for j in range(8):
                l = [(0, 0, 512, [(ident.t[:, :], mcmp.t[:, 0, j * 512:(j + 1) * 512], 0, 512, [ident.res, mcmp.res])])]
                if j >= 4:
                    l.append((1, 0, 512, [(ident.t[:, :], mcmp.t[:, 1, j * 512:(j + 1) * 512], 0, 512, [ident.res, mcmp.res])]))
                cmp_specs.append(l)
            slc_specs = causal_specs(extra=lambda j, kt, c0, c1: [
                (expand.t[0:128, kt * 128:(kt + 1) * 128], selmT.t[0:128, j * 512 + c0:j * 512 + c1], c0, c1,
                 [expand.res, selmT.res])])
            win_specs = []
            for j in range(8):
                l = []
                for dp in range(4):
                    kt = 4 * j - 4 + dp
                    if kt < 0:
                        continue
                    l.append((kt, 0, 128 * (dp + 1),
                              [(ident.t[:, :], mband.t[:, :], 128 * dp, 128 * dp + 128, [ident.res, mband.res])]))
                for d in range(4):
                    l.append((4 * j + d, 128 * d, 512,
                              [(ident.t[:, :], mcaus.t[:, :], 128 * d, 128 * d + 128, [ident.res, mcaus.res])]))
                win_specs.append(l)

            def load_q(h):
                qb = k.nxt("nQ", Qb)
                sc.dma(sp, qb.t[0:64, :], NqT[h * 64:(h + 1) * 64, :], writes=[qb.res])
                sc.dma(sp, qb.t[64:70, :], I("augq")[8 + h, :, :], writes=[qb.res])
                return qb

            for g in range(4):
                sc.dma(sp, kwb.t[0:64, :], kwT[g * 64:(g + 1) * 64, :], writes=[kwb.res])
                sc.dma(sp, vwb.t[:, :, 0:64], vw_d[:, g * 64:(g + 1) * 64].rearrange("(t p) c -> p t c", p=128), writes=[vwb.res])
                for hh in range(4):
                    h = 4 * g + hh
                    qb = load_q(h)

                    cst, cst_res, cdn = cst2[hh % 2], cst_res2[hh % 2], cdn2[hh % 2]

                    def ev_cmp(qt, ac, h=h, hh=hh, cst=cst, cst_res=cst_res):
                        sc.op(dve, lambda: V.tensor_copy(out=cst.t[:, qt, :], in_=ac.t[:, 0:129]), reads=[ac.res], writes=[cst_res[qt]])

                    attn_pass(es, qb.t, Kc.t[:, g, :], KDIM, Vc.t[:, g, :, :], 129, cmp_specs, ev_cmp,
                              [qb.res, Kc.res, Vc.res], pbufs)
                    sc.op(dve, lambda: V.tensor_scalar(out=cdn.t[:, :, 0:1], in0=cst.t[:, :, 128:129], scalar1=1e-30, scalar2=None,
                                                       op0=ALU.max), reads=cst_res, writes=[cdn.res])
                    sc.op(dve, lambda: V.reciprocal(out=cdn.t[:, :, 1:2], in_=cdn.t[:, :, 0:1]), reads=[], writes=[cdn.res])
                    sc.op(dve, lambda: V.tensor_tensor(out=cdn.t[:, :, 2:3], in0=cdn.t[:, :, 1:2], in1=gates.t[:, :, 3 * h:3 * h + 1],
                                                       op=ALU.mult), reads=[gates.res], writes=[cdn.res])
                    sc.op(dve, lambda: V.tensor_tensor(out=O.t[:, :, hh * 64:(hh + 1) * 64], in0=cst.t[:, :, 0:64],
                                                       in1=cdn.t[:, :, 2:3].to_broadcast([128, 32, 64]), op=ALU.mult),
                          reads=[cdn.res] + cst_res, writes=[O.res])
                    if hh == 0:
                        sc.op(dve, lambda: V.tensor_tensor(out=imp.t[:, :, :], in0=cst.t[:, :, 64:128],
                                                           in1=cdn.t[:, :, 1:2].to_broadcast([128, 32, 64]), op=ALU.mult),
                              reads=[cdn.res] + cst_res, writes=[imp.res])
                    else:
                        sc.op(dve, lambda: V.tensor_tensor(out=cst.t[:, :, 64:128], in0=cst.t[:, :, 64:128],
                                                           in1=cdn.t[:, :, 1:2].to_broadcast([128, 32, 64]), op=ALU.mult),
                              reads=[cdn.res], writes=cst_res)
                        sc.op(dve, lambda: V.tensor_tensor(out=imp.t[:, :, :], in0=imp.t[:, :, :], in1=cst.t[:, :, 64:128], op=ALU.add),
                              reads=cst_res, writes=[imp.res])
                def sel_vec(q8):
                    sls = []
                    for i8 in range(8):
                        qt = q8 * 8 + i8
                        im = k.nxt("nim", imb)
                        im2 = k.nxt("nim2", im2b)
                        mm = k.nxt("nm8", m8)
                        sl = k.nxt("nsel", selb)
                        sls.append(sl)
                        sc.op(pool, lambda: G.tensor_tensor(out=im.t[:, :], in0=imp.t[:, qt, :], in1=keep.t[:, qt, :], op=ALU.mult),
                              reads=[imp.res, keep.res], writes=[im.res])
                        sc.op(pool, lambda: G.tensor_tensor(out=im.t[:, :], in0=im.t[:, :], in1=force.t[:, qt, :], op=ALU.add),
                              reads=[force.res], writes=[im.res])
                        sc.op(dve, lambda: V.max(out=mm.t[:, 0:8], in_=im.t[:, :]), reads=[im.res], writes=[mm.res])
                        sc.op(dve, lambda: V.match_replace(out=im2.t[:, :], in_to_replace=mm.t[:, 0:8], in_values=im.t[:, :],
                                                           imm_value=-3.0e38), reads=[im.res, mm.res], writes=[im2.res])
                        sc.op(dve, lambda: V.max(out=mm.t[:, 8:16], in_=im2.t[:, :]), reads=[im2.res], writes=[mm.res])
                        sc.op(dve, lambda: V.tensor_scalar(out=sl.t[:, :], in0=im.t[:, :], scalar1=mm.t[:, 15:16], scalar2=NEGB,
                                                           op0=ALU.is_lt, op1=ALU.mult), reads=[im.res, mm.res], writes=[sl.res])
                    return sls

                def sel_pe(q8, sls):
                    for i8, sl in enumerate(sls):
                        sc.op(pe, lambda: T.transpose(out=tbank.t[0:64, i8 * 128:(i8 + 1) * 128], in_=sl.t[:, :],
                                                      identity=ident.t[:, :]), reads=[sl.res, ident.res], writes=[tbank.res])
                    sc.op(dve, lambda: V.tensor_copy(out=selmT.t[0:64, q8 * 1024:(q8 + 1) * 1024], in_=tbank.t[0:64, :]),
                          reads=[tbank.res], writes=[selmT.res])

                for hh in range(4):
                    h = 4 * g + hh
                    qb = load_q(h)
                    sls = sel_vec(hh)

                    def ev_win(qt, ac, h=h, hh=hh):
                        sm = k.nxt("nsm", small)
                        sc.op(dve, lambda: V.reciprocal(out=sm.t[:, 1:2], in_=ac.t[:, 64:65]), reads=[ac.res], writes=[sm.res])
                        sc.op(dve, lambda: V.tensor_tensor(out=sm.t[:, 2:3], in0=sm.t[:, 1:2],
                                                           in1=gates.t[:, qt, 3 * h + 2:3 * h + 3], op=ALU.mult),
                              reads=[sm.res, gates.res], writes=[sm.res])
                        sc.op(dve, lambda: V.scalar_tensor_tensor(out=O.t[:, qt, hh * 64:(hh + 1) * 64], in0=ac.t[:, 0:64],
                                                                  scalar=sm.t[:, 2:3], in1=O.t[:, qt, hh * 64:(hh + 1) * 64],
                                                                  op0=ALU.mult, op1=ALU.add),
                              reads=[ac.res, sm.res], writes=[O.res])

                    attn_pass(es, qb.t, kwb.t, KDIM, vwb.t, 65, win_specs, ev_win, [qb.res, kwb.res, vwb.res], pbufs)
                    sel_pe(hh, sls)
                sc.dma(sp, ksb.t[0:64, :], ksT[g * 64:(g + 1) * 64, :], writes=[ksb.res])
                sc.dma(sp, vsb.t[:, :, 0:64], vs_d[:, g * 64:(g + 1) * 64].rearrange("(t p) c -> p t c", p=128), writes=[vsb.res])
                for hh in range(4):
                    h = 4 * g + hh
                    qb = load_q(h)

                    def ev_slc(qt, ac, h=h, hh=hh):
                        sm = k.nxt("nsm", small)
                        sc.op(dve, lambda: V.reciprocal(out=sm.t[:, 1:2], in_=ac.t[:, 64:65]), reads=[ac.res], writes=[sm.res])
                        sc.op(dve, lambda: V.tensor_tensor(out=sm.t[:, 2:3], in0=sm.t[:, 1:2],
                                                           in1=gates.t[:, qt, 3 * h + 1:3 * h + 2], op=ALU.mult),
                              reads=[sm.res, gates.res], writes=[sm.res])
                        sc.op(dve, lambda: V.scalar_tensor_tensor(out=ost.t[:, qt, hh * 64:(hh + 1) * 64], in0=ac.t[:, 0:64],
                                                                  scalar=sm.t[:, 2:3], in1=O.t[:, qt, hh * 64:(hh + 1) * 64],
                                                                  op0=ALU.mult, op1=ALU.add),
                              reads=[ac.res, sm.res, O.res], writes=[ost.res])

                    SB[0] = sbanks + [tb32]
                    attn_pass(es, qb.t, ksb.t, KDIM, vsb.t, 65, slc_specs, ev_slc, [qb.res, ksb.res, vsb.res], pbufs)
                    SB[0] = sbanks
                sc.dma(pool, onsa_d[:, g * 256:(g + 1) * 256].rearrange("(t p) c -> p t c", p=128), ost.t[:, :, :],
                       reads=[ost.res])
            sc.barrier()

    k.phase3 = phase3
    def layer_norm_rows(src, dst_tmp, gb, bb, out_ap, smalls, writes_res, out_reads=()):
        st = k.nxt("lnst", smalls)
        sc.op(dve, lambda: V.bn_stats(out=st.t[:, 0:6], in_=src.t[:, 0:512]), reads=[src.res], writes=[st.res])
        sc.op(dve, lambda: V.bn_stats(out=st.t[:, 6:12], in_=src.t[:, 512:1024]), reads=[src.res], writes=[st.res])
        sc.op(dve, lambda: V.bn_aggr(out=st.t[:, 12:14], in_=st.t[:, 0:12]), reads=[st.res], writes=[st.res])
        sc.op(act, lambda: A.activation(out=st.t[:, 14:15], in_=st.t[:, 13:14], func=AF.Ln, bias=epsb.t[:, 0:1], scale=1.0),
              reads=[st.res, epsb.res], writes=[st.res])
        sc.op(act, lambda: A.activation(out=st.t[:, 14:15], in_=st.t[:, 14:15], func=AF.Exp, scale=-0.5),
              reads=[st.res], writes=[st.res])
        sc.op(dve, lambda: V.tensor_scalar(out=dst_tmp.t[:, :], in0=src.t[:, :], scalar1=st.t[:, 12:13], scalar2=st.t[:, 14:15],
                                           op0=ALU.subtract, op1=ALU.mult), reads=[src.res, st.res], writes=[dst_tmp.res])
        sc.op(dve, lambda: V.tensor_tensor(out=dst_tmp.t[:, :], in0=dst_tmp.t[:, :], in1=gb.t[:, :], op=ALU.mult),
              reads=[gb.res], writes=[dst_tmp.res])
        sc.op(dve, lambda: V.tensor_tensor(out=out_ap, in0=dst_tmp.t[:, :], in1=bb.t[:, :], op=ALU.add),
              reads=[dst_tmp.res, bb.res] + list(out_reads), writes=writes_res)

    def phase4():
        with contextlib.ExitStack() as es:
            def wload(name, src_ap, kc, n):
                b = k.sb(es, name, [128, kc, n], BF16)
                sc.dma(pool, b.t[:, :, :], src_ap.rearrange("(kc p) n -> p kc n", p=128), writes=[b.res])
                return b
            Wmg = wload("Wmg", I("w_mgate"), 8, 2048)
            Wd = wload("Wd", I("w_br_diff"), 8, 1024)
            Wn = wload("Wn", I("w_br_nsa"), 8, 1024)
            Wo = wload("Wo", I("w_o"), 8, 1024)
            Wpg = wload("Wpg", I("w_ple_gate"), 8, 1024)
            Wpp = wload("Wpp", I("w_ple_proj"), 2, 1024)
            Wr = wload("Wr", I("w_router"), 8, 32)
            bmg = k.sb(es, "bmg", [1, 2048], BF16); sc.dma(pool, bmg.t[:, :], I("b_mgate")[:, :], writes=[bmg.res])
            bpg = k.sb(es, "bpg", [1, 1024], BF16); sc.dma(pool, bpg.t[:, :], I("b_ple_gate")[:, :], writes=[bpg.res])
            brt = k.sb(es, "brt", [1, 32], BF16); sc.dma(pool, brt.t[:, :], I("b_router")[:, :], writes=[brt.res])
            b2a = k.sb(es, "b2a", [32, 1024], BF16); sc.dma(pool, b2a.t[:, :], I("b_e2")[:, :], writes=[b2a.res])
            ones = k.sb(es, "ones", [1, 128], BF16); sc.op(pool, lambda: G.memset(ones.t[:, :], 1.0), writes=[ones.res])
            g1b = bcast_load(es, "g1b", I("ln1_g")[:, :], 1024)
            b1b = bcast_load(es, "b1b", I("ln1_b")[:, :], 1024)

            def rot(name, shape, dt, n=2):
                return [k.sb(es, f"{name}{i}", shape, dt) for i in range(n)]
            od_b = rot("od", [128, 1024], BF16); on_b = rot("on", [128, 1024], BF16)
            odT_b = rot("odT", [128, 1024], BF16, 1); onT_b = rot("onT", [128, 1024], BF16, 1)
            xt_b = rot("xt", [128, 1024], F32); xTt_b = rot("xTt", [128, 8, 128], BF16)
            pTt_b = rot("pTt", [128, 2, 128], BF16)
            sgd_b = rot("sgd", [128, 1024], F32, 1); sgn_b = rot("sgn", [128, 1024], F32, 1)
            mixbf_b = rot("mixbf", [128, 1024], BF16); mixT_b = rot("mixT", [128, 1024], BF16, 1)
            r_b = rot("rr", [128, 1024], F32, 1); h_b = rot("hh", [128, 1024], F32)
            hbf_b = rot("hbf", [128, 1024], BF16); hT_b = rot("hTt", [128, 1024], BF16)
            spg_b = rot("spg", [128, 1024], F32, 1); base_b = rot("base", [128, 1024], F32)
            lnst = rot("lnst", [128, 16], F32, 4)
            lg_b = rot("lg", [128, 32], F32); e_b = rot("eb", [128, 32], F32); msk_b = rot("msk", [128, 32], F32)
            m8_b = rot("rm8", [128, 8], F32); rs_b = rot("rs", [128, 2], F32)
            gt_b = rot("gt", [128, 32], F32); gtbf_b = rot("gtbf", [128, 32], BF16); gT_b = rot("gT", [32, 128], BF16)
            st = {}
            triu = k.sb(es, "triu", [128, 128], BF16); sc.dma(sp, triu.t[:, :], I("triu")[:, :], writes=[triu.res])
            ones128 = k.sb(es, "ones128", [128, 128], BF16); sc.dma(sp, ones128.t[:, :], I("ones128")[:, :], writes=[ones128.res])
            iota32 = k.sb(es, "iota32", [128, 32], F32); sc.dma(sp, iota32.t[:, :], I("iota32")[:, :], writes=[iota32.res])
            trp = k.sb(es, "trp", [128, 1], F32); sc.dma(sp, trp.t[:, :], I("trp")[:, :], writes=[trp.res])
            cumrep = k.sb(es, "cumrep", [128, 32], F32); sc.op(pool, lambda: G.memset(cumrep.t[:, :], 0.0), writes=[cumrep.res])
            idxu_b = rot("idxu", [128, 8], U32); idxf_b = rot("idxf", [128, 4], F32); mkbf_b = rot("mkbf", [128, 32], BF16)
            rank_b = rot("rank", [128, 32], F32); oh_b = rot("oh", [128, 32], F32); rk_b = rot("rk", [128, 4], F32)
            val_b = rot("val", [128, 4], F32); t1_b = rot("t1", [128, 4], F32); e4_b = rot("e4", [128, 4], F32)

            def transposes(src, dst, eng):
                for i in range(8):
                    sc.op(pe, lambda: T.transpose(out=tbank.t[:, i * 128:(i + 1) * 128], in_=src.t[:, i * 128:(i + 1) * 128],
                                                  identity=ident.t[:, :]), reads=[src.res, ident.res], writes=[tbank.res])
                if eng is act:
                    sc.op(act, lambda: A.copy(out=dst.t[:, :], in_=tbank.t[:, :]), reads=[tbank.res], writes=[dst.res])
                else:
                    sc.op(dve, lambda: V.tensor_copy(out=dst.t[:, :], in_=tbank.t[:, :]), reads=[tbank.res], writes=[dst.res])

            def mm_tok(lhs_fn, nk, W, c0, bias=None, lhs_reads=(), n=512):
                bk = k.nxt("bank", banks)
                for kc in range(nk):
                    sc.op(pe, lambda: T.matmul(bk.t[:, 0:n], lhsT=lhs_fn(kc), rhs=W.t[:, kc, c0:c0 + n],
                                               start=(kc == 0), stop=(kc == nk - 1 and bias is None)),
                          reads=[W.res] + list(lhs_reads), writes=[bk.res])
                if bias is not None:
                    sc.op(pe, lambda: T.matmul(bk.t[:, 0:n], lhsT=ones.t[0:1, :], rhs=bias.t[0:1, c0:c0 + n],
                                               start=False, stop=True), reads=[ones.res, bias.res], writes=[bk.res])
                return bk

            def stage_a(tt):
                d = st[tt] = {}
                od = d["od"] = k.nxt("od", od_b); on = d["on"] = k.nxt("on", on_b)
                xt = d["xt"] = k.nxt("xt", xt_b); xTt = k.nxt("xTt", xTt_b); pTt = d["pTt"] = k.nxt("pTt", pTt_b)
                rows = slice(tt * 128, (tt + 1) * 128)
                sc.dma(sp, od.t[:, :], odiff_d[rows, :], writes=[od.res])
                sc.dma(sp, on.t[:, :], onsa_d[rows, :], writes=[on.res])
                sc.dma(sp, xt.t[:, :], I("x")[rows, :], writes=[xt.res])
                sc.dma(pool, xTt.t[:, :, :], I("xT")[:, rows].rearrange("(kc p) t -> p kc t", p=128), writes=[xTt.res])
                sc.dma(pool, pTt.t[:, :, :], I("pT")[:, rows].rearrange("(kc p) t -> p kc t", p=128), writes=[pTt.res])
                odT = k.nxt("odT", odT_b); onT = k.nxt("onT", onT_b)
                sgd = k.nxt("sgd", sgd_b); sgn = k.nxt("sgn", sgn_b)
                mix = sgd; t2 = sgn; mixbf = d["mixbf"] = k.nxt("mixbf", mixbf_b)
                for ch in range(4):
                    bk = mm_tok(lambda kc: xTt.t[:, kc, :], 8, Wmg, ch * 512, bias=bmg, lhs_reads=[xTt.res])
                    dst = sgd if ch < 2 else sgn
                    sc.op(act, lambda: A.activation(out=dst.t[:, (ch % 2) * 512:(ch % 2) * 512 + 512], in_=bk.t[:, :],
                                                    func=AF.Sigmoid), reads=[bk.res], writes=[dst.res])
                transposes(od, odT, act)
                transposes(on, onT, dve)
                for ch in range(2):
                    bk = mm_tok(lambda kc: odT.t[:, kc * 128:(kc + 1) * 128], 8, Wd, ch * 512, lhs_reads=[odT.res])
                    sc.op(dve, lambda: V.tensor_tensor(out=mix.t[:, ch * 512:(ch + 1) * 512], in0=sgd.t[:, ch * 512:(ch + 1) * 512],
                                                       in1=bk.t[:, :], op=ALU.mult), reads=[sgd.res, bk.res], writes=[mix.res])
                for ch in range(2):
                    bk = mm_tok(lambda kc: onT.t[:, kc * 128:(kc + 1) * 128], 8, Wn, ch * 512, lhs_reads=[onT.res])
                    sc.op(dve, lambda: V.tensor_tensor(out=t2.t[:, ch * 512:(ch + 1) * 512], in0=sgn.t[:, ch * 512:(ch + 1) * 512],
                                                       in1=bk.t[:, :], op=ALU.mult), reads=[sgn.res, bk.res], writes=[t2.res])
                sc.op(pool, lambda: G.tensor_tensor(out=mixbf.t[:, :], in0=mix.t[:, :], in1=t2.t[:, :], op=ALU.add),
                      reads=[mix.res, t2.res], writes=[mixbf.res])

            def stage_b(tt):
                d = st[tt]
                mixT = k.nxt("mixT", mixT_b); r = k.nxt("rr", r_b); h = d["h"] = k.nxt("hh", h_b)
                hbf = d["hbf"] = k.nxt("hbf", hbf_b); hT = d["hT"] = k.nxt("hTt", hT_b)
                xt = d["xt"]
                transposes(d["mixbf"], mixT, act)
                for ch in range(2):
                    bk = mm_tok(lambda kc: mixT.t[:, kc * 128:(kc + 1) * 128], 8, Wo, ch * 512, lhs_reads=[mixT.res])
                    sc.op(dve, lambda: V.scalar_tensor_tensor(out=r.t[:, ch * 512:(ch + 1) * 512], in0=xt.t[:, ch * 512:(ch + 1) * 512],
                                                              scalar=ALPHA, in1=bk.t[:, :], op0=ALU.mult, op1=ALU.add),
                          reads=[xt.res, bk.res], writes=[r.res])
                layer_norm_rows(r, r, g1b, b1b, h.t[:, :], lnst, [h.res])
                sc.op(act, lambda: A.copy(out=hbf.t[:, :], in_=h.t[:, :]), reads=[h.res], writes=[hbf.res])

            def stage_b2(tt):
                d = st[tt]
                hbf, hT = d["hbf"], d["hT"]
                transposes(hbf, hT, dve)
                sc.dma(sp, hT_d[:, tt * 128:(tt + 1) * 128].rearrange("(kc p) t -> p kc t", p=128),
                       hT.t[:, :].rearrange("p (kc t) -> p kc t", kc=8), reads=[hT.res])

            def stage_c(tt):
                d = st[tt]
                h, hT, pTt = d["h"], d["hT"], d["pTt"]
                spg = k.nxt("spg", spg_b); base = d["base"] = k.nxt("base", base_b)
                rows = slice(tt * 128, (tt + 1) * 128)
                for ch in range(2):
                    bk = mm_tok(lambda kc: hT.t[:, kc * 128:(kc + 1) * 128], 8, Wpg, ch * 512, bias=bpg, lhs_reads=[hT.res])
                    sc.op(act, lambda: A.activation(out=spg.t[:, ch * 512:(ch + 1) * 512], in_=bk.t[:, :], func=AF.Sigmoid),
                          reads=[bk.res], writes=[spg.res])
                for ch in range(2):
                    bk = mm_tok(lambda kc: pTt.t[:, kc, :], 2, Wpp, ch * 512, lhs_reads=[pTt.res])
                    sc.op(dve, lambda: V.tensor_tensor(out=spg.t[:, ch * 512:(ch + 1) * 512], in0=spg.t[:, ch * 512:(ch + 1) * 512],
                                                       in1=bk.t[:, :], op=ALU.mult), reads=[bk.res], writes=[spg.res])
                sc.op(dve, lambda: V.scalar_tensor_tensor(out=base.t[:, :], in0=h.t[:, :], scalar=ALPHA, in1=spg.t[:, :],
                                                          op0=ALU.mult, op1=ALU.add), reads=[h.res, spg.res], writes=[base.res])
                bk = mm_tok(lambda kc: hT.t[:, kc * 128:(kc + 1) * 128], 8, Wr, 0, bias=brt, lhs_reads=[hT.res], n=32)
                lg = k.nxt("lg", lg_b); e_ = k.nxt("eb", e_b); mk = k.nxt("msk", msk_b); m8_ = k.nxt("rm8", m8_b)
                rs = k.nxt("rs", rs_b); gt = k.nxt("gt", gt_b); gtbf = k.nxt("gtbf", gtbf_b); gT = k.nxt("gT", gT_b)
                sc.op(dve, lambda: V.tensor_copy(out=lg.t[:, :], in_=bk.t[:, 0:32]), reads=[bk.res], writes=[lg.res])
                sc.op(dve, lambda: V.max(out=m8_.t[:, 0:8], in_=lg.t[:, :]), reads=[lg.res], writes=[m8_.res])
                sc.op(dve, lambda: V.tensor_scalar(out=e_.t[:, :], in0=lg.t[:, :], scalar1=m8_.t[:, 0:1], scalar2=None,
                                                   op0=ALU.subtract), reads=[lg.res, m8_.res], writes=[e_.res])
                sc.op(act, lambda: A.activation(out=e_.t[:, :], in_=e_.t[:, :], func=AF.Exp), reads=[], writes=[e_.res])
                sc.op(dve, lambda: V.tensor_scalar(out=mk.t[:, :], in0=lg.t[:, :], scalar1=m8_.t[:, 3:4], scalar2=None,
                                                   op0=ALU.is_ge), reads=[lg.res, m8_.res], writes=[mk.res])
                sc.op(dve, lambda: V.tensor_tensor(out=e_.t[:, :], in0=e_.t[:, :], in1=mk.t[:, :], op=ALU.mult),
                      reads=[mk.res], writes=[e_.res])
                sc.op(dve, lambda: V.reduce_sum(out=rs.t[:, 0:1], in_=e_.t[:, :], axis=AX.X), reads=[e_.res], writes=[rs.res])
                sc.op(dve, lambda: V.reciprocal(out=rs.t[:, 1:2], in_=rs.t[:, 0:1]), reads=[], writes=[rs.res])
                sc.op(dve, lambda: V.tensor_scalar(out=gt.t[:, :], in0=e_.t[:, :], scalar1=rs.t[:, 1:2], scalar2=None,
                                                   op0=ALU.mult), reads=[e_.res, rs.res], writes=[gt.res])
                idxu = k.nxt("idxu", idxu_b); idxf = d["idxf"] = k.nxt("idxf", idxf_b); mkbf = d["mkbf"] = k.nxt("mkbf", mkbf_b)
                e4 = d["e4"] = k.nxt("e4", e4_b)
                d["rs"] = rs; d["gtbf"] = gtbf; d["gT"] = gT
                sc.op(dve, lambda: V.max_index(out=idxu.t[:, :], in_max=m8_.t[:, :], in_values=lg.t[:, :]),
                      reads=[m8_.res, lg.res], writes=[idxu.res])
                sc.op(dve, lambda: V.tensor_copy(out=idxf.t[:, :], in_=idxu.t[:, 0:4]), reads=[idxu.res], writes=[idxf.res])
                sc.op(pool, lambda: G.tensor_copy(out=mkbf.t[:, :], in_=mk.t[:, :]), reads=[mk.res], writes=[mkbf.res])
                sc.op(pool, lambda: G.tensor_copy(out=gtbf.t[:, :], in_=gt.t[:, :]), reads=[gt.res], writes=[gtbf.res])
                sc.op(dve, lambda: V.tensor_scalar(out=e4.t[:, :], in0=m8_.t[:, 0:4], scalar1=m8_.t[:, 0:1], scalar2=None,
                                                   op0=ALU.subtract), reads=[m8_.res], writes=[e4.res])
                sc.op(act, lambda: A.activation(out=e4.t[:, :], in_=e4.t[:, :], func=AF.Exp), reads=[], writes=[e4.res])

            def stage_d1(tt):
                d = st[tt]
                idxf, mkbf, e4, rs, gtbf, gT, hbf = d["idxf"], d["mkbf"], d["e4"], d["rs"], d["gtbf"], d["gT"], d["hbf"]
                rank = k.nxt("rank", rank_b); oh = k.nxt("oh", oh_b); rk = k.nxt("rk", rk_b)
                val = k.nxt("val", val_b); t1 = k.nxt("t1", t1_b)
                bkr = k.nxt("bank", banks)
                sc.op(pe, lambda: T.matmul(bkr.t[:, 0:32], lhsT=triu.t[:, :], rhs=mkbf.t[:, :], start=True, stop=True),
                      reads=[triu.res, mkbf.res], writes=[bkr.res])
                bkc = k.nxt("bank", banks)
                sc.op(pe, lambda: T.matmul(bkc.t[:, 0:32], lhsT=ones128.t[:, :], rhs=mkbf.t[:, :], start=True, stop=True),
                      reads=[ones128.res, mkbf.res], writes=[bkc.res])
                sc.op(pe, lambda: T.transpose(out=tbank.t[0:32, 0:128], in_=gtbf.t[:, :], identity=ident.t[:, :]),
                      reads=[gtbf.res, ident.res], writes=[tbank.res])
                sc.op(act, lambda: A.copy(out=gT.t[:, :], in_=tbank.t[0:32, 0:128]), reads=[tbank.res], writes=[gT.res])
                sc.op(dve, lambda: V.tensor_tensor(out=rank.t[:, :], in0=cumrep.t[:, :], in1=bkr.t[:, 0:32], op=ALU.add),
                      reads=[cumrep.res, bkr.res], writes=[rank.res])
                sc.op(dve, lambda: V.tensor_tensor(out=cumrep.t[:, :], in0=cumrep.t[:, :], in1=bkc.t[:, 0:32], op=ALU.add),
                      reads=[bkc.res], writes=[cumrep.res])
                for kk in range(4):
                    sc.op(dve, lambda: V.tensor_scalar(out=oh.t[:, :], in0=iota32.t[:, :], scalar1=idxf.t[:, kk:kk + 1], scalar2=None,
                                                       op0=ALU.is_equal), reads=[iota32.res, idxf.res], writes=[oh.res])
                    sc.op(dve, lambda: V.tensor_tensor(out=oh.t[:, :], in0=oh.t[:, :], in1=rank.t[:, :], op=ALU.mult),
                          reads=[rank.res], writes=[oh.res])
                    sc.op(dve, lambda: V.reduce_sum(out=rk.t[:, kk:kk + 1], in_=oh.t[:, :], axis=AX.X), reads=[oh.res], writes=[rk.res])
                sc.op(dve, lambda: V.tensor_scalar(out=val.t[:, :], in0=rk.t[:, :], scalar1=float(CAP), scalar2=None, op0=ALU.is_lt),
                      reads=[rk.res], writes=[val.res])
                sc.op(dve, lambda: V.scalar_tensor_tensor(out=t1.t[:, :], in0=idxf.t[:, :], scalar=float(CAP), in1=rk.t[:, :],
                                                          op0=ALU.mult, op1=ALU.add), reads=[idxf.res, rk.res], writes=[t1.res])
                sc.op(dve, lambda: V.scalar_tensor_tensor(out=t1.t[:, :], in0=t1.t[:, :], scalar=trp.t[:, 0:1], in1=val.t[:, :],
                                                          op0=ALU.subtract, op1=ALU.mult), reads=[trp.res, val.res], writes=[t1.res])
                sc.op(dve, lambda: V.tensor_scalar(out=dest_all.t[:, tt, :], in0=t1.t[:, :], scalar1=trp.t[:, 0:1], scalar2=None,
                                                   op0=ALU.add), reads=[t1.res, trp.res], writes=[dest_res[tt]])
                sc.op(dve, lambda: V.scalar_tensor_tensor(out=gk_all.t[:, tt, :], in0=e4.t[:, :], scalar=rs.t[:, 1:2], in1=val.t[:, :],
                                                          op0=ALU.mult, op1=ALU.mult), reads=[e4.res, rs.res, val.res], writes=[gk_res[tt]])
                for kk in range(4):
                    sc.dma(pool, lambda: G.indirect_dma_start(
                        out=xs_d[:, :], out_offset=bass.IndirectOffsetOnAxis(ap=dest_all.t[:, tt, kk:kk + 1], axis=0),
                        in_=hbf.t[:, :], in_offset=None), None, reads=[hbf.res, dest_res[tt]])

            def stage_d2(tt):
                d = st.pop(tt)
                gT, base = d["gT"], d["base"]
                rows = slice(tt * 128, (tt + 1) * 128)
                for ch in range(2):
                    bk2 = k.nxt("bank", banks)
                    sc.op(pe, lambda: T.matmul(bk2.t[:, :], lhsT=gT.t[0:32, :], rhs=b2a.t[0:32, ch * 512:(ch + 1) * 512],
                                               start=True, stop=True), reads=[gT.res, b2a.res], writes=[bk2.res])
                    sc.op(dve, lambda: V.tensor_tensor(out=base.t[:, ch * 512:(ch + 1) * 512], in0=base.t[:, ch * 512:(ch + 1) * 512],
                                                       in1=bk2.t[:, :], op=ALU.add), reads=[bk2.res], writes=[base.res])
                sc.dma(sp, base_d[rows, :], base.t[:, :], reads=[base.res])

            for it in range(NQT + 3):
                if 0 <= it - 3 < NQT:
                    stage_d1(it - 3)
                if 0 <= it - 2 < NQT:
                    stage_c(it - 2)
                if 0 <= it - 1 < NQT:
                    stage_b(it - 1)
                if it < NQT:
                    stage_a(it)
                if 0 <= it - 1 < NQT:
                    stage_b2(it - 1)
                if 0 <= it - 3 < NQT:
                    stage_d2(it - 3)
            sc.barrier()

    k.phase4 = phase4
    def phase5():
        NST = CAP // 128
        groups = [(g0, min(512, CAP - g0)) for g0 in range(0, CAP, 512)]
        with contextlib.ExitStack() as es:
            W1b = [k.sb(es, f"W1b{i}", [128, 8, 2048], BF16) for i in range(2)]
            W1res = [[Res() for _ in range(4)] for _ in range(2)]
            W2b = [k.sb(es, f"W2b{i}", [128, 8, 1024], BF16) for i in range(2)]
            W2res = [[Res() for _ in range(2)] for _ in range(2)]
            stg = [k.sb(es, f"wstg{i}", [128, 8, 512], F32) for i in range(2)]
            xTe2 = [k.sb(es, f"xTe{i}", [128, 8, CAP], BF16) for i in range(2)]
            actT = k.sb(es, "actT", [128, 8, CAP], BF16)
            xsl = [k.sb(es, f"xsl{i}", [128, 1024], BF16) for i in range(4)]
            yst = [k.sb(es, f"yst{i}", [128, 1024], F32) for i in range(2)]
            b1T = k.sb(es, "b1T", [128, 512], F32)
            sc.dma(sp, b1T.t[:, :], I("b_e1T")[:, :], writes=[b1T.res])
            gt_ = [k.sb(es, f"mg{i}", [128, 512], F32) for i in range(3)]
            sg_ = [k.sb(es, f"msg{i}", [128, 512], F32) for i in range(3)]
            lt_ = [k.sb(es, f"ml{i}", [128, 512], F32) for i in range(3)]
            w_e1, w_e2 = I("w_e1"), I("w_e2")
            sc.op(pool, lambda: G.memset(yst[0].t[:, :], 0.0), writes=[yst[0].res])
            sc.dma(sp, ys_d[32 * CAP:32 * CAP + 128, :], yst[0].t[:, :], reads=[yst[0].res])

            wseq = []
            for e_ in range(32):
                for blk in (0, 2, 1, 3):
                    wseq.append((e_, 1, blk))
                for blk in range(2):
                    wseq.append((e_, 2, blk))
            wstate = {"dma": 0, "cast": 0}

            def w_dma():
                n = wstate["dma"]
                if n >= len(wseq):
                    return
                wstate["dma"] += 1
                e_, kind, blk = wseq[n]
                s_ = stg[n % 2]
                src = (w_e1 if kind == 1 else w_e2)[e_, :, blk * 512:(blk + 1) * 512].rearrange("(kc p) n -> p kc n", p=128)
                sc.dma(sp, s_.t[:, :, :], src, writes=[s_.res])

            NPIECE = 2

            def w_cast_piece():
                n = wstate["cast"]
                if n >= len(wseq):
                    return
                j = wstate.get("piece", 0)
                e_, kind, blk = wseq[n]
                s_ = stg[n % 2]
                if kind == 1:
                    dst, rs_ = W1b[e_ % 2], W1res[e_ % 2][blk]
                else:
                    dst, rs_ = W2b[e_ % 2], W2res[e_ % 2][blk]
                kc0 = j * 4
                sc.op(act, lambda: A.copy(out=dst.t[:, kc0:kc0 + 4, blk * 512:(blk + 1) * 512], in_=s_.t[:, kc0:kc0 + 4, :]),
                      reads=[s_.res], writes=[rs_])
                if j + 1 == NPIECE:
                    wstate["piece"] = 0
                    wstate["cast"] += 1
                    w_dma()
                else:
                    wstate["piece"] = j + 1

            def w_cast():
                for _ in range(NPIECE):
                    w_cast_piece()

            w_dma(); w_dma()
            for _ in range(6):
                w_cast()
            def xs_tile(e_, s_i):
                xt_ = xTe2[e_ % 2]
                xl = k.nxt("xsl", xsl)
                r0 = e_ * CAP + s_i * 128
                sc.dma(sp, xl.t[:, :], xs_d[r0:r0 + 128, :], writes=[xl.res])
                for i in range(8):
                    sc.op(pe, lambda: T.transpose(out=tbank.t[:, i * 128:(i + 1) * 128], in_=xl.t[:, i * 128:(i + 1) * 128],
                                                  identity=ident.t[:, :]), reads=[xl.res, ident.res], writes=[tbank.res])
                sc.op(dve, lambda: V.tensor_copy(out=xt_.t[:, :, s_i * 128:(s_i + 1) * 128],
                                                 in_=tbank.t[:, :].rearrange("p (kc t) -> p kc t", kc=8)),
                      reads=[tbank.res], writes=[xt_.res])

            for s_i in range(NST):
                xs_tile(0, s_i)
            for e in range(32):
                w1 = W1b[e % 2]
                xTe = xTe2[e % 2]
                gi = 0
                for (g0, gn) in groups:
                    for fc in range(8):
                        if gi in (0, 2, 3, 5, 6, 8, 9, 11, 12, 14):
                            w_cast_piece()
                        gi += 1
                        bg = k.nxt("bank", banks)
                        bl = k.nxt("bank", banks)
                        for (bk, c0) in ((bg, fc * 128), (bl, 1024 + fc * 128)):
                            blk = c0 // 512
                            for kc in range(8):
                                sc.op(pe, lambda: T.matmul(bk.t[:, 0:gn], lhsT=w1.t[:, kc, c0:c0 + 128],
                                                           rhs=xTe.t[:, kc, g0:g0 + gn], start=(kc == 0), stop=(kc == 7)),
                                      reads=[W1res[e % 2][blk], xTe.res], writes=[bk.res])
                        g_ = k.nxt("mg", gt_); s_ = k.nxt("msg", sg_); l_ = k.nxt("ml", lt_)
                        cg = e * 16 + fc
                        cl = e * 16 + 8 + fc
                        sc.op(dve, lambda: V.tensor_scalar(out=g_.t[:, 0:gn], in0=bg.t[:, 0:gn], scalar1=b1T.t[:, cg:cg + 1], scalar2=7.0,
                                                           op0=ALU.add, op1=ALU.min), reads=[bg.res, b1T.res], writes=[g_.res])
                        sc.op(act, lambda: A.activation(out=s_.t[:, 0:gn], in_=g_.t[:, 0:gn], func=AF.Sigmoid, scale=1.702),
                              reads=[g_.res], writes=[s_.res])
                        sc.op(dve, lambda: V.tensor_scalar(out=l_.t[:, 0:gn], in0=bl.t[:, 0:gn], scalar1=b1T.t[:, cl:cl + 1], scalar2=-7.0,
                                                           op0=ALU.add, op1=ALU.max), reads=[bl.res, b1T.res], writes=[l_.res])
                        sc.op(dve, lambda: V.tensor_scalar(out=l_.t[:, 0:gn], in0=l_.t[:, 0:gn], scalar1=7.0, scalar2=1.0,
                                                           op0=ALU.min, op1=ALU.add), reads=[], writes=[l_.res])
                        sc.op(dve, lambda: V.tensor_tensor(out=g_.t[:, 0:gn], in0=g_.t[:, 0:gn], in1=s_.t[:, 0:gn], op=ALU.mult),
                              reads=[s_.res], writes=[g_.res])
                        sc.op(pool, lambda: G.tensor_tensor(out=actT.t[:, fc, g0:g0 + gn], in0=g_.t[:, 0:gn], in1=l_.t[:, 0:gn], op=ALU.mult),
                              reads=[g_.res, l_.res], writes=[actT.res])
                for s_i in range(NST):
                    if s_i in (1, 3):
                        w_cast_piece()
                    ys_ = k.nxt("yst", yst)
                    for ch in range(2):
                        bk = k.nxt("bank", banks)
                        for fc in range(8):
                            sc.op(pe, lambda: T.matmul(bk.t[:, :], lhsT=actT.t[:, fc, s_i * 128:(s_i + 1) * 128],
                                                       rhs=W2b[e % 2].t[:, fc, ch * 512:(ch + 1) * 512], start=(fc == 0), stop=(fc == 7)),
                                  reads=[actT.res, W2res[e % 2][ch]], writes=[bk.res])
                        evac_copy(ys_.t[:, ch * 512:(ch + 1) * 512], bk.t[:, :], [bk.res], [ys_.res])
                    r0 = e * CAP + s_i * 128
                    sc.dma(act, ys_d[r0:r0 + 128, :], ys_.t[:, :], reads=[ys_.res])
                    if e + 1 < 32:
                        xs_tile(e + 1, s_i)
            sc.barrier()
        with contextlib.ExitStack() as es:
            g2b = bcast_load(es, "g2b", I("ln2_g")[:, :], 1024)
            b2b = bcast_load(es, "b2b", I("ln2_b")[:, :], 1024)
            accb = [k.sb(es, f"cacc{i}", [128, 1024], F32) for i in range(4)]
            yb = [k.sb(es, f"cy{i}", [128, 1024], F32) for i in range(12)]
            lnst = [k.sb(es, f"lnst5{i}", [128, 16], F32) for i in range(4)]
            for tt in range(NQT):
                ac = k.nxt("cacc", accb)
                rows = slice(tt * 128, (tt + 1) * 128)
                sc.dma(sp, ac.t[:, :], base_d[rows, :], writes=[ac.res])
                for kk in range(4):
                    y_ = k.nxt("cy", yb)
                    sc.dma(pool, lambda: G.indirect_dma_start(
                        out=y_.t[:, :], out_offset=None, in_=ys_d[:, :],
                        in_offset=bass.IndirectOffsetOnAxis(ap=dest_all.t[:, tt, kk:kk + 1], axis=0)), None,
                        reads=[dest_res[tt]], writes=[y_.res])
                    sc.op(dve, lambda: V.scalar_tensor_tensor(out=ac.t[:, :], in0=y_.t[:, :], scalar=gk_all.t[:, tt, kk:kk + 1],
                                                              in1=ac.t[:, :], op0=ALU.mult, op1=ALU.add),
                          reads=[y_.res, gk_res[tt]], writes=[ac.res])
                st_ = k.nxt("lnst5", lnst)
                sc.op(dve, lambda: V.bn_stats(out=st_.t[:, 0:6], in_=ac.t[:, 0:512]), reads=[ac.res], writes=[st_.res])
                sc.op(dve, lambda: V.bn_stats(out=st_.t[:, 6:12], in_=ac.t[:, 512:1024]), reads=[ac.res], writes=[st_.res])
                sc.op(dve, lambda: V.bn_aggr(out=st_.t[:, 12:14], in_=st_.t[:, 0:12]), reads=[], writes=[st_.res])
                sc.op(act, lambda: A.activation(out=st_.t[:, 14:15], in_=st_.t[:, 13:14], func=AF.Ln, bias=epsb.t[:, 0:1], scale=1.0),
                      reads=[st_.res, epsb.res], writes=[st_.res])
                sc.op(act, lambda: A.activation(out=st_.t[:, 14:15], in_=st_.t[:, 14:15], func=AF.Exp, scale=-0.5),
                      reads=[], writes=[st_.res])
                sc.op(dve, lambda: V.scalar_tensor_tensor(out=st_.t[:, 15:16], in0=st_.t[:, 12:13], scalar=-1.0, in1=st_.t[:, 14:15],
                                                          op0=ALU.mult, op1=ALU.mult), reads=[], writes=[st_.res])
                sc.op(act, lambda: A.activation(out=ac.t[:, :], in_=ac.t[:, :], func=AF.Identity, bias=st_.t[:, 15:16],
                                                scale=st_.t[:, 14:15]), reads=[st_.res], writes=[ac.res])
                sc.op(dve, lambda: V.tensor_tensor(out=ac.t[:, :], in0=ac.t[:, :], in1=g2b.t[:, :], op=ALU.mult),
                      reads=[g2b.res], writes=[ac.res])
                sc.op(dve, lambda: V.tensor_tensor(out=ac.t[:, :], in0=ac.t[:, :], in1=b2b.t[:, :], op=ALU.add),
                      reads=[b2b.res], writes=[ac.res])
                sc.dma(sp, out_d[rows, :], ac.t[:, :], reads=[ac.res])
            sc.barrier()

    k.phase5 = phase5
    k.phase1 = phase1
    return k


def prep_inputs(inp):
    f = lambda a: np.ascontiguousarray(np.asarray(a, np.float32))
    sh = {}
    sh["w_in"] = f(inp["w_in"][0])
    for n in ("diff_lq1", "diff_lk1", "diff_lq2", "diff_lk2", "diff_subln_g", "b_mgate", "ln1_g", "ln1_b",
              "b_router", "b_ple_gate", "ln2_g", "ln2_b"):
        sh[n] = f(inp[n][0]).reshape(1, -1)
    sh["nsa_pos_kT"] = f(np.asarray(inp["nsa_pos_k"][0]).T)
    sh["nsa_pos_vT"] = f(np.asarray(inp["nsa_pos_v"][0]).T)
    sh["nsa_phi_k1"] = f(np.asarray(inp["nsa_phi_k1"][0]).reshape(32, 64, 256).transpose(1, 0, 2))
    for nm in ("k", "v"):
        w = np.asarray(inp["nsa_phi_%s1" % nm][0]).reshape(16, 2, 64, 256)
        sh["nsa_phi_%s1s" % nm] = f(w.transpose(1, 2, 0, 3).reshape(128, 16, 256))
        ps = np.asarray(inp["nsa_pos_%s" % nm][0]).reshape(16, 2, 64)
        sh["nsa_pos_%sTs" % nm] = f(ps.transpose(1, 2, 0).reshape(128, 16))
    sh["nsa_phi_v1"] = f(np.asarray(inp["nsa_phi_v1"][0]).reshape(32, 64, 256).transpose(1, 0, 2))
    for n in ("nsa_phi_k2", "nsa_phi_v2", "w_br_diff", "w_br_nsa", "w_mgate", "w_o", "w_router", "w_e1", "w_e2",
              "b_e2", "w_ple_gate", "w_ple_proj"):
        sh[n] = f(inp[n][0])
    sh["b_e1T"] = f(np.asarray(inp["b_e1"][0]).reshape(32, 16, 128).transpose(2, 0, 1).reshape(128, 512))
    sh.update(_const_tables())
    x = np.asarray(inp["x"], np.float32)
    p = np.asarray(inp["p"], np.float32)
    maps = []
    for b in range(8):
        m = dict(sh)
        m["x"] = f(x[b])
        m["xT"] = f(x[b].T)
        m["pT"] = f(p[0, b].T)
        maps.append(m)
    return maps


_CACHE = {}


def kernel(**inputs):
    if "nc" not in _CACHE:
        kb = build()
        kb.phase1(); kb.phase2(); kb.phase3(); kb.phase4(); kb.phase5()
        _CACHE["nc"] = kb
    kb = _CACHE["nc"]
    maps = prep_inputs(inputs)
    maps = [{n: m[n] for n in kb.din} for m in maps]
    res = run_bass_kernel_spmd(kb.nc, maps, core_ids=list(range(8)))
    out = np.stack([np.asarray(res.results[b]["out"], np.float32) for b in range(8)], axis=0)
    return out
```
